# Optimizing a Trainium2 kernel written in Bass

```python
import math
import jax, jax.numpy as jnp
from jax import lax
import numpy as np

D_MODEL = 1024
BATCH = 4
SEQ = 4096
DEPTH = 1
DEC_BATCH = 128
DEC_SEQ = 1
PAST_LEN = 2048
PAGE_SIZE = 128

A_HEADS = 8
A_HEAD_DIM = 64
A_WIDTH = A_HEADS * A_HEAD_DIM
BRANCHES = ((128, 1), (512, 4), (2048, 16))
MAX_WINDOW = 2048
REL_BUCKETS = 32
REL_MAX_DIST = 2048
B_HEADS = 4
B_HEAD_DIM = 128
B_WIDTH = B_HEADS * B_HEAD_DIM
CONV_WIDTH = 4
CONV_DIM = 3 * B_WIDTH
CHUNK = 64
MIX_WIDTH = A_WIDTH + B_WIDTH
COL_QA = 0
COL_KA = A_WIDTH
COL_VA = 2 * A_WIDTH
COL_UB = 3 * A_WIDTH
COL_ZB = COL_UB + CONV_DIM
COL_AB = COL_ZB + B_WIDTH
COL_BB = COL_AB + B_HEADS
IN_COLS = COL_BB + B_HEADS
N_GROUPS = 4
EXPERTS_PER_GROUP = 8
N_EXPERTS = N_GROUPS * EXPERTS_PER_GROUP
TOP_K = 2
D_EXPERT = 512
MOE_BLOCK = 128
DN_ALPHA = (2 * DEPTH) ** 0.25
DN_BETA = (8 * DEPTH) ** -0.25
LN_EPS = 1e-5
RMS_EPS = 1e-6

kernel_name = 'hybrid_dilated_deltanet_hmoe_step'


def layer_norm(x, g, b):
    xf = x.astype(jnp.float32)
    mu = jnp.mean(xf, axis=-1, keepdims=True)
    var = jnp.mean(jnp.square(xf - mu), axis=-1, keepdims=True)
    y = (xf - mu) * lax.rsqrt(var + LN_EPS) * g.astype(jnp.float32) + b.astype(jnp.float32)
    return y.astype(x.dtype)


def l2_normalize(t):
    return t * lax.rsqrt(jnp.sum(t * t, axis=-1, keepdims=True) + 1e-6)


def rel_bucket(dist):
    max_exact = REL_BUCKETS // 2
    n = jnp.maximum(dist, 0)
    ratio = jnp.maximum(n, 1).astype(jnp.float32) / max_exact
    large = max_exact + (jnp.log(ratio) / math.log(REL_MAX_DIST / max_exact)
                         * (REL_BUCKETS - max_exact)).astype(jnp.int32)
    return jnp.where(n < max_exact, n, jnp.minimum(large, REL_BUCKETS - 1))


def split_proj(p):
    N, T = p.shape[0], p.shape[1]
    heads = lambda t: t.reshape(N, T, A_HEADS, A_HEAD_DIM)
    return (heads(p[..., COL_QA:COL_KA]), heads(p[..., COL_KA:COL_VA]), heads(p[..., COL_VA:COL_UB]),
            p[..., COL_UB:COL_ZB], p[..., COL_ZB:COL_AB], p[..., COL_AB:COL_BB], p[..., COL_BB:IN_COLS])


def dilated_branch_prompt(q, k, v, rel_bias, window, dil):
    B, S, H, Dh = q.shape
    band = window // dil
    L = S // dil
    nb = -(-L // band)
    Lp = nb * band

    def to_blocks(t):
        t = jnp.swapaxes(t.reshape(B, L, dil, H, Dh), 1, 2)
        t = jnp.pad(t, ((0, 0), (0, 0), (0, Lp - L), (0, 0), (0, 0)))
        return t.reshape(B, dil, nb, band, H, Dh)

    def with_prev(t):
        prev = jnp.pad(t, ((0, 0), (0, 0), (1, 0), (0, 0), (0, 0), (0, 0)))[:, :, :nb]
        return jnp.concatenate([prev, t], axis=3)

    def from_blocks(t):
        t = t.reshape(B, dil, Lp, *t.shape[4:])[:, :, :L]
        t = jnp.swapaxes(t, 1, 2)
        return t.reshape(B, S, *t.shape[3:])

    qb = to_blocks(q)
    kk = with_prev(to_blocks(k))
    vv = with_prev(to_blocks(v))
    i = jnp.arange(band)[:, None]
    j = jnp.arange(2 * band)[None, :]
    off = band + i - j
    in_band = (off >= 0) & (off <= band)
    first = (jnp.arange(nb)[:, None, None] == 0) & (j[None] < band)
    mask = in_band[None] & ~first
    bias = jnp.transpose(rel_bias[rel_bucket(jnp.maximum(off, 0) * dil)], (2, 0, 1)).astype(jnp.float32)
    s = jnp.einsum('brnqhd,brnkhd->brnhqk', qb, kk, preferred_element_type=jnp.float32) * (Dh ** -0.5) + bias
    s = jnp.where(mask[:, None], s, -jnp.inf)
    m = jnp.max(s, axis=-1, keepdims=True)
    pr = jnp.exp(s - m)
    l = jnp.sum(pr, axis=-1, keepdims=True)
    o = jnp.einsum('brnhqk,brnkhd->brnqhd', pr, vv.astype(jnp.float32)) / jnp.transpose(l, (0, 1, 2, 4, 3, 5))
    lse = jnp.transpose((m + jnp.log(l))[..., 0], (0, 1, 2, 4, 3))
    return from_blocks(o), from_blocks(lse)


def dilated_branch_cached(q, k_all, v_all, rel_bias, window, dil):
    N, T, H, Dh = q.shape
    P = k_all.shape[1] - T
    dist = jnp.arange(window // dil + 1) * dil
    idx = P + jnp.arange(T)[:, None] - dist[None, :]
    valid = idx >= 0
    idx = jnp.maximum(idx, 0)
    kg = k_all[:, idx]
    vg = v_all[:, idx]
    bias = jnp.transpose(rel_bias[rel_bucket(dist)]).astype(jnp.float32)
    s = jnp.einsum('nthd,ntjhd->nthj', q, kg, preferred_element_type=jnp.float32) * (Dh ** -0.5) + bias
    s = jnp.where(valid[None, :, None, :], s, -jnp.inf)
    m = jnp.max(s, axis=-1, keepdims=True)
    pr = jnp.exp(s - m)
    l = jnp.sum(pr, axis=-1, keepdims=True)
    o = jnp.einsum('nthj,ntjhd->nthd', pr, vg.astype(jnp.float32)) / l
    return o, (m + jnp.log(l))[..., 0]


def merge_branches(branches):
    outs = jnp.stack([b[0] for b in branches], axis=0)
    w = jax.nn.softmax(jnp.stack([b[1] for b in branches], axis=0), axis=0)
    return jnp.sum(w[..., None] * outs, axis=0)


def short_conv(u_ext, conv_w, T):
    acc = u_ext[:, 0:T] * conv_w[0]
    for i in range(1, CONV_WIDTH):
        acc = acc + u_ext[:, i:i + T] * conv_w[i]
    return jax.nn.silu(acc)


def deltanet_features(conv_out, a_raw, b_raw, a_log, dt_bias):
    N, T = conv_out.shape[0], conv_out.shape[1]
    c = conv_out.astype(jnp.float32).reshape(N, T, 3, B_HEADS, B_HEAD_DIM)
    q = l2_normalize(c[:, :, 0]) * (B_HEAD_DIM ** -0.5)
    k = l2_normalize(c[:, :, 1])
    v = c[:, :, 2]
    g = -jnp.exp(a_log.astype(jnp.float32)) * jax.nn.softplus(a_raw.astype(jnp.float32) + dt_bias.astype(jnp.float32))
    beta = jax.nn.sigmoid(b_raw.astype(jnp.float32))
    return q, k, v, g, beta


def gated_delta_chunked(q, k, v, g, beta):
    B, S, H, Dk = q.shape
    Dv = v.shape[-1]
    n = S // CHUNK

    def blocks(t):
        return jnp.moveaxis(t.reshape(B, n, CHUNK, H, *t.shape[3:]), 3, 1)

    qc, kc, vc, gr, bc = blocks(q), blocks(k), blocks(v), blocks(g), blocks(beta)
    gc = jnp.cumsum(gr, axis=-1)
    causal = jnp.tril(jnp.ones((CHUNK, CHUNK), dtype=bool))
    strict = jnp.tril(jnp.ones((CHUNK, CHUNK), dtype=bool), -1)
    decay = jnp.exp(jnp.where(causal, gc[..., :, None] - gc[..., None, :], -jnp.inf))
    kk = jnp.einsum('bhnid,bhnjd->bhnij', kc, kc)
    tmat = jnp.where(strict, bc[..., :, None] * kk * decay, 0.0) + jnp.eye(CHUNK, dtype=jnp.float32)
    rhs = jnp.concatenate([vc * bc[..., None], kc * (bc * jnp.exp(gc))[..., None]], axis=-1)
    sol = lax.linalg.triangular_solve(tmat, rhs, left_side=True, lower=True, unit_diagonal=True)
    u_val, w_key = sol[..., :Dv], sol[..., Dv:]
    qk = jnp.einsum('bhnid,bhnjd->bhnij', qc, kc) * decay
    q_dec = qc * jnp.exp(gc)[..., None]
    k_tail = kc * jnp.exp(gc[..., -1:] - gc)[..., None]
    g_last = jnp.exp(gc[..., -1])

    def step(state, xs):
        u_i, w_i, qk_i, qd_i, kt_i, gl_i = xs
        v_new = u_i - jnp.einsum('bhcd,bhde->bhce', w_i, state)
        out = jnp.einsum('bhcd,bhde->bhce', qd_i, state) + jnp.einsum('bhcs,bhse->bhce', qk_i, v_new)
        state = state * gl_i[..., None, None] + jnp.einsum('bhcd,bhce->bhde', kt_i, v_new)
        return state, out

    xs = tuple(jnp.moveaxis(t, 2, 0) for t in (u_val, w_key, qk, q_dec, k_tail, g_last))
    state, out = lax.scan(step, jnp.zeros((B, H, Dk, Dv), jnp.float32), xs)
    out = jnp.transpose(out, (1, 0, 3, 2, 4)).reshape(B, S, H, Dv)
    return out, state


def gated_delta_recurrent(state, q, k, v, g, beta):
    def step(st, xs):
        q_t, k_t, v_t, g_t, b_t = xs
        st = st * jnp.exp(g_t)[..., None, None]
        mem = jnp.einsum('nhd,nhde->nhe', k_t, st)
        st = st + jnp.einsum('nhd,nhe->nhde', k_t, (v_t - mem) * b_t[..., None])
        return st, jnp.einsum('nhd,nhde->nhe', q_t, st)

    xs = tuple(jnp.moveaxis(t, 1, 0) for t in (q, k, v, g, beta))
    state, out = lax.scan(step, state.astype(jnp.float32), xs)
    return jnp.moveaxis(out, 0, 1), state


def deltanet_out(o, z, o_norm_g):
    N, T = o.shape[0], o.shape[1]
    o = o * lax.rsqrt(jnp.mean(o * o, axis=-1, keepdims=True) + RMS_EPS) * o_norm_g.astype(jnp.float32)
    z = z.astype(jnp.float32).reshape(N, T, B_HEADS, B_HEAD_DIM)
    return (o * jax.nn.silu(z)).reshape(N, T, B_WIDTH)


def hier_moe(h, w_group, b_group, w_router, b_router, w_gate, w_up, w_down):
    T, D = h.shape
    hf = h.astype(jnp.float32)
    g_logits = hf @ w_group.astype(jnp.float32) + b_group.astype(jnp.float32)
    g_idx = jnp.argmax(g_logits, axis=-1)
    p_group = jnp.take_along_axis(jax.nn.softmax(g_logits, axis=-1), g_idx[:, None], axis=-1)[:, 0]
    e_logits = (hf @ w_router.astype(jnp.float32)).reshape(T, N_GROUPS, EXPERTS_PER_GROUP) + b_router.astype(jnp.float32)
    e_logits = jnp.take_along_axis(e_logits, g_idx[:, None, None], axis=1)[:, 0]
    top_v, top_i = lax.top_k(e_logits, TOP_K)
    gate = (p_group[:, None] * jax.nn.softmax(top_v, axis=-1)).reshape(-1)
    expert = (g_idx[:, None] * EXPERTS_PER_GROUP + top_i).reshape(-1).astype(jnp.int32)
    tok = jnp.repeat(jnp.arange(T, dtype=jnp.int32), TOP_K)
    n_assign = T * TOP_K
    order = jnp.argsort(expert)
    e_s, tok_s, gate_s = expert[order], tok[order], gate[order]
    counts = jax.ops.segment_sum(jnp.ones_like(expert), expert, num_segments=N_EXPERTS)
    padded = (counts + MOE_BLOCK - 1) // MOE_BLOCK * MOE_BLOCK
    pad_end = jnp.cumsum(padded)
    pad_start = pad_end - padded
    cnt_start = jnp.cumsum(counts) - counts
    dest = pad_start[e_s] + jnp.arange(n_assign, dtype=jnp.int32) - cnt_start[e_s]
    n_blocks = -(-(n_assign + N_EXPERTS * (MOE_BLOCK - 1)) // MOE_BLOCK)
    slots = n_blocks * MOE_BLOCK
    slot_tok = jnp.full((slots,), T, jnp.int32).at[dest].set(tok_s)
    slot_gate = jnp.zeros((slots,), jnp.float32).at[dest].set(gate_s)
    blk_start = jnp.arange(n_blocks, dtype=jnp.int32) * MOE_BLOCK
    blk_expert = jnp.minimum(jnp.sum(pad_end[None, :] <= blk_start[:, None], axis=1), N_EXPERTS - 1)
    h_pad = jnp.concatenate([h, jnp.zeros((1, D), h.dtype)], axis=0)

    def expert_block(args):
        toks, e = args
        xb = h_pad[toks]
        return (jax.nn.silu(xb @ w_gate[e]) * (xb @ w_up[e])) @ w_down[e]

    y_slots = lax.map(expert_block, (slot_tok.reshape(n_blocks, MOE_BLOCK), blk_expert))
    y_slots = y_slots.reshape(slots, D).astype(jnp.float32) * slot_gate[:, None]
    y = jnp.zeros((T + 1, D), jnp.float32).at[slot_tok].add(y_slots)[:T]
    return y.astype(h.dtype)


def finish_layer(x, mix, w_out, ln1_g, ln1_b, w_group, b_group, w_router, b_router, w_gate, w_up, w_down, ln2_g, ln2_b):
    N, T, D = x.shape
    h = layer_norm(DN_ALPHA * x + mix.astype(x.dtype) @ w_out, ln1_g, ln1_b)
    f = hier_moe(h.reshape(N * T, D), w_group, b_group, w_router, b_router, w_gate, w_up, w_down).reshape(N, T, D)
    return layer_norm(DN_ALPHA * h + f, ln2_g, ln2_b)


def setup_inputs(seed: int = 0) -> dict:
    key = jax.random.key(seed)
    ks = jax.random.split(key, 28)
    nrm = lambda k, shape, s: jax.random.normal(k, shape, jnp.float32) * s
    win_buf = min(MAX_WINDOW, PAST_LEN)
    x_prompt = nrm(ks[0], (BATCH, SEQ, D_MODEL), 1.0)
    x_sample = nrm(ks[1], (DEC_BATCH, DEC_SEQ, D_MODEL), 1.0)
    cache_a_k = nrm(ks[2], (DEPTH, DEC_BATCH, win_buf, A_HEADS, A_HEAD_DIM), 1.0)
    cache_a_v = nrm(ks[3], (DEPTH, DEC_BATCH, win_buf, A_HEADS, A_HEAD_DIM), DN_BETA)
    state_b_ssm = nrm(ks[4], (DEPTH, DEC_BATCH, B_HEADS, B_HEAD_DIM, B_HEAD_DIM), 0.1)
    state_b_conv = nrm(ks[5], (DEPTH, DEC_BATCH, CONV_WIDTH - 1, CONV_DIM), 1.0)
    v_cols = jnp.zeros((IN_COLS,), bool).at[COL_VA:COL_UB].set(True).at[COL_UB + 2 * B_WIDTH:COL_ZB].set(True)
    w_in = nrm(ks[6], (DEPTH, D_MODEL, IN_COLS), D_MODEL ** -0.5) * jnp.where(v_cols, DN_BETA, 1.0)
    rel_bias = nrm(ks[7], (REL_BUCKETS, A_HEADS), 0.5)
    conv_w = nrm(ks[8], (DEPTH, CONV_WIDTH, CONV_DIM), CONV_WIDTH ** -0.5)
    a_log = jnp.log(jax.random.uniform(ks[9], (DEPTH, B_HEADS), jnp.float32, 1.0, 16.0))
    dt = jnp.exp(jax.random.uniform(ks[10], (DEPTH, B_HEADS), jnp.float32, math.log(1e-3), math.log(1e-1)))
    dt_bias = dt + jnp.log(-jnp.expm1(-dt))
    o_norm_g = 1.0 + nrm(ks[11], (DEPTH, B_HEAD_DIM), 0.02)
    w_out = nrm(ks[12], (DEPTH, MIX_WIDTH, D_MODEL), MIX_WIDTH ** -0.5 * DN_BETA)
    ln1_g = 1.0 + nrm(ks[13], (DEPTH, D_MODEL), 0.02)
    ln1_b = nrm(ks[14], (DEPTH, D_MODEL), 0.02)
    w_group = nrm(ks[15], (DEPTH, D_MODEL, N_GROUPS), D_MODEL ** -0.5)
    b_group = nrm(ks[16], (DEPTH, N_GROUPS), 0.01)
    w_router = nrm(ks[17], (DEPTH, D_MODEL, N_EXPERTS), D_MODEL ** -0.5)
    b_router = nrm(ks[18], (DEPTH, N_GROUPS, EXPERTS_PER_GROUP), 0.01)
    w_gate = nrm(ks[19], (DEPTH, N_EXPERTS, D_MODEL, D_EXPERT), D_MODEL ** -0.5)
    w_up = nrm(ks[20], (DEPTH, N_EXPERTS, D_MODEL, D_EXPERT), D_MODEL ** -0.5 * DN_BETA)
    w_down = nrm(ks[21], (DEPTH, N_EXPERTS, D_EXPERT, D_MODEL), D_EXPERT ** -0.5 * DN_BETA)
    ln2_g = 1.0 + nrm(ks[22], (DEPTH, D_MODEL), 0.02)
    ln2_b = nrm(ks[23], (DEPTH, D_MODEL), 0.02)
    return {'x_prompt': x_prompt, 'x_sample': x_sample, 'cache_a_k': cache_a_k, 'cache_a_v': cache_a_v,
            'state_b_ssm': state_b_ssm, 'state_b_conv': state_b_conv, 'w_in': w_in, 'rel_bias': rel_bias,
            'conv_w': conv_w, 'a_log': a_log, 'dt_bias': dt_bias, 'o_norm_g': o_norm_g, 'w_out': w_out,
            'ln1_g': ln1_g, 'ln1_b': ln1_b, 'w_group': w_group, 'b_group': b_group, 'w_router': w_router,
            'b_router': b_router, 'w_gate': w_gate, 'w_up': w_up, 'w_down': w_down, 'ln2_g': ln2_g, 'ln2_b': ln2_b}


def reference(x_prompt, x_sample, cache_a_k, cache_a_v, state_b_ssm, state_b_conv, w_in, rel_bias, conv_w,
              a_log, dt_bias, o_norm_g, w_out, ln1_g, ln1_b, w_group, b_group, w_router, b_router,
              w_gate, w_up, w_down, ln2_g, ln2_b):
    hp, hs = x_prompt, x_sample
    B, S = hp.shape[0], hp.shape[1]
    N, T = hs.shape[0], hs.shape[1]
    win_p = min(MAX_WINDOW, S)
    kp_l, vp_l, ks_l, vs_l, sp_l, ss_l, cp_l, cs_l = [], [], [], [], [], [], [], []
    for l in range(DEPTH):
        lw = (w_out[l], ln1_g[l], ln1_b[l], w_group[l], b_group[l], w_router[l], b_router[l],
              w_gate[l], w_up[l], w_down[l], ln2_g[l], ln2_b[l])
        qa, ka, va, ub, zb, ab, bb = split_proj(hp @ w_in[l])
        oa = merge_branches([dilated_branch_prompt(qa, ka, va, rel_bias, w, d) for (w, d) in BRANCHES])
        u_ext = jnp.pad(ub, ((0, 0), (CONV_WIDTH - 1, 0), (0, 0)))
        q, k, v, g, beta = deltanet_features(short_conv(u_ext, conv_w[l], S), ab, bb, a_log[l], dt_bias[l])
        ob, st_p = gated_delta_chunked(q, k, v, g, beta)
        mix = jnp.concatenate([oa.reshape(B, S, A_WIDTH), deltanet_out(ob, zb, o_norm_g[l])], axis=-1)
        hp_next = finish_layer(hp, mix, *lw)
        kp_l.append(ka[:, S - win_p:])
        vp_l.append(va[:, S - win_p:])
        sp_l.append(st_p)
        cp_l.append(u_ext[:, S:])
        qa, ka, va, ub, zb, ab, bb = split_proj(hs @ w_in[l])
        k_all = jnp.concatenate([cache_a_k[l].astype(ka.dtype), ka], axis=1)
        v_all = jnp.concatenate([cache_a_v[l].astype(va.dtype), va], axis=1)
        oa = merge_branches([dilated_branch_cached(qa, k_all, v_all, rel_bias, w, d) for (w, d) in BRANCHES])
        u_ext = jnp.concatenate([state_b_conv[l].astype(ub.dtype), ub], axis=1)
        q, k, v, g, beta = deltanet_features(short_conv(u_ext, conv_w[l], T), ab, bb, a_log[l], dt_bias[l])
        ob, st_s = gated_delta_recurrent(state_b_ssm[l], q, k, v, g, beta)
        mix = jnp.concatenate([oa.reshape(N, T, A_WIDTH), deltanet_out(ob, zb, o_norm_g[l])], axis=-1)
        hs_next = finish_layer(hs, mix, *lw)
        ks_l.append(ka)
        vs_l.append(va)
        ss_l.append(st_s)
        cs_l.append(u_ext[:, T:])
        hp, hs = hp_next, hs_next
    return (hp, hs, jnp.stack(kp_l), jnp.stack(vp_l), jnp.stack(ks_l), jnp.stack(vs_l),
            jnp.stack(sp_l), jnp.stack(ss_l), jnp.stack(cp_l), jnp.stack(cs_l))
```

```python
import math
import os
from contextlib import ExitStack

import numpy as np
import concourse.bass as bass
import concourse.mybir as mybir
from concourse.bass_utils import run_bass_kernel_spmd

F32 = mybir.dt.float32
BF16 = mybir.dt.bfloat16
I32 = mybir.dt.int32
U32 = mybir.dt.uint32
AF = mybir.ActivationFunctionType
ALU = mybir.AluOpType
AX = mybir.AxisListType

NCORES = 8
D = 1024
SEQ = 4096
HALF = 2048
EXT = 4096
NS = 16
A_HEADS, A_HD = 8, 64
B_HEADS, B_HD = 4, 128
COL_QA, COL_KA, COL_VA, COL_UB = 0, 512, 1024, 1536
COL_ZB = COL_UB + 1536
COL_AB = COL_ZB + 512
COL_BB = COL_AB + 4
IN_COLS = COL_BB + 4
BRANCHES = ((128, 1), (512, 4), (2048, 16))
NEG = -30000.0
ENGS = ("tensor", "vector", "scalar", "gpsimd", "sync")


class Sched:
    def __init__(self, nc, stack, same_engine_wait=True):
        self.nc = nc
        self.stack = stack
        self.q = {e: [] for e in ENGS}
        self.cnt = {e: 0 for e in ENGS}
        self.sem = {e: stack.enter_context(nc.semaphore("s_" + e)) for e in ENGS}
        self.waited = {e: {} for e in ENGS}
        self.same_engine_wait = same_engine_wait
        self.dma_sems = {}
        self.ninst = 0

    def _wait(self, eng, tok):
        if tok is None:
            return
        if tok[0] == "E":
            _, src, val = tok
            if src == eng and not self.same_engine_wait:
                return
            key = "E" + src
            sem = self.sem[src]
        else:
            _, slot, val = tok
            key = "D" + slot
            sem = self.dma_sems[slot][0]
        if self.waited[eng].get(key, 0) >= val:
            return
        self.waited[eng][key] = val
        self.q[eng].append(lambda e, sem=sem, val=val: e.wait_ge(sem, val))

    def op(self, eng, fn, deps=()):
        for d in deps:
            self._wait(eng, d)
        self.cnt[eng] += 1
        c = self.cnt[eng]
        sem = self.sem[eng]
        self.q[eng].append(lambda e, fn=fn, sem=sem: fn(e).then_inc(sem, 1))
        self.ninst += 1
        return ("E", eng, c)

    def dmaop(self, eng, slot, fn, deps=()):
        for d in deps:
            self._wait(eng, d)
        if slot not in self.dma_sems:
            self.dma_sems[slot] = [self.stack.enter_context(self.nc.semaphore("d_" + slot)), 0]
        ent = self.dma_sems[slot]
        ent[1] += 16
        sem = ent[0]
        self.q[eng].append(lambda e, fn=fn, sem=sem: fn(e).then_inc(sem, 16))
        self.ninst += 1
        return ("D", slot, ent[1])

    def barrier(self):
        toks = [("E", e, self.cnt[e]) for e in ENGS if self.cnt[e] > 0]
        toks += [("D", slot, ent[1]) for slot, ent in self.dma_sems.items() if ent[1] > 0]
        for e in ENGS:
            for t in toks:
                self._wait(e, t)

    def finish(self, final_tokens):
        best = {}
        for t in final_tokens:
            if t is None:
                continue
            key = (t[0], t[1])
            if key not in best or best[key][2] < t[2]:
                best[key] = t
        for t in best.values():
            self._wait("sync", t)
        with self.nc.Block() as block:
            @block.tensor
            def _(e):
                for f in self.q["tensor"]:
                    f(e)

            @block.vector
            def _(e):
                for f in self.q["vector"]:
                    f(e)

            @block.scalar
            def _(e):
                for f in self.q["scalar"]:
                    f(e)

            @block.gpsimd
            def _(e):
                for f in self.q["gpsimd"]:
                    f(e)

            @block.sync
            def _(e):
                for f in self.q["sync"]:
                    f(e)


class Buf:
    _n = 0

    def __init__(self, name=None):
        Buf._n += 1
        self.name = name or ("b%d" % Buf._n)
        self.writer = None
        self.readers = {}

    def add_reader(self, tok):
        key = tok[1]
        if key not in self.readers or self.readers[key][2] < tok[2]:
            self.readers[key] = tok


class K:
    def __init__(self, S):
        self.S = S

    def _deps(self, reads, writes, deps):
        d = list(deps)
        for b in reads:
            d.append(b.writer)
        for b in writes:
            d.extend(b.readers.values())
            d.append(b.writer)
        return d

    def _commit(self, tok, reads, writes):
        for b in reads:
            b.add_reader(tok)
        for b in writes:
            b.writer = tok
            b.readers = {}

    def op(self, eng, fn, reads=(), writes=(), deps=()):
        tok = self.S.op(eng, fn, self._deps(reads, writes, deps))
        self._commit(tok, reads, writes)
        return tok

    def acc(self, eng, fn, reads=(), acc=(), deps=()):
        d = list(deps)
        for b in reads:
            d.append(b.writer)
        tok = self.S.op(eng, fn, d)
        for b in reads:
            b.add_reader(tok)
        for b in acc:
            b.writer = tok
        return tok

    def gload(self, dst, src, writes, reads=()):
        A, L = dst.shape[1], dst.shape[2]
        slot = writes[0].name
        d = self._deps(reads, writes, ())
        tok = None
        for a0 in range(0, A, 4):
            for l0 in range(0, L, 512):
                tok = self.S.dmaop("gpsimd", slot, lambda e, a0=a0, l0=l0, L=L: e.dma_start(
                    out=dst[:, a0:a0 + 4, l0:min(L, l0 + 512)], in_=src[:, a0:a0 + 4, l0:min(L, l0 + 512)]), d)
        self._commit(tok, reads, writes)
        return tok

    def dma(self, eng, out, in_, reads=(), writes=(), deps=(), slot=None, **kw):
        if slot is None:
            slot = (writes[0] if writes else reads[0]).name
        tok = self.S.dmaop(eng, slot, lambda e: e.dma_start(out=out, in_=in_, **kw), self._deps(reads, writes, deps))
        self._commit(tok, reads, writes)
        return tok


def _rel_bucket_np(dist):
    n = np.maximum(dist, 0)
    ratio = np.maximum(n, 1).astype(np.float32) / np.float32(16)
    large = 16 + (np.log(ratio) / np.float32(math.log(2048 / 16)) * np.float32(16)).astype(np.int32)
    return np.where(n < 16, n, np.minimum(large, 31))


def _bias_tiles(rel_bias):
    kp = np.arange(128)[:, None, None]
    kt = np.arange(2)[None, :, None]
    i = np.arange(128)[None, None, :]
    off = 128 + i - (128 * kt + kp)
    valid = (off >= 0) & (off <= 128)
    out = np.empty((128, 24, 256), np.float32)
    for h in range(A_HEADS):
        for br, (_, dil) in enumerate(BRANCHES):
            bk = _rel_bucket_np(np.maximum(off, 0) * dil)
            vals = rel_bias[bk, h]
            out[:, h * 3 + br, :] = np.where(valid, vals, np.float32(NEG)).reshape(128, 256)
    return out


def sl(start, step, n=128):
    return slice(start, start + (n - 1) * step + 1, step)


def tokset(ti, dil):
    nblk, r = divmod(ti, dil)
    start = r + dil * 128 * nblk
    return start, dil


def build(debug=(), stage=99, nexp=32, with_sample=True):
    nc = bass.Bass("TRN2", target_bir_lowering=False)
    dram = lambda n, s, dt=F32, kind="ExternalInput": nc.dram_tensor(n, list(s), dt, kind=kind).ap()
    xT = dram("xT", [D, EXT])
    xo = dram("xo", [HALF, D])
    valid = dram("valid", [128, 32])
    w_in = dram("w_in", [D, IN_COLS])
    bt = dram("bt", [128, 24, 256])
    cst_d = dram("cst", [128, 7, 128])
    prm_d = dram("prm", [128, 184])
    ssm_p = dram("ssm_p", [4, 128, 128], kind="ExternalOutput")
    conv_p = dram("conv_p", [3, 1536], kind="ExternalOutput")
    kT_d = dram("kT_d", [128, 4, EXT], BF16, kind="Internal")
    vT_d = dram("vT_d", [128, 4, EXT], BF16, kind="Internal")
    qT_d = dram("qT_d", [128, 4, HALF], BF16, kind="Internal")
    gz_d = dram("gz_d", [16, 128, 512], BF16, kind="Internal")
    mixA_d = dram("mixA_d", [128, 4, HALF], BF16, kind="Internal")
    mixB_d = dram("mixB_d", [128, 4, HALF], BF16, kind="Internal")
    w_out = dram("w_out", [D, D])
    lnp_d = dram("lnp", [128, 4, D])
    wr_d = dram("wr", [D, 36])
    rb_d = dram("rb", [128, 36])
    w_gate = dram("w_gate", [32, D, 512])
    w_up = dram("w_up", [32, D, 512])
    w_down = dram("w_down", [32, 512, D])
    xs_pad = dram("xs_pad", [128, D])
    y_out = dram("y_out", [HALF, D], kind="ExternalOutput")
    ys_out = dram("ys_out", [NS, D], kind="ExternalOutput")
    mixS_d = dram("mixS_d", [128, 8, 128], BF16, kind="Internal")
    xsT = dram("xsT", [D, NS])
    ck = dram("ck", [NS, 2048, 512])
    cv = dram("cv", [NS, 2048, 512])
    sst = dram("sst", [NS * 4, 128, 128])
    scv = dram("scv", [NS, 3, 1536])
    sbias_d = dram("sbias", [128, 3, 129])
    cwr_d = dram("cwr", [NS, 4, 1536])
    sprm_d = dram("sprm", [NS * 4, 130])
    ps_d = dram("ps_d", [NS, IN_COLS], kind="Internal")
    cs_d = dram("cs_d", [NS, 1536], kind="Internal")
    ms_d = dram("ms_d", [NS, D], kind="Internal")
    knew = dram("knew", [NS, 512], kind="ExternalOutput")
    vnew = dram("vnew", [NS, 512], kind="ExternalOutput")
    ssm_s = dram("ssm_s", [NS * 4, 128, 128], kind="ExternalOutput")
    conv_s = dram("conv_s", [NS, 3, 1536], kind="ExternalOutput")
    kwin = dram("kwin", [HALF, 512], kind="ExternalOutput")
    vwin = dram("vwin", [HALF, 512], kind="ExternalOutput")
    dbg_out = {}
    for name, shape in debug:
        dbg_out[name] = dram("dbg_" + name, shape, kind="ExternalOutput")

    final = []
    with ExitStack() as st:
        S = Sched(nc, st)
        k = K(S)
        sb = lambda n, s, dt=F32: st.enter_context(nc.sbuf_tensor(n, list(s), dt))
        psT = [st.enter_context(nc.psum_tensor("ps%d" % i, [128, 512], F32)) for i in range(8)]
        psB = [Buf("ps%d" % i) for i in range(8)]

        ones_f = sb("ones_f", [128, 128])
        b_ones = Buf()
        k.op("gpsimd", lambda e: e.memset(ones_f[:], 1.0), writes=[b_ones])
        eps6 = sb("eps6", [128, 1])
        b_eps = Buf()
        k.op("gpsimd", lambda e: e.memset(eps6[:], 1e-6), writes=[b_eps])
        cst = sb("cst_sb", [128, 7, 128])
        b_cst = Buf("cst")
        k.dma("sync", cst[:], cst_d, writes=[b_cst])
        eps5 = sb("eps5", [128, 1])
        b_eps5 = Buf()
        k.op("gpsimd", lambda e: e.memset(eps5[:], 1e-5), writes=[b_eps5])
        b_mixA_d, b_mixB_d, b_mixS_d = Buf("mixA_d"), Buf("mixB_d"), Buf("mixS_d")
        valid_sb = sb("valid_sb", [128, 32])
        b_valid = Buf("valid")
        k.dma("sync", valid_sb[:], valid, writes=[b_valid])

        if with_sample:
            C = type("Ctx", (), {})()
            C.nc, C.k, C.S, C.final = nc, k, S, final
            C.psT, C.psB, C.cst, C.b_cst, C.eps6, C.b_eps = psT, psB, cst, b_cst, eps6, b_eps
            C.w_v = w_in.rearrange("(k p) c -> p k c", p=128)
            C.xsT, C.ck, C.cv, C.sst, C.scv, C.sbias_d, C.cwr_d, C.sprm_d = xsT, ck, cv, sst, scv, sbias_d, cwr_d, sprm_d
            C.ps_d, C.cs_d, C.ms_d = ps_d, cs_d, ms_d
            C.knew, C.vnew, C.ssm_s, C.conv_s = knew, vnew, ssm_s, conv_s
            C.mixS_d, C.b_mixS_d = mixS_d, b_mixS_d
            phase_s(C)
        sx = ExitStack()
        xTb = sx.enter_context(nc.sbuf_tensor("xTb", [128, 8, EXT], BF16))
        b_x = [Buf("xTb%d" % c) for c in range(8)]
        xT_v = xT.rearrange("(k p) t -> p k t", p=128)
        for c in range(8):
            k.gload(xTb[:, :, 512 * c:512 * (c + 1)], xT_v[:, :, 512 * c:512 * (c + 1)], writes=[b_x[c]])
        bx_of_tile = lambda ti_nat: b_x[ti_nat // 4]

        def x_bufs(start, step):
            lo, hi = start, start + step * 127
            return [b_x[c] for c in range(lo // 512, hi // 512 + 1)]

        with ExitStack() as sa:
            sba = lambda n, s, dt=F32: sa.enter_context(nc.sbuf_tensor(n, list(s), dt))
            mixA = sba("mixA", [128, 4, HALF], BF16)
            b_mixA = [Buf() for _ in range(4)]
            ETb = sba("ETb", [128, 24, 256], BF16)
            b_ET = Buf("ET")
            btst = sba("btst", [128, 6, 256])
            b_btst = Buf("btst")
            for g in range(4 if stage >= -1 else 0):
                k.dma("sync", btst[:], bt[:, 6 * g:6 * g + 6, :], writes=[b_btst])
                k.op("scalar", lambda e, g=g: e.activation(out=ETb[:, 6 * g:6 * g + 6, :], in_=btst[:], func=AF.Exp),
                     reads=[b_btst], writes=[b_ET])
            QT = sba("QT", [128, 2, HALF], BF16)
            KT = sba("KT", [128, 2, EXT], BF16)
            Vaug = sba("Vaug", [128, 32, 4, 65], BF16)
            acc = sba("acc", [65, 4, HALF])
            wq = sba("wq", [128, 8, 256], BF16)
            wk = sba("wk", [128, 8, 256], BF16)
            wv = sba("wv", [128, 8, 256], BF16)
            b_wq, b_wk, b_wv = Buf("wq"), Buf("wk"), Buf("wv")
            b_QT = [Buf() for _ in range(2)]
            b_KT = [Buf() for _ in range(2)]
            b_V = [Buf() for _ in range(32)]
            b_acc = [Buf() for _ in range(4)]
            b_rrow = Buf()
            stg = [sba("stg%d" % i, [128, 256]) for i in range(2)]
            b_stg = [Buf("stg%d" % i) for i in range(2)]
            exb = [sba("exb%d" % i, [128, 512]) for i in range(2)]
            b_ex = [Buf() for _ in range(2)]
            ptb = [sba("ptb%d" % i, [128, 512], BF16) for i in range(2)]
            b_pt = [Buf() for _ in range(2)]
            w_v = w_in.rearrange("(k p) c -> p k c", p=128)
            ctr = {"ps": 0, "stg": 0, "s": 0, "o": 0, "ev": 0}

            def proj_ps():
                i = ctr["ps"] % 2
                ctr["ps"] += 1
                return psT[i], psB[i]

            for hh2 in range(2):
                k.gload(wq[:], w_v[:, :, COL_QA + 256 * hh2:COL_QA + 256 * hh2 + 256], writes=[b_wq])
                k.gload(wk[:], w_v[:, :, COL_KA + 256 * hh2:COL_KA + 256 * hh2 + 256], writes=[b_wk])
                k.gload(wv[:], w_v[:, :, COL_VA + 256 * hh2:COL_VA + 256 * hh2 + 256], writes=[b_wv])
                for jj in range(2 if stage >= 0 else 0):
                    for tc in range(4 if os.environ.get('KQ','1')=='1' else 0):
                        pt_, pb_ = proj_ps()
                        for kk in range(8):
                            fn = lambda e, kk=kk, jj=jj, tc=tc, pt_=pt_: e.matmul(
                                pt_[:, :], lhsT=wq[:, kk, 128 * jj:128 * jj + 128],
                                rhs=xTb[:, kk, HALF + 512 * tc:HALF + 512 * tc + 512], start=(kk == 0), stop=(kk == 7))
                            if kk == 0:
                                k.op("tensor", fn, reads=[b_wq, b_x[4 + tc]], writes=[pb_])
                            else:
                                k.acc("tensor", fn, reads=[b_wq, b_x[4 + tc]], acc=[pb_])
                        k.op("vector", lambda e, jj=jj, tc=tc, pt_=pt_: e.tensor_scalar(
                            out=QT[:, jj, 512 * tc:512 * tc + 512], in0=pt_[:, :], scalar1=0.125, scalar2=None, op0=ALU.mult),
                            reads=[pb_], writes=[b_QT[jj]])
                    for tc in range(8 if os.environ.get('KK','1')=='1' else 0):
                        pt_, pb_ = proj_ps()
                        for kk in range(8):
                            fn = lambda e, kk=kk, jj=jj, tc=tc, pt_=pt_: e.matmul(
                                pt_[:, :], lhsT=wk[:, kk, 128 * jj:128 * jj + 128],
                                rhs=xTb[:, kk, 512 * tc:512 * tc + 512], start=(kk == 0), stop=(kk == 7))
                            if kk == 0:
                                k.op("tensor", fn, reads=[b_wk, b_x[tc]], writes=[pb_])
                            else:
                                k.acc("tensor", fn, reads=[b_wk, b_x[tc]], acc=[pb_])
                        k.op("vector", lambda e, jj=jj, tc=tc, pt_=pt_: e.tensor_copy(
                            out=KT[:, jj, 512 * tc:512 * tc + 512], in_=pt_[:, :]),
                            reads=[pb_], writes=[b_KT[jj]])
                for ti in range(16, 32 if stage >= -2 else 16):
                    pt_, pb_ = proj_ps()
                    for kk in range(8):
                        fn = lambda e, kk=kk, ti=ti, pt_=pt_: e.matmul(
                            pt_[:, 0:256], lhsT=xTb[:, kk, 128 * ti:128 * ti + 128], rhs=wk[:, kk, :],
                            start=(kk == 0), stop=(kk == 7))
                        if kk == 0:
                            k.op("tensor", fn, reads=[b_wk, b_x[ti // 4]], writes=[pb_])
                        else:
                            k.acc("tensor", fn, reads=[b_wk, b_x[ti // 4]], acc=[pb_])
                    si = ctr["stg"] % 2
                    ctr["stg"] += 1
                    k.op("scalar", lambda e, si=si, pt_=pt_: e.activation(out=stg[si][:], in_=pt_[:, 0:256], func=AF.Copy),
                         reads=[pb_], writes=[b_stg[si]])
                    final.append(k.dma("sync", kwin[128 * (ti - 16):128 * (ti - 16) + 128, 256 * hh2:256 * hh2 + 256],
                                       stg[si][:], reads=[b_stg[si]]))
                for br, (_, dil) in enumerate(BRANCHES):
                    if stage < 1:
                        break
                    k.op("gpsimd", lambda e: e.tensor_copy(
                        out=Vaug[:, :, :, 64], in_=valid_sb[:, :].unsqueeze(2).to_broadcast([128, 32, 4])),
                        reads=[b_valid], writes=b_V)
                    for ti in range(32):
                        start, step = tokset(ti, dil)
                        pt_, pb_ = proj_ps()
                        for kk in range(8):
                            fn = lambda e, kk=kk, start=start, step=step, pt_=pt_: e.matmul(
                                pt_[:, 0:256], lhsT=xTb[:, kk, sl(start, step)], rhs=wv[:, kk, :],
                                start=(kk == 0), stop=(kk == 7))
                            if kk == 0:
                                k.op("tensor", fn, reads=[b_wv] + x_bufs(start, step), writes=[pb_])
                            else:
                                k.acc("tensor", fn, reads=[b_wv] + x_bufs(start, step), acc=[pb_])
                        ev = "vector"
                        src = pt_[:, 0:256].rearrange("p (h d) -> p h d", h=4)
                        if ev == "vector":
                            k.op("vector", lambda e, ti=ti, src=src: e.tensor_copy(out=Vaug[:, ti, :, 0:64], in_=src),
                                 reads=[pb_], writes=[b_V[ti]])
                        else:
                            k.op("scalar", lambda e, ti=ti, src=src: e.activation(out=Vaug[:, ti, :, 0:64], in_=src, func=AF.Copy),
                                 reads=[pb_], writes=[b_V[ti]])
                        if br == 0 and ti >= 16:
                            si = ctr["stg"] % 2
                            ctr["stg"] += 1
                            k.op("vector", lambda e, si=si, pt_=pt_: e.tensor_copy(out=stg[si][:], in_=pt_[:, 0:256]),
                                 reads=[pb_], writes=[b_stg[si]])
                            final.append(k.dma("sync", vwin[128 * (ti - 16):128 * (ti - 16) + 128, 256 * hh2:256 * hh2 + 256],
                                               stg[si][:], reads=[b_stg[si]]))
                    for hl in range(4):
                        if stage < 2:
                            break
                        h = 4 * hh2 + hl
                        jj, pb = hl // 2, 64 * (hl % 2)
                        for ti0 in range(16, 32, 2):
                            sidx = 2 + ctr["s"] % 2
                            ctr["s"] += 1
                            pS, bS = psT[sidx], psB[sidx]
                            first = True
                            for a in range(2):
                                ti = ti0 + a
                                qs, qstep = tokset(ti, dil)
                                qs -= HALF
                                for kt in range(2):
                                    tk = ti - dil * (1 - kt)
                                    ks, kstep = tokset(tk, dil)
                                    fn = lambda e, a=a, kt=kt, ks=ks, kstep=kstep, qs=qs, qstep=qstep, pS=pS, jj=jj, pb=pb: e.matmul(
                                        pS[:, a * 256 + kt * 128:a * 256 + kt * 128 + 128],
                                        lhsT=KT[pb:pb + 64, jj, sl(ks, kstep)],
                                        rhs=QT[pb:pb + 64, jj, sl(qs, qstep)], start=True, stop=True)
                                    if first:
                                        k.op("tensor", fn, reads=[b_KT[jj], b_QT[jj]], writes=[bS])
                                        first = False
                                    else:
                                        k.acc("tensor", fn, reads=[b_KT[jj], b_QT[jj]], acc=[bS])
                            ei = ctr["ev"] % 2
                            ctr["ev"] += 1
                            k.op("scalar", lambda e, ei=ei, pS=pS: e.activation(out=exb[ei][:], in_=pS[:, :], func=AF.Exp),
                                 reads=[bS], writes=[b_ex[ei]])
                            k.op("vector", lambda e, ei=ei, h=h, br=br: e.tensor_tensor(
                                out=ptb[ei][:].rearrange("p (a c) -> p a c", a=2),
                                in0=exb[ei][:].rearrange("p (a c) -> p a c", a=2),
                                in1=ETb[:, h * 3 + br:h * 3 + br + 1, :].to_broadcast([128, 2, 256]), op=ALU.mult),
                                reads=[b_ex[ei], b_ET], writes=[b_pt[ei]])
                            oidx = 4 + ctr["o"] % 2
                            ctr["o"] += 1
                            pO, bO = psT[oidx], psB[oidx]
                            first = True
                            for a in range(2):
                                ti = ti0 + a
                                for kt in range(2):
                                    tk = ti - dil * (1 - kt)
                                    fn = lambda e, a=a, kt=kt, tk=tk, hl=hl, ei=ei, pO=pO: e.matmul(
                                        pO[0:65, a * 128:a * 128 + 128], lhsT=Vaug[:, tk, hl, 0:65],
                                        rhs=ptb[ei][:, a * 256 + kt * 128:a * 256 + kt * 128 + 128],
                                        start=(kt == 0), stop=(kt == 1))
                                    if first:
                                        k.op("tensor", fn, reads=[b_pt[ei], b_V[tk]], writes=[bO])
                                        first = False
                                    else:
                                        k.acc("tensor", fn, reads=[b_pt[ei], b_V[tk]], acc=[bO])
                            qs0, qstep = tokset(ti0, dil)
                            qs1, _ = tokset(ti0 + 1, dil)
                            qs0 -= HALF
                            qs1 -= HALF
                            dst = bass.AP(acc, hl * HALF + qs0, [[4 * HALF, 65], [qs1 - qs0, 2], [qstep, 128]])
                            srcO = pO[0:65, 0:256].rearrange("p (a c) -> p a c", a=2)
                            if br == 0:
                                k.op("vector", lambda e, dst=dst, srcO=srcO: e.tensor_copy(out=dst, in_=srcO),
                                     reads=[bO], writes=[b_acc[hl]])
                            else:
                                k.op("vector", lambda e, dst=dst, srcO=srcO: e.tensor_tensor(out=dst, in0=srcO, in1=dst, op=ALU.add),
                                     reads=[bO], writes=[b_acc[hl]])
                if stage < 3:
                    continue
                k.op("vector", lambda e: e.reciprocal(out=acc[64:65, :, :], in_=acc[64:65, :, :]), reads=b_acc, writes=[b_rrow])
                for hl in range(4):
                    jj, pb = hl // 2, 64 * (hl % 2)
                    for c in range(4):
                        pt_, pb_ = psT[6 + c % 2], psB[6 + c % 2]
                        k.op("tensor", lambda e, hl=hl, c=c, pt_=pt_: e.matmul(
                            pt_[0:64, :], lhsT=ones_f[64:65, 0:64], rhs=acc[64:65, hl, 512 * c:512 * c + 512], start=True, stop=True),
                            reads=[b_rrow, b_ones], writes=[pb_])
                        k.op("vector", lambda e, hl=hl, c=c, pt_=pt_, pb=pb, jj=jj, hh2=hh2: e.tensor_tensor(
                            out=mixA[pb:pb + 64, 2 * hh2 + jj, 512 * c:512 * c + 512], in0=acc[0:64, hl, 512 * c:512 * c + 512],
                            in1=pt_[0:64, :], op=ALU.mult), reads=[pb_, b_acc[hl]], writes=[b_mixA[2 * hh2 + jj]])

            if stage >= 3:
                for pp in range(4):
                    k.dma("sync", mixA_d[:, pp], mixA[:, pp], reads=[b_mixA[pp]], writes=[b_mixA_d])
        S.barrier()
        if stage >= 4:
            C = type("Ctx", (), {})()
            C.nc, C.k, C.S, C.final = nc, k, S, final
            C.xTb, C.b_x, C.w_v = xTb, b_x, w_in.rearrange("(k p) c -> p k c", p=128)
            C.psT, C.psB, C.cst, C.b_cst, C.ones_f, C.b_ones = psT, psB, cst, b_cst, ones_f, b_ones
            C.eps6, C.b_eps, C.prm_d = eps6, b_eps, prm_d
            C.kT_d, C.vT_d, C.qT_d, C.gz_d = kT_d, vT_d, qT_d, gz_d
            C.b_kT_d, C.b_vT_d, C.b_qT_d, C.b_gz_d = Buf("kT_d"), Buf("vT_d"), Buf("qT_d"), Buf("gz_d")
            C.mixB_d, C.b_mixB_d, C.ssm_p, C.conv_p = mixB_d, b_mixB_d, ssm_p, conv_p
            phase_b(C)
        sx.close()
        S.barrier()
        if stage >= 5:
            C = type("Ctx", (), {})()
            C.nc, C.k, C.S, C.final = nc, k, S, final
            C.psT, C.psB, C.cst, C.b_cst = psT, psB, cst, b_cst
            C.eps5, C.b_eps5 = eps5, b_eps5
            C.NT = 17 if with_sample else 16
            C.NEXP = nexp
            C.lnp_d, C.w_out, C.wr_d, C.rb_d = lnp_d, w_out, wr_d, rb_d
            C.w_gate, C.w_up, C.w_down = w_gate, w_up, w_down
            C.mixA_d, C.mixB_d, C.b_mixA_d, C.b_mixB_d = mixA_d, mixB_d, b_mixA_d, b_mixB_d
            C.mixS_d, C.b_mixS_d, C.xs_pad, C.xo = mixS_d, b_mixS_d, xs_pad, xo
            C.y_out, C.ys_out = y_out, ys_out
            phase_c(C)
        for nm, src_d, bsrc in (("mixA", mixA_d, b_mixA_d), ("mixB", mixB_d, b_mixB_d)):
            if nm in dbg_out:
                dstg_b = sb("dstg_b" + nm, [128, HALF], BF16)
                dstg = sb("dstg" + nm, [128, HALF])
                b_db, b_df = Buf("dstg_b" + nm), Buf("dstg" + nm)
                for pp in range(4):
                    k.dma("sync", dstg_b[:], src_d[:, pp], reads=[bsrc], writes=[b_db])
                    k.op("vector", lambda e, dstg=dstg, dstg_b=dstg_b: e.tensor_copy(out=dstg[:], in_=dstg_b[:]), reads=[b_db], writes=[b_df])
                    final.append(k.dma("sync", dbg_out[nm][:, pp], dstg[:], reads=[b_df]))
        S.finish(final)
    return nc


def phase_b(C):
    nc, k, S = C.nc, C.k, C.S
    xTb, b_x, w_v = C.xTb, C.b_x, C.w_v
    psT, psB = C.psT, C.psB
    cst, b_cst = C.cst, C.b_cst
    ones_f = C.ones_f
    IDENT, NEGID, LMASK, CM0, CM1, NEGM, STRICT = range(7)
    final = C.final
    kT_d, vT_d, qT_d, gz_d = C.kT_d, C.vT_d, C.qT_d, C.gz_d

    with ExitStack() as sB:
        sbb = lambda n, s, dt=F32: sB.enter_context(nc.sbuf_tensor(n, list(s), dt))
        prm = sbb("prm_sb", [128, 8 + 128 + 48])
        b_prm = Buf("prm")
        k.dma("sync", prm[:], C.prm_d, writes=[b_prm])
        identb = sbb("identb", [128, 128], BF16)
        b_identb = Buf()
        k.op("vector", lambda e: e.tensor_copy(out=identb[:], in_=cst[:, IDENT, :]), reads=[b_cst], writes=[b_identb])
        convp_sb = sbb("convp_sb", [128, 12, 3])
        b_convp = Buf("convp")

        ab_sb = sbb("ab_sb", [128, 32, 8])
        b_ab = Buf()
        with ExitStack() as s1:
            sb1 = lambda n, s, dt=F32: s1.enter_context(nc.sbuf_tensor(n, list(s), dt))
            wab = sb1("wab", [128, 8, 8], BF16)
            wz = sb1("wz", [128, 8, 512], BF16)
            b_wab, b_wz = Buf("wab"), Buf("wz")
            k.gload(wab[:], w_v[:, :, COL_AB:COL_AB + 8], writes=[b_wab])
            k.gload(wz[:], w_v[:, :, COL_ZB:COL_ZB + 512], writes=[b_wz])
            zst = [sb1("zst%d" % i, [128, 512]) for i in range(2)]
            b_zst = [Buf() for _ in range(2)]
            gzb = [sb1("gzb%d" % i, [128, 512], BF16) for i in range(2)]
            b_gzb = [Buf("gzb%d" % i) for i in range(2)]
            for ti in range(32):
                pt_, pb_ = psT[ti % 2], psB[ti % 2]
                for kk in range(8):
                    fn = lambda e, kk=kk, ti=ti, pt_=pt_: e.matmul(pt_[:, 0:8], lhsT=xTb[:, kk, 128 * ti:128 * ti + 128],
                                                                 rhs=wab[:, kk, :], start=(kk == 0), stop=(kk == 7))
                    if kk == 0:
                        k.op("tensor", fn, reads=[b_wab, b_x[ti // 4]], writes=[pb_])
                    else:
                        k.acc("tensor", fn, reads=[b_wab, b_x[ti // 4]], acc=[pb_])
                k.op("vector", lambda e, ti=ti, pt_=pt_: e.tensor_copy(out=ab_sb[:, ti, :], in_=pt_[:, 0:8]), reads=[pb_], writes=[b_ab])
            for ti in range(16, 32):
                i2 = ti % 2
                pt_, pb_ = psT[2 + i2], psB[2 + i2]
                for kk in range(8):
                    fn = lambda e, kk=kk, ti=ti, pt_=pt_: e.matmul(pt_[:, :], lhsT=xTb[:, kk, 128 * ti:128 * ti + 128],
                                                                 rhs=wz[:, kk, :], start=(kk == 0), stop=(kk == 7))
                    if kk == 0:
                        k.op("tensor", fn, reads=[b_wz, b_x[ti // 4]], writes=[pb_])
                    else:
                        k.acc("tensor", fn, reads=[b_wz, b_x[ti // 4]], acc=[pb_])
                k.op("scalar", lambda e, i2=i2, pt_=pt_: e.activation(out=zst[i2][:], in_=pt_[:, :], func=AF.Silu), reads=[pb_], writes=[b_zst[i2]])
                k.op("gpsimd", lambda e, i2=i2: e.tensor_tensor(
                    out=gzb[i2][:].rearrange("p (h e) -> p h e", h=4), in0=zst[i2][:].rearrange("p (h e) -> p h e", h=4),
                    in1=prm[:, 8:136].unsqueeze(1).to_broadcast([128, 4, 128]), op=ALU.mult),
                    reads=[b_zst[i2], b_prm], writes=[b_gzb[i2]])
                k.dma("sync", gz_d[ti - 16], gzb[i2][:], reads=[b_gzb[i2]], writes=[C.b_gz_d])

        S.barrier()
        gt = sbb("gt", [128, 12, 128])
        b_gt = [Buf() for _ in range(12)]
        G, BETA, GC, EGC, ETAIL, BEGE, EGL0, EGL1, GCL, TMP, NEGA, TMP2 = range(12)
        v3 = lambda idx: gt[:, idx, :].rearrange("p (t h) -> p t h", h=4)
        k.op("scalar", lambda e: e.activation(out=gt[:, NEGA, 0:4], in_=prm[:, 0:4], func=AF.Exp), reads=[b_prm], writes=[b_gt[NEGA]])
        k.op("vector", lambda e: e.tensor_tensor(out=v3(TMP), in0=ab_sb[:, :, 0:4], in1=prm[:, 4:8].unsqueeze(1).to_broadcast([128, 32, 4]), op=ALU.add),
             reads=[b_ab, b_prm], writes=[b_gt[TMP]])
        k.op("scalar", lambda e: e.activation(out=gt[:, TMP, :], in_=gt[:, TMP, :], func=AF.Exp), reads=[b_gt[TMP]], writes=[b_gt[TMP]])
        k.op("scalar", lambda e: e.activation(out=gt[:, TMP, :], in_=gt[:, TMP, :], func=AF.Ln, bias=1.0), reads=[b_gt[TMP]], writes=[b_gt[TMP]])
        k.op("vector", lambda e: e.scalar_tensor_tensor(out=v3(G), in0=v3(TMP), scalar=-1.0, in1=gt[:, NEGA, 0:4].unsqueeze(1).to_broadcast([128, 32, 4]),
                                                       op0=ALU.mult, op1=ALU.mult), reads=[b_gt[TMP], b_gt[NEGA]], writes=[b_gt[G]])
        k.op("scalar", lambda e: e.activation(out=v3(BETA), in_=ab_sb[:, :, 4:8], func=AF.Sigmoid), reads=[b_ab], writes=[b_gt[BETA]])
        for (mask, dst, bank) in ((LMASK, GC, 0), (CM0, EGL0, 1), (CM1, EGL1, 2)):
            k.op("tensor", lambda e, mask=mask, bank=bank: e.matmul(psT[bank][:, 0:128], lhsT=cst[:, mask, :], rhs=gt[:, G, :], start=True, stop=True),
                 reads=[b_cst, b_gt[G]], writes=[psB[bank]])
            k.op("vector", lambda e, dst=dst, bank=bank: e.tensor_copy(out=gt[:, dst, :], in_=psT[bank][:, 0:128]), reads=[psB[bank]], writes=[b_gt[dst]])
        k.op("vector", lambda e: e.tensor_copy(out=gt[0:64, GCL, :], in_=gt[0:64, EGL0, :]), reads=[b_gt[EGL0]], writes=[b_gt[GCL]])
        k.op("vector", lambda e: e.tensor_copy(out=gt[64:128, GCL, :], in_=gt[64:128, EGL1, :]), reads=[b_gt[EGL1], b_gt[GCL]], writes=[b_gt[GCL]])
        k.op("vector", lambda e: e.tensor_tensor(out=gt[:, TMP2, :], in0=gt[:, GCL, :], in1=gt[:, GC, :], op=ALU.subtract),
             reads=[b_gt[GCL], b_gt[GC]], writes=[b_gt[TMP2]])
        k.op("scalar", lambda e: e.activation(out=gt[:, ETAIL, :], in_=gt[:, TMP2, :], func=AF.Exp), reads=[b_gt[TMP2]], writes=[b_gt[ETAIL]])
        k.op("scalar", lambda e: e.activation(out=gt[:, EGC, :], in_=gt[:, GC, :], func=AF.Exp), reads=[b_gt[GC]], writes=[b_gt[EGC]])
        k.op("scalar", lambda e: e.activation(out=gt[:, EGL0, :], in_=gt[:, EGL0, :], func=AF.Exp), reads=[b_gt[EGL0], b_gt[GCL]], writes=[b_gt[EGL0]])
        k.op("scalar", lambda e: e.activation(out=gt[:, EGL1, :], in_=gt[:, EGL1, :], func=AF.Exp), reads=[b_gt[EGL1], b_gt[GCL]], writes=[b_gt[EGL1]])
        k.op("vector", lambda e: e.tensor_tensor(out=gt[:, BEGE, :], in0=gt[:, BETA, :], in1=gt[:, EGC, :], op=ALU.mult),
             reads=[b_gt[BETA], b_gt[EGC]], writes=[b_gt[BEGE]])

        with ExitStack() as s2:
            sb2 = lambda n, s, dt=F32: s2.enter_context(nc.sbuf_tensor(n, list(s), dt))
            wu = [sb2("wu%d" % i, [128, 8, 128], BF16) for i in range(2)]
            b_wu = [Buf("wu%d" % i) for i in range(2)]
            ub = [sb2("ub%d" % i, [128, 515]) for i in range(2)]
            b_ub = [Buf() for _ in range(2)]
            cb = [sb2("cb%d" % i, [128, 512]) for i in range(2)]
            b_cb = [Buf() for _ in range(2)]
            sq = [sb2("sq%d" % i, [128, 512]) for i in range(2)]
            b_sq = [Buf() for _ in range(2)]
            rt = [sb2("rt%d" % i, [128, 512]) for i in range(2)]
            b_rt = [Buf() for _ in range(2)]
            ob = [sb2("ob%d" % i, [128, 512], BF16) for i in range(2)]
            b_ob = [Buf("ob%d" % i) for i in range(2)]
            cnt = 0
            for th in range(12):
                ty, hb = divmod(th, 4)
                wi = th % 2
                c0 = COL_UB + 128 * th
                k.gload(wu[wi][:], w_v[:, :, c0:c0 + 128], writes=[b_wu[wi]])
                chunks = range(4, 8) if ty == 0 else range(8)
                dst_d = (qT_d, kT_d, vT_d)[ty]
                b_dst = (C.b_qT_d, C.b_kT_d, C.b_vT_d)[ty]
                cw = lambda i, th=th: prm[:, 136 + 4 * th + i:136 + 4 * th + i + 1]
                first = True
                for tc in chunks:
                    ci = cnt % 2
                    cnt += 1
                    pt_, pb_ = psT[ci], psB[ci]
                    if first:
                        if ty == 0:
                            hp, hpb = psT[2], psB[2]
                            for kk in range(8):
                                fn = lambda e, kk=kk, wi=wi, hp=hp: e.matmul(hp[:, 0:4], lhsT=wu[wi][:, kk, :], rhs=xTb[:, kk, HALF - 4:HALF],
                                                                            start=(kk == 0), stop=(kk == 7))
                                if kk == 0:
                                    k.op("tensor", fn, reads=[b_wu[wi], b_x[3]], writes=[hpb])
                                else:
                                    k.acc("tensor", fn, reads=[b_wu[wi], b_x[3]], acc=[hpb])
                            k.op("vector", lambda e, ci=ci, hp=hp: e.tensor_copy(out=ub[ci][:, 0:3], in_=hp[:, 1:4]), reads=[hpb], writes=[b_ub[ci]])
                        else:
                            k.op("vector", lambda e, ci=ci: e.memset(ub[ci][:, 0:3], 0.0), writes=[b_ub[ci]])
                        first = False
                    else:
                        k.op("vector", lambda e, ci=ci: e.tensor_copy(out=ub[ci][:, 0:3], in_=ub[1 - ci][:, 512:515]),
                             reads=[b_ub[1 - ci]], writes=[b_ub[ci]])
                    for kk in range(8):
                        fn = lambda e, kk=kk, wi=wi, tc=tc, pt_=pt_: e.matmul(pt_[:, :], lhsT=wu[wi][:, kk, :], rhs=xTb[:, kk, 512 * tc:512 * tc + 512],
                                                                            start=(kk == 0), stop=(kk == 7))
                        if kk == 0:
                            k.op("tensor", fn, reads=[b_wu[wi], b_x[tc]], writes=[pb_])
                        else:
                            k.acc("tensor", fn, reads=[b_wu[wi], b_x[tc]], acc=[pb_])
                    k.op("scalar", lambda e, ci=ci, pt_=pt_: e.activation(out=ub[ci][:, 3:515], in_=pt_[:, :], func=AF.Copy), reads=[pb_], writes=[b_ub[ci]])
                    if tc == 7:
                        k.op("gpsimd", lambda e, ci=ci, th=th: e.tensor_copy(out=convp_sb[:, th, :], in_=ub[ci][:, 512:515]), reads=[b_ub[ci]], writes=[b_convp])
                    k.op("vector", lambda e, ci=ci, cw=cw: e.tensor_scalar(out=cb[ci][:], in0=ub[ci][:, 3:515], scalar1=cw(3), scalar2=None, op0=ALU.mult),
                         reads=[b_ub[ci], b_prm], writes=[b_cb[ci]])
                    for i in range(3):
                        k.op("vector", lambda e, ci=ci, cw=cw, i=i: e.scalar_tensor_tensor(out=cb[ci][:], in0=ub[ci][:, i:i + 512], scalar=cw(i), in1=cb[ci][:],
                                                                                       op0=ALU.mult, op1=ALU.add), reads=[b_ub[ci], b_cb[ci]], writes=[b_cb[ci]])
                    k.op("scalar", lambda e, ci=ci: e.activation(out=cb[ci][:], in_=cb[ci][:], func=AF.Silu), reads=[b_cb[ci]], writes=[b_cb[ci]])
                    if ty == 2:
                        k.op("gpsimd", lambda e, ci=ci: e.tensor_copy(out=ob[ci][:], in_=cb[ci][:]), reads=[b_cb[ci]], writes=[b_ob[ci]])
                    else:
                        k.op("gpsimd", lambda e, ci=ci: e.tensor_tensor(out=sq[ci][:], in0=cb[ci][:], in1=cb[ci][:], op=ALU.mult), reads=[b_cb[ci]], writes=[b_sq[ci]])
                        np_, npb = psT[4 + ci], psB[4 + ci]
                        k.op("tensor", lambda e, ci=ci, np_=np_: e.matmul(np_[:, :], lhsT=ones_f[:], rhs=sq[ci][:], start=True, stop=True),
                             reads=[b_sq[ci], C.b_ones], writes=[npb])
                        k.op("scalar", lambda e, ci=ci, np_=np_: e.activation(out=rt[ci][:], in_=np_[:, :], func=AF.Sqrt, bias=C.eps6[:, 0:1]),
                             reads=[npb, C.b_eps], writes=[b_rt[ci]])
                        k.op("vector", lambda e, ci=ci: e.reciprocal(out=rt[ci][:], in_=rt[ci][:]), reads=[b_rt[ci]], writes=[b_rt[ci]])
                        sc = (128.0 ** -0.5) if ty == 0 else 1.0
                        k.op("vector", lambda e, ci=ci, sc=sc: e.scalar_tensor_tensor(out=ob[ci][:], in0=cb[ci][:], scalar=sc, in1=rt[ci][:], op0=ALU.mult, op1=ALU.mult),
                             reads=[b_cb[ci], b_rt[ci]], writes=[b_ob[ci]])
                    t0 = 512 * tc - (HALF if ty == 0 else 0)
                    k.dma("sync", dst_d[:, hb, t0:t0 + 512], ob[ci][:], reads=[b_ob[ci]], writes=[b_dst])
            convp_v = C.conv_p.rearrange("r (c p) -> p c r", p=128)
            for th in range(12):
                final.append(k.dma("sync", convp_v[:, th, :], convp_sb[:, th, :], reads=[b_convp], slot="convp", allow_slow_non_contiguous=True))

        S.barrier()
        sC = sB
        sbc = lambda n, s, dt=F32: sC.enter_context(nc.sbuf_tensor(n, list(s), dt))
        f_slots = [(psT[b][:, 0:128], psB[b]) for b in range(6)]
        psbf = [psT[6].bitcast(BF16), psT[7].bitcast(BF16)]
        h_slots = [(psbf[b][:, 0:128], psB[6 + b]) for b in range(2)]
        cnts = {"f": 0, "h": 0}

        def fslot():
            s_ = f_slots[cnts["f"] % len(f_slots)]
            cnts["f"] += 1
            return s_

        def hslot():
            s_ = h_slots[cnts["h"] % len(h_slots)]
            cnts["h"] += 1
            return s_

        class Pool_:
            def __init__(self, name, n, dt):
                self.t = [sbc("%s%d" % (name, i), [128, 128], dt) for i in range(n)]
                self.b = [Buf() for _ in range(n)]
                self.i = 0

            def get(self):
                j = self.i % len(self.t)
                self.i += 1
                return self.t[j], self.b[j]

        PF = Pool_("pf", 24, F32)
        PH = Pool_("ph", 48, BF16)
        Sst = [sbc("Sst%d" % h, [128, 128]) for h in range(4)]
        Sbf = [sbc("Sbf%d" % h, [128, 128], BF16) for h in range(4)]
        b_S = [Buf() for _ in range(4)]
        b_Sb = [Buf() for _ in range(4)]
        for h in range(4):
            k.op("vector", lambda e, h=h: e.memset(Sst[h][:], 0.0), writes=[b_S[h]])
            k.op("vector", lambda e, h=h: e.memset(Sbf[h][:], 0.0), writes=[b_Sb[h]])
        kt_t = [sbc("kt_t%d" % i, [128, 4, 128], BF16) for i in range(2)]
        vt_t = [sbc("vt_t%d" % i, [128, 4, 128], BF16) for i in range(2)]
        qt_t = [sbc("qt_t%d" % i, [128, 4, 128], BF16) for i in range(2)]
        gz_t = [sbc("gz_t%d" % i, [128, 512], BF16) for i in range(2)]
        b_kt = [Buf("kt_t%d" % i) for i in range(2)]
        b_vt = [Buf("vt_t%d" % i) for i in range(2)]
        b_qt = [Buf("qt_t%d" % i) for i in range(2)]
        b_gzt = [Buf("gz_t%d" % i) for i in range(2)]
        Ukeep = [[sbc("Uk%d_%d" % (i, h), [128, 128]) for h in range(4)] for i in range(2)]
        WTkeep = [[sbc("WTk%d_%d" % (i, h), [128, 128], BF16) for h in range(4)] for i in range(2)]
        ktlkeep = [[sbc("ktlk%d_%d" % (i, h), [128, 128], BF16) for h in range(4)] for i in range(2)]
        qkTkeep = [[sbc("qkTk%d_%d" % (i, h), [128, 128], BF16) for h in range(4)] for i in range(2)]
        b_Uk = [[Buf() for h in range(4)] for i in range(2)]
        b_WTk = [[Buf() for h in range(4)] for i in range(2)]
        b_ktlk = [[Buf() for h in range(4)] for i in range(2)]
        b_qkTk = [[Buf() for h in range(4)] for i in range(2)]
        ss = sbc("ss", [128, 8])
        b_ss = [Buf() for _ in range(8)]
        junk = sbc("junk", [128, 128])
        b_junk = Buf()
        mxs = [sbc("mxs%d" % i, [128, 4, 128], BF16) for i in range(2)]
        b_mxs = [Buf("mxs%d" % i) for i in range(2)]
        gt_ = gt
        col = lambda idx, c_: gt_[:, idx, c_:c_ + 1]

        for ti in range(32):
            own = ti >= 16
            bi = ti % 2
            k.dma("sync", kt_t[bi][:], kT_d[:, :, 128 * ti:128 * ti + 128], reads=[C.b_kT_d], writes=[b_kt[bi]])
            k.dma("sync", vt_t[bi][:], vT_d[:, :, 128 * ti:128 * ti + 128], reads=[C.b_vT_d], writes=[b_vt[bi]])
            if own:
                k.dma("sync", qt_t[bi][:], qT_d[:, :, 128 * (ti - 16):128 * (ti - 16) + 128], reads=[C.b_qT_d], writes=[b_qt[bi]])
                k.dma("sync", gz_t[bi][:], gz_d[ti - 16], reads=[C.b_gz_d], writes=[b_gzt[bi]])
            HS = []
            for hb in range(4):
                c_ = ti * 4 + hb
                kT = kt_t[bi][:, hb, :]
                vT = vt_t[bi][:, hb, :]
                qT = qt_t[bi][:, hb, :]
                rd_k, rd_v, rd_q = [b_kt[bi]], [b_vt[bi]], [b_qt[bi]]
                nd, b_nd = PF.get()
                k.op("gpsimd", lambda e, nd=nd, c_=c_: e.tensor_scalar(out=nd[:], in0=cst[:, NEGID, :], scalar1=col(GC, c_), scalar2=None, op0=ALU.mult),
                     reads=[b_cst, b_gt[GC]], writes=[b_nd])
                pD, bD = fslot()
                k.op("tensor", lambda e, pD=pD, nd=nd: e.matmul(pD, lhsT=ones_f[:], rhs=nd[:], start=True, stop=False), reads=[b_nd, C.b_ones], writes=[bD])
                k.acc("tensor", lambda e, pD=pD: e.matmul(pD, lhsT=cst[:, IDENT, :], rhs=cst[:, NEGM, :], start=False, stop=True), reads=[b_cst], acc=[bD])
                Ec, b_Ec = PF.get()
                k.op("scalar", lambda e, Ec=Ec, pD=pD, c_=c_: e.activation(out=Ec[:], in_=pD, func=AF.Exp, bias=col(GC, c_)),
                     reads=[bD, b_gt[GC]], writes=[b_Ec])
                Es, b_Es = PF.get()
                k.op("gpsimd", lambda e, Es=Es, Ec=Ec: e.tensor_tensor(out=Es[:], in0=Ec[:], in1=cst[:, STRICT, :], op=ALU.mult),
                     reads=[b_Ec, b_cst], writes=[b_Es])
                pK, bK = fslot()
                k.op("tensor", lambda e, pK=pK, kT=kT: e.matmul(pK, lhsT=kT, rhs=kT, start=True, stop=True), reads=rd_k, writes=[bK])
                A, b_A = PH.get()
                k.op("vector", lambda e, A=A, pK=pK, Es=Es, c_=c_: e.scalar_tensor_tensor(out=A[:], in0=pK, scalar=col(BETA, c_), in1=Es[:], op0=ALU.mult, op1=ALU.mult),
                     reads=[bK, b_Es, b_gt[BETA]], writes=[b_A])
                pT_, bT_ = hslot()
                k.op("tensor", lambda e, pT_=pT_, A=A: e.transpose(pT_, A[:], identb[:]), reads=[b_A, b_identb], writes=[bT_])
                Bm, b_Bm = PH.get()
                k.op("vector", lambda e, Bm=Bm, pT_=pT_: e.tensor_copy(out=Bm[:], in_=pT_), reads=[bT_], writes=[b_Bm])
                P, b_P = PH.get()
                k.op("vector", lambda e, P=P, pT_=pT_: e.tensor_tensor(out=P[:], in0=cst[:, IDENT, :], in1=pT_, op=ALU.subtract), reads=[bT_, b_cst], writes=[b_P])
                X, b_X, Y, b_Y = A, b_A, Bm, b_Bm
                for m in range(1, 6):
                    pX, bX = fslot()
                    k.op("tensor", lambda e, pX=pX, X=X, Y=Y: e.matmul(pX, lhsT=Y[:], rhs=X[:], start=True, stop=True), reads=[b_X, b_Y], writes=[bX])
                    if m < 5:
                        pY, bY = fslot()
                        k.op("tensor", lambda e, pY=pY, X=X, Y=Y: e.matmul(pY, lhsT=X[:], rhs=Y[:], start=True, stop=True), reads=[b_X, b_Y], writes=[bY])
                    Xn, b_Xn = PH.get()
                    k.op("vector", lambda e, Xn=Xn, pX=pX: e.tensor_copy(out=Xn[:], in_=pX), reads=[bX], writes=[b_Xn])
                    if m < 5:
                        Yn, b_Yn = PH.get()
                        k.op("vector", lambda e, Yn=Yn, pY=pY: e.tensor_copy(out=Yn[:], in_=pY), reads=[bY], writes=[b_Yn])
                    pP, bP = fslot()
                    k.op("tensor", lambda e, pP=pP, Xn=Xn, P=P: e.matmul(pP, lhsT=Xn[:], rhs=P[:], start=True, stop=True), reads=[b_Xn, b_P], writes=[bP])
                    Pn, b_Pn = PH.get()
                    k.op("vector", lambda e, Pn=Pn, pP=pP, P=P: e.tensor_tensor(out=Pn[:], in0=pP, in1=P[:], op=ALU.add), reads=[bP, b_P], writes=[b_Pn])
                    P, b_P = Pn, b_Pn
                    X, b_X = Xn, b_Xn
                    if m < 5:
                        Y, b_Y = Yn, b_Yn
                pk_, bk_ = hslot()
                k.op("tensor", lambda e, pk_=pk_, kT=kT: e.transpose(pk_, kT, identb[:]), reads=rd_k + [b_identb], writes=[bk_])
                pv_, bv_ = hslot()
                k.op("tensor", lambda e, pv_=pv_, vT=vT: e.transpose(pv_, vT, identb[:]), reads=rd_v + [b_identb], writes=[bv_])
                Ru, b_Ru = PH.get()
                k.op("vector", lambda e, Ru=Ru, pv_=pv_, c_=c_: e.tensor_scalar(out=Ru[:], in0=pv_, scalar1=col(BETA, c_), scalar2=None, op0=ALU.mult),
                     reads=[bv_, b_gt[BETA]], writes=[b_Ru])
                Rw, b_Rw = PH.get()
                k.op("vector", lambda e, Rw=Rw, pk_=pk_, c_=c_: e.tensor_scalar(out=Rw[:], in0=pk_, scalar1=col(BEGE, c_), scalar2=None, op0=ALU.mult),
                     reads=[bk_, b_gt[BEGE]], writes=[b_Rw])
                ktl, b_ktl = ktlkeep[bi][hb], b_ktlk[bi][hb]
                k.op("vector", lambda e, ktl=ktl, pk_=pk_, c_=c_: e.tensor_scalar(out=ktl[:], in0=pk_, scalar1=col(ETAIL, c_), scalar2=None, op0=ALU.mult),
                     reads=[bk_, b_gt[ETAIL]], writes=[b_ktl])
                pU, bU = fslot()
                k.op("tensor", lambda e, pU=pU, P=P, Ru=Ru: e.matmul(pU, lhsT=P[:], rhs=Ru[:], start=True, stop=True), reads=[b_P, b_Ru], writes=[bU])
                U, b_U = Ukeep[bi][hb], b_Uk[bi][hb]
                k.op("vector", lambda e, U=U, pU=pU: e.tensor_copy(out=U[:], in_=pU), reads=[bU], writes=[b_U])
                pW, bW = fslot()
                k.op("tensor", lambda e, pW=pW, P=P, Rw=Rw: e.matmul(pW, lhsT=Rw[:], rhs=P[:], start=True, stop=True), reads=[b_P, b_Rw], writes=[bW])
                WT, b_WT = WTkeep[bi][hb], b_WTk[bi][hb]
                k.op("vector", lambda e, WT=WT, pW=pW: e.tensor_copy(out=WT[:], in_=pW), reads=[bW], writes=[b_WT])
                qkT = b_qkT = None
                if own:
                    pQ, bQ = fslot()
                    k.op("tensor", lambda e, pQ=pQ, qT=qT, kT=kT: e.matmul(pQ, lhsT=qT, rhs=kT, start=True, stop=True), reads=rd_q + rd_k, writes=[bQ])
                    qk, b_qk = PH.get()
                    k.op("vector", lambda e, qk=qk, pQ=pQ, Ec=Ec: e.tensor_tensor(out=qk[:], in0=pQ, in1=Ec[:], op=ALU.mult), reads=[bQ, b_Ec], writes=[b_qk])
                    pq2, bq2 = hslot()
                    k.op("tensor", lambda e, pq2=pq2, qk=qk: e.transpose(pq2, qk[:], identb[:]), reads=[b_qk, b_identb], writes=[bq2])
                    qkT, b_qkT = qkTkeep[bi][hb], b_qkTk[bi][hb]
                    k.op("vector", lambda e, qkT=qkT, pq2=pq2: e.tensor_copy(out=qkT[:], in_=pq2), reads=[bq2], writes=[b_qkT])
                HS.append(dict(U=U, b_U=b_U, WT=WT, b_WT=b_WT, ktl=ktl, b_ktl=b_ktl, qkT=qkT, b_qkT=b_qkT, qT=qT, rd_q=rd_q, c_=c_))
            outs = []
            if own:
                for hb in range(4):
                    o_, b_o = PF.get()
                    outs.append((o_, b_o))
            for ch in range(2):
                r0 = 64 * ch
                for hb in range(4):
                    H = HS[hb]
                    c_ = H["c_"]
                    pV, bV = fslot()
                    k.op("tensor", lambda e, pV=pV, H=H, hb=hb, r0=r0: e.matmul(pV[r0:r0 + 64, :], lhsT=H["WT"][:, r0:r0 + 64], rhs=Sbf[hb][:], start=True, stop=True),
                         reads=[H["b_WT"], b_Sb[hb]], writes=[bV])
                    vn, b_vn = PH.get()
                    k.op("vector", lambda e, vn=vn, pV=pV, H=H, r0=r0: e.tensor_tensor(out=vn[r0:r0 + 64, :], in0=H["U"][r0:r0 + 64, :], in1=pV[r0:r0 + 64, :], op=ALU.subtract),
                         reads=[bV, H["b_U"]], writes=[b_vn])
                    if own:
                        o_, b_o = outs[hb]
                        p1, b1 = fslot()
                        k.op("tensor", lambda e, p1=p1, H=H, hb=hb, r0=r0: e.matmul(p1[r0:r0 + 64, :], lhsT=H["qT"][:, r0:r0 + 64], rhs=Sbf[hb][:], start=True, stop=True),
                             reads=H["rd_q"] + [b_Sb[hb]], writes=[b1])
                        p2, b2 = fslot()
                        k.op("tensor", lambda e, p2=p2, H=H, vn=vn, r0=r0: e.matmul(p2[r0:r0 + 64, :], lhsT=H["qkT"][r0:r0 + 64, r0:r0 + 64], rhs=vn[r0:r0 + 64, :], start=True, stop=True),
                             reads=[H["b_qkT"], b_vn], writes=[b2])
                        o2, b_o2 = PF.get()
                        k.op("vector", lambda e, o2=o2, p2=p2, r0=r0: e.tensor_copy(out=o2[r0:r0 + 64, :], in_=p2[r0:r0 + 64, :]), reads=[b2], writes=[b_o2])
                        k.op("vector", lambda e, o_=o_, p1=p1, o2=o2, r0=r0, c_=c_: e.scalar_tensor_tensor(
                            out=o_[r0:r0 + 64, :], in0=p1[r0:r0 + 64, :], scalar=gt_[r0:r0 + 64, EGC, c_:c_ + 1], in1=o2[r0:r0 + 64, :], op0=ALU.mult, op1=ALU.add),
                            reads=[b1, b_o2, b_gt[EGC]], writes=[b_o])
                    pS, bS_ = fslot()
                    k.op("tensor", lambda e, pS=pS, H=H, vn=vn, r0=r0: e.matmul(pS, lhsT=H["ktl"][r0:r0 + 64, :], rhs=vn[r0:r0 + 64, :], start=True, stop=True),
                         reads=[H["b_ktl"], b_vn], writes=[bS_])
                    egl = EGL0 if ch == 0 else EGL1
                    k.op("vector", lambda e, pS=pS, hb=hb, egl=egl, c_=c_: e.scalar_tensor_tensor(
                        out=Sst[hb][:], in0=Sst[hb][:], scalar=col(egl, c_), in1=pS, op0=ALU.mult, op1=ALU.add),
                        reads=[bS_, b_gt[egl], b_S[hb]], writes=[b_S[hb]])
                    k.op("scalar", lambda e, hb=hb: e.activation(out=Sbf[hb][:], in_=Sst[hb][:], func=AF.Copy), reads=[b_S[hb]], writes=[b_Sb[hb]])
            if own:
                for hb in range(4):
                    o_, b_o = outs[hb]
                    si = (ti * 4 + hb) % 8
                    k.op("scalar", lambda e, o_=o_, si=si: e.activation(out=junk[:], in_=o_[:], func=AF.Square, accum_out=ss[:, si:si + 1]),
                         reads=[b_o], writes=[b_junk, b_ss[si]])
                    k.op("scalar", lambda e, si=si: e.activation(out=ss[:, si:si + 1], in_=ss[:, si:si + 1], func=AF.Sqrt, scale=1.0 / 128.0, bias=C.eps6[:, 0:1]),
                         reads=[b_ss[si], C.b_eps], writes=[b_ss[si]])
                    k.op("vector", lambda e, si=si: e.reciprocal(out=ss[:, si:si + 1], in_=ss[:, si:si + 1]), reads=[b_ss[si]], writes=[b_ss[si]])
                    on, b_on = PH.get()
                    k.op("vector", lambda e, on=on, o_=o_, si=si, hb=hb, bi=bi: e.scalar_tensor_tensor(
                        out=on[:], in0=o_[:], scalar=ss[:, si:si + 1], in1=gz_t[bi][:, 128 * hb:128 * hb + 128], op0=ALU.mult, op1=ALU.mult),
                        reads=[b_o, b_ss[si], b_gzt[bi]], writes=[b_on])
                    pm, bm = hslot()
                    k.op("tensor", lambda e, pm=pm, on=on: e.transpose(pm, on[:], identb[:]), reads=[b_on, b_identb], writes=[bm])
                    k.op("vector", lambda e, pm=pm, hb=hb, bi=bi: e.tensor_copy(out=mxs[bi][:, hb, :], in_=pm),
                         reads=[bm], writes=[b_mxs[bi]])
                k.dma("sync", C.mixB_d[:, :, 128 * (ti - 16):128 * (ti - 16) + 128], mxs[bi][:], reads=[b_mxs[bi]], writes=[C.b_mixB_d])
        for hb in range(4):
            final.append(k.dma("sync", C.ssm_p[hb], Sst[hb][:], reads=[b_S[hb]], slot="ssmp"))

def phase_c(C):
    nc, k, S = C.nc, C.k, C.S
    psT, psB = C.psT, C.psB
    cst, b_cst = C.cst, C.b_cst
    IDENT = 0
    final = C.final
    NT = C.NT
    NTOK = NT * 128
    ALPHA = 2.0 ** 0.25
    KC = int(os.environ.get('KC', '99'))

    with ExitStack() as sC:
        sbc = lambda n, s, dt=F32: sC.enter_context(nc.sbuf_tensor(n, list(s), dt))
        hTb = sbc("hTb", [128, 8, NTOK], BF16)
        b_hT = [Buf() for _ in range(NT)]
        yacc = sbc("yacc", [128, NT, 1024])
        b_y = [Buf() for _ in range(NT)]
        G = sbc("G", [128, NT, 32])
        b_G = [Buf() for _ in range(NT)]
        st6_c3 = sbc("st6b", [128, 2, 2, 6])
        mv_c3 = sbc("mvb", [128, 2, 2])
        lnp = sbc("lnp_sb", [128, 2, 1024])
        b_lnp = Buf("lnp")

        with ExitStack() as s1:
            sb1 = lambda n, s, dt=F32: s1.enter_context(nc.sbuf_tensor(n, list(s), dt))
            k.dma("sync", lnp[:], C.lnp_d[:, 0:2, :], writes=[b_lnp])
            wo = sb1("wo", [128, 8, 1024], BF16)
            b_wo = Buf("wo")
            k.gload(wo[:], C.w_out.rearrange("(k p) c -> p k c", p=128), writes=[b_wo])
            wr = sb1("wr_sb", [128, 8, 36])
            b_wr = Buf("wr")
            k.dma("sync", wr[:], C.wr_d.rearrange("(k p) c -> p k c", p=128), writes=[b_wr])
            rb = sb1("rb_sb", [128, 36])
            b_rb = Buf("rb")
            k.dma("sync", rb[:], C.rb_d, writes=[b_rb])
            mx = [sb1("mx%d" % i, [128, 8, 128], BF16) for i in range(2)]
            b_mx = [Buf("mx%d" % i) for i in range(2)]
            xt = [sb1("xt%d" % i, [128, 1024]) for i in range(2)]
            b_xt = [Buf("xt%d" % i) for i in range(2)]
            rr = [sb1("rr%d" % i, [128, 1024]) for i in range(2)]
            b_rr = [Buf() for _ in range(2)]
            hh = [sb1("hh%d" % i, [128, 1024]) for i in range(2)]
            b_hh = [Buf() for _ in range(2)]
            hTf = [sb1("hTf%d" % i, [128, 8, 128]) for i in range(2)]
            b_hTf = [Buf() for _ in range(2)]
            st6 = sb1("st6", [128, 2, 2, 6])
            mv = sb1("mv", [128, 2, 2])
            b_st = [Buf() for _ in range(2)]
            b_mv = [Buf() for _ in range(2)]
            tmpr = sb1("tmpr", [128, 2, 32])
            b_tmp = [Buf() for _ in range(2)]
            sm = sb1("sm", [128, 2, 96])
            b_sm = [Buf() for _ in range(2)]
            for ti in range(NT if KC >= 6 else 0):
                bi = ti % 2
                is_s = ti >= 16
                if not is_s:
                    k.dma("sync", mx[bi][:, 0:4, :], C.mixA_d[:, :, 128 * ti:128 * ti + 128], reads=[C.b_mixA_d], writes=[b_mx[bi]])
                    k.dma("sync", mx[bi][:, 4:8, :], C.mixB_d[:, :, 128 * ti:128 * ti + 128], reads=[C.b_mixB_d], writes=[b_mx[bi]])
                    k.dma("sync", xt[bi][:], C.xo[128 * ti:128 * ti + 128, :], writes=[b_xt[bi]])
                else:
                    k.dma("sync", mx[bi][:], C.mixS_d, reads=[C.b_mixS_d], writes=[b_mx[bi]])
                    k.dma("sync", xt[bi][:], C.xs_pad, writes=[b_xt[bi]])
                for half in range(2 if KC >= 7 else 0):
                    pt_, pb_ = psT[half], psB[half]
                    for kk in range(8):
                        fn = lambda e, kk=kk, bi=bi, half=half, pt_=pt_: e.matmul(pt_[:, :], lhsT=mx[bi][:, kk, :], rhs=wo[:, kk, 512 * half:512 * half + 512],
                                                                                start=(kk == 0), stop=(kk == 7))
                        if kk == 0:
                            k.op("tensor", fn, reads=[b_mx[bi], b_wo], writes=[pb_])
                        else:
                            k.acc("tensor", fn, reads=[b_mx[bi], b_wo], acc=[pb_])
                    if KC < 8:
                        continue
                    k.op("vector", lambda e, bi=bi, half=half, pt_=pt_: e.scalar_tensor_tensor(
                        out=rr[bi][:, 512 * half:512 * half + 512], in0=xt[bi][:, 512 * half:512 * half + 512], scalar=ALPHA, in1=pt_[:, :],
                        op0=ALU.mult, op1=ALU.add), reads=[pb_, b_xt[bi]], writes=[b_rr[bi]])
                    k.op("vector", lambda e, bi=bi, half=half: e.bn_stats(out=st6[:, bi, half, :], in_=rr[bi][:, 512 * half:512 * half + 512]),
                         reads=[b_rr[bi]], writes=[b_st[bi]])
                if KC < 11:
                    continue
                k.op("vector", lambda e, bi=bi: e.bn_aggr(out=mv[:, bi, :], in_=st6[:, bi, :, :].rearrange("p a b -> p (a b)")), reads=[b_st[bi]], writes=[b_mv[bi]])
                k.op("scalar", lambda e, bi=bi: e.activation(out=mv[:, bi, 1:2], in_=mv[:, bi, 1:2], func=AF.Sqrt, bias=C.eps5[:, 0:1]), reads=[b_mv[bi], C.b_eps5], writes=[b_mv[bi]])
                k.op("vector", lambda e, bi=bi: e.reciprocal(out=mv[:, bi, 1:2], in_=mv[:, bi, 1:2]), reads=[b_mv[bi]], writes=[b_mv[bi]])
                k.op("vector", lambda e, bi=bi: e.tensor_scalar(out=hh[bi][:], in0=rr[bi][:], scalar1=mv[:, bi, 0:1], scalar2=mv[:, bi, 1:2], op0=ALU.subtract, op1=ALU.mult),
                     reads=[b_rr[bi], b_mv[bi]], writes=[b_hh[bi]])
                k.op("gpsimd", lambda e, bi=bi: e.tensor_tensor(out=hh[bi][:], in0=hh[bi][:], in1=lnp[:, 0, :], op=ALU.mult), reads=[b_hh[bi], b_lnp], writes=[b_hh[bi]])
                k.op("gpsimd", lambda e, bi=bi: e.tensor_tensor(out=hh[bi][:], in0=hh[bi][:], in1=lnp[:, 1, :], op=ALU.add), reads=[b_hh[bi], b_lnp], writes=[b_hh[bi]])
                k.op("gpsimd", lambda e, bi=bi, ti=ti: e.tensor_scalar(out=yacc[:, ti, :], in0=hh[bi][:], scalar1=ALPHA, scalar2=None, op0=ALU.mult),
                     reads=[b_hh[bi]], writes=[b_y[ti]])
                if KC < 12:
                    continue
                for g4 in range(2):
                    pt_, pb_ = psT[2 + g4], psB[2 + g4]
                    for j in range(4):
                        kk = 4 * g4 + j
                        fn = lambda e, kk=kk, j=j, bi=bi, pt_=pt_: e.transpose(pt_[:, 128 * j:128 * j + 128], hh[bi][:, 128 * kk:128 * kk + 128], cst[:, IDENT, :])
                        if j == 0:
                            k.op("tensor", fn, reads=[b_hh[bi], b_cst], writes=[pb_])
                        else:
                            k.acc("tensor", fn, reads=[b_hh[bi], b_cst], acc=[pb_])
                    k.op("vector", lambda e, g4=g4, bi=bi, pt_=pt_: e.tensor_copy(out=hTf[bi][:, 4 * g4:4 * g4 + 4, :], in_=pt_[:, :].rearrange("p (a c) -> p a c", a=4)),
                         reads=[pb_], writes=[b_hTf[bi]])
                    k.op("vector", lambda e, g4=g4, ti=ti, pt_=pt_: e.tensor_copy(out=hTb[:, 4 * g4:4 * g4 + 4, 128 * ti:128 * ti + 128], in_=pt_[:, :].rearrange("p (a c) -> p a c", a=4)),
                         reads=[pb_], writes=[b_hT[ti]])
                if KC < 13:
                    continue
                pl, plb = psT[4], psB[4]
                for kk in range(8):
                    fn = lambda e, kk=kk, bi=bi: e.matmul(pl[:, 0:36], lhsT=hTf[bi][:, kk, :], rhs=wr[:, kk, :], start=(kk == 0), stop=(kk == 7))
                    if kk == 0:
                        k.op("tensor", fn, reads=[b_hTf[bi], b_wr], writes=[plb])
                    else:
                        k.acc("tensor", fn, reads=[b_hTf[bi], b_wr], acc=[plb])
                if KC < 14:
                    continue
                R = lambda a, b_, bi=bi: sm[:, bi, a:b_]
                T3 = tmpr[:, bi, :].rearrange("p (g e) -> p g e", g=4)
                sm_b, tmp_b = b_sm[bi], b_tmp[bi]

                def VV(eng, method, reads, writes, **aps):
                    k.op(eng, lambda e, aps=aps, method=method: getattr(e, method)(**aps), reads=reads, writes=writes)
                VV("vector", "tensor_tensor", [plb, b_rb], [sm_b], out=R(0, 36), in0=pl[:, 0:36], in1=rb[:], op=ALU.add)
                VV("vector", "tensor_reduce", [sm_b], [sm_b], out=R(36, 37), in_=R(0, 4), axis=AX.X, op=ALU.max)
                VV("vector", "tensor_scalar", [sm_b], [sm_b], out=R(40, 44), in0=R(0, 4), scalar1=R(36, 37), scalar2=None, op0=ALU.is_equal)
                VV("vector", "tensor_scalar", [sm_b], [sm_b], out=R(37, 38), in0=R(36, 37), scalar1=-1.0, scalar2=None, op0=ALU.mult)
                VV("scalar", "activation", [sm_b], [sm_b], out=R(89, 93), in_=R(0, 4), func=AF.Exp, bias=R(37, 38), accum_out=R(38, 39))
                VV("vector", "reciprocal", [sm_b], [sm_b], out=R(38, 39), in_=R(38, 39))
                VV("vector", "tensor_tensor", [sm_b], [tmp_b], out=T3, in0=R(4, 36).rearrange("p (g e) -> p g e", g=4),
                   in1=R(40, 44).unsqueeze(2).to_broadcast([128, 4, 8]), op=ALU.mult)
                VV("vector", "tensor_reduce", [tmp_b], [sm_b], out=R(44, 52), in_=T3.rearrange("p g e -> p e g"), axis=AX.X, op=ALU.add)
                VV("vector", "tensor_reduce", [sm_b], [sm_b], out=R(52, 53), in_=R(44, 52), axis=AX.X, op=ALU.max)
                VV("vector", "tensor_scalar", [sm_b], [sm_b], out=R(54, 62), in0=R(44, 52), scalar1=R(52, 53), scalar2=None, op0=ALU.is_equal)
                VV("vector", "scalar_tensor_tensor", [sm_b], [sm_b], out=R(62, 70), in0=R(54, 62), scalar=-1e30, in1=R(44, 52), op0=ALU.mult, op1=ALU.add)
                VV("vector", "tensor_reduce", [sm_b], [sm_b], out=R(53, 54), in_=R(62, 70), axis=AX.X, op=ALU.max)
                VV("vector", "tensor_scalar", [sm_b], [sm_b], out=R(70, 78), in0=R(62, 70), scalar1=R(53, 54), scalar2=None, op0=ALU.is_equal)
                VV("vector", "tensor_tensor", [sm_b], [sm_b], out=R(78, 79), in0=R(53, 54), in1=R(52, 53), op=ALU.subtract)
                VV("scalar", "activation", [sm_b], [sm_b], out=R(78, 79), in_=R(78, 79), func=AF.Exp)
                VV("vector", "tensor_scalar", [sm_b], [sm_b], out=R(79, 80), in0=R(78, 79), scalar1=1.0, scalar2=None, op0=ALU.add)
                VV("vector", "reciprocal", [sm_b], [sm_b], out=R(79, 80), in_=R(79, 80))
                VV("vector", "tensor_tensor", [sm_b], [sm_b], out=R(80, 81), in0=R(78, 79), in1=R(79, 80), op=ALU.mult)
                VV("vector", "tensor_scalar", [sm_b], [sm_b], out=R(79, 81), in0=R(79, 81), scalar1=R(38, 39), scalar2=None, op0=ALU.mult)
                VV("vector", "tensor_scalar", [sm_b], [sm_b], out=R(81, 89), in0=R(54, 62), scalar1=R(79, 80), scalar2=None, op0=ALU.mult)
                VV("vector", "scalar_tensor_tensor", [sm_b], [sm_b], out=R(81, 89), in0=R(70, 78), scalar=R(80, 81), in1=R(81, 89), op0=ALU.mult, op1=ALU.add)
                VV("vector", "tensor_tensor", [sm_b], [b_G[ti]], out=G[:, ti, :].rearrange("p (g e) -> p g e", g=4),
                   in0=R(40, 44).unsqueeze(2).to_broadcast([128, 4, 8]), in1=R(81, 89).unsqueeze(1).to_broadcast([128, 4, 8]), op=ALU.mult)
        S.barrier()

        with ExitStack() as s2:
            sb2 = lambda n, s, dt=F32: s2.enter_context(nc.sbuf_tensor(n, list(s), dt))
            wg = [sb2("wg%d" % i, [128, 8, 512], BF16) for i in range(2)]
            wu_ = [sb2("wup%d" % i, [128, 8, 512], BF16) for i in range(2)]
            wd = [sb2("wd%d" % i, [128, 4, 1024], BF16) for i in range(2)]
            b_wg = [Buf("wg%d" % i) for i in range(2)]
            b_wu = [Buf("wup%d" % i) for i in range(2)]
            b_wd = [Buf("wd%d" % i) for i in range(2)]
            sg = [sb2("sg%d" % i, [128, 512]) for i in range(2)]
            b_sg = [Buf() for _ in range(2)]
            act = [sb2("act%d" % i, [128, 4, 512], BF16) for i in range(2)]
            b_act = [Buf() for _ in range(2)]
            chunks = [(512 * c, 512) for c in range(4)] + ([(2048, 128)] if NT > 16 else [])
            cc = 0
            fcnt = 0
            for ex in range(C.NEXP if KC >= 20 else 0):
                wi = ex % 2
                k.gload(wg[wi][:], C.w_gate[ex].rearrange("(k p) f -> p k f", p=128), writes=[b_wg[wi]])
                k.gload(wu_[wi][:], C.w_up[ex].rearrange("(k p) f -> p k f", p=128), writes=[b_wu[wi]])
                k.gload(wd[wi][:], C.w_down[ex].rearrange("(k p) d -> p k d", p=128), writes=[b_wd[wi]])
                for (t0, tn) in chunks:
                    ai = cc % 2
                    cc += 1
                    tiles = list(range(t0 // 128, (t0 + tn) // 128))
                    rd_h = [b_hT[t] for t in tiles]
                    for f in range(4):
                        pg, pgb = psT[0 + fcnt % 2], psB[0 + fcnt % 2]
                        pu, pub = psT[2 + fcnt % 2], psB[2 + fcnt % 2]
                        si = fcnt % 2
                        fcnt += 1
                        for (pp, ppb, ww, bw) in ((pg, pgb, wg[wi], b_wg[wi]), (pu, pub, wu_[wi], b_wu[wi])):
                            for kk in range(8):
                                fn = lambda e, kk=kk, pp=pp, ww=ww, f=f, t0=t0, tn=tn: e.matmul(pp[:, 0:tn], lhsT=ww[:, kk, 128 * f:128 * f + 128], rhs=hTb[:, kk, t0:t0 + tn],
                                                                                            start=(kk == 0), stop=(kk == 7))
                                if kk == 0:
                                    k.op("tensor", fn, reads=[bw] + rd_h, writes=[ppb])
                                else:
                                    k.acc("tensor", fn, reads=[bw] + rd_h, acc=[ppb])
                        k.op("scalar", lambda e, si=si, pg=pg, tn=tn: e.activation(out=sg[si][:, 0:tn], in_=pg[:, 0:tn], func=AF.Silu), reads=[pgb], writes=[b_sg[si]])
                        k.op("vector", lambda e, si=si, ai=ai, f=f, pu=pu, tn=tn: e.tensor_tensor(out=act[ai][:, f, 0:tn], in0=sg[si][:, 0:tn], in1=pu[:, 0:tn], op=ALU.mult),
                             reads=[b_sg[si], pub], writes=[b_act[ai]])
                    for tt, tile in enumerate(tiles):
                        for half in range(2):
                            py, pyb = psT[4 + (tt * 2 + half) % 4], psB[4 + (tt * 2 + half) % 4]
                            for f in range(4):
                                fn = lambda e, f=f, ai=ai, tt=tt, half=half, py=py, wi=wi: e.matmul(py[:, :], lhsT=act[ai][:, f, 128 * tt:128 * tt + 128],
                                                                                               rhs=wd[wi][:, f, 512 * half:512 * half + 512], start=(f == 0), stop=(f == 3))
                                if f == 0:
                                    k.op("tensor", fn, reads=[b_act[ai], b_wd[wi]], writes=[pyb])
                                else:
                                    k.acc("tensor", fn, reads=[b_act[ai], b_wd[wi]], acc=[pyb])
                            k.op("vector", lambda e, tile=tile, half=half, py=py, ex=ex: e.scalar_tensor_tensor(
                                out=yacc[:, tile, 512 * half:512 * half + 512], in0=py[:, :], scalar=G[:, tile, ex:ex + 1], in1=yacc[:, tile, 512 * half:512 * half + 512],
                                op0=ALU.mult, op1=ALU.add), reads=[pyb, b_G[tile], b_y[tile]], writes=[b_y[tile]])
        S.barrier()

        with ExitStack() as s3:
            sb3 = lambda n, s, dt=F32: s3.enter_context(nc.sbuf_tensor(n, list(s), dt))
            k.dma("sync", lnp[:], C.lnp_d[:, 2:4, :], writes=[b_lnp])
            st6, mv = st6_c3, mv_c3
            b_st = [Buf() for _ in range(2)]
            b_mv = [Buf() for _ in range(2)]
            yo = [sb3("yo%d" % i, [128, 1024]) for i in range(2)]
            b_yo = [Buf("yo%d" % i) for i in range(2)]
            for ti in range(NT if KC >= 30 else 0):
                bi = ti % 2
                for half in range(2):
                    k.op("vector", lambda e, bi=bi, half=half, ti=ti: e.bn_stats(out=st6[:, bi, half, :], in_=yacc[:, ti, 512 * half:512 * half + 512]),
                         reads=[b_y[ti]], writes=[b_st[bi]])
                k.op("vector", lambda e, bi=bi: e.bn_aggr(out=mv[:, bi, :], in_=st6[:, bi, :, :].rearrange("p a b -> p (a b)")), reads=[b_st[bi]], writes=[b_mv[bi]])
                k.op("scalar", lambda e, bi=bi: e.activation(out=mv[:, bi, 1:2], in_=mv[:, bi, 1:2], func=AF.Sqrt, bias=C.eps5[:, 0:1]), reads=[b_mv[bi], C.b_eps5], writes=[b_mv[bi]])
                k.op("vector", lambda e, bi=bi: e.reciprocal(out=mv[:, bi, 1:2], in_=mv[:, bi, 1:2]), reads=[b_mv[bi]], writes=[b_mv[bi]])
                k.op("vector", lambda e, bi=bi, ti=ti: e.tensor_scalar(out=yo[bi][:], in0=yacc[:, ti, :], scalar1=mv[:, bi, 0:1], scalar2=mv[:, bi, 1:2], op0=ALU.subtract, op1=ALU.mult),
                     reads=[b_y[ti], b_mv[bi]], writes=[b_yo[bi]])
                k.op("gpsimd", lambda e, bi=bi: e.tensor_tensor(out=yo[bi][:], in0=yo[bi][:], in1=lnp[:, 0, :], op=ALU.mult), reads=[b_yo[bi], b_lnp], writes=[b_yo[bi]])
                k.op("gpsimd", lambda e, bi=bi: e.tensor_tensor(out=yo[bi][:], in0=yo[bi][:], in1=lnp[:, 1, :], op=ALU.add), reads=[b_yo[bi], b_lnp], writes=[b_yo[bi]])
                if ti < 16:
                    final.append(k.dma("sync", C.y_out[128 * ti:128 * ti + 128, :], yo[bi][:], reads=[b_yo[bi]]))
                else:
                    final.append(k.dma("sync", C.ys_out, yo[bi][0:NS, :], reads=[b_yo[bi]]))


def phase_s(C):
    nc, k, S = C.nc, C.k, C.S
    psT, psB = C.psT, C.psB
    final = C.final
    ps_d, cs_d, ms_d = C.ps_d, C.cs_d, C.ms_d
    b_ps_d, b_cs_d, b_ms_d = Buf("ps_d"), Buf("cs_d"), Buf("ms_d")
    w_v = C.w_v

    def VV(eng, method, reads, writes, **aps):
        return k.op(eng, lambda e, aps=aps, method=method: getattr(e, method)(**aps), reads=reads, writes=writes)

    with ExitStack() as s1:
        sb1 = lambda n, s, dt=F32: s1.enter_context(nc.sbuf_tensor(n, list(s), dt))
        xsTb = sb1("xsTb", [128, 8, NS], BF16)
        b_xs = Buf("xsTb")
        k.gload(xsTb[:], C.xsT.rearrange("(k p) t -> p k t", p=128), writes=[b_xs])
        wS = [sb1("wS%d" % i, [128, 8, 512], BF16) for i in range(2)]
        b_wS = [Buf("wS%d" % i) for i in range(2)]
        p_s = sb1("p_s", [NS, IN_COLS])
        b_p = Buf("p_s")
        for cchunk in range(8):
            c0 = 512 * cchunk
            cn = min(512, IN_COLS - c0)
            wi = cchunk % 2
            k.gload(wS[wi][:, :, 0:cn], w_v[:, :, c0:c0 + cn], writes=[b_wS[wi]])
            pt_, pb_ = psT[wi], psB[wi]
            for kk in range(8):
                fn = lambda e, kk=kk, wi=wi, cn=cn, pt_=pt_: e.matmul(pt_[0:NS, 0:cn], lhsT=xsTb[:, kk, :], rhs=wS[wi][:, kk, 0:cn], start=(kk == 0), stop=(kk == 7))
                if kk == 0:
                    k.op("tensor", fn, reads=[b_xs, b_wS[wi]], writes=[pb_])
                else:
                    k.acc("tensor", fn, reads=[b_xs, b_wS[wi]], acc=[pb_])
            VV("vector", "tensor_copy", [pb_], [b_p], out=p_s[:, c0:c0 + cn], in_=pt_[0:NS, 0:cn])
        k.dma("sync", ps_d, p_s[:], reads=[b_p], writes=[b_ps_d])
        final.append(k.dma("sync", C.knew, p_s[:, COL_KA:COL_KA + 512], reads=[b_p], slot="knew"))
        final.append(k.dma("sync", C.vnew, p_s[:, COL_VA:COL_VA + 512], reads=[b_p], slot="vnew"))
        final.append(k.dma("sync", C.conv_s[:, 2, :], p_s[:, COL_UB:COL_UB + 1536], reads=[b_p], slot="convs"))
        cst_ = sb1("cst_", [NS, 3, 1536])
        b_cst_ = Buf("cst_")
        k.dma("sync", cst_[:], C.scv, writes=[b_cst_])
        final.append(k.dma("sync", C.conv_s[:, 0:2, :], cst_[:, 1:3, :], reads=[b_cst_], slot="convs"))
        cwr = sb1("cwr_sb", [NS, 4, 1536])
        b_cwr = Buf("cwr")
        k.dma("sync", cwr[:], C.cwr_d, writes=[b_cwr])
        cacc = sb1("cacc", [NS, 1536])
        ctmp = sb1("ctmp", [NS, 1536])
        b_ca, b_ct = Buf("cacc"), Buf()
        VV("vector", "tensor_tensor", [b_p, b_cwr], [b_ca], out=cacc[:], in0=p_s[:, COL_UB:COL_UB + 1536], in1=cwr[:, 3, :], op=ALU.mult)
        for i in range(3):
            VV("vector", "tensor_tensor", [b_cst_, b_cwr], [b_ct], out=ctmp[:], in0=cst_[:, i, :], in1=cwr[:, i, :], op=ALU.mult)
            VV("vector", "tensor_tensor", [b_ct, b_ca], [b_ca], out=cacc[:], in0=cacc[:], in1=ctmp[:], op=ALU.add)
        VV("scalar", "activation", [b_ca], [b_ca], out=cacc[:], in_=cacc[:], func=AF.Silu)
        k.dma("sync", cs_d, cacc[:], reads=[b_ca], writes=[b_cs_d])
    S.barrier()

    with ExitStack() as s2:
        sb2 = lambda n, s, dt=F32: s2.enter_context(nc.sbuf_tensor(n, list(s), dt))
        qkv = sb2("qkv_nh", [128, 3, 64])
        b_qkv = Buf("qkv_nh")
        for j, c0 in enumerate((COL_QA, COL_KA, COL_VA)):
            k.dma("sync", qkv[:, j, :], bass.AP(ps_d.tensor, c0, [[IN_COLS, NS], [64, 8], [1, 64]]), reads=[b_ps_d], writes=[b_qkv])
        sbias = sb2("sbias_sb", [128, 3, 129])
        b_sb = Buf("sbias")
        k.dma("sync", sbias[:], C.sbias_d, writes=[b_sb])
        Kb = sb2("Kb", [128, 128, 64])
        Vb = sb2("Vb", [128, 128, 64])
        b_Kb, b_Vb = Buf("Kb"), Buf("Vb")
        tmpS = sb2("tmpS", [128, 128, 64])
        b_tmp = Buf()
        sc = sb2("sc", [128, 3, 129])
        b_sc = Buf()
        sm = sb2("smS", [128, 16])
        b_sm = Buf()
        oacc = sb2("oacc", [128, 64])
        otmp = sb2("otmp", [128, 64])
        b_oa, b_ot = Buf("oacc"), Buf()
        VV("vector", "tensor_tensor", [b_qkv], [b_ot], out=otmp[:], in0=qkv[:, 0, :], in1=qkv[:, 1, :], op=ALU.mult)
        VV("vector", "tensor_reduce", [b_ot], [b_sm], out=sm[:, 0:1], in_=otmp[:], axis=AX.X, op=ALU.add)
        for br, (_, dil) in enumerate(BRANCHES):
            for n in range(NS):
                src = bass.AP(C.ck.tensor, n * 2048 * 512 + (2048 - 128 * dil) * 512, [[64, 8], [dil * 512, 128], [1, 64]])
                k.dma("sync", Kb[8 * n:8 * n + 8, :, :], src, writes=[b_Kb]) if n == 0 else k.S.dmaop(
                    "sync", "Kb", lambda e, n=n, src=src: e.dma_start(out=Kb[8 * n:8 * n + 8, :, :], in_=src), [])
            b_Kb.writer = ("D", "Kb", k.S.dma_sems["Kb"][1])
            VV("vector", "tensor_tensor", [b_Kb, b_qkv], [b_tmp], out=tmpS[:], in0=Kb[:], in1=qkv[:, 0, :].unsqueeze(1).to_broadcast([128, 128, 64]), op=ALU.mult)
            VV("vector", "tensor_reduce", [b_tmp], [b_sc], out=sc[:, br, 0:128], in_=tmpS[:], axis=AX.X, op=ALU.add)
            VV("vector", "tensor_copy", [b_sm], [b_sc], out=sc[:, br, 128:129], in_=sm[:, 0:1])
        VV("vector", "scalar_tensor_tensor", [b_sc, b_sb], [b_sc], out=sc[:].rearrange("p a b -> p (a b)"), in0=sc[:].rearrange("p a b -> p (a b)"),
           scalar=0.125, in1=sbias[:].rearrange("p a b -> p (a b)"), op0=ALU.mult, op1=ALU.add)
        VV("vector", "tensor_reduce", [b_sc], [b_sm], out=sm[:, 1:2], in_=sc[:].rearrange("p a b -> p (a b)"), axis=AX.X, op=ALU.max)
        VV("vector", "tensor_scalar", [b_sm], [b_sm], out=sm[:, 2:3], in0=sm[:, 1:2], scalar1=-1.0, scalar2=None, op0=ALU.mult)
        VV("scalar", "activation", [b_sc, b_sm], [b_sc, b_sm], out=sc[:].rearrange("p a b -> p (a b)"), in_=sc[:].rearrange("p a b -> p (a b)"), func=AF.Exp,
           bias=sm[:, 2:3], accum_out=sm[:, 3:4])
        VV("vector", "reciprocal", [b_sm], [b_sm], out=sm[:, 3:4], in_=sm[:, 3:4])
        VV("vector", "tensor_reduce", [b_sc], [b_sm], out=sm[:, 4:5], in_=sc[:, :, 128], axis=AX.X, op=ALU.add)
        VV("vector", "tensor_scalar", [b_qkv, b_sm], [b_oa], out=oacc[:], in0=qkv[:, 2, :], scalar1=sm[:, 4:5], scalar2=None, op0=ALU.mult)
        for br, (_, dil) in enumerate(BRANCHES):
            for n in range(NS):
                src = bass.AP(C.cv.tensor, n * 2048 * 512 + (2048 - 128 * dil) * 512, [[64, 8], [dil * 512, 128], [1, 64]])
                if n == 0:
                    k.dma("sync", Vb[8 * n:8 * n + 8, :, :], src, writes=[b_Vb])
                else:
                    k.S.dmaop("sync", "Vb", lambda e, n=n, src=src: e.dma_start(out=Vb[8 * n:8 * n + 8, :, :], in_=src), [])
            b_Vb.writer = ("D", "Vb", k.S.dma_sems["Vb"][1])
            VV("vector", "tensor_tensor", [b_Vb, b_sc], [b_tmp], out=tmpS[:], in0=Vb[:], in1=sc[:, br, 0:128].unsqueeze(2).to_broadcast([128, 128, 64]), op=ALU.mult)
            VV("vector", "tensor_reduce", [b_tmp], [b_ot], out=otmp[:], in_=tmpS[:].rearrange("p i d -> p d i"), axis=AX.X, op=ALU.add)
            VV("vector", "tensor_tensor", [b_ot, b_oa], [b_oa], out=oacc[:], in0=oacc[:], in1=otmp[:], op=ALU.add)
        VV("vector", "tensor_scalar", [b_oa, b_sm], [b_oa], out=oacc[:], in0=oacc[:], scalar1=sm[:, 3:4], scalar2=None, op0=ALU.mult)
        k.dma("sync", bass.AP(ms_d.tensor, 0, [[D, NS], [64, 8], [1, 64]]), oacc[:], reads=[b_oa], writes=[b_ms_d])
    S.barrier()

    with ExitStack() as s3:
        sb3 = lambda n, s, dt=F32: s3.enter_context(nc.sbuf_tensor(n, list(s), dt))
        NP = NS * 4
        St = sb3("St", [NP, 128, 128])
        b_St = Buf("St")
        for q4 in range(4):
            k.dma("sync", St[:, 32 * q4:32 * q4 + 32, :], C.sst[:, 32 * q4:32 * q4 + 32, :], writes=[b_St]) if q4 == 0 else k.S.dmaop(
                "sync", "St", lambda e, q4=q4: e.dma_start(out=St[:, 32 * q4:32 * q4 + 32, :], in_=C.sst[:, 32 * q4:32 * q4 + 32, :]), [])
        b_St.writer = ("D", "St", k.S.dma_sems["St"][1])
        T2 = sb3("T2", [NP, 128, 128])
        b_T2 = Buf()
        c3 = sb3("c3", [NP, 3, 128])
        b_c3 = Buf("c3")
        for ty in range(3):
            k.dma("sync", c3[:, ty, :], bass.AP(cs_d.tensor, 512 * ty, [[1536, NS], [128, 4], [1, 128]]), reads=[b_cs_d], writes=[b_c3])
        zz = sb3("zz", [NP, 128])
        b_zz = Buf("zz")
        k.dma("sync", zz[:], bass.AP(ps_d.tensor, COL_ZB, [[IN_COLS, NS], [128, 4], [1, 128]]), reads=[b_ps_d], writes=[b_zz])
        ab = sb3("ab_s", [NP, 2])
        b_ab = Buf("ab_s")
        k.dma("sync", ab[:, 0:1], bass.AP(ps_d.tensor, COL_AB, [[IN_COLS, NS], [1, 4], [1, 1]]), reads=[b_ps_d], writes=[b_ab])
        k.dma("sync", ab[:, 1:2], bass.AP(ps_d.tensor, COL_BB, [[IN_COLS, NS], [1, 4], [1, 1]]), reads=[b_ps_d], writes=[b_ab])
        sp = sb3("sprm_sb", [NP, 2 + 128])
        b_sp = Buf("sprm")
        k.dma("sync", sp[:], C.sprm_d, writes=[b_sp])
        w = sb3("wS3", [NP, 24])
        b_w = Buf()
        jk = sb3("jkS3", [NP, 128])
        b_jk = Buf()
        VV("vector", "tensor_tensor", [b_ab, b_sp], [b_w], out=w[:, 0:1], in0=ab[:, 0:1], in1=sp[:, 1:2], op=ALU.add)
        VV("scalar", "activation", [b_w], [b_w], out=w[:, 0:1], in_=w[:, 0:1], func=AF.Exp)
        VV("scalar", "activation", [b_w], [b_w], out=w[:, 0:1], in_=w[:, 0:1], func=AF.Ln, bias=1.0)
        VV("scalar", "activation", [b_sp], [b_w], out=w[:, 1:2], in_=sp[:, 0:1], func=AF.Exp)
        VV("vector", "scalar_tensor_tensor", [b_w], [b_w], out=w[:, 2:3], in0=w[:, 0:1], scalar=-1.0, in1=w[:, 1:2], op0=ALU.mult, op1=ALU.mult)
        VV("scalar", "activation", [b_w], [b_w], out=w[:, 3:4], in_=w[:, 2:3], func=AF.Exp)
        VV("scalar", "activation", [b_ab], [b_w], out=w[:, 4:5], in_=ab[:, 1:2], func=AF.Sigmoid)
        for j in range(2):
            VV("scalar", "activation", [b_c3], [b_jk, b_w], out=jk[:], in_=c3[:, j, :], func=AF.Square, accum_out=w[:, 5 + j:6 + j])
            VV("scalar", "activation", [b_w, C.b_eps], [b_w], out=w[:, 5 + j:6 + j], in_=w[:, 5 + j:6 + j], func=AF.Sqrt, bias=C.eps6[0:NP, 0:1])
            VV("vector", "reciprocal", [b_w], [b_w], out=w[:, 5 + j:6 + j], in_=w[:, 5 + j:6 + j])
        VV("vector", "tensor_scalar", [b_c3, b_w], [b_c3], out=c3[:, 0, :], in0=c3[:, 0, :], scalar1=w[:, 5:6], scalar2=128.0 ** -0.5, op0=ALU.mult, op1=ALU.mult)
        VV("vector", "tensor_scalar", [b_c3, b_w], [b_c3], out=c3[:, 1, :], in0=c3[:, 1, :], scalar1=w[:, 6:7], scalar2=None, op0=ALU.mult)
        mem = sb3("memS", [NP, 4, 128])
        b_mem = Buf()
        VV("vector", "tensor_tensor", [b_St, b_c3], [b_T2], out=T2[:], in0=St[:], in1=c3[:, 1, :].unsqueeze(2).to_broadcast([NP, 128, 128]), op=ALU.mult)
        VV("vector", "tensor_reduce", [b_T2], [b_mem], out=mem[:, 0, :], in_=T2[:].rearrange("p d e -> p e d"), axis=AX.X, op=ALU.add)
        VV("vector", "scalar_tensor_tensor", [b_mem, b_w, b_c3], [b_mem], out=mem[:, 1, :], in0=mem[:, 0, :], scalar=w[:, 3:4], in1=c3[:, 2, :], op0=ALU.mult, op1=ALU.subtract)
        VV("vector", "tensor_scalar", [b_mem, b_w], [b_mem], out=mem[:, 1, :], in0=mem[:, 1, :], scalar1=w[:, 4:5], scalar2=-1.0, op0=ALU.mult, op1=ALU.mult)
        VV("vector", "tensor_tensor", [b_c3, b_mem], [b_T2], out=T2[:], in0=c3[:, 1, :].unsqueeze(2).to_broadcast([NP, 128, 128]),
           in1=mem[:, 1, :].unsqueeze(1).to_broadcast([NP, 128, 128]), op=ALU.mult)
        VV("vector", "scalar_tensor_tensor", [b_St, b_T2, b_w], [b_St], out=St[:], in0=St[:], scalar=w[:, 3:4], in1=T2[:], op0=ALU.mult, op1=ALU.add)
        for q4 in range(4):
            final.append(k.dma("sync", C.ssm_s[:, 32 * q4:32 * q4 + 32, :], St[:, 32 * q4:32 * q4 + 32, :], reads=[b_St], slot="ssms"))
        VV("vector", "tensor_tensor", [b_St, b_c3], [b_T2], out=T2[:], in0=St[:], in1=c3[:, 0, :].unsqueeze(2).to_broadcast([NP, 128, 128]), op=ALU.mult)
        VV("vector", "tensor_reduce", [b_T2], [b_mem], out=mem[:, 2, :], in_=T2[:].rearrange("p d e -> p e d"), axis=AX.X, op=ALU.add)
        VV("scalar", "activation", [b_mem], [b_jk, b_w], out=jk[:], in_=mem[:, 2, :], func=AF.Square, accum_out=w[:, 8:9])
        VV("scalar", "activation", [b_w, C.b_eps], [b_w], out=w[:, 8:9], in_=w[:, 8:9], func=AF.Sqrt, scale=1.0 / 128.0, bias=C.eps6[0:NP, 0:1])
        VV("vector", "reciprocal", [b_w], [b_w], out=w[:, 8:9], in_=w[:, 8:9])
        VV("scalar", "activation", [b_zz], [b_zz], out=zz[:], in_=zz[:], func=AF.Silu)
        VV("vector", "tensor_tensor", [b_zz, b_sp], [b_zz], out=zz[:], in0=zz[:], in1=sp[:, 2:130], op=ALU.mult)
        VV("vector", "scalar_tensor_tensor", [b_mem, b_w, b_zz], [b_mem], out=mem[:, 3, :], in0=mem[:, 2, :], scalar=w[:, 8:9], in1=zz[:], op0=ALU.mult, op1=ALU.mult)
        k.dma("sync", bass.AP(ms_d.tensor, 512, [[D, NS], [128, 4], [1, 128]]), mem[:, 3, :], reads=[b_mem], writes=[b_ms_d])
    S.barrier()

    with ExitStack() as s4:
        sb4 = lambda n, s, dt=F32: s4.enter_context(nc.sbuf_tensor(n, list(s), dt))
        msf = sb4("msf", [128, 1024])
        b_msf = Buf("msf")
        VV("vector", "memset", [], [b_msf], ap=msf[:], constant=0.0)
        k.dma("sync", msf[0:NS, :], ms_d, reads=[b_ms_d], writes=[b_msf])
        mxS = sb4("mxS", [128, 8, 128], BF16)
        b_mxS = Buf("mxS")
        for g4 in range(2):
            pt_, pb_ = psT[2 + g4], psB[2 + g4]
            for j in range(4):
                kk = 4 * g4 + j
                fn = lambda e, kk=kk, j=j, pt_=pt_: e.transpose(pt_[:, 128 * j:128 * j + 128], msf[:, 128 * kk:128 * kk + 128], C.cst[:, 0, :])
                if j == 0:
                    k.op("tensor", fn, reads=[b_msf, C.b_cst], writes=[pb_])
                else:
                    k.acc("tensor", fn, reads=[b_msf, C.b_cst], acc=[pb_])
            VV("vector", "tensor_copy", [pb_], [b_mxS], out=mxS[:, 4 * g4:4 * g4 + 4, :], in_=pt_[:, :].rearrange("p (a c) -> p a c", a=4))
        k.dma("sync", C.mixS_d, mxS[:], reads=[b_mxS], writes=[C.b_mixS_d])
    S.barrier()


def _consts():
    t = np.arange(128)
    same = (t[:, None] // 64) == (t[None, :] // 64)
    cst = np.zeros((128, 7, 128), np.float32)
    cst[:, 0] = np.eye(128)
    cst[:, 1] = -np.eye(128)
    cst[:, 2] = (same & (t[:, None] <= t[None, :]))
    cst[:, 3] = (t[:, None] < 64) * np.ones((1, 128))
    cst[:, 4] = (t[:, None] >= 64) * np.ones((1, 128))
    cst[:, 5] = np.where(same & (t[None, :] <= t[:, None]), 0.0, NEG)
    cst[:, 6] = (same & (t[None, :] < t[:, None]))
    return cst


def _params(inp):
    prm = np.zeros((128, 184), np.float32)
    prm[:, 0:4] = inp["a_log"][0][None, :]
    prm[:, 4:8] = inp["dt_bias"][0][None, :]
    prm[:, 8:136] = inp["o_norm_g"][0][None, :]
    cw = inp["conv_w"][0]
    prm[:, 136:184] = cw.reshape(4, 12, 128).transpose(2, 1, 0).reshape(128, 48)
    return prm


def _sample_bias(rel_bias):
    out = np.empty((128, 3, 129), np.float32)
    h = np.arange(128) % 8
    for br, (_, dil) in enumerate(BRANCHES):
        dist = np.concatenate([dil * (128 - np.arange(128)), [0]])
        out[:, br, :] = rel_bias[_rel_bucket_np(dist)][:, h].T
    return out


def _core_inputs(c, inp, bt):
    b, hf = divmod(c, 2)
    x = inp["x_prompt"][b]
    xT = np.zeros((D, EXT), np.float32)
    if hf == 1:
        xT[:, :] = x.T
    else:
        xT[:, HALF:] = x[:HALF].T
    valid = np.ones((128, 32), np.float32)
    if hf == 0:
        valid[:, :16] = 0.0
    return {
        "xT": np.ascontiguousarray(xT),
        "xo": np.ascontiguousarray(x[HALF * hf:HALF * hf + HALF]),
        "valid": valid,
        "w_in": np.ascontiguousarray(inp["w_in"][0]),
        "bt": bt,
        "cst": _consts(),
        "prm": _params(inp),
        "w_out": np.ascontiguousarray(inp["w_out"][0]),
        "lnp": np.ascontiguousarray(np.broadcast_to(np.stack([inp["ln1_g"][0], inp["ln1_b"][0], inp["ln2_g"][0], inp["ln2_b"][0]])[None], (128, 4, D))),
        "wr": np.ascontiguousarray(np.concatenate([inp["w_group"][0], inp["w_router"][0]], axis=1)),
        "rb": np.ascontiguousarray(np.broadcast_to(np.concatenate([inp["b_group"][0], inp["b_router"][0].reshape(-1)])[None], (128, 36))),
        "w_gate": np.ascontiguousarray(inp["w_gate"][0]),
        "w_up": np.ascontiguousarray(inp["w_up"][0]),
        "w_down": np.ascontiguousarray(inp["w_down"][0]),
        "xsT": np.ascontiguousarray(inp["x_sample"][NS * c:NS * c + NS, 0, :].T),
        "ck": np.ascontiguousarray(inp["cache_a_k"][0, NS * c:NS * c + NS].reshape(NS, 2048, 512)),
        "cv": np.ascontiguousarray(inp["cache_a_v"][0, NS * c:NS * c + NS].reshape(NS, 2048, 512)),
        "sst": np.ascontiguousarray(inp["state_b_ssm"][0, NS * c:NS * c + NS].reshape(NS * 4, 128, 128)),
        "scv": np.ascontiguousarray(inp["state_b_conv"][0, NS * c:NS * c + NS]),
        "sbias": _sample_bias(inp["rel_bias"].astype(np.float32)),
        "cwr": np.ascontiguousarray(np.broadcast_to(inp["conv_w"][0][None], (NS, 4, 1536))),
        "sprm": np.ascontiguousarray(np.concatenate([np.tile(inp["a_log"][0], NS)[:, None], np.tile(inp["dt_bias"][0], NS)[:, None],
                                                       np.broadcast_to(inp["o_norm_g"][0][None], (NS * 4, 128))], axis=1)),
        "xs_pad": np.ascontiguousarray(np.concatenate([inp["x_sample"][NS * c:NS * c + NS, 0, :], np.zeros((128 - NS, D), np.float32)], axis=0)),
    }


def kernel(**inputs):
    inp = {k_: np.asarray(v) for k_, v in inputs.items()}
    bt = _bias_tiles(inp["rel_bias"].astype(np.float32))
    nc = build()
    in_maps = [_core_inputs(c, inp, bt) for c in range(NCORES)]
    res = run_bass_kernel_spmd(nc, in_maps, core_ids=list(range(NCORES)))
    r = res.results
    y_prompt = np.stack([np.concatenate([r[2 * b]["y_out"], r[2 * b + 1]["y_out"]], axis=0) for b in range(4)])
    k_win = np.stack([r[2 * b + 1]["kwin"].reshape(HALF, 8, 64) for b in range(4)])[None]
    v_win = np.stack([r[2 * b + 1]["vwin"].reshape(HALF, 8, 64) for b in range(4)])[None]
    ssm_p = np.stack([r[2 * b + 1]["ssm_p"] for b in range(4)])[None]
    conv_p = np.stack([r[2 * b + 1]["conv_p"] for b in range(4)])[None]
    y_sample = np.concatenate([r[c]["ys_out"] for c in range(NCORES)], axis=0)[:, None, :]
    k_new = np.concatenate([r[c]["knew"] for c in range(NCORES)], axis=0).reshape(1, 128, 1, 8, 64)
    v_new = np.concatenate([r[c]["vnew"] for c in range(NCORES)], axis=0).reshape(1, 128, 1, 8, 64)
    ssm_s = np.concatenate([r[c]["ssm_s"] for c in range(NCORES)], axis=0).reshape(1, 128, 4, 128, 128)
    conv_s = np.concatenate([r[c]["conv_s"] for c in range(NCORES)], axis=0)[None]
    return (y_prompt, y_sample, k_win, v_win, k_new, v_new, ssm_p, ssm_s, conv_p, conv_s)
```

```python
import math
import os
from contextlib import ExitStack

import numpy as np
import concourse.bass as bass
import concourse.mybir as mybir
from concourse.bass_utils import run_bass_kernel_spmd

F32 = mybir.dt.float32
BF16 = mybir.dt.bfloat16
I32 = mybir.dt.int32
U32 = mybir.dt.uint32
AF = mybir.ActivationFunctionType
ALU = mybir.AluOpType
AX = mybir.AxisListType

NCORES = 8
D = 1024
SEQ = 4096
HALF = 2048
EXT = 4096
NS = 16
A_HEADS, A_HD = 8, 64
B_HEADS, B_HD = 4, 128
COL_QA, COL_KA, COL_VA, COL_UB = 0, 512, 1024, 1536
COL_ZB = COL_UB + 1536
COL_AB = COL_ZB + 512
COL_BB = COL_AB + 4
IN_COLS = COL_BB + 4
BRANCHES = ((128, 1), (512, 4), (2048, 16))
NEG = -30000.0
ENGS = ("tensor", "vector", "scalar", "gpsimd", "sync")


class Sched:
    def __init__(self, nc, stack, same_engine_wait=True):
        self.nc = nc
        self.stack = stack
        self.q = {e: [] for e in ENGS}
        self.cnt = {e: 0 for e in ENGS}
        self.sem = {e: stack.enter_context(nc.semaphore("s_" + e)) for e in ENGS}
        self.waited = {e: {} for e in ENGS}
        self.same_engine_wait = same_engine_wait
        self.dma_sems = {}
        self.ninst = 0

    def _wait(self, eng, tok):
        if tok is None:
            return
        if tok[0] == "E":
            _, src, val = tok
            if src == eng and not self.same_engine_wait:
                return
            key = "E" + src
            sem = self.sem[src]
        else:
            _, slot, val = tok
            key = "D" + slot
            sem = self.dma_sems[slot][0]
        if self.waited[eng].get(key, 0) >= val:
            return
        self.waited[eng][key] = val
        self.q[eng].append(lambda e, sem=sem, val=val: e.wait_ge(sem, val))

    def op(self, eng, fn, deps=()):
        for d in deps:
            self._wait(eng, d)
        self.cnt[eng] += 1
        c = self.cnt[eng]
        sem = self.sem[eng]
        self.q[eng].append(lambda e, fn=fn, sem=sem: fn(e).then_inc(sem, 1))
        self.ninst += 1
        return ("E", eng, c)

    def dmaop(self, eng, slot, fn, deps=()):
        for d in deps:
            self._wait(eng, d)
        if slot not in self.dma_sems:
            self.dma_sems[slot] = [self.stack.enter_context(self.nc.semaphore("d_" + slot)), 0]
        ent = self.dma_sems[slot]
        ent[1] += 16
        sem = ent[0]
        self.q[eng].append(lambda e, fn=fn, sem=sem: fn(e).then_inc(sem, 16))
        self.ninst += 1
        return ("D", slot, ent[1])

    def barrier(self):
        toks = [("E", e, self.cnt[e]) for e in ENGS if self.cnt[e] > 0]
        toks += [("D", slot, ent[1]) for slot, ent in self.dma_sems.items() if ent[1] > 0]
        for e in ENGS:
            for t in toks:
                self._wait(e, t)

    def finish(self, final_tokens):
        best = {}
        for t in final_tokens:
            if t is None:
                continue
            key = (t[0], t[1])
            if key not in best or best[key][2] < t[2]:
                best[key] = t
        for t in best.values():
            self._wait("sync", t)
        with self.nc.Block() as block:
            @block.tensor
            def _(e):
                for f in self.q["tensor"]:
                    f(e)

            @block.vector
            def _(e):
                for f in self.q["vector"]:
                    f(e)

            @block.scalar
            def _(e):
                for f in self.q["scalar"]:
                    f(e)

            @block.gpsimd
            def _(e):
                for f in self.q["gpsimd"]:
                    f(e)

            @block.sync
            def _(e):
                for f in self.q["sync"]:
                    f(e)


class Buf:
    _n = 0

    def __init__(self, name=None):
        Buf._n += 1
        self.name = name or ("b%d" % Buf._n)
        self.writer = None
        self.readers = {}

    def add_reader(self, tok):
        key = tok[1]
        if key not in self.readers or self.readers[key][2] < tok[2]:
            self.readers[key] = tok


class K:
    def __init__(self, S):
        self.S = S

    def _deps(self, reads, writes, deps):
        d = list(deps)
        for b in reads:
            d.append(b.writer)
        for b in writes:
            d.extend(b.readers.values())
            d.append(b.writer)
        return d

    def _commit(self, tok, reads, writes):
        for b in reads:
            b.add_reader(tok)
        for b in writes:
            b.writer = tok
            b.readers = {}

    def op(self, eng, fn, reads=(), writes=(), deps=()):
        tok = self.S.op(eng, fn, self._deps(reads, writes, deps))
        self._commit(tok, reads, writes)
        return tok

    def acc(self, eng, fn, reads=(), acc=(), deps=()):
        d = list(deps)
        for b in reads:
            d.append(b.writer)
        tok = self.S.op(eng, fn, d)
        for b in reads:
            b.add_reader(tok)
        for b in acc:
            b.writer = tok
        return tok

    def gload(self, dst, src, writes, reads=()):
        A, L = dst.shape[1], dst.shape[2]
        slot = writes[0].name
        d = self._deps(reads, writes, ())
        tok = None
        for a0 in range(0, A, 4):
            for l0 in range(0, L, 512):
                tok = self.S.dmaop("gpsimd", slot, lambda e, a0=a0, l0=l0, L=L: e.dma_start(
                    out=dst[:, a0:a0 + 4, l0:min(L, l0 + 512)], in_=src[:, a0:a0 + 4, l0:min(L, l0 + 512)]), d)
        self._commit(tok, reads, writes)
        return tok

    def dma(self, eng, out, in_, reads=(), writes=(), deps=(), slot=None, **kw):
        if slot is None:
            slot = (writes[0] if writes else reads[0]).name
        tok = self.S.dmaop(eng, slot, lambda e: e.dma_start(out=out, in_=in_, **kw), self._deps(reads, writes, deps))
        self._commit(tok, reads, writes)
        return tok


def _rel_bucket_np(dist):
    n = np.maximum(dist, 0)
    ratio = np.maximum(n, 1).astype(np.float32) / np.float32(16)
    large = 16 + (np.log(ratio) / np.float32(math.log(2048 / 16)) * np.float32(16)).astype(np.int32)
    return np.where(n < 16, n, np.minimum(large, 31))


def _bias_tiles(rel_bias):
    kp = np.arange(128)[:, None, None]
    kt = np.arange(2)[None, :, None]
    i = np.arange(128)[None, None, :]
    off = 128 + i - (128 * kt + kp)
    valid = (off >= 0) & (off <= 128)
    out = np.empty((128, 24, 256), np.float32)
    for h in range(A_HEADS):
        for br, (_, dil) in enumerate(BRANCHES):
            bk = _rel_bucket_np(np.maximum(off, 0) * dil)
            vals = rel_bias[bk, h]
            out[:, h * 3 + br, :] = np.where(valid, vals, np.float32(NEG)).reshape(128, 256)
    return out


def sl(start, step, n=128):
    return slice(start, start + (n - 1) * step + 1, step)


def tokset(ti, dil):
    nblk, r = divmod(ti, dil)
    start = r + dil * 128 * nblk
    return start, dil


def build(debug=(), stage=99, nexp=32, with_sample=True):
    nc = bass.Bass("TRN2", target_bir_lowering=False)
    dram = lambda n, s, dt=F32, kind="ExternalInput": nc.dram_tensor(n, list(s), dt, kind=kind).ap()
    xT = dram("xT", [D, EXT])
    xo = dram("xo", [HALF, D])
    valid = dram("valid", [128, 32])
    w_in = dram("w_in", [D, IN_COLS])
    bt = dram("bt", [128, 24, 256])
    cst_d = dram("cst", [128, 7, 128])
    prm_d = dram("prm", [128, 184])
    ssm_p = dram("ssm_p", [4, 128, 128], kind="ExternalOutput")
    conv_p = dram("conv_p", [3, 1536], kind="ExternalOutput")
    kT_d = dram("kT_d", [128, 4, EXT], BF16, kind="Internal")
    vT_d = dram("vT_d", [128, 4, EXT], BF16, kind="Internal")
    qT_d = dram("qT_d", [128, 4, HALF], BF16, kind="Internal")
    gz_d = dram("gz_d", [16, 128, 512], BF16, kind="Internal")
    mixA_d = dram("mixA_d", [128, 4, HALF], BF16, kind="Internal")
    mixB_d = dram("mixB_d", [128, 4, HALF], BF16, kind="Internal")
    w_out = dram("w_out", [D, D])
    lnp_d = dram("lnp", [128, 4, D])
    wr_d = dram("wr", [D, 36])
    rb_d = dram("rb", [128, 36])
    w_gate = dram("w_gate", [32, D, 512])
    w_up = dram("w_up", [32, D, 512])
    w_down = dram("w_down", [32, 512, D])
    xs_pad = dram("xs_pad", [128, D])
    y_out = dram("y_out", [HALF, D], kind="ExternalOutput")
    ys_out = dram("ys_out", [NS, D], kind="ExternalOutput")
    mixS_d = dram("mixS_d", [128, 8, 128], BF16, kind="Internal")
    xsT = dram("xsT", [D, NS])
    ck = dram("ck", [NS, 2048, 512])
    cv = dram("cv", [NS, 2048, 512])
    sst = dram("sst", [NS * 4, 128, 128])
    scv = dram("scv", [NS, 3, 1536])
    sbias_d = dram("sbias", [128, 3, 129])
    cwr_d = dram("cwr", [NS, 4, 1536])
    sprm_d = dram("sprm", [NS * 4, 130])
    ps_d = dram("ps_d", [NS, IN_COLS], kind="Internal")
    cs_d = dram("cs_d", [NS, 1536], kind="Internal")
    ms_d = dram("ms_d", [NS, D], kind="Internal")
    knew = dram("knew", [NS, 512], kind="ExternalOutput")
    vnew = dram("vnew", [NS, 512], kind="ExternalOutput")
    ssm_s = dram("ssm_s", [NS * 4, 128, 128], kind="ExternalOutput")
    conv_s = dram("conv_s", [NS, 3, 1536], kind="ExternalOutput")
    kwin = dram("kwin", [HALF, 512], kind="ExternalOutput")
    vwin = dram("vwin", [HALF, 512], kind="ExternalOutput")
    dbg_out = {}
    for name, shape in debug:
        dbg_out[name] = dram("dbg_" + name, shape, kind="ExternalOutput")

    final = []
    with ExitStack() as st:
        S = Sched(nc, st, same_engine_wait=(os.environ.get("SEW", "1") == "1"))
        k = K(S)
        sb = lambda n, s, dt=F32: st.enter_context(nc.sbuf_tensor(n, list(s), dt))
        psT = [st.enter_context(nc.psum_tensor("ps%d" % i, [128, 512], F32)) for i in range(8)]
        psB = [Buf("ps%d" % i) for i in range(8)]

        ones_f = sb("ones_f", [128, 128])
        b_ones = Buf()
        k.op("gpsimd", lambda e: e.memset(ones_f[:], 1.0), writes=[b_ones])
        eps6 = sb("eps6", [128, 1])
        b_eps = Buf()
        k.op("gpsimd", lambda e: e.memset(eps6[:], 1e-6), writes=[b_eps])
        cst = sb("cst_sb", [128, 7, 128])
        b_cst = Buf("cst")
        k.dma("sync", cst[:], cst_d, writes=[b_cst])
        eps5 = sb("eps5", [128, 1])
        b_eps5 = Buf()
        k.op("gpsimd", lambda e: e.memset(eps5[:], 1e-5), writes=[b_eps5])
        b_mixA_d, b_mixB_d, b_mixS_d = Buf("mixA_d"), Buf("mixB_d"), Buf("mixS_d")
        valid_sb = sb("valid_sb", [128, 32])
        b_valid = Buf("valid")
        k.dma("sync", valid_sb[:], valid, writes=[b_valid])

        if with_sample:
            C = type("Ctx", (), {})()
            C.nc, C.k, C.S, C.final = nc, k, S, final
            C.psT, C.psB, C.cst, C.b_cst, C.eps6, C.b_eps = psT, psB, cst, b_cst, eps6, b_eps
            C.w_v = w_in.rearrange("(k p) c -> p k c", p=128)
            C.xsT, C.ck, C.cv, C.sst, C.scv, C.sbias_d, C.cwr_d, C.sprm_d = xsT, ck, cv, sst, scv, sbias_d, cwr_d, sprm_d
            C.ps_d, C.cs_d, C.ms_d = ps_d, cs_d, ms_d
            C.knew, C.vnew, C.ssm_s, C.conv_s = knew, vnew, ssm_s, conv_s
            C.mixS_d, C.b_mixS_d = mixS_d, b_mixS_d
            phase_s(C)
        sx = ExitStack()
        xTb = sx.enter_context(nc.sbuf_tensor("xTb", [128, 8, EXT], BF16))
        b_x = [Buf("xTb%d" % c) for c in range(8)]
        xT_v = xT.rearrange("(k p) t -> p k t", p=128)
        for c in range(8):
            k.gload(xTb[:, :, 512 * c:512 * (c + 1)], xT_v[:, :, 512 * c:512 * (c + 1)], writes=[b_x[c]])
        bx_of_tile = lambda ti_nat: b_x[ti_nat // 4]

        def x_bufs(start, step):
            lo, hi = start, start + step * 127
            return [b_x[c] for c in range(lo // 512, hi // 512 + 1)]

        with ExitStack() as sa:
            sba = lambda n, s, dt=F32: sa.enter_context(nc.sbuf_tensor(n, list(s), dt))
            mixA = sba("mixA", [128, 4, HALF], BF16)
            b_mixA = [Buf() for _ in range(4)]
            ETb = sba("ETb", [128, 24, 256], BF16)
            b_ET = Buf("ET")
            btst = sba("btst", [128, 6, 256])
            b_btst = Buf("btst")
            for g in range(4 if stage >= -1 else 0):
                k.dma("sync", btst[:], bt[:, 6 * g:6 * g + 6, :], writes=[b_btst])
                k.op("scalar", lambda e, g=g: e.activation(out=ETb[:, 6 * g:6 * g + 6, :], in_=btst[:], func=AF.Exp),
                     reads=[b_btst], writes=[b_ET])
            QT = sba("QT", [128, 2, HALF], BF16)
            KT = sba("KT", [128, 2, EXT], BF16)
            Vaug = sba("Vaug", [128, 32, 4, 65], BF16)
            acc = sba("acc", [65, 4, HALF])
            wq = sba("wq", [128, 8, 256], BF16)
            wk = sba("wk", [128, 8, 256], BF16)
            wv = sba("wv", [128, 8, 256], BF16)
            b_wq, b_wk, b_wv = Buf("wq"), Buf("wk"), Buf("wv")
            b_QT = [Buf() for _ in range(2)]
            b_KT = [Buf() for _ in range(2)]
            b_V = [Buf() for _ in range(32)]
            b_acc = [Buf() for _ in range(4)]
            b_rrow = Buf()
            stg = [sba("stg%d" % i, [128, 256]) for i in range(2)]
            b_stg = [Buf("stg%d" % i) for i in range(2)]
            exb = [sba("exb%d" % i, [128, 512]) for i in range(2)]
            b_ex = [Buf() for _ in range(2)]
            ptb = [sba("ptb%d" % i, [128, 512], BF16) for i in range(2)]
            b_pt = [Buf() for _ in range(2)]
            w_v = w_in.rearrange("(k p) c -> p k c", p=128)
            ctr = {"ps": 0, "stg": 0, "s": 0, "o": 0, "ev": 0}

            def proj_ps():
                i = ctr["ps"] % 2
                ctr["ps"] += 1
                return psT[i], psB[i]

            for hh2 in range(2):
                k.gload(wq[:], w_v[:, :, COL_QA + 256 * hh2:COL_QA + 256 * hh2 + 256], writes=[b_wq])
                k.gload(wk[:], w_v[:, :, COL_KA + 256 * hh2:COL_KA + 256 * hh2 + 256], writes=[b_wk])
                k.gload(wv[:], w_v[:, :, COL_VA + 256 * hh2:COL_VA + 256 * hh2 + 256], writes=[b_wv])
                for jj in range(2 if stage >= 0 else 0):
                    for tc in range(4 if os.environ.get('KQ','1')=='1' else 0):
                        pt_, pb_ = proj_ps()
                        for kk in range(8):
                            fn = lambda e, kk=kk, jj=jj, tc=tc, pt_=pt_: e.matmul(
                                pt_[:, :], lhsT=wq[:, kk, 128 * jj:128 * jj + 128],
                                rhs=xTb[:, kk, HALF + 512 * tc:HALF + 512 * tc + 512], start=(kk == 0), stop=(kk == 7))
                            if kk == 0:
                                k.op("tensor", fn, reads=[b_wq, b_x[4 + tc]], writes=[pb_])
                            else:
                                k.acc("tensor", fn, reads=[b_wq, b_x[4 + tc]], acc=[pb_])
                        k.op("vector", lambda e, jj=jj, tc=tc, pt_=pt_: e.tensor_scalar(
                            out=QT[:, jj, 512 * tc:512 * tc + 512], in0=pt_[:, :], scalar1=0.125, scalar2=None, op0=ALU.mult),
                            reads=[pb_], writes=[b_QT[jj]])
                    for tc in range(8 if os.environ.get('KK','1')=='1' else 0):
                        pt_, pb_ = proj_ps()
                        for kk in range(8):
                            fn = lambda e, kk=kk, jj=jj, tc=tc, pt_=pt_: e.matmul(
                                pt_[:, :], lhsT=wk[:, kk, 128 * jj:128 * jj + 128],
                                rhs=xTb[:, kk, 512 * tc:512 * tc + 512], start=(kk == 0), stop=(kk == 7))
                            if kk == 0:
                                k.op("tensor", fn, reads=[b_wk, b_x[tc]], writes=[pb_])
                            else:
                                k.acc("tensor", fn, reads=[b_wk, b_x[tc]], acc=[pb_])
                        k.op("vector", lambda e, jj=jj, tc=tc, pt_=pt_: e.tensor_copy(
                            out=KT[:, jj, 512 * tc:512 * tc + 512], in_=pt_[:, :]),
                            reads=[pb_], writes=[b_KT[jj]])
                for ti in range(16, 32 if stage >= -2 else 16):
                    pt_, pb_ = proj_ps()
                    for kk in range(8):
                        fn = lambda e, kk=kk, ti=ti, pt_=pt_: e.matmul(
                            pt_[:, 0:256], lhsT=xTb[:, kk, 128 * ti:128 * ti + 128], rhs=wk[:, kk, :],
                            start=(kk == 0), stop=(kk == 7))
                        if kk == 0:
                            k.op("tensor", fn, reads=[b_wk, b_x[ti // 4]], writes=[pb_])
                        else:
                            k.acc("tensor", fn, reads=[b_wk, b_x[ti // 4]], acc=[pb_])
                    si = ctr["stg"] % 2
                    ctr["stg"] += 1
                    k.op("scalar", lambda e, si=si, pt_=pt_: e.activation(out=stg[si][:], in_=pt_[:, 0:256], func=AF.Copy),
                         reads=[pb_], writes=[b_stg[si]])
                    final.append(k.dma("sync", kwin[128 * (ti - 16):128 * (ti - 16) + 128, 256 * hh2:256 * hh2 + 256],
                                       stg[si][:], reads=[b_stg[si]]))
                for br, (_, dil) in enumerate(BRANCHES):
                    if stage < 1:
                        break
                    k.op("gpsimd", lambda e: e.tensor_copy(
                        out=Vaug[:, :, :, 64], in_=valid_sb[:, :].unsqueeze(2).to_broadcast([128, 32, 4])),
                        reads=[b_valid], writes=b_V)
                    for ti in range(32):
                        start, step = tokset(ti, dil)
                        pt_, pb_ = proj_ps()
                        for kk in range(8):
                            fn = lambda e, kk=kk, start=start, step=step, pt_=pt_: e.matmul(
                                pt_[:, 0:256], lhsT=xTb[:, kk, sl(start, step)], rhs=wv[:, kk, :],
                                start=(kk == 0), stop=(kk == 7))
                            if kk == 0:
                                k.op("tensor", fn, reads=[b_wv] + x_bufs(start, step), writes=[pb_])
                            else:
                                k.acc("tensor", fn, reads=[b_wv] + x_bufs(start, step), acc=[pb_])
                        ev = "vector"
                        src = pt_[:, 0:256].rearrange("p (h d) -> p h d", h=4)
                        if ev == "vector":
                            k.op("vector", lambda e, ti=ti, src=src: e.tensor_copy(out=Vaug[:, ti, :, 0:64], in_=src),
                                 reads=[pb_], writes=[b_V[ti]])
                        else:
                            k.op("scalar", lambda e, ti=ti, src=src: e.activation(out=Vaug[:, ti, :, 0:64], in_=src, func=AF.Copy),
                                 reads=[pb_], writes=[b_V[ti]])
                        if br == 0 and ti >= 16:
                            si = ctr["stg"] % 2
                            ctr["stg"] += 1
                            k.op("vector", lambda e, si=si, pt_=pt_: e.tensor_copy(out=stg[si][:], in_=pt_[:, 0:256]),
                                 reads=[pb_], writes=[b_stg[si]])
                            final.append(k.dma("sync", vwin[128 * (ti - 16):128 * (ti - 16) + 128, 256 * hh2:256 * hh2 + 256],
                                               stg[si][:], reads=[b_stg[si]]))
                    for hl in range(4):
                        if stage < 2:
                            break
                        h = 4 * hh2 + hl
                        jj, pb = hl // 2, 64 * (hl % 2)
                        for ti0 in range(16, 32, 2):
                            sidx = 2 + ctr["s"] % 2
                            ctr["s"] += 1
                            pS, bS = psT[sidx], psB[sidx]
                            first = True
                            for a in range(2):
                                ti = ti0 + a
                                qs, qstep = tokset(ti, dil)
                                qs -= HALF
                                for kt in range(2):
                                    tk = ti - dil * (1 - kt)
                                    ks, kstep = tokset(tk, dil)
                                    fn = lambda e, a=a, kt=kt, ks=ks, kstep=kstep, qs=qs, qstep=qstep, pS=pS, jj=jj, pb=pb: e.matmul(
                                        pS[:, a * 256 + kt * 128:a * 256 + kt * 128 + 128],
                                        lhsT=KT[pb:pb + 64, jj, sl(ks, kstep)],
                                        rhs=QT[pb:pb + 64, jj, sl(qs, qstep)], start=True, stop=True)
                                    if first:
                                        k.op("tensor", fn, reads=[b_KT[jj], b_QT[jj]], writes=[bS])
                                        first = False
                                    else:
                                        k.acc("tensor", fn, reads=[b_KT[jj], b_QT[jj]], acc=[bS])
                            ei = ctr["ev"] % 2
                            ctr["ev"] += 1
                            k.op("scalar", lambda e, ei=ei, pS=pS: e.activation(out=exb[ei][:], in_=pS[:, :], func=AF.Exp),
                                 reads=[bS], writes=[b_ex[ei]])
                            k.op("vector", lambda e, ei=ei, h=h, br=br: e.tensor_tensor(
                                out=ptb[ei][:].rearrange("p (a c) -> p a c", a=2),
                                in0=exb[ei][:].rearrange("p (a c) -> p a c", a=2),
                                in1=ETb[:, h * 3 + br:h * 3 + br + 1, :].to_broadcast([128, 2, 256]), op=ALU.mult),
                                reads=[b_ex[ei], b_ET], writes=[b_pt[ei]])
                            oidx = 4 + ctr["o"] % 2
                            ctr["o"] += 1
                            pO, bO = psT[oidx], psB[oidx]
                            first = True
                            for a in range(2):
                                ti = ti0 + a
                                for kt in range(2):
                                    tk = ti - dil * (1 - kt)
                                    fn = lambda e, a=a, kt=kt, tk=tk, hl=hl, ei=ei, pO=pO: e.matmul(
                                        pO[0:65, a * 128:a * 128 + 128], lhsT=Vaug[:, tk, hl, 0:65],
                                        rhs=ptb[ei][:, a * 256 + kt * 128:a * 256 + kt * 128 + 128],
                                        start=(kt == 0), stop=(kt == 1))
                                    if first:
                                        k.op("tensor", fn, reads=[b_pt[ei], b_V[tk]], writes=[bO])
                                        first = False
                                    else:
                                        k.acc("tensor", fn, reads=[b_pt[ei], b_V[tk]], acc=[bO])
                            qs0, qstep = tokset(ti0, dil)
                            qs1, _ = tokset(ti0 + 1, dil)
                            qs0 -= HALF
                            qs1 -= HALF
                            dst = bass.AP(acc, hl * HALF + qs0, [[4 * HALF, 65], [qs1 - qs0, 2], [qstep, 128]])
                            srcO = pO[0:65, 0:256].rearrange("p (a c) -> p a c", a=2)
                            if br == 0:
                                k.op("vector", lambda e, dst=dst, srcO=srcO: e.tensor_copy(out=dst, in_=srcO),
                                     reads=[bO], writes=[b_acc[hl]])
                            else:
                                k.op("vector", lambda e, dst=dst, srcO=srcO: e.tensor_tensor(out=dst, in0=srcO, in1=dst, op=ALU.add),
                                     reads=[bO], writes=[b_acc[hl]])
                if stage < 3:
                    continue
                k.op("vector", lambda e: e.reciprocal(out=acc[64:65, :, :], in_=acc[64:65, :, :]), reads=b_acc, writes=[b_rrow])
                for hl in range(4):
                    jj, pb = hl // 2, 64 * (hl % 2)
                    for c in range(4):
                        pt_, pb_ = psT[6 + c % 2], psB[6 + c % 2]
                        k.op("tensor", lambda e, hl=hl, c=c, pt_=pt_: e.matmul(
                            pt_[0:64, :], lhsT=ones_f[64:65, 0:64], rhs=acc[64:65, hl, 512 * c:512 * c + 512], start=True, stop=True),
                            reads=[b_rrow, b_ones], writes=[pb_])
                        k.op("vector", lambda e, hl=hl, c=c, pt_=pt_, pb=pb, jj=jj, hh2=hh2: e.tensor_tensor(
                            out=mixA[pb:pb + 64, 2 * hh2 + jj, 512 * c:512 * c + 512], in0=acc[0:64, hl, 512 * c:512 * c + 512],
                            in1=pt_[0:64, :], op=ALU.mult), reads=[pb_, b_acc[hl]], writes=[b_mixA[2 * hh2 + jj]])

            if stage >= 3:
                for pp in range(4):
                    k.dma("sync", mixA_d[:, pp], mixA[:, pp], reads=[b_mixA[pp]], writes=[b_mixA_d])
        S.barrier()
        if stage >= 4:
            C = type("Ctx", (), {})()
            C.nc, C.k, C.S, C.final = nc, k, S, final
            C.xTb, C.b_x, C.w_v = xTb, b_x, w_in.rearrange("(k p) c -> p k c", p=128)
            C.psT, C.psB, C.cst, C.b_cst, C.ones_f, C.b_ones = psT, psB, cst, b_cst, ones_f, b_ones
            C.eps6, C.b_eps, C.prm_d = eps6, b_eps, prm_d
            C.kT_d, C.vT_d, C.qT_d, C.gz_d = kT_d, vT_d, qT_d, gz_d
            C.b_kT_d, C.b_vT_d, C.b_qT_d, C.b_gz_d = Buf("kT_d"), Buf("vT_d"), Buf("qT_d"), Buf("gz_d")
            C.mixB_d, C.b_mixB_d, C.ssm_p, C.conv_p = mixB_d, b_mixB_d, ssm_p, conv_p
            phase_b(C)
        sx.close()
        S.barrier()
        if stage >= 5:
            C = type("Ctx", (), {})()
            C.nc, C.k, C.S, C.final = nc, k, S, final
            C.psT, C.psB, C.cst, C.b_cst = psT, psB, cst, b_cst
            C.eps5, C.b_eps5 = eps5, b_eps5
            C.NT = 17 if with_sample else 16
            C.NEXP = nexp
            C.lnp_d, C.w_out, C.wr_d, C.rb_d = lnp_d, w_out, wr_d, rb_d
            C.w_gate, C.w_up, C.w_down = w_gate, w_up, w_down
            C.mixA_d, C.mixB_d, C.b_mixA_d, C.b_mixB_d = mixA_d, mixB_d, b_mixA_d, b_mixB_d
            C.mixS_d, C.b_mixS_d, C.xs_pad, C.xo = mixS_d, b_mixS_d, xs_pad, xo
            C.y_out, C.ys_out = y_out, ys_out
            phase_c(C)
        for nm, src_d, bsrc in (("mixA", mixA_d, b_mixA_d), ("mixB", mixB_d, b_mixB_d)):
            if nm in dbg_out:
                dstg_b = sb("dstg_b" + nm, [128, HALF], BF16)
                dstg = sb("dstg" + nm, [128, HALF])
                b_db, b_df = Buf("dstg_b" + nm), Buf("dstg" + nm)
                for pp in range(4):
                    k.dma("sync", dstg_b[:], src_d[:, pp], reads=[bsrc], writes=[b_db])
                    k.op("vector", lambda e, dstg=dstg, dstg_b=dstg_b: e.tensor_copy(out=dstg[:], in_=dstg_b[:]), reads=[b_db], writes=[b_df])
                    final.append(k.dma("sync", dbg_out[nm][:, pp], dstg[:], reads=[b_df]))
        S.finish(final)
    return nc


def phase_b(C):
    nc, k, S = C.nc, C.k, C.S
    xTb, b_x, w_v = C.xTb, C.b_x, C.w_v
    psT, psB = C.psT, C.psB
    cst, b_cst = C.cst, C.b_cst
    ones_f = C.ones_f
    IDENT, NEGID, LMASK, CM0, CM1, NEGM, STRICT = range(7)
    final = C.final
    kT_d, vT_d, qT_d, gz_d = C.kT_d, C.vT_d, C.qT_d, C.gz_d

    with ExitStack() as sB:
        sbb = lambda n, s, dt=F32: sB.enter_context(nc.sbuf_tensor(n, list(s), dt))
        prm = sbb("prm_sb", [128, 8 + 128 + 48])
        b_prm = Buf("prm")
        k.dma("sync", prm[:], C.prm_d, writes=[b_prm])
        identb = sbb("identb", [128, 128], BF16)
        b_identb = Buf()
        k.op("vector", lambda e: e.tensor_copy(out=identb[:], in_=cst[:, IDENT, :]), reads=[b_cst], writes=[b_identb])
        convp_sb = sbb("convp_sb", [128, 12, 3])
        b_convp = Buf("convp")

        ab_sb = sbb("ab_sb", [128, 32, 8])
        b_ab = Buf()
        with ExitStack() as s1:
            sb1 = lambda n, s, dt=F32: s1.enter_context(nc.sbuf_tensor(n, list(s), dt))
            wab = sb1("wab", [128, 8, 8], BF16)
            wz = sb1("wz", [128, 8, 512], BF16)
            b_wab, b_wz = Buf("wab"), Buf("wz")
            k.gload(wab[:], w_v[:, :, COL_AB:COL_AB + 8], writes=[b_wab])
            k.gload(wz[:], w_v[:, :, COL_ZB:COL_ZB + 512], writes=[b_wz])
            zst = [sb1("zst%d" % i, [128, 512]) for i in range(2)]
            b_zst = [Buf() for _ in range(2)]
            gzb = [sb1("gzb%d" % i, [128, 512], BF16) for i in range(2)]
            b_gzb = [Buf("gzb%d" % i) for i in range(2)]
            for ti in range(32):
                pt_, pb_ = psT[ti % 2], psB[ti % 2]
                for kk in range(8):
                    fn = lambda e, kk=kk, ti=ti, pt_=pt_: e.matmul(pt_[:, 0:8], lhsT=xTb[:, kk, 128 * ti:128 * ti + 128],
                                                                 rhs=wab[:, kk, :], start=(kk == 0), stop=(kk == 7))
                    if kk == 0:
                        k.op("tensor", fn, reads=[b_wab, b_x[ti // 4]], writes=[pb_])
                    else:
                        k.acc("tensor", fn, reads=[b_wab, b_x[ti // 4]], acc=[pb_])
                k.op("vector", lambda e, ti=ti, pt_=pt_: e.tensor_copy(out=ab_sb[:, ti, :], in_=pt_[:, 0:8]), reads=[pb_], writes=[b_ab])
            for ti in range(16, 32):
                i2 = ti % 2
                pt_, pb_ = psT[2 + i2], psB[2 + i2]
                for kk in range(8):
                    fn = lambda e, kk=kk, ti=ti, pt_=pt_: e.matmul(pt_[:, :], lhsT=xTb[:, kk, 128 * ti:128 * ti + 128],
                                                                 rhs=wz[:, kk, :], start=(kk == 0), stop=(kk == 7))
                    if kk == 0:
                        k.op("tensor", fn, reads=[b_wz, b_x[ti // 4]], writes=[pb_])
                    else:
                        k.acc("tensor", fn, reads=[b_wz, b_x[ti // 4]], acc=[pb_])
                k.op("scalar", lambda e, i2=i2, pt_=pt_: e.activation(out=zst[i2][:], in_=pt_[:, :], func=AF.Silu), reads=[pb_], writes=[b_zst[i2]])
                k.op("gpsimd", lambda e, i2=i2: e.tensor_tensor(
                    out=gzb[i2][:].rearrange("p (h e) -> p h e", h=4), in0=zst[i2][:].rearrange("p (h e) -> p h e", h=4),
                    in1=prm[:, 8:136].unsqueeze(1).to_broadcast([128, 4, 128]), op=ALU.mult),
                    reads=[b_zst[i2], b_prm], writes=[b_gzb[i2]])
                k.dma("sync", gz_d[ti - 16], gzb[i2][:], reads=[b_gzb[i2]], writes=[C.b_gz_d])

        S.barrier()
        gt = sbb("gt", [128, 12, 128])
        b_gt = [Buf() for _ in range(12)]
        G, BETA, GC, EGC, ETAIL, BEGE, EGL0, EGL1, GCL, TMP, NEGA, TMP2 = range(12)
        v3 = lambda idx: gt[:, idx, :].rearrange("p (t h) -> p t h", h=4)
        k.op("scalar", lambda e: e.activation(out=gt[:, NEGA, 0:4], in_=prm[:, 0:4], func=AF.Exp), reads=[b_prm], writes=[b_gt[NEGA]])
        k.op("vector", lambda e: e.tensor_tensor(out=v3(TMP), in0=ab_sb[:, :, 0:4], in1=prm[:, 4:8].unsqueeze(1).to_broadcast([128, 32, 4]), op=ALU.add),
             reads=[b_ab, b_prm], writes=[b_gt[TMP]])
        k.op("scalar", lambda e: e.activation(out=gt[:, TMP, :], in_=gt[:, TMP, :], func=AF.Exp), reads=[b_gt[TMP]], writes=[b_gt[TMP]])
        k.op("scalar", lambda e: e.activation(out=gt[:, TMP, :], in_=gt[:, TMP, :], func=AF.Ln, bias=1.0), reads=[b_gt[TMP]], writes=[b_gt[TMP]])
        k.op("vector", lambda e: e.scalar_tensor_tensor(out=v3(G), in0=v3(TMP), scalar=-1.0, in1=gt[:, NEGA, 0:4].unsqueeze(1).to_broadcast([128, 32, 4]),
                                                       op0=ALU.mult, op1=ALU.mult), reads=[b_gt[TMP], b_gt[NEGA]], writes=[b_gt[G]])
        k.op("scalar", lambda e: e.activation(out=v3(BETA), in_=ab_sb[:, :, 4:8], func=AF.Sigmoid), reads=[b_ab], writes=[b_gt[BETA]])
        for (mask, dst, bank) in ((LMASK, GC, 0), (CM0, EGL0, 1), (CM1, EGL1, 2)):
            k.op("tensor", lambda e, mask=mask, bank=bank: e.matmul(psT[bank][:, 0:128], lhsT=cst[:, mask, :], rhs=gt[:, G, :], start=True, stop=True),
                 reads=[b_cst, b_gt[G]], writes=[psB[bank]])
            k.op("vector", lambda e, dst=dst, bank=bank: e.tensor_copy(out=gt[:, dst, :], in_=psT[bank][:, 0:128]), reads=[psB[bank]], writes=[b_gt[dst]])
        k.op("vector", lambda e: e.tensor_copy(out=gt[0:64, GCL, :], in_=gt[0:64, EGL0, :]), reads=[b_gt[EGL0]], writes=[b_gt[GCL]])
        k.op("vector", lambda e: e.tensor_copy(out=gt[64:128, GCL, :], in_=gt[64:128, EGL1, :]), reads=[b_gt[EGL1], b_gt[GCL]], writes=[b_gt[GCL]])
        k.op("vector", lambda e: e.tensor_tensor(out=gt[:, TMP2, :], in0=gt[:, GCL, :], in1=gt[:, GC, :], op=ALU.subtract),
             reads=[b_gt[GCL], b_gt[GC]], writes=[b_gt[TMP2]])
        k.op("scalar", lambda e: e.activation(out=gt[:, ETAIL, :], in_=gt[:, TMP2, :], func=AF.Exp), reads=[b_gt[TMP2]], writes=[b_gt[ETAIL]])
        k.op("scalar", lambda e: e.activation(out=gt[:, EGC, :], in_=gt[:, GC, :], func=AF.Exp), reads=[b_gt[GC]], writes=[b_gt[EGC]])
        k.op("scalar", lambda e: e.activation(out=gt[:, EGL0, :], in_=gt[:, EGL0, :], func=AF.Exp), reads=[b_gt[EGL0], b_gt[GCL]], writes=[b_gt[EGL0]])
        k.op("scalar", lambda e: e.activation(out=gt[:, EGL1, :], in_=gt[:, EGL1, :], func=AF.Exp), reads=[b_gt[EGL1], b_gt[GCL]], writes=[b_gt[EGL1]])
        k.op("vector", lambda e: e.tensor_tensor(out=gt[:, BEGE, :], in0=gt[:, BETA, :], in1=gt[:, EGC, :], op=ALU.mult),
             reads=[b_gt[BETA], b_gt[EGC]], writes=[b_gt[BEGE]])

        with ExitStack() as s2:
            sb2 = lambda n, s, dt=F32: s2.enter_context(nc.sbuf_tensor(n, list(s), dt))
            wu = [sb2("wu%d" % i, [128, 8, 128], BF16) for i in range(2)]
            b_wu = [Buf("wu%d" % i) for i in range(2)]
            ub = [sb2("ub%d" % i, [128, 515]) for i in range(2)]
            b_ub = [Buf() for _ in range(2)]
            cb = [sb2("cb%d" % i, [128, 512]) for i in range(2)]
            b_cb = [Buf() for _ in range(2)]
            sq = [sb2("sq%d" % i, [128, 512]) for i in range(2)]
            b_sq = [Buf() for _ in range(2)]
            rt = [sb2("rt%d" % i, [128, 512]) for i in range(2)]
            b_rt = [Buf() for _ in range(2)]
            ob = [sb2("ob%d" % i, [128, 512], BF16) for i in range(2)]
            b_ob = [Buf("ob%d" % i) for i in range(2)]
            cnt = 0
            for th in range(12):
                ty, hb = divmod(th, 4)
                wi = th % 2
                c0 = COL_UB + 128 * th
                k.gload(wu[wi][:], w_v[:, :, c0:c0 + 128], writes=[b_wu[wi]])
                chunks = range(4, 8) if ty == 0 else range(8)
                dst_d = (qT_d, kT_d, vT_d)[ty]
                b_dst = (C.b_qT_d, C.b_kT_d, C.b_vT_d)[ty]
                cw = lambda i, th=th: prm[:, 136 + 4 * th + i:136 + 4 * th + i + 1]
                first = True
                for tc in chunks:
                    ci = cnt % 2
                    cnt += 1
                    pt_, pb_ = psT[ci], psB[ci]
                    if first:
                        if ty == 0:
                            hp, hpb = psT[2], psB[2]
                            for kk in range(8):
                                fn = lambda e, kk=kk, wi=wi, hp=hp: e.matmul(hp[:, 0:4], lhsT=wu[wi][:, kk, :], rhs=xTb[:, kk, HALF - 4:HALF],
                                                                            start=(kk == 0), stop=(kk == 7))
                                if kk == 0:
                                    k.op("tensor", fn, reads=[b_wu[wi], b_x[3]], writes=[hpb])
                                else:
                                    k.acc("tensor", fn, reads=[b_wu[wi], b_x[3]], acc=[hpb])
                            k.op("vector", lambda e, ci=ci, hp=hp: e.tensor_copy(out=ub[ci][:, 0:3], in_=hp[:, 1:4]), reads=[hpb], writes=[b_ub[ci]])
                        else:
                            k.op("vector", lambda e, ci=ci: e.memset(ub[ci][:, 0:3], 0.0), writes=[b_ub[ci]])
                        first = False
                    else:
                        k.op("vector", lambda e, ci=ci: e.tensor_copy(out=ub[ci][:, 0:3], in_=ub[1 - ci][:, 512:515]),
                             reads=[b_ub[1 - ci]], writes=[b_ub[ci]])
                    for kk in range(8):
                        fn = lambda e, kk=kk, wi=wi, tc=tc, pt_=pt_: e.matmul(pt_[:, :], lhsT=wu[wi][:, kk, :], rhs=xTb[:, kk, 512 * tc:512 * tc + 512],
                                                                            start=(kk == 0), stop=(kk == 7))
                        if kk == 0:
                            k.op("tensor", fn, reads=[b_wu[wi], b_x[tc]], writes=[pb_])
                        else:
                            k.acc("tensor", fn, reads=[b_wu[wi], b_x[tc]], acc=[pb_])
                    k.op("scalar", lambda e, ci=ci, pt_=pt_: e.activation(out=ub[ci][:, 3:515], in_=pt_[:, :], func=AF.Copy), reads=[pb_], writes=[b_ub[ci]])
                    if tc == 7:
                        k.op("gpsimd", lambda e, ci=ci, th=th: e.tensor_copy(out=convp_sb[:, th, :], in_=ub[ci][:, 512:515]), reads=[b_ub[ci]], writes=[b_convp])
                    k.op("vector", lambda e, ci=ci, cw=cw: e.tensor_scalar(out=cb[ci][:], in0=ub[ci][:, 3:515], scalar1=cw(3), scalar2=None, op0=ALU.mult),
                         reads=[b_ub[ci], b_prm], writes=[b_cb[ci]])
                    for i in range(3):
                        k.op("vector", lambda e, ci=ci, cw=cw, i=i: e.scalar_tensor_tensor(out=cb[ci][:], in0=ub[ci][:, i:i + 512], scalar=cw(i), in1=cb[ci][:],
                                                                                       op0=ALU.mult, op1=ALU.add), reads=[b_ub[ci], b_cb[ci]], writes=[b_cb[ci]])
                    k.op("scalar", lambda e, ci=ci: e.activation(out=cb[ci][:], in_=cb[ci][:], func=AF.Silu), reads=[b_cb[ci]], writes=[b_cb[ci]])
                    if ty == 2:
                        k.op("gpsimd", lambda e, ci=ci: e.tensor_copy(out=ob[ci][:], in_=cb[ci][:]), reads=[b_cb[ci]], writes=[b_ob[ci]])
                    else:
                        k.op("gpsimd", lambda e, ci=ci: e.tensor_tensor(out=sq[ci][:], in0=cb[ci][:], in1=cb[ci][:], op=ALU.mult), reads=[b_cb[ci]], writes=[b_sq[ci]])
                        np_, npb = psT[4 + ci], psB[4 + ci]
                        k.op("tensor", lambda e, ci=ci, np_=np_: e.matmul(np_[:, :], lhsT=ones_f[:], rhs=sq[ci][:], start=True, stop=True),
                             reads=[b_sq[ci], C.b_ones], writes=[npb])
                        k.op("scalar", lambda e, ci=ci, np_=np_: e.activation(out=rt[ci][:], in_=np_[:, :], func=AF.Sqrt, bias=C.eps6[:, 0:1]),
                             reads=[npb, C.b_eps], writes=[b_rt[ci]])
                        k.op("vector", lambda e, ci=ci: e.reciprocal(out=rt[ci][:], in_=rt[ci][:]), reads=[b_rt[ci]], writes=[b_rt[ci]])
                        sc = (128.0 ** -0.5) if ty == 0 else 1.0
                        k.op("vector", lambda e, ci=ci, sc=sc: e.scalar_tensor_tensor(out=ob[ci][:], in0=cb[ci][:], scalar=sc, in1=rt[ci][:], op0=ALU.mult, op1=ALU.mult),
                             reads=[b_cb[ci], b_rt[ci]], writes=[b_ob[ci]])
                    t0 = 512 * tc - (HALF if ty == 0 else 0)
                    k.dma("sync", dst_d[:, hb, t0:t0 + 512], ob[ci][:], reads=[b_ob[ci]], writes=[b_dst])
            convp_v = C.conv_p.rearrange("r (c p) -> p c r", p=128)
            for th in range(12):
                final.append(k.dma("sync", convp_v[:, th, :], convp_sb[:, th, :], reads=[b_convp], slot="convp", allow_slow_non_contiguous=True))

        S.barrier()
        sC = sB
        sbc = lambda n, s, dt=F32: sC.enter_context(nc.sbuf_tensor(n, list(s), dt))
        f_slots = [(psT[b][:, 0:128], psB[b]) for b in range(6)]
        psbf = [psT[6].bitcast(BF16), psT[7].bitcast(BF16)]
        h_slots = [(psbf[b][:, 0:128], psB[6 + b]) for b in range(2)]
        cnts = {"f": 0, "h": 0}

        def fslot():
            s_ = f_slots[cnts["f"] % len(f_slots)]
            cnts["f"] += 1
            return s_

        def hslot():
            s_ = h_slots[cnts["h"] % len(h_slots)]
            cnts["h"] += 1
            return s_

        class Pool_:
            def __init__(self, name, n, dt):
                self.t = [sbc("%s%d" % (name, i), [128, 128], dt) for i in range(n)]
                self.b = [Buf() for _ in range(n)]
                self.i = 0

            def get(self):
                j = self.i % len(self.t)
                self.i += 1
                return self.t[j], self.b[j]

        PF = Pool_("pf", 40, F32)
        PH = Pool_("ph", 96, BF16)
        Sst = [sbc("Sst%d" % h, [128, 128]) for h in range(4)]
        Sbf = [sbc("Sbf%d" % h, [128, 128], BF16) for h in range(4)]
        b_S = [Buf() for _ in range(4)]
        b_Sb = [Buf() for _ in range(4)]
        for h in range(4):
            k.op("vector", lambda e, h=h: e.memset(Sst[h][:], 0.0), writes=[b_S[h]])
            k.op("vector", lambda e, h=h: e.memset(Sbf[h][:], 0.0), writes=[b_Sb[h]])
        kt_t = [sbc("kt_t%d" % i, [128, 4, 128], BF16) for i in range(2)]
        vt_t = [sbc("vt_t%d" % i, [128, 4, 128], BF16) for i in range(2)]
        qt_t = [sbc("qt_t%d" % i, [128, 4, 128], BF16) for i in range(2)]
        gz_t = [sbc("gz_t%d" % i, [128, 512], BF16) for i in range(2)]
        b_kt = [Buf("kt_t%d" % i) for i in range(2)]
        b_vt = [Buf("vt_t%d" % i) for i in range(2)]
        b_qt = [Buf("qt_t%d" % i) for i in range(2)]
        b_gzt = [Buf("gz_t%d" % i) for i in range(2)]
        Ukeep = [[sbc("Uk%d_%d" % (i, h), [128, 128]) for h in range(4)] for i in range(2)]
        WTkeep = [[sbc("WTk%d_%d" % (i, h), [128, 128], BF16) for h in range(4)] for i in range(2)]
        ktlkeep = [[sbc("ktlk%d_%d" % (i, h), [128, 128], BF16) for h in range(4)] for i in range(2)]
        qkTkeep = [[sbc("qkTk%d_%d" % (i, h), [128, 128], BF16) for h in range(4)] for i in range(2)]
        b_Uk = [[Buf() for h in range(4)] for i in range(2)]
        b_WTk = [[Buf() for h in range(4)] for i in range(2)]
        b_ktlk = [[Buf() for h in range(4)] for i in range(2)]
        b_qkTk = [[Buf() for h in range(4)] for i in range(2)]
        ss = sbc("ss", [128, 8])
        b_ss = [Buf() for _ in range(8)]
        junk = sbc("junk", [128, 128])
        b_junk = Buf()
        mxs = [sbc("mxs%d" % i, [128, 4, 128], BF16) for i in range(2)]
        b_mxs = [Buf("mxs%d" % i) for i in range(2)]
        gt_ = gt
        col = lambda idx, c_: gt_[:, idx, c_:c_ + 1]

        def make_tile(ti):
            own = ti >= 16
            bi = ti % 2
            k.dma("sync", kt_t[bi][:], kT_d[:, :, 128 * ti:128 * ti + 128], reads=[C.b_kT_d], writes=[b_kt[bi]])
            k.dma("sync", vt_t[bi][:], vT_d[:, :, 128 * ti:128 * ti + 128], reads=[C.b_vT_d], writes=[b_vt[bi]])
            if own:
                k.dma("sync", qt_t[bi][:], qT_d[:, :, 128 * (ti - 16):128 * (ti - 16) + 128], reads=[C.b_qT_d], writes=[b_qt[bi]])
                k.dma("sync", gz_t[bi][:], gz_d[ti - 16], reads=[C.b_gz_d], writes=[b_gzt[bi]])
            HS = [None] * 4
            def stage1(hb):
                c_ = ti * 4 + hb
                kT = kt_t[bi][:, hb, :]
                vT = vt_t[bi][:, hb, :]
                qT = qt_t[bi][:, hb, :]
                rd_k, rd_v, rd_q = [b_kt[bi]], [b_vt[bi]], [b_qt[bi]]
                nd, b_nd = PF.get()
                k.op("gpsimd", lambda e, nd=nd, c_=c_: e.tensor_scalar(out=nd[:], in0=cst[:, NEGID, :], scalar1=col(GC, c_), scalar2=None, op0=ALU.mult),
                     reads=[b_cst, b_gt[GC]], writes=[b_nd])
                pD, bD = fslot()
                yield
                k.op("tensor", lambda e, pD=pD, nd=nd: e.matmul(pD, lhsT=ones_f[:], rhs=nd[:], start=True, stop=False), reads=[b_nd, C.b_ones], writes=[bD])
                k.acc("tensor", lambda e, pD=pD: e.matmul(pD, lhsT=cst[:, IDENT, :], rhs=cst[:, NEGM, :], start=False, stop=True), reads=[b_cst], acc=[bD])
                Ec, b_Ec = PF.get()
                k.op("scalar", lambda e, Ec=Ec, pD=pD, c_=c_: e.activation(out=Ec[:], in_=pD, func=AF.Exp, bias=col(GC, c_)),
                     reads=[bD, b_gt[GC]], writes=[b_Ec])
                Es, b_Es = PF.get()
                k.op("gpsimd", lambda e, Es=Es, Ec=Ec: e.tensor_tensor(out=Es[:], in0=Ec[:], in1=cst[:, STRICT, :], op=ALU.mult),
                     reads=[b_Ec, b_cst], writes=[b_Es])
                pK, bK = fslot()
                yield
                k.op("tensor", lambda e, pK=pK, kT=kT: e.matmul(pK, lhsT=kT, rhs=kT, start=True, stop=True), reads=rd_k, writes=[bK])
                A, b_A = PH.get()
                k.op("vector", lambda e, A=A, pK=pK, Es=Es, c_=c_: e.scalar_tensor_tensor(out=A[:], in0=pK, scalar=col(BETA, c_), in1=Es[:], op0=ALU.mult, op1=ALU.mult),
                     reads=[bK, b_Es, b_gt[BETA]], writes=[b_A])
                pT_, bT_ = hslot()
                yield
                k.op("tensor", lambda e, pT_=pT_, A=A: e.transpose(pT_, A[:], identb[:]), reads=[b_A, b_identb], writes=[bT_])
                Bm, b_Bm = PH.get()
                k.op("vector", lambda e, Bm=Bm, pT_=pT_: e.tensor_copy(out=Bm[:], in_=pT_), reads=[bT_], writes=[b_Bm])
                P, b_P = PH.get()
                k.op("vector", lambda e, P=P, pT_=pT_: e.tensor_tensor(out=P[:], in0=cst[:, IDENT, :], in1=pT_, op=ALU.subtract), reads=[bT_, b_cst], writes=[b_P])
                X, b_X, Y, b_Y = A, b_A, Bm, b_Bm
                for m in range(1, 6):
                    pX, bX = fslot()
                    yield
                    k.op("tensor", lambda e, pX=pX, X=X, Y=Y: e.matmul(pX, lhsT=Y[:], rhs=X[:], start=True, stop=True), reads=[b_X, b_Y], writes=[bX])
                    Xn, b_Xn = PH.get()
                    k.op("vector", lambda e, Xn=Xn, pX=pX: e.tensor_copy(out=Xn[:], in_=pX), reads=[bX], writes=[b_Xn])
                    if m < 5:
                        pY, bY = fslot()
                        yield
                        k.op("tensor", lambda e, pY=pY, X=X, Y=Y: e.matmul(pY, lhsT=X[:], rhs=Y[:], start=True, stop=True), reads=[b_X, b_Y], writes=[bY])
                        Yn, b_Yn = PH.get()
                        k.op("vector", lambda e, Yn=Yn, pY=pY: e.tensor_copy(out=Yn[:], in_=pY), reads=[bY], writes=[b_Yn])
                    pP, bP = fslot()
                    yield
                    k.op("tensor", lambda e, pP=pP, Xn=Xn, P=P: e.matmul(pP, lhsT=Xn[:], rhs=P[:], start=True, stop=True), reads=[b_Xn, b_P], writes=[bP])
                    Pn, b_Pn = PH.get()
                    k.op("vector", lambda e, Pn=Pn, pP=pP, P=P: e.tensor_tensor(out=Pn[:], in0=pP, in1=P[:], op=ALU.add), reads=[bP, b_P], writes=[b_Pn])
                    P, b_P = Pn, b_Pn
                    X, b_X = Xn, b_Xn
                    if m < 5:
                        Y, b_Y = Yn, b_Yn
                pk_, bk_ = hslot()
                yield
                k.op("tensor", lambda e, pk_=pk_, kT=kT: e.transpose(pk_, kT, identb[:]), reads=rd_k + [b_identb], writes=[bk_])
                Rw, b_Rw = PH.get()
                k.op("vector", lambda e, Rw=Rw, pk_=pk_, c_=c_: e.tensor_scalar(out=Rw[:], in0=pk_, scalar1=col(BEGE, c_), scalar2=None, op0=ALU.mult),
                     reads=[bk_, b_gt[BEGE]], writes=[b_Rw])
                ktl, b_ktl = ktlkeep[bi][hb], b_ktlk[bi][hb]
                k.op("vector", lambda e, ktl=ktl, pk_=pk_, c_=c_: e.tensor_scalar(out=ktl[:], in0=pk_, scalar1=col(ETAIL, c_), scalar2=None, op0=ALU.mult),
                     reads=[bk_, b_gt[ETAIL]], writes=[b_ktl])
                pv_, bv_ = hslot()
                yield
                k.op("tensor", lambda e, pv_=pv_, vT=vT: e.transpose(pv_, vT, identb[:]), reads=rd_v + [b_identb], writes=[bv_])
                Ru, b_Ru = PH.get()
                k.op("vector", lambda e, Ru=Ru, pv_=pv_, c_=c_: e.tensor_scalar(out=Ru[:], in0=pv_, scalar1=col(BETA, c_), scalar2=None, op0=ALU.mult),
                     reads=[bv_, b_gt[BETA]], writes=[b_Ru])
                pU, bU = fslot()
                yield
                k.op("tensor", lambda e, pU=pU, P=P, Ru=Ru: e.matmul(pU, lhsT=P[:], rhs=Ru[:], start=True, stop=True), reads=[b_P, b_Ru], writes=[bU])
                U, b_U = Ukeep[bi][hb], b_Uk[bi][hb]
                k.op("vector", lambda e, U=U, pU=pU: e.tensor_copy(out=U[:], in_=pU), reads=[bU], writes=[b_U])
                pW, bW = fslot()
                yield
                k.op("tensor", lambda e, pW=pW, P=P, Rw=Rw: e.matmul(pW, lhsT=Rw[:], rhs=P[:], start=True, stop=True), reads=[b_P, b_Rw], writes=[bW])
                WT, b_WT = WTkeep[bi][hb], b_WTk[bi][hb]
                k.op("vector", lambda e, WT=WT, pW=pW: e.tensor_copy(out=WT[:], in_=pW), reads=[bW], writes=[b_WT])
                qkT = b_qkT = None
                if own:
                    pQ, bQ = fslot()
                    yield
                    k.op("tensor", lambda e, pQ=pQ, qT=qT, kT=kT: e.matmul(pQ, lhsT=qT, rhs=kT, start=True, stop=True), reads=rd_q + rd_k, writes=[bQ])
                    qk, b_qk = PH.get()
                    k.op("vector", lambda e, qk=qk, pQ=pQ, Ec=Ec: e.tensor_tensor(out=qk[:], in0=pQ, in1=Ec[:], op=ALU.mult), reads=[bQ, b_Ec], writes=[b_qk])
                    pq2, bq2 = hslot()
                    yield
                    k.op("tensor", lambda e, pq2=pq2, qk=qk: e.transpose(pq2, qk[:], identb[:]), reads=[b_qk, b_identb], writes=[bq2])
                    qkT, b_qkT = qkTkeep[bi][hb], b_qkTk[bi][hb]
                    k.op("vector", lambda e, qkT=qkT, pq2=pq2: e.tensor_copy(out=qkT[:], in_=pq2), reads=[bq2], writes=[b_qkT])
                HS[hb] = (dict(U=U, b_U=b_U, WT=WT, b_WT=b_WT, ktl=ktl, b_ktl=b_ktl, qkT=qkT, b_qkT=b_qkT, qT=qT, rd_q=rd_q, c_=c_))
            def stage23():
                outs = []
                if own:
                    for hb in range(4):
                        o_, b_o = PF.get()
                        outs.append((o_, b_o))
                for ch in range(2):
                    r0 = 64 * ch
                    for hb in range(4):
                        H = HS[hb]
                        c_ = H["c_"]
                        yield
                        pV, bV = fslot()
                        k.op("tensor", lambda e, pV=pV, H=H, hb=hb, r0=r0: e.matmul(pV[r0:r0 + 64, :], lhsT=H["WT"][:, r0:r0 + 64], rhs=Sbf[hb][:], start=True, stop=True),
                             reads=[H["b_WT"], b_Sb[hb]], writes=[bV])
                        vn, b_vn = PH.get()
                        k.op("vector", lambda e, vn=vn, pV=pV, H=H, r0=r0: e.tensor_tensor(out=vn[r0:r0 + 64, :], in0=H["U"][r0:r0 + 64, :], in1=pV[r0:r0 + 64, :], op=ALU.subtract),
                             reads=[bV, H["b_U"]], writes=[b_vn])
                        if own:
                            o_, b_o = outs[hb]
                            yield
                            p1, b1 = fslot()
                            k.op("tensor", lambda e, p1=p1, H=H, hb=hb, r0=r0: e.matmul(p1[r0:r0 + 64, :], lhsT=H["qT"][:, r0:r0 + 64], rhs=Sbf[hb][:], start=True, stop=True),
                                 reads=H["rd_q"] + [b_Sb[hb]], writes=[b1])
                            p2, b2 = fslot()
                            k.op("tensor", lambda e, p2=p2, H=H, vn=vn, r0=r0: e.matmul(p2[r0:r0 + 64, :], lhsT=H["qkT"][r0:r0 + 64, r0:r0 + 64], rhs=vn[r0:r0 + 64, :], start=True, stop=True),
                                 reads=[H["b_qkT"], b_vn], writes=[b2])
                            o2, b_o2 = PF.get()
                            k.op("vector", lambda e, o2=o2, p2=p2, r0=r0: e.tensor_copy(out=o2[r0:r0 + 64, :], in_=p2[r0:r0 + 64, :]), reads=[b2], writes=[b_o2])
                            k.op("vector", lambda e, o_=o_, p1=p1, o2=o2, r0=r0, c_=c_: e.scalar_tensor_tensor(
                                out=o_[r0:r0 + 64, :], in0=p1[r0:r0 + 64, :], scalar=gt_[r0:r0 + 64, EGC, c_:c_ + 1], in1=o2[r0:r0 + 64, :], op0=ALU.mult, op1=ALU.add),
                                reads=[b1, b_o2, b_gt[EGC]], writes=[b_o])
                        yield
                        pS, bS_ = fslot()
                        k.op("tensor", lambda e, pS=pS, H=H, vn=vn, r0=r0: e.matmul(pS, lhsT=H["ktl"][r0:r0 + 64, :], rhs=vn[r0:r0 + 64, :], start=True, stop=True),
                             reads=[H["b_ktl"], b_vn], writes=[bS_])
                        egl = EGL0 if ch == 0 else EGL1
                        k.op("vector", lambda e, pS=pS, hb=hb, egl=egl, c_=c_: e.scalar_tensor_tensor(
                            out=Sst[hb][:], in0=Sst[hb][:], scalar=col(egl, c_), in1=pS, op0=ALU.mult, op1=ALU.add),
                            reads=[bS_, b_gt[egl], b_S[hb]], writes=[b_S[hb]])
                        k.op("scalar", lambda e, hb=hb: e.activation(out=Sbf[hb][:], in_=Sst[hb][:], func=AF.Copy), reads=[b_S[hb]], writes=[b_Sb[hb]])
                if own:
                    for hb in range(4):
                        o_, b_o = outs[hb]
                        si = (ti * 4 + hb) % 8
                        yield
                        k.op("scalar", lambda e, o_=o_, si=si: e.activation(out=junk[:], in_=o_[:], func=AF.Square, accum_out=ss[:, si:si + 1]),
                             reads=[b_o], writes=[b_junk, b_ss[si]])
                        k.op("scalar", lambda e, si=si: e.activation(out=ss[:, si:si + 1], in_=ss[:, si:si + 1], func=AF.Sqrt, scale=1.0 / 128.0, bias=C.eps6[:, 0:1]),
                             reads=[b_ss[si], C.b_eps], writes=[b_ss[si]])
                        k.op("vector", lambda e, si=si: e.reciprocal(out=ss[:, si:si + 1], in_=ss[:, si:si + 1]), reads=[b_ss[si]], writes=[b_ss[si]])
                        on, b_on = PH.get()
                        k.op("vector", lambda e, on=on, o_=o_, si=si, hb=hb, bi=bi: e.scalar_tensor_tensor(
                            out=on[:], in0=o_[:], scalar=ss[:, si:si + 1], in1=gz_t[bi][:, 128 * hb:128 * hb + 128], op0=ALU.mult, op1=ALU.mult),
                            reads=[b_o, b_ss[si], b_gzt[bi]], writes=[b_on])
                        yield
                        pm, bm = hslot()
                        k.op("tensor", lambda e, pm=pm, on=on: e.transpose(pm, on[:], identb[:]), reads=[b_on, b_identb], writes=[bm])
                        k.op("vector", lambda e, pm=pm, hb=hb, bi=bi: e.tensor_copy(out=mxs[bi][:, hb, :], in_=pm),
                             reads=[bm], writes=[b_mxs[bi]])
                    k.dma("sync", C.mixB_d[:, :, 128 * (ti - 16):128 * (ti - 16) + 128], mxs[bi][:], reads=[b_mxs[bi]], writes=[C.b_mixB_d])
            return [stage1(hb) for hb in range(4)], stage23


        pending = None
        for ti in range(33):
            gens = []
            nxt = None
            if ti < 32:
                s1, nxt = make_tile(ti)
                gens += s1
            if pending is not None:
                gens.append(pending())
            while gens:
                for g_ in list(gens):
                    try:
                        next(g_)
                    except StopIteration:
                        gens.remove(g_)
            pending = nxt
        for hb in range(4):
            final.append(k.dma("sync", C.ssm_p[hb], Sst[hb][:], reads=[b_S[hb]], slot="ssmp"))

def phase_c(C):
    nc, k, S = C.nc, C.k, C.S
    psT, psB = C.psT, C.psB
    cst, b_cst = C.cst, C.b_cst
    IDENT = 0
    final = C.final
    NT = C.NT
    NTOK = NT * 128
    ALPHA = 2.0 ** 0.25
    KC = int(os.environ.get('KC', '99'))

    with ExitStack() as sC:
        sbc = lambda n, s, dt=F32: sC.enter_context(nc.sbuf_tensor(n, list(s), dt))
        hTb = sbc("hTb", [128, 8, NTOK], BF16)
        b_hT = [Buf() for _ in range(NT)]
        yacc = sbc("yacc", [128, NT, 1024])
        b_y = [Buf() for _ in range(NT)]
        G = sbc("G", [128, NT, 32])
        b_G = [Buf() for _ in range(NT)]
        st6_c3 = sbc("st6b", [128, 2, 2, 6])
        mv_c3 = sbc("mvb", [128, 2, 2])
        lnp = sbc("lnp_sb", [128, 2, 1024])
        b_lnp = Buf("lnp")

        with ExitStack() as s1:
            sb1 = lambda n, s, dt=F32: s1.enter_context(nc.sbuf_tensor(n, list(s), dt))
            k.dma("sync", lnp[:], C.lnp_d[:, 0:2, :], writes=[b_lnp])
            wo = sb1("wo", [128, 8, 1024], BF16)
            b_wo = Buf("wo")
            k.gload(wo[:], C.w_out.rearrange("(k p) c -> p k c", p=128), writes=[b_wo])
            wr = sb1("wr_sb", [128, 8, 36])
            b_wr = Buf("wr")
            k.dma("sync", wr[:], C.wr_d.rearrange("(k p) c -> p k c", p=128), writes=[b_wr])
            rb = sb1("rb_sb", [128, 36])
            b_rb = Buf("rb")
            k.dma("sync", rb[:], C.rb_d, writes=[b_rb])
            mx = [sb1("mx%d" % i, [128, 8, 128], BF16) for i in range(2)]
            b_mx = [Buf("mx%d" % i) for i in range(2)]
            xt = [sb1("xt%d" % i, [128, 1024]) for i in range(2)]
            b_xt = [Buf("xt%d" % i) for i in range(2)]
            rr = [sb1("rr%d" % i, [128, 1024]) for i in range(2)]
            b_rr = [Buf() for _ in range(2)]
            hh = [sb1("hh%d" % i, [128, 1024]) for i in range(2)]
            b_hh = [Buf() for _ in range(2)]
            hTf = [sb1("hTf%d" % i, [128, 8, 128]) for i in range(2)]
            b_hTf = [Buf() for _ in range(2)]
            st6 = sb1("st6", [128, 2, 2, 6])
            mv = sb1("mv", [128, 2, 2])
            b_st = [Buf() for _ in range(2)]
            b_mv = [Buf() for _ in range(2)]
            tmpr = sb1("tmpr", [128, 2, 32])
            b_tmp = [Buf() for _ in range(2)]
            sm = sb1("sm", [128, 2, 96])
            b_sm = [Buf() for _ in range(2)]
            for ti in range(NT if KC >= 6 else 0):
                bi = ti % 2
                is_s = ti >= 16
                if not is_s:
                    k.dma("sync", mx[bi][:, 0:4, :], C.mixA_d[:, :, 128 * ti:128 * ti + 128], reads=[C.b_mixA_d], writes=[b_mx[bi]])
                    k.dma("sync", mx[bi][:, 4:8, :], C.mixB_d[:, :, 128 * ti:128 * ti + 128], reads=[C.b_mixB_d], writes=[b_mx[bi]])
                    k.dma("sync", xt[bi][:], C.xo[128 * ti:128 * ti + 128, :], writes=[b_xt[bi]])
                else:
                    k.dma("sync", mx[bi][:], C.mixS_d, reads=[C.b_mixS_d], writes=[b_mx[bi]])
                    k.dma("sync", xt[bi][:], C.xs_pad, writes=[b_xt[bi]])
                for half in range(2 if KC >= 7 else 0):
                    pt_, pb_ = psT[half], psB[half]
                    for kk in range(8):
                        fn = lambda e, kk=kk, bi=bi, half=half, pt_=pt_: e.matmul(pt_[:, :], lhsT=mx[bi][:, kk, :], rhs=wo[:, kk, 512 * half:512 * half + 512],
                                                                                start=(kk == 0), stop=(kk == 7))
                        if kk == 0:
                            k.op("tensor", fn, reads=[b_mx[bi], b_wo], writes=[pb_])
                        else:
                            k.acc("tensor", fn, reads=[b_mx[bi], b_wo], acc=[pb_])
                    if KC < 8:
                        continue
                    k.op("vector", lambda e, bi=bi, half=half, pt_=pt_: e.scalar_tensor_tensor(
                        out=rr[bi][:, 512 * half:512 * half + 512], in0=xt[bi][:, 512 * half:512 * half + 512], scalar=ALPHA, in1=pt_[:, :],
                        op0=ALU.mult, op1=ALU.add), reads=[pb_, b_xt[bi]], writes=[b_rr[bi]])
                    k.op("vector", lambda e, bi=bi, half=half: e.bn_stats(out=st6[:, bi, half, :], in_=rr[bi][:, 512 * half:512 * half + 512]),
                         reads=[b_rr[bi]], writes=[b_st[bi]])
                if KC < 11:
                    continue
                k.op("vector", lambda e, bi=bi: e.bn_aggr(out=mv[:, bi, :], in_=st6[:, bi, :, :].rearrange("p a b -> p (a b)")), reads=[b_st[bi]], writes=[b_mv[bi]])
                k.op("scalar", lambda e, bi=bi: e.activation(out=mv[:, bi, 1:2], in_=mv[:, bi, 1:2], func=AF.Sqrt, bias=C.eps5[:, 0:1]), reads=[b_mv[bi], C.b_eps5], writes=[b_mv[bi]])
                k.op("vector", lambda e, bi=bi: e.reciprocal(out=mv[:, bi, 1:2], in_=mv[:, bi, 1:2]), reads=[b_mv[bi]], writes=[b_mv[bi]])
                k.op("vector", lambda e, bi=bi: e.tensor_scalar(out=hh[bi][:], in0=rr[bi][:], scalar1=mv[:, bi, 0:1], scalar2=mv[:, bi, 1:2], op0=ALU.subtract, op1=ALU.mult),
                     reads=[b_rr[bi], b_mv[bi]], writes=[b_hh[bi]])
                k.op("gpsimd", lambda e, bi=bi: e.tensor_tensor(out=hh[bi][:], in0=hh[bi][:], in1=lnp[:, 0, :], op=ALU.mult), reads=[b_hh[bi], b_lnp], writes=[b_hh[bi]])
                k.op("gpsimd", lambda e, bi=bi: e.tensor_tensor(out=hh[bi][:], in0=hh[bi][:], in1=lnp[:, 1, :], op=ALU.add), reads=[b_hh[bi], b_lnp], writes=[b_hh[bi]])
                k.op("gpsimd", lambda e, bi=bi, ti=ti: e.tensor_scalar(out=yacc[:, ti, :], in0=hh[bi][:], scalar1=ALPHA, scalar2=None, op0=ALU.mult),
                     reads=[b_hh[bi]], writes=[b_y[ti]])
                if KC < 12:
                    continue
                for g4 in range(2):
                    pt_, pb_ = psT[2 + g4], psB[2 + g4]
                    for j in range(4):
                        kk = 4 * g4 + j
                        fn = lambda e, kk=kk, j=j, bi=bi, pt_=pt_: e.transpose(pt_[:, 128 * j:128 * j + 128], hh[bi][:, 128 * kk:128 * kk + 128], cst[:, IDENT, :])
                        if j == 0:
                            k.op("tensor", fn, reads=[b_hh[bi], b_cst], writes=[pb_])
                        else:
                            k.acc("tensor", fn, reads=[b_hh[bi], b_cst], acc=[pb_])
                    k.op("vector", lambda e, g4=g4, bi=bi, pt_=pt_: e.tensor_copy(out=hTf[bi][:, 4 * g4:4 * g4 + 4, :], in_=pt_[:, :].rearrange("p (a c) -> p a c", a=4)),
                         reads=[pb_], writes=[b_hTf[bi]])
                    k.op("vector", lambda e, g4=g4, ti=ti, pt_=pt_: e.tensor_copy(out=hTb[:, 4 * g4:4 * g4 + 4, 128 * ti:128 * ti + 128], in_=pt_[:, :].rearrange("p (a c) -> p a c", a=4)),
                         reads=[pb_], writes=[b_hT[ti]])
                if KC < 13:
                    continue
                pl, plb = psT[4], psB[4]
                for kk in range(8):
                    fn = lambda e, kk=kk, bi=bi: e.matmul(pl[:, 0:36], lhsT=hTf[bi][:, kk, :], rhs=wr[:, kk, :], start=(kk == 0), stop=(kk == 7))
                    if kk == 0:
                        k.op("tensor", fn, reads=[b_hTf[bi], b_wr], writes=[plb])
                    else:
                        k.acc("tensor", fn, reads=[b_hTf[bi], b_wr], acc=[plb])
                if KC < 14:
                    continue
                R = lambda a, b_, bi=bi: sm[:, bi, a:b_]
                T3 = tmpr[:, bi, :].rearrange("p (g e) -> p g e", g=4)
                sm_b, tmp_b = b_sm[bi], b_tmp[bi]

                def VV(eng, method, reads, writes, **aps):
                    k.op(eng, lambda e, aps=aps, method=method: getattr(e, method)(**aps), reads=reads, writes=writes)
                VV("vector", "tensor_tensor", [plb, b_rb], [sm_b], out=R(0, 36), in0=pl[:, 0:36], in1=rb[:], op=ALU.add)
                VV("vector", "tensor_reduce", [sm_b], [sm_b], out=R(36, 37), in_=R(0, 4), axis=AX.X, op=ALU.max)
                VV("vector", "tensor_scalar", [sm_b], [sm_b], out=R(40, 44), in0=R(0, 4), scalar1=R(36, 37), scalar2=None, op0=ALU.is_equal)
                VV("vector", "tensor_scalar", [sm_b], [sm_b], out=R(37, 38), in0=R(36, 37), scalar1=-1.0, scalar2=None, op0=ALU.mult)
                VV("scalar", "activation", [sm_b], [sm_b], out=R(89, 93), in_=R(0, 4), func=AF.Exp, bias=R(37, 38), accum_out=R(38, 39))
                VV("vector", "reciprocal", [sm_b], [sm_b], out=R(38, 39), in_=R(38, 39))
                VV("vector", "tensor_tensor", [sm_b], [tmp_b], out=T3, in0=R(4, 36).rearrange("p (g e) -> p g e", g=4),
                   in1=R(40, 44).unsqueeze(2).to_broadcast([128, 4, 8]), op=ALU.mult)
                VV("vector", "tensor_reduce", [tmp_b], [sm_b], out=R(44, 52), in_=T3.rearrange("p g e -> p e g"), axis=AX.X, op=ALU.add)
                VV("vector", "tensor_reduce", [sm_b], [sm_b], out=R(52, 53), in_=R(44, 52), axis=AX.X, op=ALU.max)
                VV("vector", "tensor_scalar", [sm_b], [sm_b], out=R(54, 62), in0=R(44, 52), scalar1=R(52, 53), scalar2=None, op0=ALU.is_equal)
                VV("vector", "scalar_tensor_tensor", [sm_b], [sm_b], out=R(62, 70), in0=R(54, 62), scalar=-1e30, in1=R(44, 52), op0=ALU.mult, op1=ALU.add)
                VV("vector", "tensor_reduce", [sm_b], [sm_b], out=R(53, 54), in_=R(62, 70), axis=AX.X, op=ALU.max)
                VV("vector", "tensor_scalar", [sm_b], [sm_b], out=R(70, 78), in0=R(62, 70), scalar1=R(53, 54), scalar2=None, op0=ALU.is_equal)
                VV("vector", "tensor_tensor", [sm_b], [sm_b], out=R(78, 79), in0=R(53, 54), in1=R(52, 53), op=ALU.subtract)
                VV("scalar", "activation", [sm_b], [sm_b], out=R(78, 79), in_=R(78, 79), func=AF.Exp)
                VV("vector", "tensor_scalar", [sm_b], [sm_b], out=R(79, 80), in0=R(78, 79), scalar1=1.0, scalar2=None, op0=ALU.add)
                VV("vector", "reciprocal", [sm_b], [sm_b], out=R(79, 80), in_=R(79, 80))
                VV("vector", "tensor_tensor", [sm_b], [sm_b], out=R(80, 81), in0=R(78, 79), in1=R(79, 80), op=ALU.mult)
                VV("vector", "tensor_scalar", [sm_b], [sm_b], out=R(79, 81), in0=R(79, 81), scalar1=R(38, 39), scalar2=None, op0=ALU.mult)
                VV("vector", "tensor_scalar", [sm_b], [sm_b], out=R(81, 89), in0=R(54, 62), scalar1=R(79, 80), scalar2=None, op0=ALU.mult)
                VV("vector", "scalar_tensor_tensor", [sm_b], [sm_b], out=R(81, 89), in0=R(70, 78), scalar=R(80, 81), in1=R(81, 89), op0=ALU.mult, op1=ALU.add)
                VV("vector", "tensor_tensor", [sm_b], [b_G[ti]], out=G[:, ti, :].rearrange("p (g e) -> p g e", g=4),
                   in0=R(40, 44).unsqueeze(2).to_broadcast([128, 4, 8]), in1=R(81, 89).unsqueeze(1).to_broadcast([128, 4, 8]), op=ALU.mult)
        S.barrier()

        with ExitStack() as s2:
            sb2 = lambda n, s, dt=F32: s2.enter_context(nc.sbuf_tensor(n, list(s), dt))
            wg = [sb2("wg%d" % i, [128, 8, 512], BF16) for i in range(2)]
            wu_ = [sb2("wup%d" % i, [128, 8, 512], BF16) for i in range(2)]
            wd = [sb2("wd%d" % i, [128, 4, 1024], BF16) for i in range(2)]
            b_wg = [Buf("wg%d" % i) for i in range(2)]
            b_wu = [Buf("wup%d" % i) for i in range(2)]
            b_wd = [Buf("wd%d" % i) for i in range(2)]
            sg = [sb2("sg%d" % i, [128, 512]) for i in range(2)]
            b_sg = [Buf() for _ in range(2)]
            act = [sb2("act%d" % i, [128, 4, 512], BF16) for i in range(2)]
            b_act = [Buf() for _ in range(2)]
            chunks = [(512 * c, 512) for c in range(4)] + ([(2048, 128)] if NT > 16 else [])
            cc = 0
            fcnt = 0
            for ex in range(C.NEXP if KC >= 20 else 0):
                wi = ex % 2
                k.gload(wg[wi][:], C.w_gate[ex].rearrange("(k p) f -> p k f", p=128), writes=[b_wg[wi]])
                k.gload(wu_[wi][:], C.w_up[ex].rearrange("(k p) f -> p k f", p=128), writes=[b_wu[wi]])
                k.gload(wd[wi][:], C.w_down[ex].rearrange("(k p) d -> p k d", p=128), writes=[b_wd[wi]])
                for (t0, tn) in chunks:
                    ai = cc % 2
                    cc += 1
                    tiles = list(range(t0 // 128, (t0 + tn) // 128))
                    rd_h = [b_hT[t] for t in tiles]
                    for f in range(4):
                        pg, pgb = psT[0 + fcnt % 2], psB[0 + fcnt % 2]
                        pu, pub = psT[2 + fcnt % 2], psB[2 + fcnt % 2]
                        si = fcnt % 2
                        fcnt += 1
                        for (pp, ppb, ww, bw) in ((pg, pgb, wg[wi], b_wg[wi]), (pu, pub, wu_[wi], b_wu[wi])):
                            for kk in range(8):
                                fn = lambda e, kk=kk, pp=pp, ww=ww, f=f, t0=t0, tn=tn: e.matmul(pp[:, 0:tn], lhsT=ww[:, kk, 128 * f:128 * f + 128], rhs=hTb[:, kk, t0:t0 + tn],
                                                                                            start=(kk == 0), stop=(kk == 7))
                                if kk == 0:
                                    k.op("tensor", fn, reads=[bw] + rd_h, writes=[ppb])
                                else:
                                    k.acc("tensor", fn, reads=[bw] + rd_h, acc=[ppb])
                        k.op("scalar", lambda e, si=si, pg=pg, tn=tn: e.activation(out=sg[si][:, 0:tn], in_=pg[:, 0:tn], func=AF.Silu), reads=[pgb], writes=[b_sg[si]])
                        k.op("vector", lambda e, si=si, ai=ai, f=f, pu=pu, tn=tn: e.tensor_tensor(out=act[ai][:, f, 0:tn], in0=sg[si][:, 0:tn], in1=pu[:, 0:tn], op=ALU.mult),
                             reads=[b_sg[si], pub], writes=[b_act[ai]])
                    for tt, tile in enumerate(tiles):
                        for half in range(2):
                            py, pyb = psT[4 + (tt * 2 + half) % 4], psB[4 + (tt * 2 + half) % 4]
                            for f in range(4):
                                fn = lambda e, f=f, ai=ai, tt=tt, half=half, py=py, wi=wi: e.matmul(py[:, :], lhsT=act[ai][:, f, 128 * tt:128 * tt + 128],
                                                                                               rhs=wd[wi][:, f, 512 * half:512 * half + 512], start=(f == 0), stop=(f == 3))
                                if f == 0:
                                    k.op("tensor", fn, reads=[b_act[ai], b_wd[wi]], writes=[pyb])
                                else:
                                    k.acc("tensor", fn, reads=[b_act[ai], b_wd[wi]], acc=[pyb])
                            k.op("vector", lambda e, tile=tile, half=half, py=py, ex=ex: e.scalar_tensor_tensor(
                                out=yacc[:, tile, 512 * half:512 * half + 512], in0=py[:, :], scalar=G[:, tile, ex:ex + 1], in1=yacc[:, tile, 512 * half:512 * half + 512],
                                op0=ALU.mult, op1=ALU.add), reads=[pyb, b_G[tile], b_y[tile]], writes=[b_y[tile]])
        S.barrier()

        with ExitStack() as s3:
            sb3 = lambda n, s, dt=F32: s3.enter_context(nc.sbuf_tensor(n, list(s), dt))
            k.dma("sync", lnp[:], C.lnp_d[:, 2:4, :], writes=[b_lnp])
            st6, mv = st6_c3, mv_c3
            b_st = [Buf() for _ in range(2)]
            b_mv = [Buf() for _ in range(2)]
            yo = [sb3("yo%d" % i, [128, 1024]) for i in range(2)]
            b_yo = [Buf("yo%d" % i) for i in range(2)]
            for ti in range(NT if KC >= 30 else 0):
                bi = ti % 2
                for half in range(2):
                    k.op("vector", lambda e, bi=bi, half=half, ti=ti: e.bn_stats(out=st6[:, bi, half, :], in_=yacc[:, ti, 512 * half:512 * half + 512]),
                         reads=[b_y[ti]], writes=[b_st[bi]])
                k.op("vector", lambda e, bi=bi: e.bn_aggr(out=mv[:, bi, :], in_=st6[:, bi, :, :].rearrange("p a b -> p (a b)")), reads=[b_st[bi]], writes=[b_mv[bi]])
                k.op("scalar", lambda e, bi=bi: e.activation(out=mv[:, bi, 1:2], in_=mv[:, bi, 1:2], func=AF.Sqrt, bias=C.eps5[:, 0:1]), reads=[b_mv[bi], C.b_eps5], writes=[b_mv[bi]])
                k.op("vector", lambda e, bi=bi: e.reciprocal(out=mv[:, bi, 1:2], in_=mv[:, bi, 1:2]), reads=[b_mv[bi]], writes=[b_mv[bi]])
                k.op("vector", lambda e, bi=bi, ti=ti: e.tensor_scalar(out=yo[bi][:], in0=yacc[:, ti, :], scalar1=mv[:, bi, 0:1], scalar2=mv[:, bi, 1:2], op0=ALU.subtract, op1=ALU.mult),
                     reads=[b_y[ti], b_mv[bi]], writes=[b_yo[bi]])
                k.op("gpsimd", lambda e, bi=bi: e.tensor_tensor(out=yo[bi][:], in0=yo[bi][:], in1=lnp[:, 0, :], op=ALU.mult), reads=[b_yo[bi], b_lnp], writes=[b_yo[bi]])
                k.op("gpsimd", lambda e, bi=bi: e.tensor_tensor(out=yo[bi][:], in0=yo[bi][:], in1=lnp[:, 1, :], op=ALU.add), reads=[b_yo[bi], b_lnp], writes=[b_yo[bi]])
                if ti < 16:
                    final.append(k.dma("sync", C.y_out[128 * ti:128 * ti + 128, :], yo[bi][:], reads=[b_yo[bi]]))
                else:
                    final.append(k.dma("sync", C.ys_out, yo[bi][0:NS, :], reads=[b_yo[bi]]))


def phase_s(C):
    nc, k, S = C.nc, C.k, C.S
    psT, psB = C.psT, C.psB
    final = C.final
    ps_d, cs_d, ms_d = C.ps_d, C.cs_d, C.ms_d
    b_ps_d, b_cs_d, b_ms_d = Buf("ps_d"), Buf("cs_d"), Buf("ms_d")
    w_v = C.w_v

    def VV(eng, method, reads, writes, **aps):
        return k.op(eng, lambda e, aps=aps, method=method: getattr(e, method)(**aps), reads=reads, writes=writes)

    with ExitStack() as s1:
        sb1 = lambda n, s, dt=F32: s1.enter_context(nc.sbuf_tensor(n, list(s), dt))
        xsTb = sb1("xsTb", [128, 8, NS], BF16)
        b_xs = Buf("xsTb")
        k.gload(xsTb[:], C.xsT.rearrange("(k p) t -> p k t", p=128), writes=[b_xs])
        wS = [sb1("wS%d" % i, [128, 8, 512], BF16) for i in range(2)]
        b_wS = [Buf("wS%d" % i) for i in range(2)]
        p_s = sb1("p_s", [NS, IN_COLS])
        b_p = Buf("p_s")
        for cchunk in range(8):
            c0 = 512 * cchunk
            cn = min(512, IN_COLS - c0)
            wi = cchunk % 2
            k.gload(wS[wi][:, :, 0:cn], w_v[:, :, c0:c0 + cn], writes=[b_wS[wi]])
            pt_, pb_ = psT[wi], psB[wi]
            for kk in range(8):
                fn = lambda e, kk=kk, wi=wi, cn=cn, pt_=pt_: e.matmul(pt_[0:NS, 0:cn], lhsT=xsTb[:, kk, :], rhs=wS[wi][:, kk, 0:cn], start=(kk == 0), stop=(kk == 7))
                if kk == 0:
                    k.op("tensor", fn, reads=[b_xs, b_wS[wi]], writes=[pb_])
                else:
                    k.acc("tensor", fn, reads=[b_xs, b_wS[wi]], acc=[pb_])
            VV("vector", "tensor_copy", [pb_], [b_p], out=p_s[:, c0:c0 + cn], in_=pt_[0:NS, 0:cn])
        k.dma("sync", ps_d, p_s[:], reads=[b_p], writes=[b_ps_d])
        final.append(k.dma("sync", C.knew, p_s[:, COL_KA:COL_KA + 512], reads=[b_p], slot="knew"))
        final.append(k.dma("sync", C.vnew, p_s[:, COL_VA:COL_VA + 512], reads=[b_p], slot="vnew"))
        final.append(k.dma("sync", C.conv_s[:, 2, :], p_s[:, COL_UB:COL_UB + 1536], reads=[b_p], slot="convs"))
        cst_ = sb1("cst_", [NS, 3, 1536])
        b_cst_ = Buf("cst_")
        k.dma("sync", cst_[:], C.scv, writes=[b_cst_])
        final.append(k.dma("sync", C.conv_s[:, 0:2, :], cst_[:, 1:3, :], reads=[b_cst_], slot="convs"))
        cwr = sb1("cwr_sb", [NS, 4, 1536])
        b_cwr = Buf("cwr")
        k.dma("sync", cwr[:], C.cwr_d, writes=[b_cwr])
        cacc = sb1("cacc", [NS, 1536])
        ctmp = sb1("ctmp", [NS, 1536])
        b_ca, b_ct = Buf("cacc"), Buf()
        VV("vector", "tensor_tensor", [b_p, b_cwr], [b_ca], out=cacc[:], in0=p_s[:, COL_UB:COL_UB + 1536], in1=cwr[:, 3, :], op=ALU.mult)
        for i in range(3):
            VV("vector", "tensor_tensor", [b_cst_, b_cwr], [b_ct], out=ctmp[:], in0=cst_[:, i, :], in1=cwr[:, i, :], op=ALU.mult)
            VV("vector", "tensor_tensor", [b_ct, b_ca], [b_ca], out=cacc[:], in0=cacc[:], in1=ctmp[:], op=ALU.add)
        VV("scalar", "activation", [b_ca], [b_ca], out=cacc[:], in_=cacc[:], func=AF.Silu)
        k.dma("sync", cs_d, cacc[:], reads=[b_ca], writes=[b_cs_d])
    S.barrier()

    with ExitStack() as s2:
        sb2 = lambda n, s, dt=F32: s2.enter_context(nc.sbuf_tensor(n, list(s), dt))
        qkv = sb2("qkv_nh", [128, 3, 64])
        b_qkv = Buf("qkv_nh")
        for j, c0 in enumerate((COL_QA, COL_KA, COL_VA)):
            k.dma("sync", qkv[:, j, :], bass.AP(ps_d.tensor, c0, [[IN_COLS, NS], [64, 8], [1, 64]]), reads=[b_ps_d], writes=[b_qkv])
        sbias = sb2("sbias_sb", [128, 3, 129])
        b_sb = Buf("sbias")
        k.dma("sync", sbias[:], C.sbias_d, writes=[b_sb])
        Kb = sb2("Kb", [128, 128, 64])
        Vb = sb2("Vb", [128, 128, 64])
        b_Kb, b_Vb = Buf("Kb"), Buf("Vb")
        tmpS = sb2("tmpS", [128, 128, 64])
        b_tmp = Buf()
        sc = sb2("sc", [128, 3, 129])
        b_sc = Buf()
        sm = sb2("smS", [128, 16])
        b_sm = Buf()
        oacc = sb2("oacc", [128, 64])
        otmp = sb2("otmp", [128, 64])
        b_oa, b_ot = Buf("oacc"), Buf()
        VV("vector", "tensor_tensor", [b_qkv], [b_ot], out=otmp[:], in0=qkv[:, 0, :], in1=qkv[:, 1, :], op=ALU.mult)
        VV("vector", "tensor_reduce", [b_ot], [b_sm], out=sm[:, 0:1], in_=otmp[:], axis=AX.X, op=ALU.add)
        for br, (_, dil) in enumerate(BRANCHES):
            for n in range(NS):
                src = bass.AP(C.ck.tensor, n * 2048 * 512 + (2048 - 128 * dil) * 512, [[64, 8], [dil * 512, 128], [1, 64]])
                k.dma("sync", Kb[8 * n:8 * n + 8, :, :], src, writes=[b_Kb]) if n == 0 else k.S.dmaop(
                    "sync", "Kb", lambda e, n=n, src=src: e.dma_start(out=Kb[8 * n:8 * n + 8, :, :], in_=src), [])
            b_Kb.writer = ("D", "Kb", k.S.dma_sems["Kb"][1])
            VV("vector", "tensor_tensor", [b_Kb, b_qkv], [b_tmp], out=tmpS[:], in0=Kb[:], in1=qkv[:, 0, :].unsqueeze(1).to_broadcast([128, 128, 64]), op=ALU.mult)
            VV("vector", "tensor_reduce", [b_tmp], [b_sc], out=sc[:, br, 0:128], in_=tmpS[:], axis=AX.X, op=ALU.add)
            VV("vector", "tensor_copy", [b_sm], [b_sc], out=sc[:, br, 128:129], in_=sm[:, 0:1])
        VV("vector", "scalar_tensor_tensor", [b_sc, b_sb], [b_sc], out=sc[:].rearrange("p a b -> p (a b)"), in0=sc[:].rearrange("p a b -> p (a b)"),
           scalar=0.125, in1=sbias[:].rearrange("p a b -> p (a b)"), op0=ALU.mult, op1=ALU.add)
        VV("vector", "tensor_reduce", [b_sc], [b_sm], out=sm[:, 1:2], in_=sc[:].rearrange("p a b -> p (a b)"), axis=AX.X, op=ALU.max)
        VV("vector", "tensor_scalar", [b_sm], [b_sm], out=sm[:, 2:3], in0=sm[:, 1:2], scalar1=-1.0, scalar2=None, op0=ALU.mult)
        VV("scalar", "activation", [b_sc, b_sm], [b_sc, b_sm], out=sc[:].rearrange("p a b -> p (a b)"), in_=sc[:].rearrange("p a b -> p (a b)"), func=AF.Exp,
           bias=sm[:, 2:3], accum_out=sm[:, 3:4])
        VV("vector", "reciprocal", [b_sm], [b_sm], out=sm[:, 3:4], in_=sm[:, 3:4])
        VV("vector", "tensor_reduce", [b_sc], [b_sm], out=sm[:, 4:5], in_=sc[:, :, 128], axis=AX.X, op=ALU.add)
        VV("vector", "tensor_scalar", [b_qkv, b_sm], [b_oa], out=oacc[:], in0=qkv[:, 2, :], scalar1=sm[:, 4:5], scalar2=None, op0=ALU.mult)
        for br, (_, dil) in enumerate(BRANCHES):
            for n in range(NS):
                src = bass.AP(C.cv.tensor, n * 2048 * 512 + (2048 - 128 * dil) * 512, [[64, 8], [dil * 512, 128], [1, 64]])
                if n == 0:
                    k.dma("sync", Vb[8 * n:8 * n + 8, :, :], src, writes=[b_Vb])
                else:
                    k.S.dmaop("sync", "Vb", lambda e, n=n, src=src: e.dma_start(out=Vb[8 * n:8 * n + 8, :, :], in_=src), [])
            b_Vb.writer = ("D", "Vb", k.S.dma_sems["Vb"][1])
            VV("vector", "tensor_tensor", [b_Vb, b_sc], [b_tmp], out=tmpS[:], in0=Vb[:], in1=sc[:, br, 0:128].unsqueeze(2).to_broadcast([128, 128, 64]), op=ALU.mult)
            VV("vector", "tensor_reduce", [b_tmp], [b_ot], out=otmp[:], in_=tmpS[:].rearrange("p i d -> p d i"), axis=AX.X, op=ALU.add)
            VV("vector", "tensor_tensor", [b_ot, b_oa], [b_oa], out=oacc[:], in0=oacc[:], in1=otmp[:], op=ALU.add)
        VV("vector", "tensor_scalar", [b_oa, b_sm], [b_oa], out=oacc[:], in0=oacc[:], scalar1=sm[:, 3:4], scalar2=None, op0=ALU.mult)
        k.dma("sync", bass.AP(ms_d.tensor, 0, [[D, NS], [64, 8], [1, 64]]), oacc[:], reads=[b_oa], writes=[b_ms_d])
    S.barrier()

    with ExitStack() as s3:
        sb3 = lambda n, s, dt=F32: s3.enter_context(nc.sbuf_tensor(n, list(s), dt))
        NP = NS * 4
        St = sb3("St", [NP, 128, 128])
        b_St = Buf("St")
        for q4 in range(4):
            k.dma("sync", St[:, 32 * q4:32 * q4 + 32, :], C.sst[:, 32 * q4:32 * q4 + 32, :], writes=[b_St]) if q4 == 0 else k.S.dmaop(
                "sync", "St", lambda e, q4=q4: e.dma_start(out=St[:, 32 * q4:32 * q4 + 32, :], in_=C.sst[:, 32 * q4:32 * q4 + 32, :]), [])
        b_St.writer = ("D", "St", k.S.dma_sems["St"][1])
        T2 = sb3("T2", [NP, 128, 128])
        b_T2 = Buf()
        c3 = sb3("c3", [NP, 3, 128])
        b_c3 = Buf("c3")
        for ty in range(3):
            k.dma("sync", c3[:, ty, :], bass.AP(cs_d.tensor, 512 * ty, [[1536, NS], [128, 4], [1, 128]]), reads=[b_cs_d], writes=[b_c3])
        zz = sb3("zz", [NP, 128])
        b_zz = Buf("zz")
        k.dma("sync", zz[:], bass.AP(ps_d.tensor, COL_ZB, [[IN_COLS, NS], [128, 4], [1, 128]]), reads=[b_ps_d], writes=[b_zz])
        ab = sb3("ab_s", [NP, 2])
        b_ab = Buf("ab_s")
        k.dma("sync", ab[:, 0:1], bass.AP(ps_d.tensor, COL_AB, [[IN_COLS, NS], [1, 4], [1, 1]]), reads=[b_ps_d], writes=[b_ab])
        k.dma("sync", ab[:, 1:2], bass.AP(ps_d.tensor, COL_BB, [[IN_COLS, NS], [1, 4], [1, 1]]), reads=[b_ps_d], writes=[b_ab])
        sp = sb3("sprm_sb", [NP, 2 + 128])
        b_sp = Buf("sprm")
        k.dma("sync", sp[:], C.sprm_d, writes=[b_sp])
        w = sb3("wS3", [NP, 24])
        b_w = Buf()
        jk = sb3("jkS3", [NP, 128])
        b_jk = Buf()
        VV("vector", "tensor_tensor", [b_ab, b_sp], [b_w], out=w[:, 0:1], in0=ab[:, 0:1], in1=sp[:, 1:2], op=ALU.add)
        VV("scalar", "activation", [b_w], [b_w], out=w[:, 0:1], in_=w[:, 0:1], func=AF.Exp)
        VV("scalar", "activation", [b_w], [b_w], out=w[:, 0:1], in_=w[:, 0:1], func=AF.Ln, bias=1.0)
        VV("scalar", "activation", [b_sp], [b_w], out=w[:, 1:2], in_=sp[:, 0:1], func=AF.Exp)
        VV("vector", "scalar_tensor_tensor", [b_w], [b_w], out=w[:, 2:3], in0=w[:, 0:1], scalar=-1.0, in1=w[:, 1:2], op0=ALU.mult, op1=ALU.mult)
        VV("scalar", "activation", [b_w], [b_w], out=w[:, 3:4], in_=w[:, 2:3], func=AF.Exp)
        VV("scalar", "activation", [b_ab], [b_w], out=w[:, 4:5], in_=ab[:, 1:2], func=AF.Sigmoid)
        for j in range(2):
            VV("scalar", "activation", [b_c3], [b_jk, b_w], out=jk[:], in_=c3[:, j, :], func=AF.Square, accum_out=w[:, 5 + j:6 + j])
            VV("scalar", "activation", [b_w, C.b_eps], [b_w], out=w[:, 5 + j:6 + j], in_=w[:, 5 + j:6 + j], func=AF.Sqrt, bias=C.eps6[0:NP, 0:1])
            VV("vector", "reciprocal", [b_w], [b_w], out=w[:, 5 + j:6 + j], in_=w[:, 5 + j:6 + j])
        VV("vector", "tensor_scalar", [b_c3, b_w], [b_c3], out=c3[:, 0, :], in0=c3[:, 0, :], scalar1=w[:, 5:6], scalar2=128.0 ** -0.5, op0=ALU.mult, op1=ALU.mult)
        VV("vector", "tensor_scalar", [b_c3, b_w], [b_c3], out=c3[:, 1, :], in0=c3[:, 1, :], scalar1=w[:, 6:7], scalar2=None, op0=ALU.mult)
        mem = sb3("memS", [NP, 4, 128])
        b_mem = Buf()
        VV("vector", "tensor_tensor", [b_St, b_c3], [b_T2], out=T2[:], in0=St[:], in1=c3[:, 1, :].unsqueeze(2).to_broadcast([NP, 128, 128]), op=ALU.mult)
        VV("vector", "tensor_reduce", [b_T2], [b_mem], out=mem[:, 0, :], in_=T2[:].rearrange("p d e -> p e d"), axis=AX.X, op=ALU.add)
        VV("vector", "scalar_tensor_tensor", [b_mem, b_w, b_c3], [b_mem], out=mem[:, 1, :], in0=mem[:, 0, :], scalar=w[:, 3:4], in1=c3[:, 2, :], op0=ALU.mult, op1=ALU.subtract)
        VV("vector", "tensor_scalar", [b_mem, b_w], [b_mem], out=mem[:, 1, :], in0=mem[:, 1, :], scalar1=w[:, 4:5], scalar2=-1.0, op0=ALU.mult, op1=ALU.mult)
        VV("vector", "tensor_tensor", [b_c3, b_mem], [b_T2], out=T2[:], in0=c3[:, 1, :].unsqueeze(2).to_broadcast([NP, 128, 128]),
           in1=mem[:, 1, :].unsqueeze(1).to_broadcast([NP, 128, 128]), op=ALU.mult)
        VV("vector", "scalar_tensor_tensor", [b_St, b_T2, b_w], [b_St], out=St[:], in0=St[:], scalar=w[:, 3:4], in1=T2[:], op0=ALU.mult, op1=ALU.add)
        for q4 in range(4):
            final.append(k.dma("sync", C.ssm_s[:, 32 * q4:32 * q4 + 32, :], St[:, 32 * q4:32 * q4 + 32, :], reads=[b_St], slot="ssms"))
        VV("vector", "tensor_tensor", [b_St, b_c3], [b_T2], out=T2[:], in0=St[:], in1=c3[:, 0, :].unsqueeze(2).to_broadcast([NP, 128, 128]), op=ALU.mult)
        VV("vector", "tensor_reduce", [b_T2], [b_mem], out=mem[:, 2, :], in_=T2[:].rearrange("p d e -> p e d"), axis=AX.X, op=ALU.add)
        VV("scalar", "activation", [b_mem], [b_jk, b_w], out=jk[:], in_=mem[:, 2, :], func=AF.Square, accum_out=w[:, 8:9])
        VV("scalar", "activation", [b_w, C.b_eps], [b_w], out=w[:, 8:9], in_=w[:, 8:9], func=AF.Sqrt, scale=1.0 / 128.0, bias=C.eps6[0:NP, 0:1])
        VV("vector", "reciprocal", [b_w], [b_w], out=w[:, 8:9], in_=w[:, 8:9])
        VV("scalar", "activation", [b_zz], [b_zz], out=zz[:], in_=zz[:], func=AF.Silu)
        VV("vector", "tensor_tensor", [b_zz, b_sp], [b_zz], out=zz[:], in0=zz[:], in1=sp[:, 2:130], op=ALU.mult)
        VV("vector", "scalar_tensor_tensor", [b_mem, b_w, b_zz], [b_mem], out=mem[:, 3, :], in0=mem[:, 2, :], scalar=w[:, 8:9], in1=zz[:], op0=ALU.mult, op1=ALU.mult)
        k.dma("sync", bass.AP(ms_d.tensor, 512, [[D, NS], [128, 4], [1, 128]]), mem[:, 3, :], reads=[b_mem], writes=[b_ms_d])
    S.barrier()

    with ExitStack() as s4:
        sb4 = lambda n, s, dt=F32: s4.enter_context(nc.sbuf_tensor(n, list(s), dt))
        msf = sb4("msf", [128, 1024])
        b_msf = Buf("msf")
        VV("vector", "memset", [], [b_msf], ap=msf[:], constant=0.0)
        k.dma("sync", msf[0:NS, :], ms_d, reads=[b_ms_d], writes=[b_msf])
        mxS = sb4("mxS", [128, 8, 128], BF16)
        b_mxS = Buf("mxS")
        for g4 in range(2):
            pt_, pb_ = psT[2 + g4], psB[2 + g4]
            for j in range(4):
                kk = 4 * g4 + j
                fn = lambda e, kk=kk, j=j, pt_=pt_: e.transpose(pt_[:, 128 * j:128 * j + 128], msf[:, 128 * kk:128 * kk + 128], C.cst[:, 0, :])
                if j == 0:
                    k.op("tensor", fn, reads=[b_msf, C.b_cst], writes=[pb_])
                else:
                    k.acc("tensor", fn, reads=[b_msf, C.b_cst], acc=[pb_])
            VV("vector", "tensor_copy", [pb_], [b_mxS], out=mxS[:, 4 * g4:4 * g4 + 4, :], in_=pt_[:, :].rearrange("p (a c) -> p a c", a=4))
        k.dma("sync", C.mixS_d, mxS[:], reads=[b_mxS], writes=[C.b_mixS_d])
    S.barrier()


def _consts():
    t = np.arange(128)
    same = (t[:, None] // 64) == (t[None, :] // 64)
    cst = np.zeros((128, 7, 128), np.float32)
    cst[:, 0] = np.eye(128)
    cst[:, 1] = -np.eye(128)
    cst[:, 2] = (same & (t[:, None] <= t[None, :]))
    cst[:, 3] = (t[:, None] < 64) * np.ones((1, 128))
    cst[:, 4] = (t[:, None] >= 64) * np.ones((1, 128))
    cst[:, 5] = np.where(same & (t[None, :] <= t[:, None]), 0.0, NEG)
    cst[:, 6] = (same & (t[None, :] < t[:, None]))
    return cst


def _params(inp):
    prm = np.zeros((128, 184), np.float32)
    prm[:, 0:4] = inp["a_log"][0][None, :]
    prm[:, 4:8] = inp["dt_bias"][0][None, :]
    prm[:, 8:136] = inp["o_norm_g"][0][None, :]
    cw = inp["conv_w"][0]
    prm[:, 136:184] = cw.reshape(4, 12, 128).transpose(2, 1, 0).reshape(128, 48)
    return prm


def _sample_bias(rel_bias):
    out = np.empty((128, 3, 129), np.float32)
    h = np.arange(128) % 8
    for br, (_, dil) in enumerate(BRANCHES):
        dist = np.concatenate([dil * (128 - np.arange(128)), [0]])
        out[:, br, :] = rel_bias[_rel_bucket_np(dist)][:, h].T
    return out


def _core_inputs(c, inp, bt):
    b, hf = divmod(c, 2)
    x = inp["x_prompt"][b]
    xT = np.zeros((D, EXT), np.float32)
    if hf == 1:
        xT[:, :] = x.T
    else:
        xT[:, HALF:] = x[:HALF].T
    valid = np.ones((128, 32), np.float32)
    if hf == 0:
        valid[:, :16] = 0.0
    return {
        "xT": np.ascontiguousarray(xT),
        "xo": np.ascontiguousarray(x[HALF * hf:HALF * hf + HALF]),
        "valid": valid,
        "w_in": np.ascontiguousarray(inp["w_in"][0]),
        "bt": bt,
        "cst": _consts(),
        "prm": _params(inp),
        "w_out": np.ascontiguousarray(inp["w_out"][0]),
        "lnp": np.ascontiguousarray(np.broadcast_to(np.stack([inp["ln1_g"][0], inp["ln1_b"][0], inp["ln2_g"][0], inp["ln2_b"][0]])[None], (128, 4, D))),
        "wr": np.ascontiguousarray(np.concatenate([inp["w_group"][0], inp["w_router"][0]], axis=1)),
        "rb": np.ascontiguousarray(np.broadcast_to(np.concatenate([inp["b_group"][0], inp["b_router"][0].reshape(-1)])[None], (128, 36))),
        "w_gate": np.ascontiguousarray(inp["w_gate"][0]),
        "w_up": np.ascontiguousarray(inp["w_up"][0]),
        "w_down": np.ascontiguousarray(inp["w_down"][0]),
        "xsT": np.ascontiguousarray(inp["x_sample"][NS * c:NS * c + NS, 0, :].T),
        "ck": np.ascontiguousarray(inp["cache_a_k"][0, NS * c:NS * c + NS].reshape(NS, 2048, 512)),
        "cv": np.ascontiguousarray(inp["cache_a_v"][0, NS * c:NS * c + NS].reshape(NS, 2048, 512)),
        "sst": np.ascontiguousarray(inp["state_b_ssm"][0, NS * c:NS * c + NS].reshape(NS * 4, 128, 128)),
        "scv": np.ascontiguousarray(inp["state_b_conv"][0, NS * c:NS * c + NS]),
        "sbias": _sample_bias(inp["rel_bias"].astype(np.float32)),
        "cwr": np.ascontiguousarray(np.broadcast_to(inp["conv_w"][0][None], (NS, 4, 1536))),
        "sprm": np.ascontiguousarray(np.concatenate([np.tile(inp["a_log"][0], NS)[:, None], np.tile(inp["dt_bias"][0], NS)[:, None],
                                                       np.broadcast_to(inp["o_norm_g"][0][None], (NS * 4, 128))], axis=1)),
        "xs_pad": np.ascontiguousarray(np.concatenate([inp["x_sample"][NS * c:NS * c + NS, 0, :], np.zeros((128 - NS, D), np.float32)], axis=0)),
    }


def kernel(**inputs):
    inp = {k_: np.asarray(v) for k_, v in inputs.items()}
    bt = _bias_tiles(inp["rel_bias"].astype(np.float32))
    nc = build()
    in_maps = [_core_inputs(c, inp, bt) for c in range(NCORES)]
    res = run_bass_kernel_spmd(nc, in_maps, core_ids=list(range(NCORES)))
    r = res.results
    y_prompt = np.stack([np.concatenate([r[2 * b]["y_out"], r[2 * b + 1]["y_out"]], axis=0) for b in range(4)])
    k_win = np.stack([r[2 * b + 1]["kwin"].reshape(HALF, 8, 64) for b in range(4)])[None]
    v_win = np.stack([r[2 * b + 1]["vwin"].reshape(HALF, 8, 64) for b in range(4)])[None]
    ssm_p = np.stack([r[2 * b + 1]["ssm_p"] for b in range(4)])[None]
    conv_p = np.stack([r[2 * b + 1]["conv_p"] for b in range(4)])[None]
    y_sample = np.concatenate([r[c]["ys_out"] for c in range(NCORES)], axis=0)[:, None, :]
    k_new = np.concatenate([r[c]["knew"] for c in range(NCORES)], axis=0).reshape(1, 128, 1, 8, 64)
    v_new = np.concatenate([r[c]["vnew"] for c in range(NCORES)], axis=0).reshape(1, 128, 1, 8, 64)
    ssm_s = np.concatenate([r[c]["ssm_s"] for c in range(NCORES)], axis=0).reshape(1, 128, 4, 128, 128)
    conv_s = np.concatenate([r[c]["conv_s"] for c in range(NCORES)], axis=0)[None]
    return (y_prompt, y_sample, k_win, v_win, k_new, v_new, ssm_p, ssm_s, conv_p, conv_s)
```

```python
import math
import os
from contextlib import ExitStack

import numpy as np
import concourse.bass as bass
import concourse.mybir as mybir
from concourse.bass_utils import run_bass_kernel_spmd

F32 = mybir.dt.float32
BF16 = mybir.dt.bfloat16
I32 = mybir.dt.int32
U32 = mybir.dt.uint32
AF = mybir.ActivationFunctionType
ALU = mybir.AluOpType
AX = mybir.AxisListType

NCORES = 8
D = 1024
SEQ = 4096
HALF = 2048
EXT = 4096
NS = 16
A_HEADS, A_HD = 8, 64
B_HEADS, B_HD = 4, 128
COL_QA, COL_KA, COL_VA, COL_UB = 0, 512, 1024, 1536
COL_ZB = COL_UB + 1536
COL_AB = COL_ZB + 512
COL_BB = COL_AB + 4
IN_COLS = COL_BB + 4
BRANCHES = ((128, 1), (512, 4), (2048, 16))
NEG = -30000.0
CAP = 640
ENGS = ("tensor", "vector", "scalar", "gpsimd", "sync")


class Sched:
    def __init__(self, nc, stack, same_engine_wait=True):
        self.nc = nc
        self.stack = stack
        self.q = {e: [] for e in ENGS}
        self.cnt = {e: 0 for e in ENGS}
        self.sem = {e: stack.enter_context(nc.semaphore("s_" + e)) for e in ENGS}
        self.waited = {e: {} for e in ENGS}
        self.same_engine_wait = same_engine_wait
        self.dma_sems = {}
        self.ninst = 0

    def _wait(self, eng, tok):
        if tok is None:
            return
        if tok[0] == "E":
            _, src, val = tok
            if src == eng and not self.same_engine_wait:
                return
            key = "E" + src
            sem = self.sem[src]
        else:
            _, slot, val = tok
            key = "D" + slot
            sem = self.dma_sems[slot][0]
        if self.waited[eng].get(key, 0) >= val:
            return
        self.waited[eng][key] = val
        self.q[eng].append(lambda e, sem=sem, val=val: e.wait_ge(sem, val))

    def op(self, eng, fn, deps=()):
        for d in deps:
            self._wait(eng, d)
        self.cnt[eng] += 1
        c = self.cnt[eng]
        sem = self.sem[eng]
        self.q[eng].append(lambda e, fn=fn, sem=sem: fn(e).then_inc(sem, 1))
        self.ninst += 1
        return ("E", eng, c)

    def dmaop(self, eng, slot, fn, deps=()):
        for d in deps:
            self._wait(eng, d)
        if slot not in self.dma_sems:
            self.dma_sems[slot] = [self.stack.enter_context(self.nc.semaphore("d_" + slot)), 0]
        ent = self.dma_sems[slot]
        ent[1] += 16
        sem = ent[0]
        self.q[eng].append(lambda e, fn=fn, sem=sem: fn(e).then_inc(sem, 16))
        self.ninst += 1
        return ("D", slot, ent[1])

    def barrier(self):
        toks = [("E", e, self.cnt[e]) for e in ENGS if self.cnt[e] > 0]
        toks += [("D", slot, ent[1]) for slot, ent in self.dma_sems.items() if ent[1] > 0]
        for e in ENGS:
            for t in toks:
                self._wait(e, t)

    def finish(self, final_tokens):
        best = {}
        for t in final_tokens:
            if t is None:
                continue
            key = (t[0], t[1])
            if key not in best or best[key][2] < t[2]:
                best[key] = t
        for t in best.values():
            self._wait("sync", t)
        with self.nc.Block() as block:
            @block.tensor
            def _(e):
                for f in self.q["tensor"]:
                    f(e)

            @block.vector
            def _(e):
                for f in self.q["vector"]:
                    f(e)

            @block.scalar
            def _(e):
                for f in self.q["scalar"]:
                    f(e)

            @block.gpsimd
            def _(e):
                for f in self.q["gpsimd"]:
                    f(e)

            @block.sync
            def _(e):
                for f in self.q["sync"]:
                    f(e)


class Buf:
    _n = 0

    def __init__(self, name=None):
        Buf._n += 1
        self.name = name or ("b%d" % Buf._n)
        self.writer = None
        self.readers = {}

    def add_reader(self, tok):
        key = tok[1]
        if key not in self.readers or self.readers[key][2] < tok[2]:
            self.readers[key] = tok


class K:
    def __init__(self, S):
        self.S = S

    def _deps(self, reads, writes, deps):
        d = list(deps)
        for b in reads:
            d.append(b.writer)
        for b in writes:
            d.extend(b.readers.values())
            d.append(b.writer)
        return d

    def _commit(self, tok, reads, writes):
        for b in reads:
            b.add_reader(tok)
        for b in writes:
            b.writer = tok
            b.readers = {}

    def op(self, eng, fn, reads=(), writes=(), deps=()):
        tok = self.S.op(eng, fn, self._deps(reads, writes, deps))
        self._commit(tok, reads, writes)
        return tok

    def acc(self, eng, fn, reads=(), acc=(), deps=()):
        d = list(deps)
        for b in reads:
            d.append(b.writer)
        tok = self.S.op(eng, fn, d)
        for b in reads:
            b.add_reader(tok)
        for b in acc:
            b.writer = tok
        return tok

    def gload(self, dst, src, writes, reads=()):
        A, L = dst.shape[1], dst.shape[2]
        slot = writes[0].name
        d = self._deps(reads, writes, ())
        tok = None
        for a0 in range(0, A, 4):
            for l0 in range(0, L, 512):
                tok = self.S.dmaop("gpsimd", slot, lambda e, a0=a0, l0=l0, L=L: e.dma_start(
                    out=dst[:, a0:a0 + 4, l0:min(L, l0 + 512)], in_=src[:, a0:a0 + 4, l0:min(L, l0 + 512)]), d)
        self._commit(tok, reads, writes)
        return tok

    def dma(self, eng, out, in_, reads=(), writes=(), deps=(), slot=None, **kw):
        if slot is None:
            slot = (writes[0] if writes else reads[0]).name
        tok = self.S.dmaop(eng, slot, lambda e: e.dma_start(out=out, in_=in_, **kw), self._deps(reads, writes, deps))
        self._commit(tok, reads, writes)
        return tok


def _rel_bucket_np(dist):
    n = np.maximum(dist, 0)
    ratio = np.maximum(n, 1).astype(np.float32) / np.float32(16)
    large = 16 + (np.log(ratio) / np.float32(math.log(2048 / 16)) * np.float32(16)).astype(np.int32)
    return np.where(n < 16, n, np.minimum(large, 31))


def _bias_tiles(rel_bias):
    kp = np.arange(128)[:, None, None]
    kt = np.arange(2)[None, :, None]
    i = np.arange(128)[None, None, :]
    off = 128 + i - (128 * kt + kp)
    valid = (off >= 0) & (off <= 128)
    out = np.empty((128, 24, 256), np.float32)
    for h in range(A_HEADS):
        for br, (_, dil) in enumerate(BRANCHES):
            bk = _rel_bucket_np(np.maximum(off, 0) * dil)
            vals = rel_bias[bk, h]
            out[:, h * 3 + br, :] = np.where(valid, vals, np.float32(NEG)).reshape(128, 256)
    return out


def sl(start, step, n=128):
    return slice(start, start + (n - 1) * step + 1, step)


def tokset(ti, dil):
    nblk, r = divmod(ti, dil)
    start = r + dil * 128 * nblk
    return start, dil


def build(debug=(), stage=99, nexp=32, with_sample=True):
    nc = bass.Bass("TRN2", target_bir_lowering=False)
    dram = lambda n, s, dt=F32, kind="ExternalInput": nc.dram_tensor(n, list(s), dt, kind=kind).ap()
    xT = dram("xT", [D, EXT])
    xo = dram("xo", [HALF, D])
    valid = dram("valid", [128, 32])
    w_in = dram("w_in", [D, IN_COLS])
    bt = dram("bt", [128, 24, 256])
    cst_d = dram("cst", [128, 8, 128])
    prm_d = dram("prm", [128, 184])
    ssm_p = dram("ssm_p", [4, 128, 128], kind="ExternalOutput")
    conv_p = dram("conv_p", [3, 1536], kind="ExternalOutput")
    kT_d = dram("kT_d", [128, 4, EXT], BF16, kind="Internal")
    vT_d = dram("vT_d", [128, 4, EXT], BF16, kind="Internal")
    qT_d = dram("qT_d", [128, 4, HALF], BF16, kind="Internal")
    gz_d = dram("gz_d", [16, 128, 512], BF16, kind="Internal")
    mixA_d = dram("mixA_d", [128, 4, HALF], BF16, kind="Internal")
    mixB_d = dram("mixB_d", [128, 4, HALF], BF16, kind="Internal")
    w_out = dram("w_out", [D, D])
    lnp_d = dram("lnp", [128, 4, D])
    wr_d = dram("wr", [D, 36])
    rb_d = dram("rb", [128, 36])
    w_gate = dram("w_gate", [32, D, 512])
    w_up = dram("w_up", [32, D, 512])
    w_down = dram("w_down", [32, 512, D])
    xs_pad = dram("xs_pad", [128, D])
    iota_d = dram("iota", [128, CAP])
    hb_d = dram("hb_d", [17 * 128, D], BF16, kind="Internal")
    y_out = dram("y_out", [HALF, D], kind="ExternalOutput")
    ys_out = dram("ys_out", [NS, D], kind="ExternalOutput")
    mixS_d = dram("mixS_d", [128, 8, 128], BF16, kind="Internal")
    xsT = dram("xsT", [D, NS])
    ck = dram("ck", [NS, 2048, 512])
    cv = dram("cv", [NS, 2048, 512])
    sst = dram("sst", [NS * 4, 128, 128])
    scv = dram("scv", [NS, 3, 1536])
    sbias_d = dram("sbias", [128, 3, 129])
    cwr_d = dram("cwr", [NS, 4, 1536])
    sprm_d = dram("sprm", [NS * 4, 130])
    ps_d = dram("ps_d", [NS, IN_COLS], kind="Internal")
    cs_d = dram("cs_d", [NS, 1536], kind="Internal")
    ms_d = dram("ms_d", [NS, D], kind="Internal")
    knew = dram("knew", [NS, 512], kind="ExternalOutput")
    vnew = dram("vnew", [NS, 512], kind="ExternalOutput")
    ssm_s = dram("ssm_s", [NS * 4, 128, 128], kind="ExternalOutput")
    conv_s = dram("conv_s", [NS, 3, 1536], kind="ExternalOutput")
    kwin = dram("kwin", [HALF, 512], kind="ExternalOutput")
    vwin = dram("vwin", [HALF, 512], kind="ExternalOutput")
    dbg_out = {}
    for name, shape in debug:
        dbg_out[name] = dram("dbg_" + name, shape, kind="ExternalOutput")

    final = []
    with ExitStack() as st:
        S = Sched(nc, st, same_engine_wait=(os.environ.get("SEW", "1") == "1"))
        k = K(S)
        sb = lambda n, s, dt=F32: st.enter_context(nc.sbuf_tensor(n, list(s), dt))
        psT = [st.enter_context(nc.psum_tensor("ps%d" % i, [128, 512], F32)) for i in range(8)]
        psB = [Buf("ps%d" % i) for i in range(8)]

        ones_f = sb("ones_f", [128, 128])
        b_ones = Buf()
        k.op("gpsimd", lambda e: e.memset(ones_f[:], 1.0), writes=[b_ones])
        eps6 = sb("eps6", [128, 1])
        b_eps = Buf()
        k.op("gpsimd", lambda e: e.memset(eps6[:], 1e-6), writes=[b_eps])
        cst = sb("cst_sb", [128, 8, 128])
        b_cst = Buf("cst")
        k.dma("sync", cst[:], cst_d, writes=[b_cst])
        eps5 = sb("eps5", [128, 1])
        b_eps5 = Buf()
        k.op("gpsimd", lambda e: e.memset(eps5[:], 1e-5), writes=[b_eps5])
        b_mixA_d, b_mixB_d, b_mixS_d = Buf("mixA_d"), Buf("mixB_d"), Buf("mixS_d")
        valid_sb = sb("valid_sb", [128, 32])
        b_valid = Buf("valid")
        k.dma("sync", valid_sb[:], valid, writes=[b_valid])

        if with_sample:
            C = type("Ctx", (), {})()
            C.nc, C.k, C.S, C.final = nc, k, S, final
            C.psT, C.psB, C.cst, C.b_cst, C.eps6, C.b_eps = psT, psB, cst, b_cst, eps6, b_eps
            C.w_v = w_in.rearrange("(k p) c -> p k c", p=128)
            C.xsT, C.ck, C.cv, C.sst, C.scv, C.sbias_d, C.cwr_d, C.sprm_d = xsT, ck, cv, sst, scv, sbias_d, cwr_d, sprm_d
            C.ps_d, C.cs_d, C.ms_d = ps_d, cs_d, ms_d
            C.knew, C.vnew, C.ssm_s, C.conv_s = knew, vnew, ssm_s, conv_s
            C.mixS_d, C.b_mixS_d = mixS_d, b_mixS_d
            phase_s(C)
        sx = ExitStack()
        xTb = sx.enter_context(nc.sbuf_tensor("xTb", [128, 8, EXT], BF16))
        b_x = [Buf("xTb%d" % c) for c in range(8)]
        xT_v = xT.rearrange("(k p) t -> p k t", p=128)
        for c in range(8):
            k.gload(xTb[:, :, 512 * c:512 * (c + 1)], xT_v[:, :, 512 * c:512 * (c + 1)], writes=[b_x[c]])
        bx_of_tile = lambda ti_nat: b_x[ti_nat // 4]

        def x_bufs(start, step):
            lo, hi = start, start + step * 127
            return [b_x[c] for c in range(lo // 512, hi // 512 + 1)]

        with ExitStack() as sa:
            sba = lambda n, s, dt=F32: sa.enter_context(nc.sbuf_tensor(n, list(s), dt))
            mixA = sba("mixA", [128, 4, HALF], BF16)
            b_mixA = [Buf() for _ in range(4)]
            ETb = sba("ETb", [128, 24, 256], BF16)
            b_ET = Buf("ET")
            btst = sba("btst", [128, 6, 256])
            b_btst = Buf("btst")
            for g in range(4 if stage >= -1 else 0):
                k.dma("sync", btst[:], bt[:, 6 * g:6 * g + 6, :], writes=[b_btst])
                k.op("scalar", lambda e, g=g: e.activation(out=ETb[:, 6 * g:6 * g + 6, :], in_=btst[:], func=AF.Exp),
                     reads=[b_btst], writes=[b_ET])
            QT = sba("QT", [128, 2, HALF], BF16)
            KT = sba("KT", [128, 2, EXT], BF16)
            Vaug = sba("Vaug", [128, 32, 4, 65], BF16)
            acc = sba("acc", [65, 4, HALF])
            wq = sba("wq", [128, 8, 256], BF16)
            wk = sba("wk", [128, 8, 256], BF16)
            wv = sba("wv", [128, 8, 256], BF16)
            b_wq, b_wk, b_wv = Buf("wq"), Buf("wk"), Buf("wv")
            b_QT = [Buf() for _ in range(2)]
            b_KT = [Buf() for _ in range(2)]
            b_V = [Buf() for _ in range(32)]
            b_acc = [Buf() for _ in range(4)]
            b_rrow = Buf()
            stg = [sba("stg%d" % i, [128, 256]) for i in range(2)]
            b_stg = [Buf("stg%d" % i) for i in range(2)]
            exb = [sba("exb%d" % i, [128, 512]) for i in range(2)]
            b_ex = [Buf() for _ in range(2)]
            ptb = [sba("ptb%d" % i, [128, 512], BF16) for i in range(2)]
            b_pt = [Buf() for _ in range(2)]
            w_v = w_in.rearrange("(k p) c -> p k c", p=128)
            ctr = {"ps": 0, "stg": 0, "s": 0, "o": 0, "ev": 0}

            def proj_ps():
                i = ctr["ps"] % 2
                ctr["ps"] += 1
                return psT[i], psB[i]

            for hh2 in range(2):
                k.gload(wq[:], w_v[:, :, COL_QA + 256 * hh2:COL_QA + 256 * hh2 + 256], writes=[b_wq])
                k.gload(wk[:], w_v[:, :, COL_KA + 256 * hh2:COL_KA + 256 * hh2 + 256], writes=[b_wk])
                k.gload(wv[:], w_v[:, :, COL_VA + 256 * hh2:COL_VA + 256 * hh2 + 256], writes=[b_wv])
                for jj in range(2 if stage >= 0 else 0):
                    for tc in range(4 if os.environ.get('KQ','1')=='1' else 0):
                        pt_, pb_ = proj_ps()
                        for kk in range(8):
                            fn = lambda e, kk=kk, jj=jj, tc=tc, pt_=pt_: e.matmul(
                                pt_[:, :], lhsT=wq[:, kk, 128 * jj:128 * jj + 128],
                                rhs=xTb[:, kk, HALF + 512 * tc:HALF + 512 * tc + 512], start=(kk == 0), stop=(kk == 7))
                            if kk == 0:
                                k.op("tensor", fn, reads=[b_wq, b_x[4 + tc]], writes=[pb_])
                            else:
                                k.acc("tensor", fn, reads=[b_wq, b_x[4 + tc]], acc=[pb_])
                        k.op("vector", lambda e, jj=jj, tc=tc, pt_=pt_: e.tensor_scalar(
                            out=QT[:, jj, 512 * tc:512 * tc + 512], in0=pt_[:, :], scalar1=0.125, scalar2=None, op0=ALU.mult),
                            reads=[pb_], writes=[b_QT[jj]])
                    for tc in range(8 if os.environ.get('KK','1')=='1' else 0):
                        pt_, pb_ = proj_ps()
                        for kk in range(8):
                            fn = lambda e, kk=kk, jj=jj, tc=tc, pt_=pt_: e.matmul(
                                pt_[:, :], lhsT=wk[:, kk, 128 * jj:128 * jj + 128],
                                rhs=xTb[:, kk, 512 * tc:512 * tc + 512], start=(kk == 0), stop=(kk == 7))
                            if kk == 0:
                                k.op("tensor", fn, reads=[b_wk, b_x[tc]], writes=[pb_])
                            else:
                                k.acc("tensor", fn, reads=[b_wk, b_x[tc]], acc=[pb_])
                        k.op("vector", lambda e, jj=jj, tc=tc, pt_=pt_: e.tensor_copy(
                            out=KT[:, jj, 512 * tc:512 * tc + 512], in_=pt_[:, :]),
                            reads=[pb_], writes=[b_KT[jj]])
                for ti in range(16, 32 if stage >= -2 else 16):
                    pt_, pb_ = proj_ps()
                    for kk in range(8):
                        fn = lambda e, kk=kk, ti=ti, pt_=pt_: e.matmul(
                            pt_[:, 0:256], lhsT=xTb[:, kk, 128 * ti:128 * ti + 128], rhs=wk[:, kk, :],
                            start=(kk == 0), stop=(kk == 7))
                        if kk == 0:
                            k.op("tensor", fn, reads=[b_wk, b_x[ti // 4]], writes=[pb_])
                        else:
                            k.acc("tensor", fn, reads=[b_wk, b_x[ti // 4]], acc=[pb_])
                    si = ctr["stg"] % 2
                    ctr["stg"] += 1
                    k.op("scalar", lambda e, si=si, pt_=pt_: e.activation(out=stg[si][:], in_=pt_[:, 0:256], func=AF.Copy),
                         reads=[pb_], writes=[b_stg[si]])
                    final.append(k.dma("sync", kwin[128 * (ti - 16):128 * (ti - 16) + 128, 256 * hh2:256 * hh2 + 256],
                                       stg[si][:], reads=[b_stg[si]]))
                for br, (_, dil) in enumerate(BRANCHES):
                    if stage < 1:
                        break
                    k.op("gpsimd", lambda e: e.tensor_copy(
                        out=Vaug[:, :, :, 64], in_=valid_sb[:, :].unsqueeze(2).to_broadcast([128, 32, 4])),
                        reads=[b_valid], writes=b_V)
                    for ti in range(32):
                        start, step = tokset(ti, dil)
                        pt_, pb_ = proj_ps()
                        for kk in range(8):
                            fn = lambda e, kk=kk, start=start, step=step, pt_=pt_: e.matmul(
                                pt_[:, 0:256], lhsT=xTb[:, kk, sl(start, step)], rhs=wv[:, kk, :],
                                start=(kk == 0), stop=(kk == 7))
                            if kk == 0:
                                k.op("tensor", fn, reads=[b_wv] + x_bufs(start, step), writes=[pb_])
                            else:
                                k.acc("tensor", fn, reads=[b_wv] + x_bufs(start, step), acc=[pb_])
                        ev = "vector"
                        src = pt_[:, 0:256].rearrange("p (h d) -> p h d", h=4)
                        if ev == "vector":
                            k.op("vector", lambda e, ti=ti, src=src: e.tensor_copy(out=Vaug[:, ti, :, 0:64], in_=src),
                                 reads=[pb_], writes=[b_V[ti]])
                        else:
                            k.op("scalar", lambda e, ti=ti, src=src: e.activation(out=Vaug[:, ti, :, 0:64], in_=src, func=AF.Copy),
                                 reads=[pb_], writes=[b_V[ti]])
                        if br == 0 and ti >= 16:
                            si = ctr["stg"] % 2
                            ctr["stg"] += 1
                            k.op("vector", lambda e, si=si, pt_=pt_: e.tensor_copy(out=stg[si][:], in_=pt_[:, 0:256]),
                                 reads=[pb_], writes=[b_stg[si]])
                            final.append(k.dma("sync", vwin[128 * (ti - 16):128 * (ti - 16) + 128, 256 * hh2:256 * hh2 + 256],
                                               stg[si][:], reads=[b_stg[si]]))
                    for hl in range(4):
                        if stage < 2:
                            break
                        h = 4 * hh2 + hl
                        jj, pb = hl // 2, 64 * (hl % 2)
                        for ti0 in range(16, 32, 2):
                            sidx = 2 + ctr["s"] % 2
                            ctr["s"] += 1
                            pS, bS = psT[sidx], psB[sidx]
                            first = True
                            for a in range(2):
                                ti = ti0 + a
                                qs, qstep = tokset(ti, dil)
                                qs -= HALF
                                for kt in range(2):
                                    tk = ti - dil * (1 - kt)
                                    ks, kstep = tokset(tk, dil)
                                    fn = lambda e, a=a, kt=kt, ks=ks, kstep=kstep, qs=qs, qstep=qstep, pS=pS, jj=jj, pb=pb: e.matmul(
                                        pS[:, a * 256 + kt * 128:a * 256 + kt * 128 + 128],
                                        lhsT=KT[pb:pb + 64, jj, sl(ks, kstep)],
                                        rhs=QT[pb:pb + 64, jj, sl(qs, qstep)], start=True, stop=True)
                                    if first:
                                        k.op("tensor", fn, reads=[b_KT[jj], b_QT[jj]], writes=[bS])
                                        first = False
                                    else:
                                        k.acc("tensor", fn, reads=[b_KT[jj], b_QT[jj]], acc=[bS])
                            ei = ctr["ev"] % 2
                            ctr["ev"] += 1
                            k.op("scalar", lambda e, ei=ei, pS=pS: e.activation(out=exb[ei][:], in_=pS[:, :], func=AF.Exp),
                                 reads=[bS], writes=[b_ex[ei]])
                            k.op("vector", lambda e, ei=ei, h=h, br=br: e.tensor_tensor(
                                out=ptb[ei][:].rearrange("p (a c) -> p a c", a=2),
                                in0=exb[ei][:].rearrange("p (a c) -> p a c", a=2),
                                in1=ETb[:, h * 3 + br:h * 3 + br + 1, :].to_broadcast([128, 2, 256]), op=ALU.mult),
                                reads=[b_ex[ei], b_ET], writes=[b_pt[ei]])
                            oidx = 4 + ctr["o"] % 2
                            ctr["o"] += 1
                            pO, bO = psT[oidx], psB[oidx]
                            first = True
                            for a in range(2):
                                ti = ti0 + a
                                for kt in range(2):
                                    tk = ti - dil * (1 - kt)
                                    fn = lambda e, a=a, kt=kt, tk=tk, hl=hl, ei=ei, pO=pO: e.matmul(
                                        pO[0:65, a * 128:a * 128 + 128], lhsT=Vaug[:, tk, hl, 0:65],
                                        rhs=ptb[ei][:, a * 256 + kt * 128:a * 256 + kt * 128 + 128],
                                        start=(kt == 0), stop=(kt == 1))
                                    if first:
                                        k.op("tensor", fn, reads=[b_pt[ei], b_V[tk]], writes=[bO])
                                        first = False
                                    else:
                                        k.acc("tensor", fn, reads=[b_pt[ei], b_V[tk]], acc=[bO])
                            qs0, qstep = tokset(ti0, dil)
                            qs1, _ = tokset(ti0 + 1, dil)
                            qs0 -= HALF
                            qs1 -= HALF
                            dst = bass.AP(acc, hl * HALF + qs0, [[4 * HALF, 65], [qs1 - qs0, 2], [qstep, 128]])
                            srcO = pO[0:65, 0:256].rearrange("p (a c) -> p a c", a=2)
                            if br == 0:
                                k.op("vector", lambda e, dst=dst, srcO=srcO: e.tensor_copy(out=dst, in_=srcO),
                                     reads=[bO], writes=[b_acc[hl]])
                            else:
                                k.op("vector", lambda e, dst=dst, srcO=srcO: e.tensor_tensor(out=dst, in0=srcO, in1=dst, op=ALU.add),
                                     reads=[bO], writes=[b_acc[hl]])
                if stage < 3:
                    continue
                k.op("vector", lambda e: e.reciprocal(out=acc[64:65, :, :], in_=acc[64:65, :, :]), reads=b_acc, writes=[b_rrow])
                for hl in range(4):
                    jj, pb = hl // 2, 64 * (hl % 2)
                    for c in range(4):
                        pt_, pb_ = psT[6 + c % 2], psB[6 + c % 2]
                        k.op("tensor", lambda e, hl=hl, c=c, pt_=pt_: e.matmul(
                            pt_[0:64, :], lhsT=ones_f[64:65, 0:64], rhs=acc[64:65, hl, 512 * c:512 * c + 512], start=True, stop=True),
                            reads=[b_rrow, b_ones], writes=[pb_])
                        k.op("vector", lambda e, hl=hl, c=c, pt_=pt_, pb=pb, jj=jj, hh2=hh2: e.tensor_tensor(
                            out=mixA[pb:pb + 64, 2 * hh2 + jj, 512 * c:512 * c + 512], in0=acc[0:64, hl, 512 * c:512 * c + 512],
                            in1=pt_[0:64, :], op=ALU.mult), reads=[pb_, b_acc[hl]], writes=[b_mixA[2 * hh2 + jj]])

            if stage >= 3:
                for pp in range(4):
                    k.dma("sync", mixA_d[:, pp], mixA[:, pp], reads=[b_mixA[pp]], writes=[b_mixA_d])
        S.barrier()
        if stage >= 4:
            C = type("Ctx", (), {})()
            C.nc, C.k, C.S, C.final = nc, k, S, final
            C.xTb, C.b_x, C.w_v = xTb, b_x, w_in.rearrange("(k p) c -> p k c", p=128)
            C.psT, C.psB, C.cst, C.b_cst, C.ones_f, C.b_ones = psT, psB, cst, b_cst, ones_f, b_ones
            C.eps6, C.b_eps, C.prm_d = eps6, b_eps, prm_d
            C.kT_d, C.vT_d, C.qT_d, C.gz_d = kT_d, vT_d, qT_d, gz_d
            C.b_kT_d, C.b_vT_d, C.b_qT_d, C.b_gz_d = Buf("kT_d"), Buf("vT_d"), Buf("qT_d"), Buf("gz_d")
            C.mixB_d, C.b_mixB_d, C.ssm_p, C.conv_p = mixB_d, b_mixB_d, ssm_p, conv_p
            phase_b(C)
        sx.close()
        S.barrier()
        if stage >= 5:
            C = type("Ctx", (), {})()
            C.nc, C.k, C.S, C.final = nc, k, S, final
            C.psT, C.psB, C.cst, C.b_cst = psT, psB, cst, b_cst
            C.eps5, C.b_eps5 = eps5, b_eps5
            C.NT = 17 if with_sample else 16
            C.NEXP = nexp
            C.lnp_d, C.w_out, C.wr_d, C.rb_d = lnp_d, w_out, wr_d, rb_d
            C.w_gate, C.w_up, C.w_down = w_gate, w_up, w_down
            C.mixA_d, C.mixB_d, C.b_mixA_d, C.b_mixB_d = mixA_d, mixB_d, b_mixA_d, b_mixB_d
            C.mixS_d, C.b_mixS_d, C.xs_pad, C.xo = mixS_d, b_mixS_d, xs_pad, xo
            C.y_out, C.ys_out = y_out, ys_out
            C.iota_d, C.hb_d, C.b_hb_d, C.ones_f, C.b_ones = iota_d, hb_d, Buf("hb_d"), ones_f, b_ones
            phase_c(C)
        for nm, src_d, bsrc in (("mixA", mixA_d, b_mixA_d), ("mixB", mixB_d, b_mixB_d)):
            if nm in dbg_out:
                dstg_b = sb("dstg_b" + nm, [128, HALF], BF16)
                dstg = sb("dstg" + nm, [128, HALF])
                b_db, b_df = Buf("dstg_b" + nm), Buf("dstg" + nm)
                for pp in range(4):
                    k.dma("sync", dstg_b[:], src_d[:, pp], reads=[bsrc], writes=[b_db])
                    k.op("vector", lambda e, dstg=dstg, dstg_b=dstg_b: e.tensor_copy(out=dstg[:], in_=dstg_b[:]), reads=[b_db], writes=[b_df])
                    final.append(k.dma("sync", dbg_out[nm][:, pp], dstg[:], reads=[b_df]))
        S.finish(final)
    return nc


def phase_b(C):
    nc, k, S = C.nc, C.k, C.S
    xTb, b_x, w_v = C.xTb, C.b_x, C.w_v
    psT, psB = C.psT, C.psB
    cst, b_cst = C.cst, C.b_cst
    ones_f = C.ones_f
    IDENT, NEGID, LMASK, CM0, CM1, NEGM, STRICT = range(7)
    final = C.final
    kT_d, vT_d, qT_d, gz_d = C.kT_d, C.vT_d, C.qT_d, C.gz_d

    with ExitStack() as sB:
        sbb = lambda n, s, dt=F32: sB.enter_context(nc.sbuf_tensor(n, list(s), dt))
        prm = sbb("prm_sb", [128, 8 + 128 + 48])
        b_prm = Buf("prm")
        k.dma("sync", prm[:], C.prm_d, writes=[b_prm])
        identb = sbb("identb", [128, 128], BF16)
        b_identb = Buf()
        k.op("vector", lambda e: e.tensor_copy(out=identb[:], in_=cst[:, IDENT, :]), reads=[b_cst], writes=[b_identb])
        convp_sb = sbb("convp_sb", [128, 12, 3])
        b_convp = Buf("convp")

        ab_sb = sbb("ab_sb", [128, 32, 8])
        b_ab = Buf()
        with ExitStack() as s1:
            sb1 = lambda n, s, dt=F32: s1.enter_context(nc.sbuf_tensor(n, list(s), dt))
            wab = sb1("wab", [128, 8, 8], BF16)
            wz = sb1("wz", [128, 8, 512], BF16)
            b_wab, b_wz = Buf("wab"), Buf("wz")
            k.gload(wab[:], w_v[:, :, COL_AB:COL_AB + 8], writes=[b_wab])
            k.gload(wz[:], w_v[:, :, COL_ZB:COL_ZB + 512], writes=[b_wz])
            zst = [sb1("zst%d" % i, [128, 512]) for i in range(2)]
            b_zst = [Buf() for _ in range(2)]
            gzb = [sb1("gzb%d" % i, [128, 512], BF16) for i in range(2)]
            b_gzb = [Buf("gzb%d" % i) for i in range(2)]
            for ti in range(32):
                pt_, pb_ = psT[ti % 2], psB[ti % 2]
                for kk in range(8):
                    fn = lambda e, kk=kk, ti=ti, pt_=pt_: e.matmul(pt_[:, 0:8], lhsT=xTb[:, kk, 128 * ti:128 * ti + 128],
                                                                 rhs=wab[:, kk, :], start=(kk == 0), stop=(kk == 7))
                    if kk == 0:
                        k.op("tensor", fn, reads=[b_wab, b_x[ti // 4]], writes=[pb_])
                    else:
                        k.acc("tensor", fn, reads=[b_wab, b_x[ti // 4]], acc=[pb_])
                k.op("vector", lambda e, ti=ti, pt_=pt_: e.tensor_copy(out=ab_sb[:, ti, :], in_=pt_[:, 0:8]), reads=[pb_], writes=[b_ab])
            for ti in range(16, 32):
                i2 = ti % 2
                pt_, pb_ = psT[2 + i2], psB[2 + i2]
                for kk in range(8):
                    fn = lambda e, kk=kk, ti=ti, pt_=pt_: e.matmul(pt_[:, :], lhsT=xTb[:, kk, 128 * ti:128 * ti + 128],
                                                                 rhs=wz[:, kk, :], start=(kk == 0), stop=(kk == 7))
                    if kk == 0:
                        k.op("tensor", fn, reads=[b_wz, b_x[ti // 4]], writes=[pb_])
                    else:
                        k.acc("tensor", fn, reads=[b_wz, b_x[ti // 4]], acc=[pb_])
                k.op("scalar", lambda e, i2=i2, pt_=pt_: e.activation(out=zst[i2][:], in_=pt_[:, :], func=AF.Silu), reads=[pb_], writes=[b_zst[i2]])
                k.op("gpsimd", lambda e, i2=i2: e.tensor_tensor(
                    out=gzb[i2][:].rearrange("p (h e) -> p h e", h=4), in0=zst[i2][:].rearrange("p (h e) -> p h e", h=4),
                    in1=prm[:, 8:136].unsqueeze(1).to_broadcast([128, 4, 128]), op=ALU.mult),
                    reads=[b_zst[i2], b_prm], writes=[b_gzb[i2]])
                k.dma("sync", gz_d[ti - 16], gzb[i2][:], reads=[b_gzb[i2]], writes=[C.b_gz_d])

        S.barrier()
        gt = sbb("gt", [128, 12, 128])
        b_gt = [Buf() for _ in range(12)]
        G, BETA, GC, EGC, ETAIL, BEGE, EGL0, EGL1, GCL, TMP, NEGA, TMP2 = range(12)
        v3 = lambda idx: gt[:, idx, :].rearrange("p (t h) -> p t h", h=4)
        k.op("scalar", lambda e: e.activation(out=gt[:, NEGA, 0:4], in_=prm[:, 0:4], func=AF.Exp), reads=[b_prm], writes=[b_gt[NEGA]])
        k.op("vector", lambda e: e.tensor_tensor(out=v3(TMP), in0=ab_sb[:, :, 0:4], in1=prm[:, 4:8].unsqueeze(1).to_broadcast([128, 32, 4]), op=ALU.add),
             reads=[b_ab, b_prm], writes=[b_gt[TMP]])
        k.op("scalar", lambda e: e.activation(out=gt[:, TMP, :], in_=gt[:, TMP, :], func=AF.Exp), reads=[b_gt[TMP]], writes=[b_gt[TMP]])
        k.op("scalar", lambda e: e.activation(out=gt[:, TMP, :], in_=gt[:, TMP, :], func=AF.Ln, bias=1.0), reads=[b_gt[TMP]], writes=[b_gt[TMP]])
        k.op("vector", lambda e: e.scalar_tensor_tensor(out=v3(G), in0=v3(TMP), scalar=-1.0, in1=gt[:, NEGA, 0:4].unsqueeze(1).to_broadcast([128, 32, 4]),
                                                       op0=ALU.mult, op1=ALU.mult), reads=[b_gt[TMP], b_gt[NEGA]], writes=[b_gt[G]])
        k.op("scalar", lambda e: e.activation(out=v3(BETA), in_=ab_sb[:, :, 4:8], func=AF.Sigmoid), reads=[b_ab], writes=[b_gt[BETA]])
        for (mask, dst, bank) in ((LMASK, GC, 0), (CM0, EGL0, 1), (CM1, EGL1, 2)):
            k.op("tensor", lambda e, mask=mask, bank=bank: e.matmul(psT[bank][:, 0:128], lhsT=cst[:, mask, :], rhs=gt[:, G, :], start=True, stop=True),
                 reads=[b_cst, b_gt[G]], writes=[psB[bank]])
            k.op("vector", lambda e, dst=dst, bank=bank: e.tensor_copy(out=gt[:, dst, :], in_=psT[bank][:, 0:128]), reads=[psB[bank]], writes=[b_gt[dst]])
        k.op("vector", lambda e: e.tensor_copy(out=gt[0:64, GCL, :], in_=gt[0:64, EGL0, :]), reads=[b_gt[EGL0]], writes=[b_gt[GCL]])
        k.op("vector", lambda e: e.tensor_copy(out=gt[64:128, GCL, :], in_=gt[64:128, EGL1, :]), reads=[b_gt[EGL1], b_gt[GCL]], writes=[b_gt[GCL]])
        k.op("vector", lambda e: e.tensor_tensor(out=gt[:, TMP2, :], in0=gt[:, GCL, :], in1=gt[:, GC, :], op=ALU.subtract),
             reads=[b_gt[GCL], b_gt[GC]], writes=[b_gt[TMP2]])
        k.op("scalar", lambda e: e.activation(out=gt[:, ETAIL, :], in_=gt[:, TMP2, :], func=AF.Exp), reads=[b_gt[TMP2]], writes=[b_gt[ETAIL]])
        k.op("scalar", lambda e: e.activation(out=gt[:, EGC, :], in_=gt[:, GC, :], func=AF.Exp), reads=[b_gt[GC]], writes=[b_gt[EGC]])
        k.op("scalar", lambda e: e.activation(out=gt[:, EGL0, :], in_=gt[:, EGL0, :], func=AF.Exp), reads=[b_gt[EGL0], b_gt[GCL]], writes=[b_gt[EGL0]])
        k.op("scalar", lambda e: e.activation(out=gt[:, EGL1, :], in_=gt[:, EGL1, :], func=AF.Exp), reads=[b_gt[EGL1], b_gt[GCL]], writes=[b_gt[EGL1]])
        k.op("vector", lambda e: e.tensor_tensor(out=gt[:, BEGE, :], in0=gt[:, BETA, :], in1=gt[:, EGC, :], op=ALU.mult),
             reads=[b_gt[BETA], b_gt[EGC]], writes=[b_gt[BEGE]])

        with ExitStack() as s2:
            sb2 = lambda n, s, dt=F32: s2.enter_context(nc.sbuf_tensor(n, list(s), dt))
            wu = [sb2("wu%d" % i, [128, 8, 128], BF16) for i in range(2)]
            b_wu = [Buf("wu%d" % i) for i in range(2)]
            ub = [sb2("ub%d" % i, [128, 515]) for i in range(2)]
            b_ub = [Buf() for _ in range(2)]
            cb = [sb2("cb%d" % i, [128, 512]) for i in range(2)]
            b_cb = [Buf() for _ in range(2)]
            sq = [sb2("sq%d" % i, [128, 512]) for i in range(2)]
            b_sq = [Buf() for _ in range(2)]
            rt = [sb2("rt%d" % i, [128, 512]) for i in range(2)]
            b_rt = [Buf() for _ in range(2)]
            ob = [sb2("ob%d" % i, [128, 512], BF16) for i in range(2)]
            b_ob = [Buf("ob%d" % i) for i in range(2)]
            cnt = 0
            for th in range(12):
                ty, hb = divmod(th, 4)
                wi = th % 2
                c0 = COL_UB + 128 * th
                k.gload(wu[wi][:], w_v[:, :, c0:c0 + 128], writes=[b_wu[wi]])
                chunks = range(4, 8) if ty == 0 else range(8)
                dst_d = (qT_d, kT_d, vT_d)[ty]
                b_dst = (C.b_qT_d, C.b_kT_d, C.b_vT_d)[ty]
                cw = lambda i, th=th: prm[:, 136 + 4 * th + i:136 + 4 * th + i + 1]
                first = True
                for tc in chunks:
                    ci = cnt % 2
                    cnt += 1
                    pt_, pb_ = psT[ci], psB[ci]
                    if first:
                        if ty == 0:
                            hp, hpb = psT[2], psB[2]
                            for kk in range(8):
                                fn = lambda e, kk=kk, wi=wi, hp=hp: e.matmul(hp[:, 0:4], lhsT=wu[wi][:, kk, :], rhs=xTb[:, kk, HALF - 4:HALF],
                                                                            start=(kk == 0), stop=(kk == 7))
                                if kk == 0:
                                    k.op("tensor", fn, reads=[b_wu[wi], b_x[3]], writes=[hpb])
                                else:
                                    k.acc("tensor", fn, reads=[b_wu[wi], b_x[3]], acc=[hpb])
                            k.op("vector", lambda e, ci=ci, hp=hp: e.tensor_copy(out=ub[ci][:, 0:3], in_=hp[:, 1:4]), reads=[hpb], writes=[b_ub[ci]])
                        else:
                            k.op("vector", lambda e, ci=ci: e.memset(ub[ci][:, 0:3], 0.0), writes=[b_ub[ci]])
                        first = False
                    else:
                        k.op("vector", lambda e, ci=ci: e.tensor_copy(out=ub[ci][:, 0:3], in_=ub[1 - ci][:, 512:515]),
                             reads=[b_ub[1 - ci]], writes=[b_ub[ci]])
                    for kk in range(8):
                        fn = lambda e, kk=kk, wi=wi, tc=tc, pt_=pt_: e.matmul(pt_[:, :], lhsT=wu[wi][:, kk, :], rhs=xTb[:, kk, 512 * tc:512 * tc + 512],
                                                                            start=(kk == 0), stop=(kk == 7))
                        if kk == 0:
                            k.op("tensor", fn, reads=[b_wu[wi], b_x[tc]], writes=[pb_])
                        else:
                            k.acc("tensor", fn, reads=[b_wu[wi], b_x[tc]], acc=[pb_])
                    k.op("scalar", lambda e, ci=ci, pt_=pt_: e.activation(out=ub[ci][:, 3:515], in_=pt_[:, :], func=AF.Copy), reads=[pb_], writes=[b_ub[ci]])
                    if tc == 7:
                        k.op("gpsimd", lambda e, ci=ci, th=th: e.tensor_copy(out=convp_sb[:, th, :], in_=ub[ci][:, 512:515]), reads=[b_ub[ci]], writes=[b_convp])
                    k.op("vector", lambda e, ci=ci, cw=cw: e.tensor_scalar(out=cb[ci][:], in0=ub[ci][:, 3:515], scalar1=cw(3), scalar2=None, op0=ALU.mult),
                         reads=[b_ub[ci], b_prm], writes=[b_cb[ci]])
                    for i in range(3):
                        k.op("vector", lambda e, ci=ci, cw=cw, i=i: e.scalar_tensor_tensor(out=cb[ci][:], in0=ub[ci][:, i:i + 512], scalar=cw(i), in1=cb[ci][:],
                                                                                       op0=ALU.mult, op1=ALU.add), reads=[b_ub[ci], b_cb[ci]], writes=[b_cb[ci]])
                    k.op("scalar", lambda e, ci=ci: e.activation(out=cb[ci][:], in_=cb[ci][:], func=AF.Silu), reads=[b_cb[ci]], writes=[b_cb[ci]])
                    if ty == 2:
                        k.op("gpsimd", lambda e, ci=ci: e.tensor_copy(out=ob[ci][:], in_=cb[ci][:]), reads=[b_cb[ci]], writes=[b_ob[ci]])
                    else:
                        k.op("gpsimd", lambda e, ci=ci: e.tensor_tensor(out=sq[ci][:], in0=cb[ci][:], in1=cb[ci][:], op=ALU.mult), reads=[b_cb[ci]], writes=[b_sq[ci]])
                        np_, npb = psT[4 + ci], psB[4 + ci]
                        k.op("tensor", lambda e, ci=ci, np_=np_: e.matmul(np_[:, :], lhsT=ones_f[:], rhs=sq[ci][:], start=True, stop=True),
                             reads=[b_sq[ci], C.b_ones], writes=[npb])
                        k.op("scalar", lambda e, ci=ci, np_=np_: e.activation(out=rt[ci][:], in_=np_[:, :], func=AF.Sqrt, bias=C.eps6[:, 0:1]),
                             reads=[npb, C.b_eps], writes=[b_rt[ci]])
                        k.op("vector", lambda e, ci=ci: e.reciprocal(out=rt[ci][:], in_=rt[ci][:]), reads=[b_rt[ci]], writes=[b_rt[ci]])
                        sc = (128.0 ** -0.5) if ty == 0 else 1.0
                        k.op("vector", lambda e, ci=ci, sc=sc: e.scalar_tensor_tensor(out=ob[ci][:], in0=cb[ci][:], scalar=sc, in1=rt[ci][:], op0=ALU.mult, op1=ALU.mult),
                             reads=[b_cb[ci], b_rt[ci]], writes=[b_ob[ci]])
                    t0 = 512 * tc - (HALF if ty == 0 else 0)
                    k.dma("sync", dst_d[:, hb, t0:t0 + 512], ob[ci][:], reads=[b_ob[ci]], writes=[b_dst])
            convp_v = C.conv_p.rearrange("r (c p) -> p c r", p=128)
            for th in range(12):
                final.append(k.dma("sync", convp_v[:, th, :], convp_sb[:, th, :], reads=[b_convp], slot="convp", allow_slow_non_contiguous=True))

        S.barrier()
        sC = sB
        sbc = lambda n, s, dt=F32: sC.enter_context(nc.sbuf_tensor(n, list(s), dt))
        f_slots = [(psT[b][:, 0:128], psB[b]) for b in range(6)]
        psbf = [psT[6].bitcast(BF16), psT[7].bitcast(BF16)]
        h_slots = [(psbf[b][:, 0:128], psB[6 + b]) for b in range(2)]
        cnts = {"f": 0, "h": 0}

        def fslot():
            s_ = f_slots[cnts["f"] % len(f_slots)]
            cnts["f"] += 1
            return s_

        def hslot():
            s_ = h_slots[cnts["h"] % len(h_slots)]
            cnts["h"] += 1
            return s_

        class Pool_:
            def __init__(self, name, n, dt):
                self.t = [sbc("%s%d" % (name, i), [128, 128], dt) for i in range(n)]
                self.b = [Buf() for _ in range(n)]
                self.i = 0

            def get(self):
                j = self.i % len(self.t)
                self.i += 1
                return self.t[j], self.b[j]

        PF = Pool_("pf", 40, F32)
        PH = Pool_("ph", 96, BF16)
        Sst = [sbc("Sst%d" % h, [128, 128]) for h in range(4)]
        Sbf = [sbc("Sbf%d" % h, [128, 128], BF16) for h in range(4)]
        b_S = [Buf() for _ in range(4)]
        b_Sb = [Buf() for _ in range(4)]
        for h in range(4):
            k.op("vector", lambda e, h=h: e.memset(Sst[h][:], 0.0), writes=[b_S[h]])
            k.op("vector", lambda e, h=h: e.memset(Sbf[h][:], 0.0), writes=[b_Sb[h]])
        kt_t = [sbc("kt_t%d" % i, [128, 4, 128], BF16) for i in range(2)]
        vt_t = [sbc("vt_t%d" % i, [128, 4, 128], BF16) for i in range(2)]
        qt_t = [sbc("qt_t%d" % i, [128, 4, 128], BF16) for i in range(2)]
        gz_t = [sbc("gz_t%d" % i, [128, 512], BF16) for i in range(2)]
        b_kt = [Buf("kt_t%d" % i) for i in range(2)]
        b_vt = [Buf("vt_t%d" % i) for i in range(2)]
        b_qt = [Buf("qt_t%d" % i) for i in range(2)]
        b_gzt = [Buf("gz_t%d" % i) for i in range(2)]
        Ukeep = [[sbc("Uk%d_%d" % (i, h), [128, 128]) for h in range(4)] for i in range(2)]
        WTkeep = [[sbc("WTk%d_%d" % (i, h), [128, 128], BF16) for h in range(4)] for i in range(2)]
        ktlkeep = [[sbc("ktlk%d_%d" % (i, h), [128, 128], BF16) for h in range(4)] for i in range(2)]
        qkTkeep = [[sbc("qkTk%d_%d" % (i, h), [128, 128], BF16) for h in range(4)] for i in range(2)]
        b_Uk = [[Buf() for h in range(4)] for i in range(2)]
        b_WTk = [[Buf() for h in range(4)] for i in range(2)]
        b_ktlk = [[Buf() for h in range(4)] for i in range(2)]
        b_qkTk = [[Buf() for h in range(4)] for i in range(2)]
        ss = sbc("ss", [128, 8])
        b_ss = [Buf() for _ in range(8)]
        junk = sbc("junk", [128, 128])
        b_junk = Buf()
        mxs = [sbc("mxs%d" % i, [128, 4, 128], BF16) for i in range(2)]
        b_mxs = [Buf("mxs%d" % i) for i in range(2)]
        gt_ = gt
        col = lambda idx, c_: gt_[:, idx, c_:c_ + 1]

        def make_tile(ti):
            own = ti >= 16
            bi = ti % 2
            k.dma("sync", kt_t[bi][:], kT_d[:, :, 128 * ti:128 * ti + 128], reads=[C.b_kT_d], writes=[b_kt[bi]])
            k.dma("sync", vt_t[bi][:], vT_d[:, :, 128 * ti:128 * ti + 128], reads=[C.b_vT_d], writes=[b_vt[bi]])
            if own:
                k.dma("sync", qt_t[bi][:], qT_d[:, :, 128 * (ti - 16):128 * (ti - 16) + 128], reads=[C.b_qT_d], writes=[b_qt[bi]])
                k.dma("sync", gz_t[bi][:], gz_d[ti - 16], reads=[C.b_gz_d], writes=[b_gzt[bi]])
            HS = [None] * 4
            def stage1(hb):
                c_ = ti * 4 + hb
                kT = kt_t[bi][:, hb, :]
                vT = vt_t[bi][:, hb, :]
                qT = qt_t[bi][:, hb, :]
                rd_k, rd_v, rd_q = [b_kt[bi]], [b_vt[bi]], [b_qt[bi]]
                nd, b_nd = PF.get()
                k.op("gpsimd", lambda e, nd=nd, c_=c_: e.tensor_scalar(out=nd[:], in0=cst[:, NEGID, :], scalar1=col(GC, c_), scalar2=None, op0=ALU.mult),
                     reads=[b_cst, b_gt[GC]], writes=[b_nd])
                pD, bD = fslot()
                yield
                k.op("tensor", lambda e, pD=pD, nd=nd: e.matmul(pD, lhsT=ones_f[:], rhs=nd[:], start=True, stop=False), reads=[b_nd, C.b_ones], writes=[bD])
                k.acc("tensor", lambda e, pD=pD: e.matmul(pD, lhsT=cst[:, IDENT, :], rhs=cst[:, NEGM, :], start=False, stop=True), reads=[b_cst], acc=[bD])
                Ec, b_Ec = PF.get()
                k.op("scalar", lambda e, Ec=Ec, pD=pD, c_=c_: e.activation(out=Ec[:], in_=pD, func=AF.Exp, bias=col(GC, c_)),
                     reads=[bD, b_gt[GC]], writes=[b_Ec])
                Es, b_Es = PF.get()
                k.op("gpsimd", lambda e, Es=Es, Ec=Ec: e.tensor_tensor(out=Es[:], in0=Ec[:], in1=cst[:, STRICT, :], op=ALU.mult),
                     reads=[b_Ec, b_cst], writes=[b_Es])
                pK, bK = fslot()
                yield
                k.op("tensor", lambda e, pK=pK, kT=kT: e.matmul(pK, lhsT=kT, rhs=kT, start=True, stop=True), reads=rd_k, writes=[bK])
                A, b_A = PH.get()
                k.op("vector", lambda e, A=A, pK=pK, Es=Es, c_=c_: e.scalar_tensor_tensor(out=A[:], in0=pK, scalar=col(BETA, c_), in1=Es[:], op0=ALU.mult, op1=ALU.mult),
                     reads=[bK, b_Es, b_gt[BETA]], writes=[b_A])
                pT_, bT_ = hslot()
                yield
                k.op("tensor", lambda e, pT_=pT_, A=A: e.transpose(pT_, A[:], identb[:]), reads=[b_A, b_identb], writes=[bT_])
                Bm, b_Bm = PH.get()
                k.op("vector", lambda e, Bm=Bm, pT_=pT_: e.tensor_copy(out=Bm[:], in_=pT_), reads=[bT_], writes=[b_Bm])
                P, b_P = PH.get()
                k.op("vector", lambda e, P=P, pT_=pT_: e.tensor_tensor(out=P[:], in0=cst[:, IDENT, :], in1=pT_, op=ALU.subtract), reads=[bT_, b_cst], writes=[b_P])
                X, b_X, Y, b_Y = A, b_A, Bm, b_Bm
                for m in range(1, 6):
                    pX, bX = fslot()
                    yield
                    k.op("tensor", lambda e, pX=pX, X=X, Y=Y: e.matmul(pX, lhsT=Y[:], rhs=X[:], start=True, stop=True), reads=[b_X, b_Y], writes=[bX])
                    Xn, b_Xn = PH.get()
                    k.op("vector", lambda e, Xn=Xn, pX=pX: e.tensor_copy(out=Xn[:], in_=pX), reads=[bX], writes=[b_Xn])
                    if m < 5:
                        pY, bY = fslot()
                        yield
                        k.op("tensor", lambda e, pY=pY, X=X, Y=Y: e.matmul(pY, lhsT=X[:], rhs=Y[:], start=True, stop=True), reads=[b_X, b_Y], writes=[bY])
                        Yn, b_Yn = PH.get()
                        k.op("vector", lambda e, Yn=Yn, pY=pY: e.tensor_copy(out=Yn[:], in_=pY), reads=[bY], writes=[b_Yn])
                    pP, bP = fslot()
                    yield
                    k.op("tensor", lambda e, pP=pP, Xn=Xn, P=P: e.matmul(pP, lhsT=Xn[:], rhs=P[:], start=True, stop=True), reads=[b_Xn, b_P], writes=[bP])
                    Pn, b_Pn = PH.get()
                    k.op("vector", lambda e, Pn=Pn, pP=pP, P=P: e.tensor_tensor(out=Pn[:], in0=pP, in1=P[:], op=ALU.add), reads=[bP, b_P], writes=[b_Pn])
                    P, b_P = Pn, b_Pn
                    X, b_X = Xn, b_Xn
                    if m < 5:
                        Y, b_Y = Yn, b_Yn
                pk_, bk_ = hslot()
                yield
                k.op("tensor", lambda e, pk_=pk_, kT=kT: e.transpose(pk_, kT, identb[:]), reads=rd_k + [b_identb], writes=[bk_])
                Rw, b_Rw = PH.get()
                k.op("vector", lambda e, Rw=Rw, pk_=pk_, c_=c_: e.tensor_scalar(out=Rw[:], in0=pk_, scalar1=col(BEGE, c_), scalar2=None, op0=ALU.mult),
                     reads=[bk_, b_gt[BEGE]], writes=[b_Rw])
                ktl, b_ktl = ktlkeep[bi][hb], b_ktlk[bi][hb]
                k.op("vector", lambda e, ktl=ktl, pk_=pk_, c_=c_: e.tensor_scalar(out=ktl[:], in0=pk_, scalar1=col(ETAIL, c_), scalar2=None, op0=ALU.mult),
                     reads=[bk_, b_gt[ETAIL]], writes=[b_ktl])
                pv_, bv_ = hslot()
                yield
                k.op("tensor", lambda e, pv_=pv_, vT=vT: e.transpose(pv_, vT, identb[:]), reads=rd_v + [b_identb], writes=[bv_])
                Ru, b_Ru = PH.get()
                k.op("vector", lambda e, Ru=Ru, pv_=pv_, c_=c_: e.tensor_scalar(out=Ru[:], in0=pv_, scalar1=col(BETA, c_), scalar2=None, op0=ALU.mult),
                     reads=[bv_, b_gt[BETA]], writes=[b_Ru])
                pU, bU = fslot()
                yield
                k.op("tensor", lambda e, pU=pU, P=P, Ru=Ru: e.matmul(pU, lhsT=P[:], rhs=Ru[:], start=True, stop=True), reads=[b_P, b_Ru], writes=[bU])
                U, b_U = Ukeep[bi][hb], b_Uk[bi][hb]
                k.op("vector", lambda e, U=U, pU=pU: e.tensor_copy(out=U[:], in_=pU), reads=[bU], writes=[b_U])
                pW, bW = fslot()
                yield
                k.op("tensor", lambda e, pW=pW, P=P, Rw=Rw: e.matmul(pW, lhsT=Rw[:], rhs=P[:], start=True, stop=True), reads=[b_P, b_Rw], writes=[bW])
                WT, b_WT = WTkeep[bi][hb], b_WTk[bi][hb]
                k.op("vector", lambda e, WT=WT, pW=pW: e.tensor_copy(out=WT[:], in_=pW), reads=[bW], writes=[b_WT])
                qkT = b_qkT = None
                if own:
                    pQ, bQ = fslot()
                    yield
                    k.op("tensor", lambda e, pQ=pQ, qT=qT, kT=kT: e.matmul(pQ, lhsT=qT, rhs=kT, start=True, stop=True), reads=rd_q + rd_k, writes=[bQ])
                    qk, b_qk = PH.get()
                    k.op("vector", lambda e, qk=qk, pQ=pQ, Ec=Ec: e.tensor_tensor(out=qk[:], in0=pQ, in1=Ec[:], op=ALU.mult), reads=[bQ, b_Ec], writes=[b_qk])
                    pq2, bq2 = hslot()
                    yield
                    k.op("tensor", lambda e, pq2=pq2, qk=qk: e.transpose(pq2, qk[:], identb[:]), reads=[b_qk, b_identb], writes=[bq2])
                    qkT, b_qkT = qkTkeep[bi][hb], b_qkTk[bi][hb]
                    k.op("vector", lambda e, qkT=qkT, pq2=pq2: e.tensor_copy(out=qkT[:], in_=pq2), reads=[bq2], writes=[b_qkT])
                HS[hb] = (dict(U=U, b_U=b_U, WT=WT, b_WT=b_WT, ktl=ktl, b_ktl=b_ktl, qkT=qkT, b_qkT=b_qkT, qT=qT, rd_q=rd_q, c_=c_))
            def stage23():
                outs = []
                if own:
                    for hb in range(4):
                        o_, b_o = PF.get()
                        outs.append((o_, b_o))
                for ch in range(2):
                    r0 = 64 * ch
                    for hb in range(4):
                        H = HS[hb]
                        c_ = H["c_"]
                        yield
                        pV, bV = fslot()
                        k.op("tensor", lambda e, pV=pV, H=H, hb=hb, r0=r0: e.matmul(pV[r0:r0 + 64, :], lhsT=H["WT"][:, r0:r0 + 64], rhs=Sbf[hb][:], start=True, stop=True),
                             reads=[H["b_WT"], b_Sb[hb]], writes=[bV])
                        vn, b_vn = PH.get()
                        k.op("vector", lambda e, vn=vn, pV=pV, H=H, r0=r0: e.tensor_tensor(out=vn[r0:r0 + 64, :], in0=H["U"][r0:r0 + 64, :], in1=pV[r0:r0 + 64, :], op=ALU.subtract),
                             reads=[bV, H["b_U"]], writes=[b_vn])
                        if own:
                            o_, b_o = outs[hb]
                            yield
                            p1, b1 = fslot()
                            k.op("tensor", lambda e, p1=p1, H=H, hb=hb, r0=r0: e.matmul(p1[r0:r0 + 64, :], lhsT=H["qT"][:, r0:r0 + 64], rhs=Sbf[hb][:], start=True, stop=True),
                                 reads=H["rd_q"] + [b_Sb[hb]], writes=[b1])
                            p2, b2 = fslot()
                            k.op("tensor", lambda e, p2=p2, H=H, vn=vn, r0=r0: e.matmul(p2[r0:r0 + 64, :], lhsT=H["qkT"][r0:r0 + 64, r0:r0 + 64], rhs=vn[r0:r0 + 64, :], start=True, stop=True),
                                 reads=[H["b_qkT"], b_vn], writes=[b2])
                            o2, b_o2 = PF.get()
                            k.op("vector", lambda e, o2=o2, p2=p2, r0=r0: e.tensor_copy(out=o2[r0:r0 + 64, :], in_=p2[r0:r0 + 64, :]), reads=[b2], writes=[b_o2])
                            k.op("vector", lambda e, o_=o_, p1=p1, o2=o2, r0=r0, c_=c_: e.scalar_tensor_tensor(
                                out=o_[r0:r0 + 64, :], in0=p1[r0:r0 + 64, :], scalar=gt_[r0:r0 + 64, EGC, c_:c_ + 1], in1=o2[r0:r0 + 64, :], op0=ALU.mult, op1=ALU.add),
                                reads=[b1, b_o2, b_gt[EGC]], writes=[b_o])
                        yield
                        pS, bS_ = fslot()
                        k.op("tensor", lambda e, pS=pS, H=H, vn=vn, r0=r0: e.matmul(pS, lhsT=H["ktl"][r0:r0 + 64, :], rhs=vn[r0:r0 + 64, :], start=True, stop=True),
                             reads=[H["b_ktl"], b_vn], writes=[bS_])
                        egl = EGL0 if ch == 0 else EGL1
                        k.op("vector", lambda e, pS=pS, hb=hb, egl=egl, c_=c_: e.scalar_tensor_tensor(
                            out=Sst[hb][:], in0=Sst[hb][:], scalar=col(egl, c_), in1=pS, op0=ALU.mult, op1=ALU.add),
                            reads=[bS_, b_gt[egl], b_S[hb]], writes=[b_S[hb]])
                        k.op("scalar", lambda e, hb=hb: e.activation(out=Sbf[hb][:], in_=Sst[hb][:], func=AF.Copy), reads=[b_S[hb]], writes=[b_Sb[hb]])
                if own:
                    for hb in range(4):
                        o_, b_o = outs[hb]
                        si = (ti * 4 + hb) % 8
                        yield
                        k.op("scalar", lambda e, o_=o_, si=si: e.activation(out=junk[:], in_=o_[:], func=AF.Square, accum_out=ss[:, si:si + 1]),
                             reads=[b_o], writes=[b_junk, b_ss[si]])
                        k.op("scalar", lambda e, si=si: e.activation(out=ss[:, si:si + 1], in_=ss[:, si:si + 1], func=AF.Sqrt, scale=1.0 / 128.0, bias=C.eps6[:, 0:1]),
                             reads=[b_ss[si], C.b_eps], writes=[b_ss[si]])
                        k.op("vector", lambda e, si=si: e.reciprocal(out=ss[:, si:si + 1], in_=ss[:, si:si + 1]), reads=[b_ss[si]], writes=[b_ss[si]])
                        on, b_on = PH.get()
                        k.op("vector", lambda e, on=on, o_=o_, si=si, hb=hb, bi=bi: e.scalar_tensor_tensor(
                            out=on[:], in0=o_[:], scalar=ss[:, si:si + 1], in1=gz_t[bi][:, 128 * hb:128 * hb + 128], op0=ALU.mult, op1=ALU.mult),
                            reads=[b_o, b_ss[si], b_gzt[bi]], writes=[b_on])
                        yield
                        pm, bm = hslot()
                        k.op("tensor", lambda e, pm=pm, on=on: e.transpose(pm, on[:], identb[:]), reads=[b_on, b_identb], writes=[bm])
                        k.op("vector", lambda e, pm=pm, hb=hb, bi=bi: e.tensor_copy(out=mxs[bi][:, hb, :], in_=pm),
                             reads=[bm], writes=[b_mxs[bi]])
                    k.dma("sync", C.mixB_d[:, :, 128 * (ti - 16):128 * (ti - 16) + 128], mxs[bi][:], reads=[b_mxs[bi]], writes=[C.b_mixB_d])
            return [stage1(hb) for hb in range(4)], stage23


        pending = None
        for ti in range(33):
            gens = []
            nxt = None
            if ti < 32:
                s1, nxt = make_tile(ti)
                gens += s1
            if pending is not None:
                gens.append(pending())
            while gens:
                for g_ in list(gens):
                    try:
                        next(g_)
                    except StopIteration:
                        gens.remove(g_)
            pending = nxt
        for hb in range(4):
            final.append(k.dma("sync", C.ssm_p[hb], Sst[hb][:], reads=[b_S[hb]], slot="ssmp"))

def phase_c(C):
    nc, k, S = C.nc, C.k, C.S
    psT, psB = C.psT, C.psB
    cst, b_cst = C.cst, C.b_cst
    IDENT = 0
    final = C.final
    NT = C.NT
    NTOK = NT * 128
    ALPHA = 2.0 ** 0.25
    KC = int(os.environ.get('KC', '99'))

    with ExitStack() as sC:
        sbc = lambda n, s, dt=F32: sC.enter_context(nc.sbuf_tensor(n, list(s), dt))
        Mg = sbc("Mg", [128, NT, 4])
        b_Mg = Buf()
        Ghl = sbc("Ghl", [128, NT, 4, 16], BF16)
        b_Ghl = Buf()
        identb = sbc("identbC", [128, 128], BF16)
        b_identb = Buf()
        k.op("vector", lambda e: e.tensor_copy(out=identb[:], in_=cst[:, IDENT, :]), reads=[b_cst], writes=[b_identb])
        yacc = sbc("yacc", [128, NT, 1024])
        b_y = [Buf() for _ in range(NT)]
        G = sbc("G", [128, NT, 32])
        b_G = [Buf() for _ in range(NT)]
        st6_c3 = sbc("st6b", [128, 2, 2, 6])
        mv_c3 = sbc("mvb", [128, 2, 2])

        with ExitStack() as s1:
            sb1 = lambda n, s, dt=F32: s1.enter_context(nc.sbuf_tensor(n, list(s), dt))
            lnp = sb1("lnp_sb", [128, 2, 1024])
            b_lnp = Buf("lnp")
            k.dma("sync", lnp[:], C.lnp_d[:, 0:2, :], writes=[b_lnp])
            wo = sb1("wo", [128, 8, 1024], BF16)
            b_wo = Buf("wo")
            k.gload(wo[:], C.w_out.rearrange("(k p) c -> p k c", p=128), writes=[b_wo])
            wr = sb1("wr_sb", [128, 8, 36])
            b_wr = Buf("wr")
            k.dma("sync", wr[:], C.wr_d.rearrange("(k p) c -> p k c", p=128), writes=[b_wr])
            rb = sb1("rb_sb", [128, 36])
            b_rb = Buf("rb")
            k.dma("sync", rb[:], C.rb_d, writes=[b_rb])
            mx = [sb1("mx%d" % i, [128, 8, 128], BF16) for i in range(2)]
            b_mx = [Buf("mx%d" % i) for i in range(2)]
            xt = [sb1("xt%d" % i, [128, 1024]) for i in range(2)]
            b_xt = [Buf("xt%d" % i) for i in range(2)]
            rr = [sb1("rr%d" % i, [128, 1024]) for i in range(2)]
            b_rr = [Buf() for _ in range(2)]
            hh = [sb1("hh%d" % i, [128, 1024]) for i in range(2)]
            b_hh = [Buf() for _ in range(2)]
            hbb = [sb1("hbb%d" % i, [128, 1024], BF16) for i in range(2)]
            b_hbb = [Buf("hbb%d" % i) for i in range(2)]
            hTf = [sb1("hTf%d" % i, [128, 8, 128]) for i in range(2)]
            b_hTf = [Buf() for _ in range(2)]
            st6 = sb1("st6", [128, 2, 2, 6])
            mv = sb1("mv", [128, 2, 2])
            b_st = [Buf() for _ in range(2)]
            b_mv = [Buf() for _ in range(2)]
            tmpr = sb1("tmpr", [128, 2, 32])
            b_tmp = [Buf() for _ in range(2)]
            sm = sb1("sm", [128, 2, 96])
            b_sm = [Buf() for _ in range(2)]
            for ti in range(NT if KC >= 6 else 0):
                bi = ti % 2
                is_s = ti >= 16
                if not is_s:
                    k.dma("sync", mx[bi][:, 0:4, :], C.mixA_d[:, :, 128 * ti:128 * ti + 128], reads=[C.b_mixA_d], writes=[b_mx[bi]])
                    k.dma("sync", mx[bi][:, 4:8, :], C.mixB_d[:, :, 128 * ti:128 * ti + 128], reads=[C.b_mixB_d], writes=[b_mx[bi]])
                    k.dma("sync", xt[bi][:], C.xo[128 * ti:128 * ti + 128, :], writes=[b_xt[bi]])
                else:
                    k.dma("sync", mx[bi][:], C.mixS_d, reads=[C.b_mixS_d], writes=[b_mx[bi]])
                    k.dma("sync", xt[bi][:], C.xs_pad, writes=[b_xt[bi]])
                for half in range(2 if KC >= 7 else 0):
                    pt_, pb_ = psT[half], psB[half]
                    for kk in range(8):
                        fn = lambda e, kk=kk, bi=bi, half=half, pt_=pt_: e.matmul(pt_[:, :], lhsT=mx[bi][:, kk, :], rhs=wo[:, kk, 512 * half:512 * half + 512],
                                                                                start=(kk == 0), stop=(kk == 7))
                        if kk == 0:
                            k.op("tensor", fn, reads=[b_mx[bi], b_wo], writes=[pb_])
                        else:
                            k.acc("tensor", fn, reads=[b_mx[bi], b_wo], acc=[pb_])
                    if KC < 8:
                        continue
                    k.op("vector", lambda e, bi=bi, half=half, pt_=pt_: e.scalar_tensor_tensor(
                        out=rr[bi][:, 512 * half:512 * half + 512], in0=xt[bi][:, 512 * half:512 * half + 512], scalar=ALPHA, in1=pt_[:, :],
                        op0=ALU.mult, op1=ALU.add), reads=[pb_, b_xt[bi]], writes=[b_rr[bi]])
                    k.op("vector", lambda e, bi=bi, half=half: e.bn_stats(out=st6[:, bi, half, :], in_=rr[bi][:, 512 * half:512 * half + 512]),
                         reads=[b_rr[bi]], writes=[b_st[bi]])
                if KC < 11:
                    continue
                k.op("vector", lambda e, bi=bi: e.bn_aggr(out=mv[:, bi, :], in_=st6[:, bi, :, :].rearrange("p a b -> p (a b)")), reads=[b_st[bi]], writes=[b_mv[bi]])
                k.op("scalar", lambda e, bi=bi: e.activation(out=mv[:, bi, 1:2], in_=mv[:, bi, 1:2], func=AF.Sqrt, bias=C.eps5[:, 0:1]), reads=[b_mv[bi], C.b_eps5], writes=[b_mv[bi]])
                k.op("vector", lambda e, bi=bi: e.reciprocal(out=mv[:, bi, 1:2], in_=mv[:, bi, 1:2]), reads=[b_mv[bi]], writes=[b_mv[bi]])
                k.op("vector", lambda e, bi=bi: e.tensor_scalar(out=hh[bi][:], in0=rr[bi][:], scalar1=mv[:, bi, 0:1], scalar2=mv[:, bi, 1:2], op0=ALU.subtract, op1=ALU.mult),
                     reads=[b_rr[bi], b_mv[bi]], writes=[b_hh[bi]])
                k.op("gpsimd", lambda e, bi=bi: e.tensor_tensor(out=hh[bi][:], in0=hh[bi][:], in1=lnp[:, 0, :], op=ALU.mult), reads=[b_hh[bi], b_lnp], writes=[b_hh[bi]])
                k.op("gpsimd", lambda e, bi=bi: e.tensor_tensor(out=hh[bi][:], in0=hh[bi][:], in1=lnp[:, 1, :], op=ALU.add), reads=[b_hh[bi], b_lnp], writes=[b_hh[bi]])
                k.op("gpsimd", lambda e, bi=bi, ti=ti: e.tensor_scalar(out=yacc[:, ti, :], in0=hh[bi][:], scalar1=ALPHA, scalar2=None, op0=ALU.mult),
                     reads=[b_hh[bi]], writes=[b_y[ti]])
                if KC < 12:
                    continue
                k.op("gpsimd", lambda e, bi=bi: e.tensor_copy(out=hbb[bi][:], in_=hh[bi][:]), reads=[b_hh[bi]], writes=[b_hbb[bi]])
                k.dma("sync", C.hb_d[128 * ti:128 * ti + 128, :], hbb[bi][:], reads=[b_hbb[bi]], writes=[C.b_hb_d])
                for g4 in range(2):
                    pt_, pb_ = psT[2 + g4], psB[2 + g4]
                    for j in range(4):
                        kk = 4 * g4 + j
                        fn = lambda e, kk=kk, j=j, bi=bi, pt_=pt_: e.transpose(pt_[:, 128 * j:128 * j + 128], hh[bi][:, 128 * kk:128 * kk + 128], cst[:, IDENT, :])
                        if j == 0:
                            k.op("tensor", fn, reads=[b_hh[bi], b_cst], writes=[pb_])
                        else:
                            k.acc("tensor", fn, reads=[b_hh[bi], b_cst], acc=[pb_])
                    k.op("vector", lambda e, g4=g4, bi=bi, pt_=pt_: e.tensor_copy(out=hTf[bi][:, 4 * g4:4 * g4 + 4, :], in_=pt_[:, :].rearrange("p (a c) -> p a c", a=4)),
                         reads=[pb_], writes=[b_hTf[bi]])
                if KC < 13:
                    continue
                pl, plb = psT[4], psB[4]
                for kk in range(8):
                    fn = lambda e, kk=kk, bi=bi: e.matmul(pl[:, 0:36], lhsT=hTf[bi][:, kk, :], rhs=wr[:, kk, :], start=(kk == 0), stop=(kk == 7))
                    if kk == 0:
                        k.op("tensor", fn, reads=[b_hTf[bi], b_wr], writes=[plb])
                    else:
                        k.acc("tensor", fn, reads=[b_hTf[bi], b_wr], acc=[plb])
                if KC < 14:
                    continue
                R = lambda a, b_, bi=bi: sm[:, bi, a:b_]
                T3 = tmpr[:, bi, :].rearrange("p (g e) -> p g e", g=4)
                sm_b, tmp_b = b_sm[bi], b_tmp[bi]

                def VV(eng, method, reads, writes, **aps):
                    k.op(eng, lambda e, aps=aps, method=method: getattr(e, method)(**aps), reads=reads, writes=writes)
                VV("vector", "tensor_tensor", [plb, b_rb], [sm_b], out=R(0, 36), in0=pl[:, 0:36], in1=rb[:], op=ALU.add)
                VV("vector", "tensor_reduce", [sm_b], [sm_b], out=R(36, 37), in_=R(0, 4), axis=AX.X, op=ALU.max)
                VV("vector", "tensor_scalar", [sm_b], [sm_b], out=R(40, 44), in0=R(0, 4), scalar1=R(36, 37), scalar2=None, op0=ALU.is_equal)
                VV("vector", "tensor_scalar", [sm_b], [sm_b], out=R(37, 38), in0=R(36, 37), scalar1=-1.0, scalar2=None, op0=ALU.mult)
                VV("scalar", "activation", [sm_b], [sm_b], out=R(89, 93), in_=R(0, 4), func=AF.Exp, bias=R(37, 38), accum_out=R(38, 39))
                VV("vector", "reciprocal", [sm_b], [sm_b], out=R(38, 39), in_=R(38, 39))
                VV("vector", "tensor_tensor", [sm_b], [tmp_b], out=T3, in0=R(4, 36).rearrange("p (g e) -> p g e", g=4),
                   in1=R(40, 44).unsqueeze(2).to_broadcast([128, 4, 8]), op=ALU.mult)
                VV("vector", "tensor_reduce", [tmp_b], [sm_b], out=R(44, 52), in_=T3.rearrange("p g e -> p e g"), axis=AX.X, op=ALU.add)
                VV("vector", "tensor_reduce", [sm_b], [sm_b], out=R(52, 53), in_=R(44, 52), axis=AX.X, op=ALU.max)
                VV("vector", "tensor_scalar", [sm_b], [sm_b], out=R(54, 62), in0=R(44, 52), scalar1=R(52, 53), scalar2=None, op0=ALU.is_equal)
                VV("vector", "scalar_tensor_tensor", [sm_b], [sm_b], out=R(62, 70), in0=R(54, 62), scalar=-1e30, in1=R(44, 52), op0=ALU.mult, op1=ALU.add)
                VV("vector", "tensor_reduce", [sm_b], [sm_b], out=R(53, 54), in_=R(62, 70), axis=AX.X, op=ALU.max)
                VV("vector", "tensor_scalar", [sm_b], [sm_b], out=R(70, 78), in0=R(62, 70), scalar1=R(53, 54), scalar2=None, op0=ALU.is_equal)
                VV("vector", "tensor_tensor", [sm_b], [sm_b], out=R(78, 79), in0=R(53, 54), in1=R(52, 53), op=ALU.subtract)
                VV("scalar", "activation", [sm_b], [sm_b], out=R(78, 79), in_=R(78, 79), func=AF.Exp)
                VV("vector", "tensor_scalar", [sm_b], [sm_b], out=R(79, 80), in0=R(78, 79), scalar1=1.0, scalar2=None, op0=ALU.add)
                VV("vector", "reciprocal", [sm_b], [sm_b], out=R(79, 80), in_=R(79, 80))
                VV("vector", "tensor_tensor", [sm_b], [sm_b], out=R(80, 81), in0=R(78, 79), in1=R(79, 80), op=ALU.mult)
                VV("vector", "tensor_scalar", [sm_b], [sm_b], out=R(79, 81), in0=R(79, 81), scalar1=R(38, 39), scalar2=None, op0=ALU.mult)
                VV("vector", "tensor_scalar", [sm_b], [sm_b], out=R(81, 89), in0=R(54, 62), scalar1=R(79, 80), scalar2=None, op0=ALU.mult)
                VV("vector", "scalar_tensor_tensor", [sm_b], [sm_b], out=R(81, 89), in0=R(70, 78), scalar=R(80, 81), in1=R(81, 89), op0=ALU.mult, op1=ALU.add)
                VV("vector", "tensor_tensor", [sm_b], [b_G[ti]], out=G[:, ti, :].rearrange("p (g e) -> p g e", g=4),
                   in0=R(40, 44).unsqueeze(2).to_broadcast([128, 4, 8]), in1=R(81, 89).unsqueeze(1).to_broadcast([128, 4, 8]), op=ALU.mult)
                G3 = G[:, ti, :].rearrange("p (g e) -> p g e", g=4)
                VV("vector", "tensor_copy", [sm_b], [b_Mg], out=Mg[:, ti, :], in_=R(40, 44))
                VV("vector", "tensor_copy", [b_G[ti]], [b_Ghl], out=Ghl[:, ti, :, 0:8], in_=G3)
                VV("vector", "tensor_tensor", [b_G[ti], b_Ghl], [tmp_b], out=T3, in0=G3, in1=Ghl[:, ti, :, 0:8], op=ALU.subtract)
                VV("vector", "tensor_copy", [tmp_b], [b_Ghl], out=Ghl[:, ti, :, 8:16], in_=T3)
        S.barrier()

        with ExitStack() as s2:
            sb2 = lambda n, s, dt=F32: s2.enter_context(nc.sbuf_tensor(n, list(s), dt))
            NST = CAP // 128
            iota = sb2("iota_sb", [128, CAP])
            b_iota = Buf("iota_sb")
            k.dma("sync", iota[:], C.iota_d, writes=[b_iota])
            Mcum = sb2("Mcum", [128, NT, 4])
            b_Mc = Buf()
            slotf = sb2("slotf", [128, NT])
            b_sl = Buf()
            t4 = sb2("t4", [128, 4])
            b_t4 = Buf()

            def VV(eng, method, reads, writes, **aps):
                return k.op(eng, lambda e, aps=aps, method=method: getattr(e, method)(**aps), reads=reads, writes=writes)
            for ti in range(NT):
                if ti == 0:
                    VV("vector", "tensor_copy", [b_Mg], [b_Mc], out=Mcum[:, 0, :], in_=Mg[:, 0, :])
                else:
                    VV("vector", "tensor_tensor", [b_Mg, b_Mc], [b_Mc], out=Mcum[:, ti, :], in0=Mcum[:, ti - 1, :], in1=Mg[:, ti, :], op=ALU.add)
            for ti in range(NT):
                pr, prb = psT[ti % 2], psB[ti % 2]
                k.op("tensor", lambda e, ti=ti, pr=pr: e.matmul(pr[:, 0:4], lhsT=cst[:, 7, :], rhs=Mg[:, ti, :], start=True, stop=(ti == 0)),
                     reads=[b_cst, b_Mg], writes=[prb])
                if ti > 0:
                    k.acc("tensor", lambda e, ti=ti, pr=pr: e.matmul(pr[:, 0:4], lhsT=C.ones_f[:], rhs=Mcum[:, ti - 1, :], start=False, stop=True),
                          reads=[C.b_ones, b_Mc], acc=[prb])
                VV("vector", "tensor_tensor", [prb, b_Mg], [b_t4], out=t4[:], in0=pr[:, 0:4], in1=Mg[:, ti, :], op=ALU.mult)
                VV("vector", "tensor_reduce", [b_t4], [b_sl], out=slotf[:, ti:ti + 1], in_=t4[:], axis=AX.X, op=ALU.add)

            Sel = sb2("Sel", [128, NT, CAP], BF16)
            b_Sel = Buf()
            hTg = sb2("hTg", [128, 8, CAP], BF16)
            b_hTg = Buf()
            Yg = sb2("Yg", [128, NST, 1024])
            b_Yg = [Buf() for _ in range(NST)]
            Ygs = sb2("Ygs", [128, NST, 1024], BF16)
            b_Ygs = Buf()
            t16 = sb2("t16", [128, 16])
            b_t16 = Buf()
            Gs = sb2("Gs", [128, NST, 8])
            b_Gs = Buf()
            hbt = [sb2("hbt%d" % i, [128, 1024], BF16) for i in range(3)]
            b_hbt = [Buf("hbt%d" % i) for i in range(3)]
            selT = [sb2("selT%d" % i, [128, 128], BF16) for i in range(4)]
            b_selT = [Buf() for _ in range(4)]
            wg = [sb2("wg%d" % i, [128, 8, 512], BF16) for i in range(2)]
            wu_ = [sb2("wup%d" % i, [128, 8, 512], BF16) for i in range(2)]
            wd = [sb2("wd%d" % i, [128, 4, 1024], BF16) for i in range(2)]
            b_wg = [Buf("wg%d" % i) for i in range(2)]
            b_wu = [Buf("wup%d" % i) for i in range(2)]
            b_wd = [Buf("wd%d" % i) for i in range(2)]
            sg = [sb2("sg%d" % i, [128, 512]) for i in range(2)]
            b_sg = [Buf() for _ in range(2)]
            act = sb2("act", [128, 4, 512], BF16)
            b_act = Buf()
            psbf = psT[7].bitcast(BF16)
            chunks = [(0, 512), (512, CAP - 512)]
            fcnt = 0
            hcnt = 0
            tcnt = 0
            for g in range(4):
                for ti in range(NT):
                    VV("vector", "tensor_scalar", [b_iota, b_sl, b_Mg], [b_Sel], out=Sel[:, ti, :], in0=iota[:], scalar1=slotf[:, ti:ti + 1],
                       scalar2=Mg[:, ti, g:g + 1], op0=ALU.is_equal, op1=ALU.mult)
                for ps_ in range(2):
                    for ti in range(NT):
                        hi = hcnt % 3
                        hcnt += 1
                        k.dma("sync", hbt[hi][:], C.hb_d[128 * ti:128 * ti + 128, :], reads=[C.b_hb_d], writes=[b_hbt[hi]])
                        for j in range(4):
                            kk = 4 * ps_ + j
                            for (bank, c0, cn) in ((j, 0, 512), (4 + j, 512, CAP - 512)):
                                fn = lambda e, bank=bank, hi=hi, kk=kk, ti=ti, c0=c0, cn=cn: e.matmul(
                                    psT[bank][:, 0:cn], lhsT=hbt[hi][:, 128 * kk:128 * kk + 128], rhs=Sel[:, ti, c0:c0 + cn], start=(ti == 0), stop=(ti == NT - 1))
                                if ti == 0:
                                    k.op("tensor", fn, reads=[b_hbt[hi], b_Sel], writes=[psB[bank]])
                                else:
                                    k.acc("tensor", fn, reads=[b_hbt[hi], b_Sel], acc=[psB[bank]])
                    for j in range(4):
                        kk = 4 * ps_ + j
                        VV("vector", "tensor_copy", [psB[j]], [b_hTg], out=hTg[:, kk, 0:512], in_=psT[j][:, 0:512])
                        VV("vector", "tensor_copy", [psB[4 + j]], [b_hTg], out=hTg[:, kk, 512:CAP], in_=psT[4 + j][:, 0:CAP - 512])
                for st in range(NST):
                    pg_, pgb_ = psT[st % 2], psB[st % 2]
                    for ti in range(NT):
                        fn = lambda e, st=st, ti=ti, g=g, pg_=pg_: e.matmul(pg_[:, 0:16], lhsT=Sel[:, ti, 128 * st:128 * st + 128], rhs=Ghl[:, ti, g, :],
                                                                           start=(ti == 0), stop=(ti == NT - 1))
                        if ti == 0:
                            k.op("tensor", fn, reads=[b_Sel, b_Ghl], writes=[pgb_])
                        else:
                            k.acc("tensor", fn, reads=[b_Sel, b_Ghl], acc=[pgb_])
                    VV("vector", "tensor_copy", [pgb_], [b_t16], out=t16[:], in_=pg_[:, 0:16])
                    VV("vector", "tensor_tensor", [b_t16], [b_Gs], out=Gs[:, st, :], in0=t16[:, 0:8], in1=t16[:, 8:16], op=ALU.add)
                for e8 in range(8):
                    ex = 8 * g + e8
                    wi = ex % 2
                    k.gload(wg[wi][:], C.w_gate[ex].rearrange("(k p) f -> p k f", p=128), writes=[b_wg[wi]])
                    k.gload(wu_[wi][:], C.w_up[ex].rearrange("(k p) f -> p k f", p=128), writes=[b_wu[wi]])
                    k.gload(wd[wi][:], C.w_down[ex].rearrange("(k p) d -> p k d", p=128), writes=[b_wd[wi]])
                    for (t0, tn) in chunks:
                        for f in range(4):
                            pg, pgb = psT[0 + fcnt % 2], psB[0 + fcnt % 2]
                            pu, pub = psT[2 + fcnt % 2], psB[2 + fcnt % 2]
                            si = fcnt % 2
                            fcnt += 1
                            for (pp, ppb, ww, bw) in ((pg, pgb, wg[wi], b_wg[wi]), (pu, pub, wu_[wi], b_wu[wi])):
                                for kk in range(8):
                                    fn = lambda e, kk=kk, pp=pp, ww=ww, f=f, t0=t0, tn=tn: e.matmul(pp[:, 0:tn], lhsT=ww[:, kk, 128 * f:128 * f + 128], rhs=hTg[:, kk, t0:t0 + tn],
                                                                                                start=(kk == 0), stop=(kk == 7))
                                    if kk == 0:
                                        k.op("tensor", fn, reads=[bw, b_hTg], writes=[ppb])
                                    else:
                                        k.acc("tensor", fn, reads=[bw, b_hTg], acc=[ppb])
                            k.op("scalar", lambda e, si=si, pg=pg, tn=tn: e.activation(out=sg[si][:, 0:tn], in_=pg[:, 0:tn], func=AF.Silu), reads=[pgb], writes=[b_sg[si]])
                            k.op("vector", lambda e, si=si, f=f, pu=pu, tn=tn: e.tensor_tensor(out=act[:, f, 0:tn], in0=sg[si][:, 0:tn], in1=pu[:, 0:tn], op=ALU.mult),
                                 reads=[b_sg[si], pub], writes=[b_act])
                        for tt in range(tn // 128):
                            st = t0 // 128 + tt
                            for half in range(2):
                                py, pyb = psT[4 + (tt * 2 + half) % 4], psB[4 + (tt * 2 + half) % 4]
                                for f in range(4):
                                    fn = lambda e, f=f, tt=tt, half=half, py=py, wi=wi: e.matmul(py[:, :], lhsT=act[:, f, 128 * tt:128 * tt + 128],
                                                                                             rhs=wd[wi][:, f, 512 * half:512 * half + 512], start=(f == 0), stop=(f == 3))
                                    if f == 0:
                                        k.op("tensor", fn, reads=[b_act, b_wd[wi]], writes=[pyb])
                                    else:
                                        k.acc("tensor", fn, reads=[b_act, b_wd[wi]], acc=[pyb])
                                dsty = Yg[:, st, 512 * half:512 * half + 512]
                                if e8 == 0:
                                    VV("vector", "tensor_scalar", [pyb, b_Gs], [b_Yg[st]], out=dsty, in0=py[:, :], scalar1=Gs[:, st, e8:e8 + 1], scalar2=None, op0=ALU.mult)
                                else:
                                    VV("vector", "scalar_tensor_tensor", [pyb, b_Gs, b_Yg[st]], [b_Yg[st]], out=dsty, in0=py[:, :], scalar=Gs[:, st, e8:e8 + 1], in1=dsty,
                                       op0=ALU.mult, op1=ALU.add)
                for st in range(NST):
                    VV("gpsimd", "tensor_copy", [b_Yg[st]], [b_Ygs], out=Ygs[:, st, :], in_=Yg[:, st, :])
                for ti in range(NT):
                    pa = [(psT[0], psB[0]), (psT[1], psB[1])] if ti % 2 == 0 else [(psT[2], psB[2]), (psT[3], psB[3])]
                    for st in range(NST):
                        sti = tcnt % 4
                        tcnt += 1
                        pt_b = psbf[:, 128 * sti:128 * sti + 128]
                        k.op("tensor", lambda e, pt_b=pt_b, ti=ti, st=st: e.transpose(pt_b, Sel[:, ti, 128 * st:128 * st + 128], identb[:]),
                             reads=[b_Sel, b_identb], writes=[psB[7]])
                        VV("vector", "tensor_copy", [psB[7]], [b_selT[sti]], out=selT[sti][:], in_=pt_b)
                        for half in range(2):
                            fn = lambda e, half=half, sti=sti, st=st, pa=pa: e.matmul(pa[half][0][:, :], lhsT=selT[sti][:], rhs=Ygs[:, st, 512 * half:512 * half + 512],
                                                                                 start=(st == 0), stop=(st == NST - 1))
                            if st == 0:
                                k.op("tensor", fn, reads=[b_selT[sti], b_Ygs], writes=[pa[half][1]])
                            else:
                                k.acc("tensor", fn, reads=[b_selT[sti], b_Ygs], acc=[pa[half][1]])
                    for half in range(2):
                        dy = yacc[:, ti, 512 * half:512 * half + 512]
                        VV("vector", "tensor_tensor", [pa[half][1], b_y[ti]], [b_y[ti]], out=dy, in0=pa[half][0][:, :], in1=dy, op=ALU.add)
        S.barrier()

        with ExitStack() as s3:
            sb3 = lambda n, s, dt=F32: s3.enter_context(nc.sbuf_tensor(n, list(s), dt))
            lnp3 = sb3("lnp_sb3", [128, 2, 1024])
            b_lnp3 = Buf("lnp3")
            k.dma("sync", lnp3[:], C.lnp_d[:, 2:4, :], writes=[b_lnp3])
            st6, mv = st6_c3, mv_c3
            b_st = [Buf() for _ in range(2)]
            b_mv = [Buf() for _ in range(2)]
            yo = [sb3("yo%d" % i, [128, 1024]) for i in range(2)]
            b_yo = [Buf("yo%d" % i) for i in range(2)]
            for ti in range(NT if KC >= 30 else 0):
                bi = ti % 2
                for half in range(2):
                    k.op("vector", lambda e, bi=bi, half=half, ti=ti: e.bn_stats(out=st6[:, bi, half, :], in_=yacc[:, ti, 512 * half:512 * half + 512]),
                         reads=[b_y[ti]], writes=[b_st[bi]])
                k.op("vector", lambda e, bi=bi: e.bn_aggr(out=mv[:, bi, :], in_=st6[:, bi, :, :].rearrange("p a b -> p (a b)")), reads=[b_st[bi]], writes=[b_mv[bi]])
                k.op("scalar", lambda e, bi=bi: e.activation(out=mv[:, bi, 1:2], in_=mv[:, bi, 1:2], func=AF.Sqrt, bias=C.eps5[:, 0:1]), reads=[b_mv[bi], C.b_eps5], writes=[b_mv[bi]])
                k.op("vector", lambda e, bi=bi: e.reciprocal(out=mv[:, bi, 1:2], in_=mv[:, bi, 1:2]), reads=[b_mv[bi]], writes=[b_mv[bi]])
                k.op("vector", lambda e, bi=bi, ti=ti: e.tensor_scalar(out=yo[bi][:], in0=yacc[:, ti, :], scalar1=mv[:, bi, 0:1], scalar2=mv[:, bi, 1:2], op0=ALU.subtract, op1=ALU.mult),
                     reads=[b_y[ti], b_mv[bi]], writes=[b_yo[bi]])
                k.op("gpsimd", lambda e, bi=bi: e.tensor_tensor(out=yo[bi][:], in0=yo[bi][:], in1=lnp3[:, 0, :], op=ALU.mult), reads=[b_yo[bi], b_lnp3], writes=[b_yo[bi]])
                k.op("gpsimd", lambda e, bi=bi: e.tensor_tensor(out=yo[bi][:], in0=yo[bi][:], in1=lnp3[:, 1, :], op=ALU.add), reads=[b_yo[bi], b_lnp3], writes=[b_yo[bi]])
                if ti < 16:
                    final.append(k.dma("sync", C.y_out[128 * ti:128 * ti + 128, :], yo[bi][:], reads=[b_yo[bi]]))
                else:
                    final.append(k.dma("sync", C.ys_out, yo[bi][0:NS, :], reads=[b_yo[bi]]))


def phase_s(C):
    nc, k, S = C.nc, C.k, C.S
    psT, psB = C.psT, C.psB
    final = C.final
    ps_d, cs_d, ms_d = C.ps_d, C.cs_d, C.ms_d
    b_ps_d, b_cs_d, b_ms_d = Buf("ps_d"), Buf("cs_d"), Buf("ms_d")
    w_v = C.w_v

    def VV(eng, method, reads, writes, **aps):
        return k.op(eng, lambda e, aps=aps, method=method: getattr(e, method)(**aps), reads=reads, writes=writes)

    with ExitStack() as s1:
        sb1 = lambda n, s, dt=F32: s1.enter_context(nc.sbuf_tensor(n, list(s), dt))
        xsTb = sb1("xsTb", [128, 8, NS], BF16)
        b_xs = Buf("xsTb")
        k.gload(xsTb[:], C.xsT.rearrange("(k p) t -> p k t", p=128), writes=[b_xs])
        wS = [sb1("wS%d" % i, [128, 8, 512], BF16) for i in range(2)]
        b_wS = [Buf("wS%d" % i) for i in range(2)]
        p_s = sb1("p_s", [NS, IN_COLS])
        b_p = Buf("p_s")
        for cchunk in range(8):
            c0 = 512 * cchunk
            cn = min(512, IN_COLS - c0)
            wi = cchunk % 2
            k.gload(wS[wi][:, :, 0:cn], w_v[:, :, c0:c0 + cn], writes=[b_wS[wi]])
            pt_, pb_ = psT[wi], psB[wi]
            for kk in range(8):
                fn = lambda e, kk=kk, wi=wi, cn=cn, pt_=pt_: e.matmul(pt_[0:NS, 0:cn], lhsT=xsTb[:, kk, :], rhs=wS[wi][:, kk, 0:cn], start=(kk == 0), stop=(kk == 7))
                if kk == 0:
                    k.op("tensor", fn, reads=[b_xs, b_wS[wi]], writes=[pb_])
                else:
                    k.acc("tensor", fn, reads=[b_xs, b_wS[wi]], acc=[pb_])
            VV("vector", "tensor_copy", [pb_], [b_p], out=p_s[:, c0:c0 + cn], in_=pt_[0:NS, 0:cn])
        k.dma("sync", ps_d, p_s[:], reads=[b_p], writes=[b_ps_d])
        final.append(k.dma("sync", C.knew, p_s[:, COL_KA:COL_KA + 512], reads=[b_p], slot="knew"))
        final.append(k.dma("sync", C.vnew, p_s[:, COL_VA:COL_VA + 512], reads=[b_p], slot="vnew"))
        final.append(k.dma("sync", C.conv_s[:, 2, :], p_s[:, COL_UB:COL_UB + 1536], reads=[b_p], slot="convs"))
        cst_ = sb1("cst_", [NS, 3, 1536])
        b_cst_ = Buf("cst_")
        k.dma("sync", cst_[:], C.scv, writes=[b_cst_])
        final.append(k.dma("sync", C.conv_s[:, 0:2, :], cst_[:, 1:3, :], reads=[b_cst_], slot="convs"))
        cwr = sb1("cwr_sb", [NS, 4, 1536])
        b_cwr = Buf("cwr")
        k.dma("sync", cwr[:], C.cwr_d, writes=[b_cwr])
        cacc = sb1("cacc", [NS, 1536])
        ctmp = sb1("ctmp", [NS, 1536])
        b_ca, b_ct = Buf("cacc"), Buf()
        VV("vector", "tensor_tensor", [b_p, b_cwr], [b_ca], out=cacc[:], in0=p_s[:, COL_UB:COL_UB + 1536], in1=cwr[:, 3, :], op=ALU.mult)
        for i in range(3):
            VV("vector", "tensor_tensor", [b_cst_, b_cwr], [b_ct], out=ctmp[:], in0=cst_[:, i, :], in1=cwr[:, i, :], op=ALU.mult)
            VV("vector", "tensor_tensor", [b_ct, b_ca], [b_ca], out=cacc[:], in0=cacc[:], in1=ctmp[:], op=ALU.add)
        VV("scalar", "activation", [b_ca], [b_ca], out=cacc[:], in_=cacc[:], func=AF.Silu)
        k.dma("sync", cs_d, cacc[:], reads=[b_ca], writes=[b_cs_d])
    S.barrier()

    with ExitStack() as s2:
        sb2 = lambda n, s, dt=F32: s2.enter_context(nc.sbuf_tensor(n, list(s), dt))
        qkv = sb2("qkv_nh", [128, 3, 64])
        b_qkv = Buf("qkv_nh")
        for j, c0 in enumerate((COL_QA, COL_KA, COL_VA)):
            k.dma("sync", qkv[:, j, :], bass.AP(ps_d.tensor, c0, [[IN_COLS, NS], [64, 8], [1, 64]]), reads=[b_ps_d], writes=[b_qkv])
        sbias = sb2("sbias_sb", [128, 3, 129])
        b_sb = Buf("sbias")
        k.dma("sync", sbias[:], C.sbias_d, writes=[b_sb])
        Kb = sb2("Kb", [128, 128, 64])
        Vb = sb2("Vb", [128, 128, 64])
        b_Kb, b_Vb = Buf("Kb"), Buf("Vb")
        tmpS = sb2("tmpS", [128, 128, 64])
        b_tmp = Buf()
        sc = sb2("sc", [128, 3, 129])
        b_sc = Buf()
        sm = sb2("smS", [128, 16])
        b_sm = Buf()
        oacc = sb2("oacc", [128, 64])
        otmp = sb2("otmp", [128, 64])
        b_oa, b_ot = Buf("oacc"), Buf()
        VV("vector", "tensor_tensor", [b_qkv], [b_ot], out=otmp[:], in0=qkv[:, 0, :], in1=qkv[:, 1, :], op=ALU.mult)
        VV("vector", "tensor_reduce", [b_ot], [b_sm], out=sm[:, 0:1], in_=otmp[:], axis=AX.X, op=ALU.add)
        for br, (_, dil) in enumerate(BRANCHES):
            for n in range(NS):
                src = bass.AP(C.ck.tensor, n * 2048 * 512 + (2048 - 128 * dil) * 512, [[64, 8], [dil * 512, 128], [1, 64]])
                k.dma("sync", Kb[8 * n:8 * n + 8, :, :], src, writes=[b_Kb]) if n == 0 else k.S.dmaop(
                    "sync", "Kb", lambda e, n=n, src=src: e.dma_start(out=Kb[8 * n:8 * n + 8, :, :], in_=src), [])
            b_Kb.writer = ("D", "Kb", k.S.dma_sems["Kb"][1])
            VV("vector", "tensor_tensor", [b_Kb, b_qkv], [b_tmp], out=tmpS[:], in0=Kb[:], in1=qkv[:, 0, :].unsqueeze(1).to_broadcast([128, 128, 64]), op=ALU.mult)
            VV("vector", "tensor_reduce", [b_tmp], [b_sc], out=sc[:, br, 0:128], in_=tmpS[:], axis=AX.X, op=ALU.add)
            VV("vector", "tensor_copy", [b_sm], [b_sc], out=sc[:, br, 128:129], in_=sm[:, 0:1])
        VV("vector", "scalar_tensor_tensor", [b_sc, b_sb], [b_sc], out=sc[:].rearrange("p a b -> p (a b)"), in0=sc[:].rearrange("p a b -> p (a b)"),
           scalar=0.125, in1=sbias[:].rearrange("p a b -> p (a b)"), op0=ALU.mult, op1=ALU.add)
        VV("vector", "tensor_reduce", [b_sc], [b_sm], out=sm[:, 1:2], in_=sc[:].rearrange("p a b -> p (a b)"), axis=AX.X, op=ALU.max)
        VV("vector", "tensor_scalar", [b_sm], [b_sm], out=sm[:, 2:3], in0=sm[:, 1:2], scalar1=-1.0, scalar2=None, op0=ALU.mult)
        VV("scalar", "activation", [b_sc, b_sm], [b_sc, b_sm], out=sc[:].rearrange("p a b -> p (a b)"), in_=sc[:].rearrange("p a b -> p (a b)"), func=AF.Exp,
           bias=sm[:, 2:3], accum_out=sm[:, 3:4])
        VV("vector", "reciprocal", [b_sm], [b_sm], out=sm[:, 3:4], in_=sm[:, 3:4])
        VV("vector", "tensor_reduce", [b_sc], [b_sm], out=sm[:, 4:5], in_=sc[:, :, 128], axis=AX.X, op=ALU.add)
        VV("vector", "tensor_scalar", [b_qkv, b_sm], [b_oa], out=oacc[:], in0=qkv[:, 2, :], scalar1=sm[:, 4:5], scalar2=None, op0=ALU.mult)
        for br, (_, dil) in enumerate(BRANCHES):
            for n in range(NS):
                src = bass.AP(C.cv.tensor, n * 2048 * 512 + (2048 - 128 * dil) * 512, [[64, 8], [dil * 512, 128], [1, 64]])
                if n == 0:
                    k.dma("sync", Vb[8 * n:8 * n + 8, :, :], src, writes=[b_Vb])
                else:
                    k.S.dmaop("sync", "Vb", lambda e, n=n, src=src: e.dma_start(out=Vb[8 * n:8 * n + 8, :, :], in_=src), [])
            b_Vb.writer = ("D", "Vb", k.S.dma_sems["Vb"][1])
            VV("vector", "tensor_tensor", [b_Vb, b_sc], [b_tmp], out=tmpS[:], in0=Vb[:], in1=sc[:, br, 0:128].unsqueeze(2).to_broadcast([128, 128, 64]), op=ALU.mult)
            VV("vector", "tensor_reduce", [b_tmp], [b_ot], out=otmp[:], in_=tmpS[:].rearrange("p i d -> p d i"), axis=AX.X, op=ALU.add)
            VV("vector", "tensor_tensor", [b_ot, b_oa], [b_oa], out=oacc[:], in0=oacc[:], in1=otmp[:], op=ALU.add)
        VV("vector", "tensor_scalar", [b_oa, b_sm], [b_oa], out=oacc[:], in0=oacc[:], scalar1=sm[:, 3:4], scalar2=None, op0=ALU.mult)
        k.dma("sync", bass.AP(ms_d.tensor, 0, [[D, NS], [64, 8], [1, 64]]), oacc[:], reads=[b_oa], writes=[b_ms_d])
    S.barrier()

    with ExitStack() as s3:
        sb3 = lambda n, s, dt=F32: s3.enter_context(nc.sbuf_tensor(n, list(s), dt))
        NP = NS * 4
        St = sb3("St", [NP, 128, 128])
        b_St = Buf("St")
        for q4 in range(4):
            k.dma("sync", St[:, 32 * q4:32 * q4 + 32, :], C.sst[:, 32 * q4:32 * q4 + 32, :], writes=[b_St]) if q4 == 0 else k.S.dmaop(
                "sync", "St", lambda e, q4=q4: e.dma_start(out=St[:, 32 * q4:32 * q4 + 32, :], in_=C.sst[:, 32 * q4:32 * q4 + 32, :]), [])
        b_St.writer = ("D", "St", k.S.dma_sems["St"][1])
        T2 = sb3("T2", [NP, 128, 128])
        b_T2 = Buf()
        c3 = sb3("c3", [NP, 3, 128])
        b_c3 = Buf("c3")
        for ty in range(3):
            k.dma("sync", c3[:, ty, :], bass.AP(cs_d.tensor, 512 * ty, [[1536, NS], [128, 4], [1, 128]]), reads=[b_cs_d], writes=[b_c3])
        zz = sb3("zz", [NP, 128])
        b_zz = Buf("zz")
        k.dma("sync", zz[:], bass.AP(ps_d.tensor, COL_ZB, [[IN_COLS, NS], [128, 4], [1, 128]]), reads=[b_ps_d], writes=[b_zz])
        ab = sb3("ab_s", [NP, 2])
        b_ab = Buf("ab_s")
        k.dma("sync", ab[:, 0:1], bass.AP(ps_d.tensor, COL_AB, [[IN_COLS, NS], [1, 4], [1, 1]]), reads=[b_ps_d], writes=[b_ab])
        k.dma("sync", ab[:, 1:2], bass.AP(ps_d.tensor, COL_BB, [[IN_COLS, NS], [1, 4], [1, 1]]), reads=[b_ps_d], writes=[b_ab])
        sp = sb3("sprm_sb", [NP, 2 + 128])
        b_sp = Buf("sprm")
        k.dma("sync", sp[:], C.sprm_d, writes=[b_sp])
        w = sb3("wS3", [NP, 24])
        b_w = Buf()
        jk = sb3("jkS3", [NP, 128])
        b_jk = Buf()
        VV("vector", "tensor_tensor", [b_ab, b_sp], [b_w], out=w[:, 0:1], in0=ab[:, 0:1], in1=sp[:, 1:2], op=ALU.add)
        VV("scalar", "activation", [b_w], [b_w], out=w[:, 0:1], in_=w[:, 0:1], func=AF.Exp)
        VV("scalar", "activation", [b_w], [b_w], out=w[:, 0:1], in_=w[:, 0:1], func=AF.Ln, bias=1.0)
        VV("scalar", "activation", [b_sp], [b_w], out=w[:, 1:2], in_=sp[:, 0:1], func=AF.Exp)
        VV("vector", "scalar_tensor_tensor", [b_w], [b_w], out=w[:, 2:3], in0=w[:, 0:1], scalar=-1.0, in1=w[:, 1:2], op0=ALU.mult, op1=ALU.mult)
        VV("scalar", "activation", [b_w], [b_w], out=w[:, 3:4], in_=w[:, 2:3], func=AF.Exp)
        VV("scalar", "activation", [b_ab], [b_w], out=w[:, 4:5], in_=ab[:, 1:2], func=AF.Sigmoid)
        for j in range(2):
            VV("scalar", "activation", [b_c3], [b_jk, b_w], out=jk[:], in_=c3[:, j, :], func=AF.Square, accum_out=w[:, 5 + j:6 + j])
            VV("scalar", "activation", [b_w, C.b_eps], [b_w], out=w[:, 5 + j:6 + j], in_=w[:, 5 + j:6 + j], func=AF.Sqrt, bias=C.eps6[0:NP, 0:1])
            VV("vector", "reciprocal", [b_w], [b_w], out=w[:, 5 + j:6 + j], in_=w[:, 5 + j:6 + j])
        VV("vector", "tensor_scalar", [b_c3, b_w], [b_c3], out=c3[:, 0, :], in0=c3[:, 0, :], scalar1=w[:, 5:6], scalar2=128.0 ** -0.5, op0=ALU.mult, op1=ALU.mult)
        VV("vector", "tensor_scalar", [b_c3, b_w], [b_c3], out=c3[:, 1, :], in0=c3[:, 1, :], scalar1=w[:, 6:7], scalar2=None, op0=ALU.mult)
        mem = sb3("memS", [NP, 4, 128])
        b_mem = Buf()
        VV("vector", "tensor_tensor", [b_St, b_c3], [b_T2], out=T2[:], in0=St[:], in1=c3[:, 1, :].unsqueeze(2).to_broadcast([NP, 128, 128]), op=ALU.mult)
        VV("vector", "tensor_reduce", [b_T2], [b_mem], out=mem[:, 0, :], in_=T2[:].rearrange("p d e -> p e d"), axis=AX.X, op=ALU.add)
        VV("vector", "scalar_tensor_tensor", [b_mem, b_w, b_c3], [b_mem], out=mem[:, 1, :], in0=mem[:, 0, :], scalar=w[:, 3:4], in1=c3[:, 2, :], op0=ALU.mult, op1=ALU.subtract)
        VV("vector", "tensor_scalar", [b_mem, b_w], [b_mem], out=mem[:, 1, :], in0=mem[:, 1, :], scalar1=w[:, 4:5], scalar2=-1.0, op0=ALU.mult, op1=ALU.mult)
        VV("vector", "tensor_tensor", [b_c3, b_mem], [b_T2], out=T2[:], in0=c3[:, 1, :].unsqueeze(2).to_broadcast([NP, 128, 128]),
           in1=mem[:, 1, :].unsqueeze(1).to_broadcast([NP, 128, 128]), op=ALU.mult)
        VV("vector", "scalar_tensor_tensor", [b_St, b_T2, b_w], [b_St], out=St[:], in0=St[:], scalar=w[:, 3:4], in1=T2[:], op0=ALU.mult, op1=ALU.add)
        for q4 in range(4):
            final.append(k.dma("sync", C.ssm_s[:, 32 * q4:32 * q4 + 32, :], St[:, 32 * q4:32 * q4 + 32, :], reads=[b_St], slot="ssms"))
        VV("vector", "tensor_tensor", [b_St, b_c3], [b_T2], out=T2[:], in0=St[:], in1=c3[:, 0, :].unsqueeze(2).to_broadcast([NP, 128, 128]), op=ALU.mult)
        VV("vector", "tensor_reduce", [b_T2], [b_mem], out=mem[:, 2, :], in_=T2[:].rearrange("p d e -> p e d"), axis=AX.X, op=ALU.add)
        VV("scalar", "activation", [b_mem], [b_jk, b_w], out=jk[:], in_=mem[:, 2, :], func=AF.Square, accum_out=w[:, 8:9])
        VV("scalar", "activation", [b_w, C.b_eps], [b_w], out=w[:, 8:9], in_=w[:, 8:9], func=AF.Sqrt, scale=1.0 / 128.0, bias=C.eps6[0:NP, 0:1])
        VV("vector", "reciprocal", [b_w], [b_w], out=w[:, 8:9], in_=w[:, 8:9])
        VV("scalar", "activation", [b_zz], [b_zz], out=zz[:], in_=zz[:], func=AF.Silu)
        VV("vector", "tensor_tensor", [b_zz, b_sp], [b_zz], out=zz[:], in0=zz[:], in1=sp[:, 2:130], op=ALU.mult)
        VV("vector", "scalar_tensor_tensor", [b_mem, b_w, b_zz], [b_mem], out=mem[:, 3, :], in0=mem[:, 2, :], scalar=w[:, 8:9], in1=zz[:], op0=ALU.mult, op1=ALU.mult)
        k.dma("sync", bass.AP(ms_d.tensor, 512, [[D, NS], [128, 4], [1, 128]]), mem[:, 3, :], reads=[b_mem], writes=[b_ms_d])
    S.barrier()

    with ExitStack() as s4:
        sb4 = lambda n, s, dt=F32: s4.enter_context(nc.sbuf_tensor(n, list(s), dt))
        msf = sb4("msf", [128, 1024])
        b_msf = Buf("msf")
        VV("vector", "memset", [], [b_msf], ap=msf[:], constant=0.0)
        k.dma("sync", msf[0:NS, :], ms_d, reads=[b_ms_d], writes=[b_msf])
        mxS = sb4("mxS", [128, 8, 128], BF16)
        b_mxS = Buf("mxS")
        for g4 in range(2):
            pt_, pb_ = psT[2 + g4], psB[2 + g4]
            for j in range(4):
                kk = 4 * g4 + j
                fn = lambda e, kk=kk, j=j, pt_=pt_: e.transpose(pt_[:, 128 * j:128 * j + 128], msf[:, 128 * kk:128 * kk + 128], C.cst[:, 0, :])
                if j == 0:
                    k.op("tensor", fn, reads=[b_msf, C.b_cst], writes=[pb_])
                else:
                    k.acc("tensor", fn, reads=[b_msf, C.b_cst], acc=[pb_])
            VV("vector", "tensor_copy", [pb_], [b_mxS], out=mxS[:, 4 * g4:4 * g4 + 4, :], in_=pt_[:, :].rearrange("p (a c) -> p a c", a=4))
        k.dma("sync", C.mixS_d, mxS[:], reads=[b_mxS], writes=[C.b_mixS_d])
    S.barrier()


def _consts():
    t = np.arange(128)
    same = (t[:, None] // 64) == (t[None, :] // 64)
    cst = np.zeros((128, 8, 128), np.float32)
    cst[:, 0] = np.eye(128)
    cst[:, 1] = -np.eye(128)
    cst[:, 2] = (same & (t[:, None] <= t[None, :]))
    cst[:, 3] = (t[:, None] < 64) * np.ones((1, 128))
    cst[:, 4] = (t[:, None] >= 64) * np.ones((1, 128))
    cst[:, 5] = np.where(same & (t[None, :] <= t[:, None]), 0.0, NEG)
    cst[:, 6] = (same & (t[None, :] < t[:, None]))
    cst[:, 7] = (t[:, None] < t[None, :])
    return cst


def _params(inp):
    prm = np.zeros((128, 184), np.float32)
    prm[:, 0:4] = inp["a_log"][0][None, :]
    prm[:, 4:8] = inp["dt_bias"][0][None, :]
    prm[:, 8:136] = inp["o_norm_g"][0][None, :]
    cw = inp["conv_w"][0]
    prm[:, 136:184] = cw.reshape(4, 12, 128).transpose(2, 1, 0).reshape(128, 48)
    return prm


def _sample_bias(rel_bias):
    out = np.empty((128, 3, 129), np.float32)
    h = np.arange(128) % 8
    for br, (_, dil) in enumerate(BRANCHES):
        dist = np.concatenate([dil * (128 - np.arange(128)), [0]])
        out[:, br, :] = rel_bias[_rel_bucket_np(dist)][:, h].T
    return out


def _core_inputs(c, inp, bt):
    b, hf = divmod(c, 2)
    x = inp["x_prompt"][b]
    xT = np.zeros((D, EXT), np.float32)
    if hf == 1:
        xT[:, :] = x.T
    else:
        xT[:, HALF:] = x[:HALF].T
    valid = np.ones((128, 32), np.float32)
    if hf == 0:
        valid[:, :16] = 0.0
    return {
        "xT": np.ascontiguousarray(xT),
        "xo": np.ascontiguousarray(x[HALF * hf:HALF * hf + HALF]),
        "valid": valid,
        "w_in": np.ascontiguousarray(inp["w_in"][0]),
        "bt": bt,
        "cst": _consts(),
        "prm": _params(inp),
        "w_out": np.ascontiguousarray(inp["w_out"][0]),
        "lnp": np.ascontiguousarray(np.broadcast_to(np.stack([inp["ln1_g"][0], inp["ln1_b"][0], inp["ln2_g"][0], inp["ln2_b"][0]])[None], (128, 4, D))),
        "wr": np.ascontiguousarray(np.concatenate([inp["w_group"][0], inp["w_router"][0]], axis=1)),
        "rb": np.ascontiguousarray(np.broadcast_to(np.concatenate([inp["b_group"][0], inp["b_router"][0].reshape(-1)])[None], (128, 36))),
        "w_gate": np.ascontiguousarray(inp["w_gate"][0]),
        "w_up": np.ascontiguousarray(inp["w_up"][0]),
        "w_down": np.ascontiguousarray(inp["w_down"][0]),
        "xsT": np.ascontiguousarray(inp["x_sample"][NS * c:NS * c + NS, 0, :].T),
        "ck": np.ascontiguousarray(inp["cache_a_k"][0, NS * c:NS * c + NS].reshape(NS, 2048, 512)),
        "cv": np.ascontiguousarray(inp["cache_a_v"][0, NS * c:NS * c + NS].reshape(NS, 2048, 512)),
        "sst": np.ascontiguousarray(inp["state_b_ssm"][0, NS * c:NS * c + NS].reshape(NS * 4, 128, 128)),
        "scv": np.ascontiguousarray(inp["state_b_conv"][0, NS * c:NS * c + NS]),
        "sbias": _sample_bias(inp["rel_bias"].astype(np.float32)),
        "cwr": np.ascontiguousarray(np.broadcast_to(inp["conv_w"][0][None], (NS, 4, 1536))),
        "sprm": np.ascontiguousarray(np.concatenate([np.tile(inp["a_log"][0], NS)[:, None], np.tile(inp["dt_bias"][0], NS)[:, None],
                                                       np.broadcast_to(inp["o_norm_g"][0][None], (NS * 4, 128))], axis=1)),
        "iota": np.ascontiguousarray(np.broadcast_to(np.arange(CAP, dtype=np.float32)[None], (128, CAP))),
        "xs_pad": np.ascontiguousarray(np.concatenate([inp["x_sample"][NS * c:NS * c + NS, 0, :], np.zeros((128 - NS, D), np.float32)], axis=0)),
    }


def kernel(**inputs):
    inp = {k_: np.asarray(v) for k_, v in inputs.items()}
    bt = _bias_tiles(inp["rel_bias"].astype(np.float32))
    nc = build()
    in_maps = [_core_inputs(c, inp, bt) for c in range(NCORES)]
    res = run_bass_kernel_spmd(nc, in_maps, core_ids=list(range(NCORES)))
    r = res.results
    y_prompt = np.stack([np.concatenate([r[2 * b]["y_out"], r[2 * b + 1]["y_out"]], axis=0) for b in range(4)])
    k_win = np.stack([r[2 * b + 1]["kwin"].reshape(HALF, 8, 64) for b in range(4)])[None]
    v_win = np.stack([r[2 * b + 1]["vwin"].reshape(HALF, 8, 64) for b in range(4)])[None]
    ssm_p = np.stack([r[2 * b + 1]["ssm_p"] for b in range(4)])[None]
    conv_p = np.stack([r[2 * b + 1]["conv_p"] for b in range(4)])[None]
    y_sample = np.concatenate([r[c]["ys_out"] for c in range(NCORES)], axis=0)[:, None, :]
    k_new = np.concatenate([r[c]["knew"] for c in range(NCORES)], axis=0).reshape(1, 128, 1, 8, 64)
    v_new = np.concatenate([r[c]["vnew"] for c in range(NCORES)], axis=0).reshape(1, 128, 1, 8, 64)
    ssm_s = np.concatenate([r[c]["ssm_s"] for c in range(NCORES)], axis=0).reshape(1, 128, 4, 128, 128)
    conv_s = np.concatenate([r[c]["conv_s"] for c in range(NCORES)], axis=0)[None]
    return (y_prompt, y_sample, k_win, v_win, k_new, v_new, ssm_p, ssm_s, conv_p, conv_s)
```

```python
import math
import os
from contextlib import ExitStack

import numpy as np
import concourse.bass as bass
import concourse.mybir as mybir
from concourse.bass_utils import run_bass_kernel_spmd

F32 = mybir.dt.float32
BF16 = mybir.dt.bfloat16
I32 = mybir.dt.int32
U32 = mybir.dt.uint32
AF = mybir.ActivationFunctionType
ALU = mybir.AluOpType
AX = mybir.AxisListType

NCORES = 8
D = 1024
SEQ = 4096
HALF = 2048
EXT = 4096
NS = 16
A_HEADS, A_HD = 8, 64
B_HEADS, B_HD = 4, 128
COL_QA, COL_KA, COL_VA, COL_UB = 0, 512, 1024, 1536
COL_ZB = COL_UB + 1536
COL_AB = COL_ZB + 512
COL_BB = COL_AB + 4
IN_COLS = COL_BB + 4
BRANCHES = ((128, 1), (512, 4), (2048, 16))
NEG = -30000.0
CAP = 640
ENGS = ("tensor", "vector", "scalar", "gpsimd", "sync")


class Sched:
    def __init__(self, nc, stack, same_engine_wait=True):
        self.nc = nc
        self.stack = stack
        self.q = {e: [] for e in ENGS}
        self.cnt = {e: 0 for e in ENGS}
        self.sem = {e: stack.enter_context(nc.semaphore("s_" + e)) for e in ENGS}
        self.waited = {e: {} for e in ENGS}
        self.same_engine_wait = same_engine_wait
        self.dma_sems = {}
        self.ninst = 0

    def _wait(self, eng, tok):
        if tok is None:
            return
        if tok[0] == "E":
            _, src, val = tok
            if src == eng and not self.same_engine_wait:
                return
            key = "E" + src
            sem = self.sem[src]
        else:
            _, slot, val = tok
            key = "D" + slot
            sem = self.dma_sems[slot][0]
        if self.waited[eng].get(key, 0) >= val:
            return
        self.waited[eng][key] = val
        self.q[eng].append(lambda e, sem=sem, val=val: e.wait_ge(sem, val))

    def op(self, eng, fn, deps=()):
        for d in deps:
            self._wait(eng, d)
        self.cnt[eng] += 1
        c = self.cnt[eng]
        sem = self.sem[eng]
        self.q[eng].append(lambda e, fn=fn, sem=sem: fn(e).then_inc(sem, 1))
        self.ninst += 1
        return ("E", eng, c)

    def dmaop(self, eng, slot, fn, deps=()):
        for d in deps:
            self._wait(eng, d)
        if slot not in self.dma_sems:
            self.dma_sems[slot] = [self.stack.enter_context(self.nc.semaphore("d_" + slot)), 0]
        ent = self.dma_sems[slot]
        ent[1] += 16
        sem = ent[0]
        self.q[eng].append(lambda e, fn=fn, sem=sem: fn(e).then_inc(sem, 16))
        self.ninst += 1
        return ("D", slot, ent[1])

    def barrier(self):
        toks = [("E", e, self.cnt[e]) for e in ENGS if self.cnt[e] > 0]
        toks += [("D", slot, ent[1]) for slot, ent in self.dma_sems.items() if ent[1] > 0]
        for e in ENGS:
            for t in toks:
                self._wait(e, t)

    def finish(self, final_tokens):
        best = {}
        for t in final_tokens:
            if t is None:
                continue
            key = (t[0], t[1])
            if key not in best or best[key][2] < t[2]:
                best[key] = t
        for t in best.values():
            self._wait("sync", t)
        with self.nc.Block() as block:
            @block.tensor
            def _(e):
                for f in self.q["tensor"]:
                    f(e)

            @block.vector
            def _(e):
                for f in self.q["vector"]:
                    f(e)

            @block.scalar
            def _(e):
                for f in self.q["scalar"]:
                    f(e)

            @block.gpsimd
            def _(e):
                for f in self.q["gpsimd"]:
                    f(e)

            @block.sync
            def _(e):
                for f in self.q["sync"]:
                    f(e)


class Buf:
    _n = 0

    def __init__(self, name=None):
        Buf._n += 1
        self.name = name or ("b%d" % Buf._n)
        self.writer = None
        self.readers = {}

    def add_reader(self, tok):
        key = tok[1]
        if key not in self.readers or self.readers[key][2] < tok[2]:
            self.readers[key] = tok


class K:
    def __init__(self, S):
        self.S = S

    def _deps(self, reads, writes, deps):
        d = list(deps)
        for b in reads:
            d.append(b.writer)
        for b in writes:
            d.extend(b.readers.values())
            d.append(b.writer)
        return d

    def _commit(self, tok, reads, writes):
        for b in reads:
            b.add_reader(tok)
        for b in writes:
            b.writer = tok
            b.readers = {}

    def op(self, eng, fn, reads=(), writes=(), deps=()):
        tok = self.S.op(eng, fn, self._deps(reads, writes, deps))
        self._commit(tok, reads, writes)
        return tok

    def acc(self, eng, fn, reads=(), acc=(), deps=()):
        d = list(deps)
        for b in reads:
            d.append(b.writer)
        tok = self.S.op(eng, fn, d)
        for b in reads:
            b.add_reader(tok)
        for b in acc:
            b.writer = tok
        return tok

    def gload(self, dst, src, writes, reads=()):
        A, L = dst.shape[1], dst.shape[2]
        slot = writes[0].name
        d = self._deps(reads, writes, ())
        tok = None
        for a0 in range(0, A, 4):
            for l0 in range(0, L, 512):
                tok = self.S.dmaop("gpsimd", slot, lambda e, a0=a0, l0=l0, L=L: e.dma_start(
                    out=dst[:, a0:a0 + 4, l0:min(L, l0 + 512)], in_=src[:, a0:a0 + 4, l0:min(L, l0 + 512)]), d)
        self._commit(tok, reads, writes)
        return tok

    def dma(self, eng, out, in_, reads=(), writes=(), deps=(), slot=None, **kw):
        if slot is None:
            slot = (writes[0] if writes else reads[0]).name
        tok = self.S.dmaop(eng, slot, lambda e: e.dma_start(out=out, in_=in_, **kw), self._deps(reads, writes, deps))
        self._commit(tok, reads, writes)
        return tok


def _rel_bucket_np(dist):
    n = np.maximum(dist, 0)
    ratio = np.maximum(n, 1).astype(np.float32) / np.float32(16)
    large = 16 + (np.log(ratio) / np.float32(math.log(2048 / 16)) * np.float32(16)).astype(np.int32)
    return np.where(n < 16, n, np.minimum(large, 31))


def _bias_tiles(rel_bias):
    kp = np.arange(128)[:, None, None]
    kt = np.arange(2)[None, :, None]
    i = np.arange(128)[None, None, :]
    off = 128 + i - (128 * kt + kp)
    valid = (off >= 0) & (off <= 128)
    out = np.empty((128, 24, 256), np.float32)
    for h in range(A_HEADS):
        for br, (_, dil) in enumerate(BRANCHES):
            bk = _rel_bucket_np(np.maximum(off, 0) * dil)
            vals = rel_bias[bk, h]
            out[:, h * 3 + br, :] = np.where(valid, vals, np.float32(NEG)).reshape(128, 256)
    return out


def sl(start, step, n=128):
    return slice(start, start + (n - 1) * step + 1, step)


def tokset(ti, dil):
    nblk, r = divmod(ti, dil)
    start = r + dil * 128 * nblk
    return start, dil


def build(debug=(), stage=99, nexp=32, with_sample=True):
    nc = bass.Bass("TRN2", target_bir_lowering=False)
    dram = lambda n, s, dt=F32, kind="ExternalInput": nc.dram_tensor(n, list(s), dt, kind=kind).ap()
    xT = dram("xT", [D, EXT])
    xo = dram("xo", [HALF, D])
    valid = dram("valid", [128, 32])
    w_in = dram("w_in", [D, IN_COLS])
    bt = dram("bt", [128, 24, 256])
    cst_d = dram("cst", [128, 8, 128])
    prm_d = dram("prm", [128, 184])
    ssm_p = dram("ssm_p", [4, 128, 128], kind="ExternalOutput")
    conv_p = dram("conv_p", [3, 1536], kind="ExternalOutput")
    kT_d = dram("kT_d", [128, 4, EXT], BF16, kind="Internal")
    vT_d = dram("vT_d", [128, 4, EXT], BF16, kind="Internal")
    qT_d = dram("qT_d", [128, 4, HALF], BF16, kind="Internal")
    gz_d = dram("gz_d", [16, 128, 512], BF16, kind="Internal")
    mixA_d = dram("mixA_d", [128, 4, HALF], BF16, kind="Internal")
    mixB_d = dram("mixB_d", [128, 4, HALF], BF16, kind="Internal")
    w_out = dram("w_out", [D, D])
    lnp_d = dram("lnp", [128, 4, D])
    wr_d = dram("wr", [D, 36])
    rb_d = dram("rb", [128, 36])
    w_gate = dram("w_gate", [32, D, 512])
    w_up = dram("w_up", [32, D, 512])
    w_down = dram("w_down", [32, 512, D])
    xs_pad = dram("xs_pad", [128, D])
    iota_d = dram("iota", [128, CAP])
    hb_d = dram("hb_d", [17 * 128, D], BF16, kind="Internal")
    y_out = dram("y_out", [HALF, D], kind="ExternalOutput")
    ys_out = dram("ys_out", [NS, D], kind="ExternalOutput")
    mixS_d = dram("mixS_d", [128, 8, 128], BF16, kind="Internal")
    xsT = dram("xsT", [D, NS])
    ck = dram("ck", [NS, 2048, 512])
    cv = dram("cv", [NS, 2048, 512])
    sst = dram("sst", [NS * 4, 128, 128])
    scv = dram("scv", [NS, 3, 1536])
    sbias_d = dram("sbias", [128, 3, 129])
    cwr_d = dram("cwr", [NS, 4, 1536])
    sprm_d = dram("sprm", [NS * 4, 130])
    ps_d = dram("ps_d", [NS, IN_COLS], kind="Internal")
    cs_d = dram("cs_d", [NS, 1536], kind="Internal")
    ms_d = dram("ms_d", [NS, D], kind="Internal")
    knew = dram("knew", [NS, 512], kind="ExternalOutput")
    vnew = dram("vnew", [NS, 512], kind="ExternalOutput")
    ssm_s = dram("ssm_s", [NS * 4, 128, 128], kind="ExternalOutput")
    conv_s = dram("conv_s", [NS, 3, 1536], kind="ExternalOutput")
    kwin = dram("kwin", [HALF, 512], kind="ExternalOutput")
    vwin = dram("vwin", [HALF, 512], kind="ExternalOutput")
    dbg_out = {}
    for name, shape in debug:
        dbg_out[name] = dram("dbg_" + name, shape, kind="ExternalOutput")

    final = []
    with ExitStack() as st:
        S = Sched(nc, st, same_engine_wait=(os.environ.get("SEW", "1") == "1"))
        k = K(S)
        sb = lambda n, s, dt=F32: st.enter_context(nc.sbuf_tensor(n, list(s), dt))
        psT = [st.enter_context(nc.psum_tensor("ps%d" % i, [128, 512], F32)) for i in range(8)]
        psB = [Buf("ps%d" % i) for i in range(8)]

        ones_f = sb("ones_f", [128, 128])
        b_ones = Buf()
        k.op("gpsimd", lambda e: e.memset(ones_f[:], 1.0), writes=[b_ones])
        eps6 = sb("eps6", [128, 1])
        b_eps = Buf()
        k.op("gpsimd", lambda e: e.memset(eps6[:], 1e-6), writes=[b_eps])
        cst = sb("cst_sb", [128, 8, 128])
        b_cst = Buf("cst")
        k.dma("sync", cst[:], cst_d, writes=[b_cst])
        eps5 = sb("eps5", [128, 1])
        b_eps5 = Buf()
        k.op("gpsimd", lambda e: e.memset(eps5[:], 1e-5), writes=[b_eps5])
        b_mixA_d, b_mixB_d, b_mixS_d = Buf("mixA_d"), Buf("mixB_d"), Buf("mixS_d")
        valid_sb = sb("valid_sb", [128, 32])
        b_valid = Buf("valid")
        k.dma("sync", valid_sb[:], valid, writes=[b_valid])

        if with_sample:
            C = type("Ctx", (), {})()
            C.nc, C.k, C.S, C.final = nc, k, S, final
            C.psT, C.psB, C.cst, C.b_cst, C.eps6, C.b_eps = psT, psB, cst, b_cst, eps6, b_eps
            C.w_v = w_in.rearrange("(k p) c -> p k c", p=128)
            C.xsT, C.ck, C.cv, C.sst, C.scv, C.sbias_d, C.cwr_d, C.sprm_d = xsT, ck, cv, sst, scv, sbias_d, cwr_d, sprm_d
            C.ps_d, C.cs_d, C.ms_d = ps_d, cs_d, ms_d
            C.knew, C.vnew, C.ssm_s, C.conv_s = knew, vnew, ssm_s, conv_s
            C.mixS_d, C.b_mixS_d = mixS_d, b_mixS_d
            phase_s(C)
        sx = ExitStack()
        xTb = sx.enter_context(nc.sbuf_tensor("xTb", [128, 8, EXT], BF16))
        b_x = [Buf("xTb%d" % c) for c in range(8)]
        xT_v = xT.rearrange("(k p) t -> p k t", p=128)
        for c in range(8):
            k.gload(xTb[:, :, 512 * c:512 * (c + 1)], xT_v[:, :, 512 * c:512 * (c + 1)], writes=[b_x[c]])
        bx_of_tile = lambda ti_nat: b_x[ti_nat // 4]

        def x_bufs(start, step):
            lo, hi = start, start + step * 127
            return [b_x[c] for c in range(lo // 512, hi // 512 + 1)]

        with ExitStack() as sa:
            sba = lambda n, s, dt=F32: sa.enter_context(nc.sbuf_tensor(n, list(s), dt))
            mixA = sba("mixA", [128, 4, HALF], BF16)
            b_mixA = [Buf() for _ in range(4)]
            ETb = sba("ETb", [128, 24, 256], BF16)
            b_ET = Buf("ET")
            btst = sba("btst", [128, 6, 256])
            b_btst = Buf("btst")
            for g in range(4 if stage >= -1 else 0):
                k.dma("sync", btst[:], bt[:, 6 * g:6 * g + 6, :], writes=[b_btst])
                k.op("scalar", lambda e, g=g: e.activation(out=ETb[:, 6 * g:6 * g + 6, :], in_=btst[:], func=AF.Exp),
                     reads=[b_btst], writes=[b_ET])
            QT = sba("QT", [128, 2, HALF], BF16)
            KT = sba("KT", [128, 2, EXT], BF16)
            Vaug = sba("Vaug", [128, 32, 4, 65], BF16)
            acc = sba("acc", [65, 4, HALF])
            wq = sba("wq", [128, 8, 256], BF16)
            wk = sba("wk", [128, 8, 256], BF16)
            wv = sba("wv", [128, 8, 256], BF16)
            b_wq, b_wk, b_wv = Buf("wq"), Buf("wk"), Buf("wv")
            b_QT = [Buf() for _ in range(2)]
            b_KT = [Buf() for _ in range(2)]
            b_V = [Buf() for _ in range(32)]
            b_acc = [Buf() for _ in range(4)]
            b_rrow = Buf()
            stg = [sba("stg%d" % i, [128, 256]) for i in range(2)]
            b_stg = [Buf("stg%d" % i) for i in range(2)]
            exb = [sba("exb%d" % i, [128, 512]) for i in range(2)]
            b_ex = [Buf() for _ in range(2)]
            ptb = [sba("ptb%d" % i, [128, 512], BF16) for i in range(2)]
            b_pt = [Buf() for _ in range(2)]
            w_v = w_in.rearrange("(k p) c -> p k c", p=128)
            ctr = {"ps": 0, "stg": 0, "s": 0, "o": 0, "ev": 0}

            def proj_ps():
                i = ctr["ps"] % 2
                ctr["ps"] += 1
                return psT[i], psB[i]

            for hh2 in range(2):
                k.gload(wq[:], w_v[:, :, COL_QA + 256 * hh2:COL_QA + 256 * hh2 + 256], writes=[b_wq])
                k.gload(wk[:], w_v[:, :, COL_KA + 256 * hh2:COL_KA + 256 * hh2 + 256], writes=[b_wk])
                k.gload(wv[:], w_v[:, :, COL_VA + 256 * hh2:COL_VA + 256 * hh2 + 256], writes=[b_wv])
                for jj in range(2 if stage >= 0 else 0):
                    for tc in range(4 if os.environ.get('KQ','1')=='1' else 0):
                        pt_, pb_ = proj_ps()
                        for kk in range(8):
                            fn = lambda e, kk=kk, jj=jj, tc=tc, pt_=pt_: e.matmul(
                                pt_[:, :], lhsT=wq[:, kk, 128 * jj:128 * jj + 128],
                                rhs=xTb[:, kk, HALF + 512 * tc:HALF + 512 * tc + 512], start=(kk == 0), stop=(kk == 7))
                            if kk == 0:
                                k.op("tensor", fn, reads=[b_wq, b_x[4 + tc]], writes=[pb_])
                            else:
                                k.acc("tensor", fn, reads=[b_wq, b_x[4 + tc]], acc=[pb_])
                        k.op("vector", lambda e, jj=jj, tc=tc, pt_=pt_: e.tensor_scalar(
                            out=QT[:, jj, 512 * tc:512 * tc + 512], in0=pt_[:, :], scalar1=0.125, scalar2=None, op0=ALU.mult),
                            reads=[pb_], writes=[b_QT[jj]])
                    for tc in range(8 if os.environ.get('KK','1')=='1' else 0):
                        pt_, pb_ = proj_ps()
                        for kk in range(8):
                            fn = lambda e, kk=kk, jj=jj, tc=tc, pt_=pt_: e.matmul(
                                pt_[:, :], lhsT=wk[:, kk, 128 * jj:128 * jj + 128],
                                rhs=xTb[:, kk, 512 * tc:512 * tc + 512], start=(kk == 0), stop=(kk == 7))
                            if kk == 0:
                                k.op("tensor", fn, reads=[b_wk, b_x[tc]], writes=[pb_])
                            else:
                                k.acc("tensor", fn, reads=[b_wk, b_x[tc]], acc=[pb_])
                        k.op("vector", lambda e, jj=jj, tc=tc, pt_=pt_: e.tensor_copy(
                            out=KT[:, jj, 512 * tc:512 * tc + 512], in_=pt_[:, :]),
                            reads=[pb_], writes=[b_KT[jj]])
                for ti in range(16, 32 if stage >= -2 else 16):
                    pt_, pb_ = proj_ps()
                    for kk in range(8):
                        fn = lambda e, kk=kk, ti=ti, pt_=pt_: e.matmul(
                            pt_[:, 0:256], lhsT=xTb[:, kk, 128 * ti:128 * ti + 128], rhs=wk[:, kk, :],
                            start=(kk == 0), stop=(kk == 7))
                        if kk == 0:
                            k.op("tensor", fn, reads=[b_wk, b_x[ti // 4]], writes=[pb_])
                        else:
                            k.acc("tensor", fn, reads=[b_wk, b_x[ti // 4]], acc=[pb_])
                    si = ctr["stg"] % 2
                    ctr["stg"] += 1
                    k.op("scalar", lambda e, si=si, pt_=pt_: e.activation(out=stg[si][:], in_=pt_[:, 0:256], func=AF.Copy),
                         reads=[pb_], writes=[b_stg[si]])
                    final.append(k.dma("sync", kwin[128 * (ti - 16):128 * (ti - 16) + 128, 256 * hh2:256 * hh2 + 256],
                                       stg[si][:], reads=[b_stg[si]]))
                for br, (_, dil) in enumerate(BRANCHES):
                    if stage < 1:
                        break
                    k.op("gpsimd", lambda e: e.tensor_copy(
                        out=Vaug[:, :, :, 64], in_=valid_sb[:, :].unsqueeze(2).to_broadcast([128, 32, 4])),
                        reads=[b_valid], writes=b_V)
                    for ti in range(32):
                        start, step = tokset(ti, dil)
                        pt_, pb_ = proj_ps()
                        for kk in range(8):
                            fn = lambda e, kk=kk, start=start, step=step, pt_=pt_: e.matmul(
                                pt_[:, 0:256], lhsT=xTb[:, kk, sl(start, step)], rhs=wv[:, kk, :],
                                start=(kk == 0), stop=(kk == 7))
                            if kk == 0:
                                k.op("tensor", fn, reads=[b_wv] + x_bufs(start, step), writes=[pb_])
                            else:
                                k.acc("tensor", fn, reads=[b_wv] + x_bufs(start, step), acc=[pb_])
                        ev = "vector"
                        src = pt_[:, 0:256].rearrange("p (h d) -> p h d", h=4)
                        if ev == "vector":
                            k.op("vector", lambda e, ti=ti, src=src: e.tensor_copy(out=Vaug[:, ti, :, 0:64], in_=src),
                                 reads=[pb_], writes=[b_V[ti]])
                        else:
                            k.op("scalar", lambda e, ti=ti, src=src: e.activation(out=Vaug[:, ti, :, 0:64], in_=src, func=AF.Copy),
                                 reads=[pb_], writes=[b_V[ti]])
                        if br == 0 and ti >= 16:
                            si = ctr["stg"] % 2
                            ctr["stg"] += 1
                            k.op("vector", lambda e, si=si, pt_=pt_: e.tensor_copy(out=stg[si][:], in_=pt_[:, 0:256]),
                                 reads=[pb_], writes=[b_stg[si]])
                            final.append(k.dma("sync", vwin[128 * (ti - 16):128 * (ti - 16) + 128, 256 * hh2:256 * hh2 + 256],
                                               stg[si][:], reads=[b_stg[si]]))
                    for hl in range(4):
                        if stage < 2:
                            break
                        h = 4 * hh2 + hl
                        jj, pb = hl // 2, 64 * (hl % 2)
                        for ti0 in range(16, 32, 2):
                            sidx = 2 + ctr["s"] % 2
                            ctr["s"] += 1
                            pS, bS = psT[sidx], psB[sidx]
                            first = True
                            for a in range(2):
                                ti = ti0 + a
                                qs, qstep = tokset(ti, dil)
                                qs -= HALF
                                for kt in range(2):
                                    tk = ti - dil * (1 - kt)
                                    ks, kstep = tokset(tk, dil)
                                    fn = lambda e, a=a, kt=kt, ks=ks, kstep=kstep, qs=qs, qstep=qstep, pS=pS, jj=jj, pb=pb: e.matmul(
                                        pS[:, a * 256 + kt * 128:a * 256 + kt * 128 + 128],
                                        lhsT=KT[pb:pb + 64, jj, sl(ks, kstep)],
                                        rhs=QT[pb:pb + 64, jj, sl(qs, qstep)], start=True, stop=True)
                                    if first:
                                        k.op("tensor", fn, reads=[b_KT[jj], b_QT[jj]], writes=[bS])
                                        first = False
                                    else:
                                        k.acc("tensor", fn, reads=[b_KT[jj], b_QT[jj]], acc=[bS])
                            ei = ctr["ev"] % 2
                            ctr["ev"] += 1
                            k.op("scalar", lambda e, ei=ei, pS=pS: e.activation(out=exb[ei][:], in_=pS[:, :], func=AF.Exp),
                                 reads=[bS], writes=[b_ex[ei]])
                            k.op("vector", lambda e, ei=ei, h=h, br=br: e.tensor_tensor(
                                out=ptb[ei][:].rearrange("p (a c) -> p a c", a=2),
                                in0=exb[ei][:].rearrange("p (a c) -> p a c", a=2),
                                in1=ETb[:, h * 3 + br:h * 3 + br + 1, :].to_broadcast([128, 2, 256]), op=ALU.mult),
                                reads=[b_ex[ei], b_ET], writes=[b_pt[ei]])
                            oidx = 4 + ctr["o"] % 2
                            ctr["o"] += 1
                            pO, bO = psT[oidx], psB[oidx]
                            first = True
                            for a in range(2):
                                ti = ti0 + a
                                for kt in range(2):
                                    tk = ti - dil * (1 - kt)
                                    fn = lambda e, a=a, kt=kt, tk=tk, hl=hl, ei=ei, pO=pO: e.matmul(
                                        pO[0:65, a * 128:a * 128 + 128], lhsT=Vaug[:, tk, hl, 0:65],
                                        rhs=ptb[ei][:, a * 256 + kt * 128:a * 256 + kt * 128 + 128],
                                        start=(kt == 0), stop=(kt == 1))
                                    if first:
                                        k.op("tensor", fn, reads=[b_pt[ei], b_V[tk]], writes=[bO])
                                        first = False
                                    else:
                                        k.acc("tensor", fn, reads=[b_pt[ei], b_V[tk]], acc=[bO])
                            qs0, qstep = tokset(ti0, dil)
                            qs1, _ = tokset(ti0 + 1, dil)
                            qs0 -= HALF
                            qs1 -= HALF
                            dst = bass.AP(acc, hl * HALF + qs0, [[4 * HALF, 65], [qs1 - qs0, 2], [qstep, 128]])
                            srcO = pO[0:65, 0:256].rearrange("p (a c) -> p a c", a=2)
                            if br == 0:
                                k.op("vector", lambda e, dst=dst, srcO=srcO: e.tensor_copy(out=dst, in_=srcO),
                                     reads=[bO], writes=[b_acc[hl]])
                            else:
                                k.op("vector", lambda e, dst=dst, srcO=srcO: e.tensor_tensor(out=dst, in0=srcO, in1=dst, op=ALU.add),
                                     reads=[bO], writes=[b_acc[hl]])
                if stage < 3:
                    continue
                k.op("vector", lambda e: e.reciprocal(out=acc[64:65, :, :], in_=acc[64:65, :, :]), reads=b_acc, writes=[b_rrow])
                for hl in range(4):
                    jj, pb = hl // 2, 64 * (hl % 2)
                    for c in range(4):
                        pt_, pb_ = psT[6 + c % 2], psB[6 + c % 2]
                        k.op("tensor", lambda e, hl=hl, c=c, pt_=pt_: e.matmul(
                            pt_[0:64, :], lhsT=ones_f[64:65, 0:64], rhs=acc[64:65, hl, 512 * c:512 * c + 512], start=True, stop=True),
                            reads=[b_rrow, b_ones], writes=[pb_])
                        k.op("vector", lambda e, hl=hl, c=c, pt_=pt_, pb=pb, jj=jj, hh2=hh2: e.tensor_tensor(
                            out=mixA[pb:pb + 64, 2 * hh2 + jj, 512 * c:512 * c + 512], in0=acc[0:64, hl, 512 * c:512 * c + 512],
                            in1=pt_[0:64, :], op=ALU.mult), reads=[pb_, b_acc[hl]], writes=[b_mixA[2 * hh2 + jj]])

            if stage >= 3:
                for pp in range(4):
                    k.dma("sync", mixA_d[:, pp], mixA[:, pp], reads=[b_mixA[pp]], writes=[b_mixA_d])
        S.barrier()
        if stage >= 4:
            C = type("Ctx", (), {})()
            C.nc, C.k, C.S, C.final = nc, k, S, final
            C.xTb, C.b_x, C.w_v = xTb, b_x, w_in.rearrange("(k p) c -> p k c", p=128)
            C.psT, C.psB, C.cst, C.b_cst, C.ones_f, C.b_ones = psT, psB, cst, b_cst, ones_f, b_ones
            C.eps6, C.b_eps, C.prm_d = eps6, b_eps, prm_d
            C.kT_d, C.vT_d, C.qT_d, C.gz_d = kT_d, vT_d, qT_d, gz_d
            C.b_kT_d, C.b_vT_d, C.b_qT_d, C.b_gz_d = Buf("kT_d"), Buf("vT_d"), Buf("qT_d"), Buf("gz_d")
            C.mixB_d, C.b_mixB_d, C.ssm_p, C.conv_p = mixB_d, b_mixB_d, ssm_p, conv_p
            phase_b(C)
        sx.close()
        S.barrier()
        if stage >= 5:
            C = type("Ctx", (), {})()
            C.nc, C.k, C.S, C.final = nc, k, S, final
            C.psT, C.psB, C.cst, C.b_cst = psT, psB, cst, b_cst
            C.eps5, C.b_eps5 = eps5, b_eps5
            C.NT = 17 if with_sample else 16
            C.NEXP = nexp
            C.lnp_d, C.w_out, C.wr_d, C.rb_d = lnp_d, w_out, wr_d, rb_d
            C.w_gate, C.w_up, C.w_down = w_gate, w_up, w_down
            C.mixA_d, C.mixB_d, C.b_mixA_d, C.b_mixB_d = mixA_d, mixB_d, b_mixA_d, b_mixB_d
            C.mixS_d, C.b_mixS_d, C.xs_pad, C.xo = mixS_d, b_mixS_d, xs_pad, xo
            C.y_out, C.ys_out = y_out, ys_out
            C.iota_d, C.hb_d, C.b_hb_d, C.ones_f, C.b_ones = iota_d, hb_d, Buf("hb_d"), ones_f, b_ones
            phase_c(C)
        for nm, src_d, bsrc in (("mixA", mixA_d, b_mixA_d), ("mixB", mixB_d, b_mixB_d)):
            if nm in dbg_out:
                dstg_b = sb("dstg_b" + nm, [128, HALF], BF16)
                dstg = sb("dstg" + nm, [128, HALF])
                b_db, b_df = Buf("dstg_b" + nm), Buf("dstg" + nm)
                for pp in range(4):
                    k.dma("sync", dstg_b[:], src_d[:, pp], reads=[bsrc], writes=[b_db])
                    k.op("vector", lambda e, dstg=dstg, dstg_b=dstg_b: e.tensor_copy(out=dstg[:], in_=dstg_b[:]), reads=[b_db], writes=[b_df])
                    final.append(k.dma("sync", dbg_out[nm][:, pp], dstg[:], reads=[b_df]))
        S.finish(final)
    return nc


def phase_b(C):
    nc, k, S = C.nc, C.k, C.S
    xTb, b_x, w_v = C.xTb, C.b_x, C.w_v
    psT, psB = C.psT, C.psB
    cst, b_cst = C.cst, C.b_cst
    ones_f = C.ones_f
    IDENT, NEGID, LMASK, CM0, CM1, NEGM, STRICT = range(7)
    final = C.final
    kT_d, vT_d, qT_d, gz_d = C.kT_d, C.vT_d, C.qT_d, C.gz_d

    with ExitStack() as sB:
        sbb = lambda n, s, dt=F32: sB.enter_context(nc.sbuf_tensor(n, list(s), dt))
        prm = sbb("prm_sb", [128, 8 + 128 + 48])
        b_prm = Buf("prm")
        k.dma("sync", prm[:], C.prm_d, writes=[b_prm])
        identb = sbb("identb", [128, 128], BF16)
        b_identb = Buf()
        k.op("vector", lambda e: e.tensor_copy(out=identb[:], in_=cst[:, IDENT, :]), reads=[b_cst], writes=[b_identb])
        convp_sb = sbb("convp_sb", [128, 12, 3])
        b_convp = Buf("convp")

        ab_sb = sbb("ab_sb", [128, 32, 8])
        b_ab = Buf()
        with ExitStack() as s1:
            sb1 = lambda n, s, dt=F32: s1.enter_context(nc.sbuf_tensor(n, list(s), dt))
            wab = sb1("wab", [128, 8, 8], BF16)
            wz = sb1("wz", [128, 8, 512], BF16)
            b_wab, b_wz = Buf("wab"), Buf("wz")
            k.gload(wab[:], w_v[:, :, COL_AB:COL_AB + 8], writes=[b_wab])
            k.gload(wz[:], w_v[:, :, COL_ZB:COL_ZB + 512], writes=[b_wz])
            zst = [sb1("zst%d" % i, [128, 512]) for i in range(2)]
            b_zst = [Buf() for _ in range(2)]
            gzb = [sb1("gzb%d" % i, [128, 512], BF16) for i in range(2)]
            b_gzb = [Buf("gzb%d" % i) for i in range(2)]
            for ti in range(32):
                pt_, pb_ = psT[ti % 2], psB[ti % 2]
                for kk in range(8):
                    fn = lambda e, kk=kk, ti=ti, pt_=pt_: e.matmul(pt_[:, 0:8], lhsT=xTb[:, kk, 128 * ti:128 * ti + 128],
                                                                 rhs=wab[:, kk, :], start=(kk == 0), stop=(kk == 7))
                    if kk == 0:
                        k.op("tensor", fn, reads=[b_wab, b_x[ti // 4]], writes=[pb_])
                    else:
                        k.acc("tensor", fn, reads=[b_wab, b_x[ti // 4]], acc=[pb_])
                k.op("vector", lambda e, ti=ti, pt_=pt_: e.tensor_copy(out=ab_sb[:, ti, :], in_=pt_[:, 0:8]), reads=[pb_], writes=[b_ab])
            for ti in range(16, 32):
                i2 = ti % 2
                pt_, pb_ = psT[2 + i2], psB[2 + i2]
                for kk in range(8):
                    fn = lambda e, kk=kk, ti=ti, pt_=pt_: e.matmul(pt_[:, :], lhsT=xTb[:, kk, 128 * ti:128 * ti + 128],
                                                                 rhs=wz[:, kk, :], start=(kk == 0), stop=(kk == 7))
                    if kk == 0:
                        k.op("tensor", fn, reads=[b_wz, b_x[ti // 4]], writes=[pb_])
                    else:
                        k.acc("tensor", fn, reads=[b_wz, b_x[ti // 4]], acc=[pb_])
                k.op("scalar", lambda e, i2=i2, pt_=pt_: e.activation(out=zst[i2][:], in_=pt_[:, :], func=AF.Silu), reads=[pb_], writes=[b_zst[i2]])
                k.op("gpsimd", lambda e, i2=i2: e.tensor_tensor(
                    out=gzb[i2][:].rearrange("p (h e) -> p h e", h=4), in0=zst[i2][:].rearrange("p (h e) -> p h e", h=4),
                    in1=prm[:, 8:136].unsqueeze(1).to_broadcast([128, 4, 128]), op=ALU.mult),
                    reads=[b_zst[i2], b_prm], writes=[b_gzb[i2]])
                k.dma("sync", gz_d[ti - 16], gzb[i2][:], reads=[b_gzb[i2]], writes=[C.b_gz_d])

        S.barrier()
        gt = sbb("gt", [128, 12, 128])
        b_gt = [Buf() for _ in range(12)]
        G, BETA, GC, EGC, ETAIL, BEGE, EGL0, EGL1, GCL, TMP, NEGA, TMP2 = range(12)
        v3 = lambda idx: gt[:, idx, :].rearrange("p (t h) -> p t h", h=4)
        k.op("scalar", lambda e: e.activation(out=gt[:, NEGA, 0:4], in_=prm[:, 0:4], func=AF.Exp), reads=[b_prm], writes=[b_gt[NEGA]])
        k.op("vector", lambda e: e.tensor_tensor(out=v3(TMP), in0=ab_sb[:, :, 0:4], in1=prm[:, 4:8].unsqueeze(1).to_broadcast([128, 32, 4]), op=ALU.add),
             reads=[b_ab, b_prm], writes=[b_gt[TMP]])
        k.op("scalar", lambda e: e.activation(out=gt[:, TMP, :], in_=gt[:, TMP, :], func=AF.Exp), reads=[b_gt[TMP]], writes=[b_gt[TMP]])
        k.op("scalar", lambda e: e.activation(out=gt[:, TMP, :], in_=gt[:, TMP, :], func=AF.Ln, bias=1.0), reads=[b_gt[TMP]], writes=[b_gt[TMP]])
        k.op("vector", lambda e: e.scalar_tensor_tensor(out=v3(G), in0=v3(TMP), scalar=-1.0, in1=gt[:, NEGA, 0:4].unsqueeze(1).to_broadcast([128, 32, 4]),
                                                       op0=ALU.mult, op1=ALU.mult), reads=[b_gt[TMP], b_gt[NEGA]], writes=[b_gt[G]])
        k.op("scalar", lambda e: e.activation(out=v3(BETA), in_=ab_sb[:, :, 4:8], func=AF.Sigmoid), reads=[b_ab], writes=[b_gt[BETA]])
        for (mask, dst, bank) in ((LMASK, GC, 0), (CM0, EGL0, 1), (CM1, EGL1, 2)):
            k.op("tensor", lambda e, mask=mask, bank=bank: e.matmul(psT[bank][:, 0:128], lhsT=cst[:, mask, :], rhs=gt[:, G, :], start=True, stop=True),
                 reads=[b_cst, b_gt[G]], writes=[psB[bank]])
            k.op("vector", lambda e, dst=dst, bank=bank: e.tensor_copy(out=gt[:, dst, :], in_=psT[bank][:, 0:128]), reads=[psB[bank]], writes=[b_gt[dst]])
        k.op("vector", lambda e: e.tensor_copy(out=gt[0:64, GCL, :], in_=gt[0:64, EGL0, :]), reads=[b_gt[EGL0]], writes=[b_gt[GCL]])
        k.op("vector", lambda e: e.tensor_copy(out=gt[64:128, GCL, :], in_=gt[64:128, EGL1, :]), reads=[b_gt[EGL1], b_gt[GCL]], writes=[b_gt[GCL]])
        k.op("vector", lambda e: e.tensor_tensor(out=gt[:, TMP2, :], in0=gt[:, GCL, :], in1=gt[:, GC, :], op=ALU.subtract),
             reads=[b_gt[GCL], b_gt[GC]], writes=[b_gt[TMP2]])
        k.op("scalar", lambda e: e.activation(out=gt[:, ETAIL, :], in_=gt[:, TMP2, :], func=AF.Exp), reads=[b_gt[TMP2]], writes=[b_gt[ETAIL]])
        k.op("scalar", lambda e: e.activation(out=gt[:, EGC, :], in_=gt[:, GC, :], func=AF.Exp), reads=[b_gt[GC]], writes=[b_gt[EGC]])
        k.op("scalar", lambda e: e.activation(out=gt[:, EGL0, :], in_=gt[:, EGL0, :], func=AF.Exp), reads=[b_gt[EGL0], b_gt[GCL]], writes=[b_gt[EGL0]])
        k.op("scalar", lambda e: e.activation(out=gt[:, EGL1, :], in_=gt[:, EGL1, :], func=AF.Exp), reads=[b_gt[EGL1], b_gt[GCL]], writes=[b_gt[EGL1]])
        k.op("vector", lambda e: e.tensor_tensor(out=gt[:, BEGE, :], in0=gt[:, BETA, :], in1=gt[:, EGC, :], op=ALU.mult),
             reads=[b_gt[BETA], b_gt[EGC]], writes=[b_gt[BEGE]])

        with ExitStack() as s2:
            sb2 = lambda n, s, dt=F32: s2.enter_context(nc.sbuf_tensor(n, list(s), dt))
            wu = [sb2("wu%d" % i, [128, 8, 128], BF16) for i in range(2)]
            b_wu = [Buf("wu%d" % i) for i in range(2)]
            ub = [sb2("ub%d" % i, [128, 515]) for i in range(4)]
            b_ub = [Buf() for _ in range(4)]
            cb = [sb2("cb%d" % i, [128, 512]) for i in range(4)]
            b_cb = [Buf() for _ in range(4)]
            sq = [sb2("sq%d" % i, [128, 512]) for i in range(4)]
            b_sq = [Buf() for _ in range(4)]
            rt = [sb2("rt%d" % i, [128, 512]) for i in range(4)]
            b_rt = [Buf() for _ in range(4)]
            ob = [sb2("ob%d" % i, [128, 512], BF16) for i in range(4)]
            b_ob = [Buf("ob%d" % i) for i in range(4)]
            cnt = 0
            pendY = []
            for th in range(12):
                ty, hb = divmod(th, 4)
                wi = th % 2
                c0 = COL_UB + 128 * th
                k.gload(wu[wi][:], w_v[:, :, c0:c0 + 128], writes=[b_wu[wi]])
                chunks = range(4, 8) if ty == 0 else range(8)
                dst_d = (qT_d, kT_d, vT_d)[ty]
                b_dst = (C.b_qT_d, C.b_kT_d, C.b_vT_d)[ty]
                cw = lambda i, th=th: prm[:, 136 + 4 * th + i:136 + 4 * th + i + 1]
                first = True
                for tc in chunks:
                    ci = cnt % 4
                    cnt += 1
                    pt_, pb_ = psT[ci], psB[ci]
                    if first:
                        if ty == 0:
                            hp, hpb = psT[(ci + 1) % 4], psB[(ci + 1) % 4]
                            for kk in range(8):
                                fn = lambda e, kk=kk, wi=wi, hp=hp: e.matmul(hp[:, 0:4], lhsT=wu[wi][:, kk, :], rhs=xTb[:, kk, HALF - 4:HALF],
                                                                            start=(kk == 0), stop=(kk == 7))
                                if kk == 0:
                                    k.op("tensor", fn, reads=[b_wu[wi], b_x[3]], writes=[hpb])
                                else:
                                    k.acc("tensor", fn, reads=[b_wu[wi], b_x[3]], acc=[hpb])
                            k.op("vector", lambda e, ci=ci, hp=hp: e.tensor_copy(out=ub[ci][:, 0:3], in_=hp[:, 1:4]), reads=[hpb], writes=[b_ub[ci]])
                        else:
                            k.op("vector", lambda e, ci=ci: e.memset(ub[ci][:, 0:3], 0.0), writes=[b_ub[ci]])
                        first = False
                    else:
                        k.op("vector", lambda e, ci=ci: e.tensor_copy(out=ub[ci][:, 0:3], in_=ub[(ci - 1) % 4][:, 512:515]),
                             reads=[b_ub[(ci - 1) % 4]], writes=[b_ub[ci]])
                    for kk in range(8):
                        fn = lambda e, kk=kk, wi=wi, tc=tc, pt_=pt_: e.matmul(pt_[:, :], lhsT=wu[wi][:, kk, :], rhs=xTb[:, kk, 512 * tc:512 * tc + 512],
                                                                            start=(kk == 0), stop=(kk == 7))
                        if kk == 0:
                            k.op("tensor", fn, reads=[b_wu[wi], b_x[tc]], writes=[pb_])
                        else:
                            k.acc("tensor", fn, reads=[b_wu[wi], b_x[tc]], acc=[pb_])
                    k.op("scalar", lambda e, ci=ci, pt_=pt_: e.activation(out=ub[ci][:, 3:515], in_=pt_[:, :], func=AF.Copy), reads=[pb_], writes=[b_ub[ci]])
                    if tc == 7:
                        k.op("gpsimd", lambda e, ci=ci, th=th: e.tensor_copy(out=convp_sb[:, th, :], in_=ub[ci][:, 512:515]), reads=[b_ub[ci]], writes=[b_convp])
                    k.op("vector", lambda e, ci=ci, cw=cw: e.tensor_scalar(out=cb[ci][:], in0=ub[ci][:, 3:515], scalar1=cw(3), scalar2=None, op0=ALU.mult),
                         reads=[b_ub[ci], b_prm], writes=[b_cb[ci]])
                    for i in range(3):
                        k.op("vector", lambda e, ci=ci, cw=cw, i=i: e.scalar_tensor_tensor(out=cb[ci][:], in0=ub[ci][:, i:i + 512], scalar=cw(i), in1=cb[ci][:],
                                                                                       op0=ALU.mult, op1=ALU.add), reads=[b_ub[ci], b_cb[ci]], writes=[b_cb[ci]])
                    k.op("scalar", lambda e, ci=ci: e.activation(out=cb[ci][:], in_=cb[ci][:], func=AF.Silu), reads=[b_cb[ci]], writes=[b_cb[ci]])
                    if ty == 2:
                        k.op("gpsimd", lambda e, ci=ci: e.tensor_copy(out=ob[ci][:], in_=cb[ci][:]), reads=[b_cb[ci]], writes=[b_ob[ci]])
                    else:
                        k.op("gpsimd", lambda e, ci=ci: e.tensor_tensor(out=sq[ci][:], in0=cb[ci][:], in1=cb[ci][:], op=ALU.mult), reads=[b_cb[ci]], writes=[b_sq[ci]])
                        np_, npb = psT[4 + ci], psB[4 + ci]
                        k.op("tensor", lambda e, ci=ci, np_=np_: e.matmul(np_[:, :], lhsT=ones_f[:], rhs=sq[ci][:], start=True, stop=True),
                             reads=[b_sq[ci], C.b_ones], writes=[npb])
                        k.op("scalar", lambda e, ci=ci, np_=np_: e.activation(out=rt[ci][:], in_=np_[:, :], func=AF.Sqrt, bias=C.eps6[:, 0:1]),
                             reads=[npb, C.b_eps], writes=[b_rt[ci]])
                    t0 = 512 * tc - (HALF if ty == 0 else 0)
                    sc = (128.0 ** -0.5) if ty == 0 else 1.0

                    def tailY(ci=ci, ty=ty, sc=sc, dst_d=dst_d, b_dst=b_dst, hb=hb, t0=t0):
                        if ty != 2:
                            k.op("vector", lambda e, ci=ci: e.reciprocal(out=rt[ci][:], in_=rt[ci][:]), reads=[b_rt[ci]], writes=[b_rt[ci]])
                            k.op("vector", lambda e, ci=ci, sc=sc: e.scalar_tensor_tensor(out=ob[ci][:], in0=cb[ci][:], scalar=sc, in1=rt[ci][:], op0=ALU.mult, op1=ALU.mult),
                                 reads=[b_cb[ci], b_rt[ci]], writes=[b_ob[ci]])
                        k.dma("sync", dst_d[:, hb, t0:t0 + 512], ob[ci][:], reads=[b_ob[ci]], writes=[b_dst])
                    pendY.append(tailY)
                    if len(pendY) > 2:
                        pendY.pop(0)()
            while pendY:
                pendY.pop(0)()
            convp_v = C.conv_p.rearrange("r (c p) -> p c r", p=128)
            for th in range(12):
                final.append(k.dma("sync", convp_v[:, th, :], convp_sb[:, th, :], reads=[b_convp], slot="convp", allow_slow_non_contiguous=True))

        S.barrier()
        sC = sB
        sbc = lambda n, s, dt=F32: sC.enter_context(nc.sbuf_tensor(n, list(s), dt))
        f_slots = [(psT[b][:, 0:128], psB[b]) for b in range(6)]
        psbf = [psT[6].bitcast(BF16), psT[7].bitcast(BF16)]
        h_slots = [(psbf[b][:, 0:128], psB[6 + b]) for b in range(2)]
        cnts = {"f": 0, "h": 0}

        def fslot():
            s_ = f_slots[cnts["f"] % len(f_slots)]
            cnts["f"] += 1
            return s_

        def hslot():
            s_ = h_slots[cnts["h"] % len(h_slots)]
            cnts["h"] += 1
            return s_

        class Pool_:
            def __init__(self, name, n, dt):
                self.t = [sbc("%s%d" % (name, i), [128, 128], dt) for i in range(n)]
                self.b = [Buf() for _ in range(n)]
                self.i = 0

            def get(self):
                j = self.i % len(self.t)
                self.i += 1
                return self.t[j], self.b[j]

        PF = Pool_("pf", 40, F32)
        PH = Pool_("ph", 96, BF16)
        Sst = [sbc("Sst%d" % h, [128, 128]) for h in range(4)]
        Sbf = [sbc("Sbf%d" % h, [128, 128], BF16) for h in range(4)]
        b_S = [Buf() for _ in range(4)]
        b_Sb = [Buf() for _ in range(4)]
        for h in range(4):
            k.op("vector", lambda e, h=h: e.memset(Sst[h][:], 0.0), writes=[b_S[h]])
            k.op("vector", lambda e, h=h: e.memset(Sbf[h][:], 0.0), writes=[b_Sb[h]])
        kt_t = [sbc("kt_t%d" % i, [128, 4, 128], BF16) for i in range(2)]
        vt_t = [sbc("vt_t%d" % i, [128, 4, 128], BF16) for i in range(2)]
        qt_t = [sbc("qt_t%d" % i, [128, 4, 128], BF16) for i in range(2)]
        gz_t = [sbc("gz_t%d" % i, [128, 512], BF16) for i in range(2)]
        b_kt = [Buf("kt_t%d" % i) for i in range(2)]
        b_vt = [Buf("vt_t%d" % i) for i in range(2)]
        b_qt = [Buf("qt_t%d" % i) for i in range(2)]
        b_gzt = [Buf("gz_t%d" % i) for i in range(2)]
        Ukeep = [[sbc("Uk%d_%d" % (i, h), [128, 128]) for h in range(4)] for i in range(2)]
        WTkeep = [[sbc("WTk%d_%d" % (i, h), [128, 128], BF16) for h in range(4)] for i in range(2)]
        ktlkeep = [[sbc("ktlk%d_%d" % (i, h), [128, 128], BF16) for h in range(4)] for i in range(2)]
        qkTkeep = [[sbc("qkTk%d_%d" % (i, h), [128, 128], BF16) for h in range(4)] for i in range(2)]
        b_Uk = [[Buf() for h in range(4)] for i in range(2)]
        b_WTk = [[Buf() for h in range(4)] for i in range(2)]
        b_ktlk = [[Buf() for h in range(4)] for i in range(2)]
        b_qkTk = [[Buf() for h in range(4)] for i in range(2)]
        ss = sbc("ss", [128, 8])
        b_ss = [Buf() for _ in range(8)]
        junk = sbc("junk", [128, 128])
        b_junk = Buf()
        mxs = [sbc("mxs%d" % i, [128, 4, 128], BF16) for i in range(2)]
        b_mxs = [Buf("mxs%d" % i) for i in range(2)]
        gt_ = gt
        col = lambda idx, c_: gt_[:, idx, c_:c_ + 1]

        def make_tile(ti):
            own = ti >= 16
            bi = ti % 2
            k.dma("sync", kt_t[bi][:], kT_d[:, :, 128 * ti:128 * ti + 128], reads=[C.b_kT_d], writes=[b_kt[bi]])
            k.dma("sync", vt_t[bi][:], vT_d[:, :, 128 * ti:128 * ti + 128], reads=[C.b_vT_d], writes=[b_vt[bi]])
            if own:
                k.dma("sync", qt_t[bi][:], qT_d[:, :, 128 * (ti - 16):128 * (ti - 16) + 128], reads=[C.b_qT_d], writes=[b_qt[bi]])
                k.dma("sync", gz_t[bi][:], gz_d[ti - 16], reads=[C.b_gz_d], writes=[b_gzt[bi]])
            HS = [None] * 4
            def stage1(hb):
                c_ = ti * 4 + hb
                kT = kt_t[bi][:, hb, :]
                vT = vt_t[bi][:, hb, :]
                qT = qt_t[bi][:, hb, :]
                rd_k, rd_v, rd_q = [b_kt[bi]], [b_vt[bi]], [b_qt[bi]]
                nd, b_nd = PF.get()
                k.op("gpsimd", lambda e, nd=nd, c_=c_: e.tensor_scalar(out=nd[:], in0=cst[:, NEGID, :], scalar1=col(GC, c_), scalar2=None, op0=ALU.mult),
                     reads=[b_cst, b_gt[GC]], writes=[b_nd])
                pD, bD = fslot()
                yield
                k.op("tensor", lambda e, pD=pD, nd=nd: e.matmul(pD, lhsT=ones_f[:], rhs=nd[:], start=True, stop=False), reads=[b_nd, C.b_ones], writes=[bD])
                k.acc("tensor", lambda e, pD=pD: e.matmul(pD, lhsT=cst[:, IDENT, :], rhs=cst[:, NEGM, :], start=False, stop=True), reads=[b_cst], acc=[bD])
                Ec, b_Ec = PF.get()
                k.op("scalar", lambda e, Ec=Ec, pD=pD, c_=c_: e.activation(out=Ec[:], in_=pD, func=AF.Exp, bias=col(GC, c_)),
                     reads=[bD, b_gt[GC]], writes=[b_Ec])
                Es, b_Es = PF.get()
                k.op("gpsimd", lambda e, Es=Es, Ec=Ec: e.tensor_tensor(out=Es[:], in0=Ec[:], in1=cst[:, STRICT, :], op=ALU.mult),
                     reads=[b_Ec, b_cst], writes=[b_Es])
                pK, bK = fslot()
                yield
                k.op("tensor", lambda e, pK=pK, kT=kT: e.matmul(pK, lhsT=kT, rhs=kT, start=True, stop=True), reads=rd_k, writes=[bK])
                A, b_A = PH.get()
                k.op("vector", lambda e, A=A, pK=pK, Es=Es, c_=c_: e.scalar_tensor_tensor(out=A[:], in0=pK, scalar=col(BETA, c_), in1=Es[:], op0=ALU.mult, op1=ALU.mult),
                     reads=[bK, b_Es, b_gt[BETA]], writes=[b_A])
                pT_, bT_ = hslot()
                yield
                k.op("tensor", lambda e, pT_=pT_, A=A: e.transpose(pT_, A[:], identb[:]), reads=[b_A, b_identb], writes=[bT_])
                Bm, b_Bm = PH.get()
                k.op("vector", lambda e, Bm=Bm, pT_=pT_: e.tensor_copy(out=Bm[:], in_=pT_), reads=[bT_], writes=[b_Bm])
                P, b_P = PH.get()
                k.op("vector", lambda e, P=P, pT_=pT_: e.tensor_tensor(out=P[:], in0=cst[:, IDENT, :], in1=pT_, op=ALU.subtract), reads=[bT_, b_cst], writes=[b_P])
                X, b_X, Y, b_Y = A, b_A, Bm, b_Bm
                for m in range(1, 6):
                    pX, bX = fslot()
                    yield
                    k.op("tensor", lambda e, pX=pX, X=X, Y=Y: e.matmul(pX, lhsT=Y[:], rhs=X[:], start=True, stop=True), reads=[b_X, b_Y], writes=[bX])
                    Xn, b_Xn = PH.get()
                    if os.environ.get("ACTEV", "0") == "1":
                        k.op("scalar", lambda e, Xn=Xn, pX=pX: e.activation(out=Xn[:], in_=pX, func=AF.Copy), reads=[bX], writes=[b_Xn])
                    else:
                        k.op("vector", lambda e, Xn=Xn, pX=pX: e.tensor_copy(out=Xn[:], in_=pX), reads=[bX], writes=[b_Xn])
                    if m < 5:
                        pY, bY = fslot()
                        yield
                        k.op("tensor", lambda e, pY=pY, X=X, Y=Y: e.matmul(pY, lhsT=X[:], rhs=Y[:], start=True, stop=True), reads=[b_X, b_Y], writes=[bY])
                        Yn, b_Yn = PH.get()
                        if os.environ.get("ACTEV", "0") == "1":
                            k.op("scalar", lambda e, Yn=Yn, pY=pY: e.activation(out=Yn[:], in_=pY, func=AF.Copy), reads=[bY], writes=[b_Yn])
                        else:
                            k.op("vector", lambda e, Yn=Yn, pY=pY: e.tensor_copy(out=Yn[:], in_=pY), reads=[bY], writes=[b_Yn])
                    pP, bP = fslot()
                    yield
                    k.op("tensor", lambda e, pP=pP, Xn=Xn, P=P: e.matmul(pP, lhsT=Xn[:], rhs=P[:], start=True, stop=True), reads=[b_Xn, b_P], writes=[bP])
                    Pn, b_Pn = PH.get()
                    k.op("vector", lambda e, Pn=Pn, pP=pP, P=P: e.tensor_tensor(out=Pn[:], in0=pP, in1=P[:], op=ALU.add), reads=[bP, b_P], writes=[b_Pn])
                    P, b_P = Pn, b_Pn
                    X, b_X = Xn, b_Xn
                    if m < 5:
                        Y, b_Y = Yn, b_Yn
                pk_, bk_ = hslot()
                yield
                k.op("tensor", lambda e, pk_=pk_, kT=kT: e.transpose(pk_, kT, identb[:]), reads=rd_k + [b_identb], writes=[bk_])
                Rw, b_Rw = PH.get()
                k.op("vector", lambda e, Rw=Rw, pk_=pk_, c_=c_: e.tensor_scalar(out=Rw[:], in0=pk_, scalar1=col(BEGE, c_), scalar2=None, op0=ALU.mult),
                     reads=[bk_, b_gt[BEGE]], writes=[b_Rw])
                ktl, b_ktl = ktlkeep[bi][hb], b_ktlk[bi][hb]
                k.op("vector", lambda e, ktl=ktl, pk_=pk_, c_=c_: e.tensor_scalar(out=ktl[:], in0=pk_, scalar1=col(ETAIL, c_), scalar2=None, op0=ALU.mult),
                     reads=[bk_, b_gt[ETAIL]], writes=[b_ktl])
                pv_, bv_ = hslot()
                yield
                k.op("tensor", lambda e, pv_=pv_, vT=vT: e.transpose(pv_, vT, identb[:]), reads=rd_v + [b_identb], writes=[bv_])
                Ru, b_Ru = PH.get()
                k.op("vector", lambda e, Ru=Ru, pv_=pv_, c_=c_: e.tensor_scalar(out=Ru[:], in0=pv_, scalar1=col(BETA, c_), scalar2=None, op0=ALU.mult),
                     reads=[bv_, b_gt[BETA]], writes=[b_Ru])
                pU, bU = fslot()
                yield
                k.op("tensor", lambda e, pU=pU, P=P, Ru=Ru: e.matmul(pU, lhsT=P[:], rhs=Ru[:], start=True, stop=True), reads=[b_P, b_Ru], writes=[bU])
                U, b_U = Ukeep[bi][hb], b_Uk[bi][hb]
                k.op("vector", lambda e, U=U, pU=pU: e.tensor_copy(out=U[:], in_=pU), reads=[bU], writes=[b_U])
                pW, bW = fslot()
                yield
                k.op("tensor", lambda e, pW=pW, P=P, Rw=Rw: e.matmul(pW, lhsT=Rw[:], rhs=P[:], start=True, stop=True), reads=[b_P, b_Rw], writes=[bW])
                WT, b_WT = WTkeep[bi][hb], b_WTk[bi][hb]
                k.op("vector", lambda e, WT=WT, pW=pW: e.tensor_copy(out=WT[:], in_=pW), reads=[bW], writes=[b_WT])
                qkT = b_qkT = None
                if own:
                    pQ, bQ = fslot()
                    yield
                    k.op("tensor", lambda e, pQ=pQ, qT=qT, kT=kT: e.matmul(pQ, lhsT=qT, rhs=kT, start=True, stop=True), reads=rd_q + rd_k, writes=[bQ])
                    qk, b_qk = PH.get()
                    k.op("vector", lambda e, qk=qk, pQ=pQ, Ec=Ec: e.tensor_tensor(out=qk[:], in0=pQ, in1=Ec[:], op=ALU.mult), reads=[bQ, b_Ec], writes=[b_qk])
                    pq2, bq2 = hslot()
                    yield
                    k.op("tensor", lambda e, pq2=pq2, qk=qk: e.transpose(pq2, qk[:], identb[:]), reads=[b_qk, b_identb], writes=[bq2])
                    qkT, b_qkT = qkTkeep[bi][hb], b_qkTk[bi][hb]
                    k.op("vector", lambda e, qkT=qkT, pq2=pq2: e.tensor_copy(out=qkT[:], in_=pq2), reads=[bq2], writes=[b_qkT])
                HS[hb] = (dict(U=U, b_U=b_U, WT=WT, b_WT=b_WT, ktl=ktl, b_ktl=b_ktl, qkT=qkT, b_qkT=b_qkT, qT=qT, rd_q=rd_q, c_=c_))
            def stage23():
                outs = []
                if own:
                    for hb in range(4):
                        o_, b_o = PF.get()
                        outs.append((o_, b_o))
                for ch in range(2):
                    r0 = 64 * ch
                    for hb in range(4):
                        H = HS[hb]
                        c_ = H["c_"]
                        yield
                        pV, bV = fslot()
                        k.op("tensor", lambda e, pV=pV, H=H, hb=hb, r0=r0: e.matmul(pV[r0:r0 + 64, :], lhsT=H["WT"][:, r0:r0 + 64], rhs=Sbf[hb][:], start=True, stop=True),
                             reads=[H["b_WT"], b_Sb[hb]], writes=[bV])
                        vn, b_vn = PH.get()
                        k.op("vector", lambda e, vn=vn, pV=pV, H=H, r0=r0: e.tensor_tensor(out=vn[r0:r0 + 64, :], in0=H["U"][r0:r0 + 64, :], in1=pV[r0:r0 + 64, :], op=ALU.subtract),
                             reads=[bV, H["b_U"]], writes=[b_vn])
                        if own:
                            o_, b_o = outs[hb]
                            yield
                            p1, b1 = fslot()
                            k.op("tensor", lambda e, p1=p1, H=H, hb=hb, r0=r0: e.matmul(p1[r0:r0 + 64, :], lhsT=H["qT"][:, r0:r0 + 64], rhs=Sbf[hb][:], start=True, stop=True),
                                 reads=H["rd_q"] + [b_Sb[hb]], writes=[b1])
                            p2, b2 = fslot()
                            k.op("tensor", lambda e, p2=p2, H=H, vn=vn, r0=r0: e.matmul(p2[r0:r0 + 64, :], lhsT=H["qkT"][r0:r0 + 64, r0:r0 + 64], rhs=vn[r0:r0 + 64, :], start=True, stop=True),
                                 reads=[H["b_qkT"], b_vn], writes=[b2])
                            o2, b_o2 = PF.get()
                            k.op("vector", lambda e, o2=o2, p2=p2, r0=r0: e.tensor_copy(out=o2[r0:r0 + 64, :], in_=p2[r0:r0 + 64, :]), reads=[b2], writes=[b_o2])
                            k.op("vector", lambda e, o_=o_, p1=p1, o2=o2, r0=r0, c_=c_: e.scalar_tensor_tensor(
                                out=o_[r0:r0 + 64, :], in0=p1[r0:r0 + 64, :], scalar=gt_[r0:r0 + 64, EGC, c_:c_ + 1], in1=o2[r0:r0 + 64, :], op0=ALU.mult, op1=ALU.add),
                                reads=[b1, b_o2, b_gt[EGC]], writes=[b_o])
                        yield
                        pS, bS_ = fslot()
                        k.op("tensor", lambda e, pS=pS, H=H, vn=vn, r0=r0: e.matmul(pS, lhsT=H["ktl"][r0:r0 + 64, :], rhs=vn[r0:r0 + 64, :], start=True, stop=True),
                             reads=[H["b_ktl"], b_vn], writes=[bS_])
                        egl = EGL0 if ch == 0 else EGL1
                        k.op("vector", lambda e, pS=pS, hb=hb, egl=egl, c_=c_: e.scalar_tensor_tensor(
                            out=Sst[hb][:], in0=Sst[hb][:], scalar=col(egl, c_), in1=pS, op0=ALU.mult, op1=ALU.add),
                            reads=[bS_, b_gt[egl], b_S[hb]], writes=[b_S[hb]])
                        k.op("scalar", lambda e, hb=hb: e.activation(out=Sbf[hb][:], in_=Sst[hb][:], func=AF.Copy), reads=[b_S[hb]], writes=[b_Sb[hb]])
                if own:
                    for hb in range(4):
                        o_, b_o = outs[hb]
                        si = (ti * 4 + hb) % 8
                        yield
                        k.op("scalar", lambda e, o_=o_, si=si: e.activation(out=junk[:], in_=o_[:], func=AF.Square, accum_out=ss[:, si:si + 1]),
                             reads=[b_o], writes=[b_junk, b_ss[si]])
                        k.op("scalar", lambda e, si=si: e.activation(out=ss[:, si:si + 1], in_=ss[:, si:si + 1], func=AF.Sqrt, scale=1.0 / 128.0, bias=C.eps6[:, 0:1]),
                             reads=[b_ss[si], C.b_eps], writes=[b_ss[si]])
                        k.op("vector", lambda e, si=si: e.reciprocal(out=ss[:, si:si + 1], in_=ss[:, si:si + 1]), reads=[b_ss[si]], writes=[b_ss[si]])
                        on, b_on = PH.get()
                        k.op("vector", lambda e, on=on, o_=o_, si=si, hb=hb, bi=bi: e.scalar_tensor_tensor(
                            out=on[:], in0=o_[:], scalar=ss[:, si:si + 1], in1=gz_t[bi][:, 128 * hb:128 * hb + 128], op0=ALU.mult, op1=ALU.mult),
                            reads=[b_o, b_ss[si], b_gzt[bi]], writes=[b_on])
                        yield
                        pm, bm = hslot()
                        k.op("tensor", lambda e, pm=pm, on=on: e.transpose(pm, on[:], identb[:]), reads=[b_on, b_identb], writes=[bm])
                        k.op("vector", lambda e, pm=pm, hb=hb, bi=bi: e.tensor_copy(out=mxs[bi][:, hb, :], in_=pm),
                             reads=[bm], writes=[b_mxs[bi]])
                    k.dma("sync", C.mixB_d[:, :, 128 * (ti - 16):128 * (ti - 16) + 128], mxs[bi][:], reads=[b_mxs[bi]], writes=[C.b_mixB_d])
            return [stage1(hb) for hb in range(4)], stage23


        pending = None
        for ti in range(33):
            gens = []
            nxt = None
            if ti < 32:
                s1, nxt = make_tile(ti)
                gens += s1
            if pending is not None:
                gens.append(pending())
            while gens:
                for g_ in list(gens):
                    try:
                        next(g_)
                    except StopIteration:
                        gens.remove(g_)
            pending = nxt
        for hb in range(4):
            final.append(k.dma("sync", C.ssm_p[hb], Sst[hb][:], reads=[b_S[hb]], slot="ssmp"))

def phase_c(C):
    nc, k, S = C.nc, C.k, C.S
    psT, psB = C.psT, C.psB
    cst, b_cst = C.cst, C.b_cst
    IDENT = 0
    final = C.final
    NT = C.NT
    NTOK = NT * 128
    ALPHA = 2.0 ** 0.25
    KC = int(os.environ.get('KC', '99'))

    with ExitStack() as sC:
        sbc = lambda n, s, dt=F32: sC.enter_context(nc.sbuf_tensor(n, list(s), dt))
        Mg = sbc("Mg", [128, NT, 4])
        b_Mg = Buf()
        Ghl = sbc("Ghl", [128, NT, 4, 16], BF16)
        b_Ghl = Buf()
        identb = sbc("identbC", [128, 128], BF16)
        b_identb = Buf()
        k.op("vector", lambda e: e.tensor_copy(out=identb[:], in_=cst[:, IDENT, :]), reads=[b_cst], writes=[b_identb])
        yacc = sbc("yacc", [128, NT, 1024])
        b_y = [Buf() for _ in range(NT)]
        G = sbc("G", [128, NT, 32])
        b_G = [Buf() for _ in range(NT)]
        st6_c3 = sbc("st6b", [128, 2, 2, 6])
        mv_c3 = sbc("mvb", [128, 2, 2])

        with ExitStack() as s1:
            sb1 = lambda n, s, dt=F32: s1.enter_context(nc.sbuf_tensor(n, list(s), dt))
            lnp = sb1("lnp_sb", [128, 2, 1024])
            b_lnp = Buf("lnp")
            k.dma("sync", lnp[:], C.lnp_d[:, 0:2, :], writes=[b_lnp])
            wo = sb1("wo", [128, 8, 1024], BF16)
            b_wo = Buf("wo")
            k.gload(wo[:], C.w_out.rearrange("(k p) c -> p k c", p=128), writes=[b_wo])
            wr = sb1("wr_sb", [128, 8, 36])
            b_wr = Buf("wr")
            k.dma("sync", wr[:], C.wr_d.rearrange("(k p) c -> p k c", p=128), writes=[b_wr])
            rb = sb1("rb_sb", [128, 36])
            b_rb = Buf("rb")
            k.dma("sync", rb[:], C.rb_d, writes=[b_rb])
            mx = [sb1("mx%d" % i, [128, 8, 128], BF16) for i in range(2)]
            b_mx = [Buf("mx%d" % i) for i in range(2)]
            xt = [sb1("xt%d" % i, [128, 1024]) for i in range(2)]
            b_xt = [Buf("xt%d" % i) for i in range(2)]
            rr = [sb1("rr%d" % i, [128, 1024]) for i in range(2)]
            b_rr = [Buf() for _ in range(2)]
            hh = [sb1("hh%d" % i, [128, 1024]) for i in range(2)]
            b_hh = [Buf() for _ in range(2)]
            hbb = [sb1("hbb%d" % i, [128, 1024], BF16) for i in range(2)]
            b_hbb = [Buf("hbb%d" % i) for i in range(2)]
            hTf = [sb1("hTf%d" % i, [128, 8, 128]) for i in range(2)]
            b_hTf = [Buf() for _ in range(2)]
            st6 = sb1("st6", [128, 2, 2, 6])
            mv = sb1("mv", [128, 2, 2])
            b_st = [Buf() for _ in range(2)]
            b_mv = [Buf() for _ in range(2)]
            tmpr = sb1("tmpr", [128, 2, 32])
            b_tmp = [Buf() for _ in range(2)]
            sm = sb1("sm", [128, 2, 96])
            b_sm = [Buf() for _ in range(2)]
            for ti in range(NT if KC >= 6 else 0):
                bi = ti % 2
                is_s = ti >= 16
                if not is_s:
                    k.dma("sync", mx[bi][:, 0:4, :], C.mixA_d[:, :, 128 * ti:128 * ti + 128], reads=[C.b_mixA_d], writes=[b_mx[bi]])
                    k.dma("sync", mx[bi][:, 4:8, :], C.mixB_d[:, :, 128 * ti:128 * ti + 128], reads=[C.b_mixB_d], writes=[b_mx[bi]])
                    k.dma("sync", xt[bi][:], C.xo[128 * ti:128 * ti + 128, :], writes=[b_xt[bi]])
                else:
                    k.dma("sync", mx[bi][:], C.mixS_d, reads=[C.b_mixS_d], writes=[b_mx[bi]])
                    k.dma("sync", xt[bi][:], C.xs_pad, writes=[b_xt[bi]])
                for half in range(2 if KC >= 7 else 0):
                    pt_, pb_ = psT[half], psB[half]
                    for kk in range(8):
                        fn = lambda e, kk=kk, bi=bi, half=half, pt_=pt_: e.matmul(pt_[:, :], lhsT=mx[bi][:, kk, :], rhs=wo[:, kk, 512 * half:512 * half + 512],
                                                                                start=(kk == 0), stop=(kk == 7))
                        if kk == 0:
                            k.op("tensor", fn, reads=[b_mx[bi], b_wo], writes=[pb_])
                        else:
                            k.acc("tensor", fn, reads=[b_mx[bi], b_wo], acc=[pb_])
                    if KC < 8:
                        continue
                    k.op("vector", lambda e, bi=bi, half=half, pt_=pt_: e.scalar_tensor_tensor(
                        out=rr[bi][:, 512 * half:512 * half + 512], in0=xt[bi][:, 512 * half:512 * half + 512], scalar=ALPHA, in1=pt_[:, :],
                        op0=ALU.mult, op1=ALU.add), reads=[pb_, b_xt[bi]], writes=[b_rr[bi]])
                    k.op("vector", lambda e, bi=bi, half=half: e.bn_stats(out=st6[:, bi, half, :], in_=rr[bi][:, 512 * half:512 * half + 512]),
                         reads=[b_rr[bi]], writes=[b_st[bi]])
                if KC < 11:
                    continue
                k.op("vector", lambda e, bi=bi: e.bn_aggr(out=mv[:, bi, :], in_=st6[:, bi, :, :].rearrange("p a b -> p (a b)")), reads=[b_st[bi]], writes=[b_mv[bi]])
                k.op("scalar", lambda e, bi=bi: e.activation(out=mv[:, bi, 1:2], in_=mv[:, bi, 1:2], func=AF.Sqrt, bias=C.eps5[:, 0:1]), reads=[b_mv[bi], C.b_eps5], writes=[b_mv[bi]])
                k.op("vector", lambda e, bi=bi: e.reciprocal(out=mv[:, bi, 1:2], in_=mv[:, bi, 1:2]), reads=[b_mv[bi]], writes=[b_mv[bi]])
                k.op("vector", lambda e, bi=bi: e.tensor_scalar(out=hh[bi][:], in0=rr[bi][:], scalar1=mv[:, bi, 0:1], scalar2=mv[:, bi, 1:2], op0=ALU.subtract, op1=ALU.mult),
                     reads=[b_rr[bi], b_mv[bi]], writes=[b_hh[bi]])
                k.op("gpsimd", lambda e, bi=bi: e.tensor_tensor(out=hh[bi][:], in0=hh[bi][:], in1=lnp[:, 0, :], op=ALU.mult), reads=[b_hh[bi], b_lnp], writes=[b_hh[bi]])
                k.op("gpsimd", lambda e, bi=bi: e.tensor_tensor(out=hh[bi][:], in0=hh[bi][:], in1=lnp[:, 1, :], op=ALU.add), reads=[b_hh[bi], b_lnp], writes=[b_hh[bi]])
                k.op("gpsimd", lambda e, bi=bi, ti=ti: e.tensor_scalar(out=yacc[:, ti, :], in0=hh[bi][:], scalar1=ALPHA, scalar2=None, op0=ALU.mult),
                     reads=[b_hh[bi]], writes=[b_y[ti]])
                if KC < 12:
                    continue
                k.op("gpsimd", lambda e, bi=bi: e.tensor_copy(out=hbb[bi][:], in_=hh[bi][:]), reads=[b_hh[bi]], writes=[b_hbb[bi]])
                k.dma("sync", C.hb_d[128 * ti:128 * ti + 128, :], hbb[bi][:], reads=[b_hbb[bi]], writes=[C.b_hb_d])
                for g4 in range(2):
                    pt_, pb_ = psT[2 + g4], psB[2 + g4]
                    for j in range(4):
                        kk = 4 * g4 + j
                        fn = lambda e, kk=kk, j=j, bi=bi, pt_=pt_: e.transpose(pt_[:, 128 * j:128 * j + 128], hh[bi][:, 128 * kk:128 * kk + 128], cst[:, IDENT, :])
                        if j == 0:
                            k.op("tensor", fn, reads=[b_hh[bi], b_cst], writes=[pb_])
                        else:
                            k.acc("tensor", fn, reads=[b_hh[bi], b_cst], acc=[pb_])
                    k.op("vector", lambda e, g4=g4, bi=bi, pt_=pt_: e.tensor_copy(out=hTf[bi][:, 4 * g4:4 * g4 + 4, :], in_=pt_[:, :].rearrange("p (a c) -> p a c", a=4)),
                         reads=[pb_], writes=[b_hTf[bi]])
                if KC < 13:
                    continue
                pl, plb = psT[4], psB[4]
                for kk in range(8):
                    fn = lambda e, kk=kk, bi=bi: e.matmul(pl[:, 0:36], lhsT=hTf[bi][:, kk, :], rhs=wr[:, kk, :], start=(kk == 0), stop=(kk == 7))
                    if kk == 0:
                        k.op("tensor", fn, reads=[b_hTf[bi], b_wr], writes=[plb])
                    else:
                        k.acc("tensor", fn, reads=[b_hTf[bi], b_wr], acc=[plb])
                if KC < 14:
                    continue
                R = lambda a, b_, bi=bi: sm[:, bi, a:b_]
                T3 = tmpr[:, bi, :].rearrange("p (g e) -> p g e", g=4)
                sm_b, tmp_b = b_sm[bi], b_tmp[bi]

                def VV(eng, method, reads, writes, **aps):
                    k.op(eng, lambda e, aps=aps, method=method: getattr(e, method)(**aps), reads=reads, writes=writes)
                VV("vector", "tensor_tensor", [plb, b_rb], [sm_b], out=R(0, 36), in0=pl[:, 0:36], in1=rb[:], op=ALU.add)
                VV("vector", "tensor_reduce", [sm_b], [sm_b], out=R(36, 37), in_=R(0, 4), axis=AX.X, op=ALU.max)
                VV("vector", "tensor_scalar", [sm_b], [sm_b], out=R(40, 44), in0=R(0, 4), scalar1=R(36, 37), scalar2=None, op0=ALU.is_equal)
                VV("vector", "tensor_scalar", [sm_b], [sm_b], out=R(37, 38), in0=R(36, 37), scalar1=-1.0, scalar2=None, op0=ALU.mult)
                VV("scalar", "activation", [sm_b], [sm_b], out=R(89, 93), in_=R(0, 4), func=AF.Exp, bias=R(37, 38), accum_out=R(38, 39))
                VV("vector", "reciprocal", [sm_b], [sm_b], out=R(38, 39), in_=R(38, 39))
                VV("vector", "tensor_tensor", [sm_b], [tmp_b], out=T3, in0=R(4, 36).rearrange("p (g e) -> p g e", g=4),
                   in1=R(40, 44).unsqueeze(2).to_broadcast([128, 4, 8]), op=ALU.mult)
                VV("vector", "tensor_reduce", [tmp_b], [sm_b], out=R(44, 52), in_=T3.rearrange("p g e -> p e g"), axis=AX.X, op=ALU.add)
                VV("vector", "tensor_reduce", [sm_b], [sm_b], out=R(52, 53), in_=R(44, 52), axis=AX.X, op=ALU.max)
                VV("vector", "tensor_scalar", [sm_b], [sm_b], out=R(54, 62), in0=R(44, 52), scalar1=R(52, 53), scalar2=None, op0=ALU.is_equal)
                VV("vector", "scalar_tensor_tensor", [sm_b], [sm_b], out=R(62, 70), in0=R(54, 62), scalar=-1e30, in1=R(44, 52), op0=ALU.mult, op1=ALU.add)
                VV("vector", "tensor_reduce", [sm_b], [sm_b], out=R(53, 54), in_=R(62, 70), axis=AX.X, op=ALU.max)
                VV("vector", "tensor_scalar", [sm_b], [sm_b], out=R(70, 78), in0=R(62, 70), scalar1=R(53, 54), scalar2=None, op0=ALU.is_equal)
                VV("vector", "tensor_tensor", [sm_b], [sm_b], out=R(78, 79), in0=R(53, 54), in1=R(52, 53), op=ALU.subtract)
                VV("scalar", "activation", [sm_b], [sm_b], out=R(78, 79), in_=R(78, 79), func=AF.Exp)
                VV("vector", "tensor_scalar", [sm_b], [sm_b], out=R(79, 80), in0=R(78, 79), scalar1=1.0, scalar2=None, op0=ALU.add)
                VV("vector", "reciprocal", [sm_b], [sm_b], out=R(79, 80), in_=R(79, 80))
                VV("vector", "tensor_tensor", [sm_b], [sm_b], out=R(80, 81), in0=R(78, 79), in1=R(79, 80), op=ALU.mult)
                VV("vector", "tensor_scalar", [sm_b], [sm_b], out=R(79, 81), in0=R(79, 81), scalar1=R(38, 39), scalar2=None, op0=ALU.mult)
                VV("vector", "tensor_scalar", [sm_b], [sm_b], out=R(81, 89), in0=R(54, 62), scalar1=R(79, 80), scalar2=None, op0=ALU.mult)
                VV("vector", "scalar_tensor_tensor", [sm_b], [sm_b], out=R(81, 89), in0=R(70, 78), scalar=R(80, 81), in1=R(81, 89), op0=ALU.mult, op1=ALU.add)
                VV("vector", "tensor_tensor", [sm_b], [b_G[ti]], out=G[:, ti, :].rearrange("p (g e) -> p g e", g=4),
                   in0=R(40, 44).unsqueeze(2).to_broadcast([128, 4, 8]), in1=R(81, 89).unsqueeze(1).to_broadcast([128, 4, 8]), op=ALU.mult)
                G3 = G[:, ti, :].rearrange("p (g e) -> p g e", g=4)
                VV("vector", "tensor_copy", [sm_b], [b_Mg], out=Mg[:, ti, :], in_=R(40, 44))
                VV("vector", "tensor_copy", [b_G[ti]], [b_Ghl], out=Ghl[:, ti, :, 0:8], in_=G3)
                VV("vector", "tensor_tensor", [b_G[ti], b_Ghl], [tmp_b], out=T3, in0=G3, in1=Ghl[:, ti, :, 0:8], op=ALU.subtract)
                VV("vector", "tensor_copy", [tmp_b], [b_Ghl], out=Ghl[:, ti, :, 8:16], in_=T3)
        S.barrier()

        with ExitStack() as s2:
            sb2 = lambda n, s, dt=F32: s2.enter_context(nc.sbuf_tensor(n, list(s), dt))
            NST = CAP // 128
            iota = sb2("iota_sb", [128, CAP])
            b_iota = Buf("iota_sb")
            k.dma("sync", iota[:], C.iota_d, writes=[b_iota])
            Mcum = sb2("Mcum", [128, NT, 4])
            b_Mc = Buf()
            slotf = sb2("slotf", [128, NT])
            b_sl = Buf()
            t4 = sb2("t4", [128, 4])
            b_t4 = Buf()

            def VV(eng, method, reads, writes, **aps):
                return k.op(eng, lambda e, aps=aps, method=method: getattr(e, method)(**aps), reads=reads, writes=writes)
            for ti in range(NT):
                if ti == 0:
                    VV("vector", "tensor_copy", [b_Mg], [b_Mc], out=Mcum[:, 0, :], in_=Mg[:, 0, :])
                else:
                    VV("vector", "tensor_tensor", [b_Mg, b_Mc], [b_Mc], out=Mcum[:, ti, :], in0=Mcum[:, ti - 1, :], in1=Mg[:, ti, :], op=ALU.add)
            for ti in range(NT):
                pr, prb = psT[ti % 2], psB[ti % 2]
                k.op("tensor", lambda e, ti=ti, pr=pr: e.matmul(pr[:, 0:4], lhsT=cst[:, 7, :], rhs=Mg[:, ti, :], start=True, stop=(ti == 0)),
                     reads=[b_cst, b_Mg], writes=[prb])
                if ti > 0:
                    k.acc("tensor", lambda e, ti=ti, pr=pr: e.matmul(pr[:, 0:4], lhsT=C.ones_f[:], rhs=Mcum[:, ti - 1, :], start=False, stop=True),
                          reads=[C.b_ones, b_Mc], acc=[prb])
                VV("vector", "tensor_tensor", [prb, b_Mg], [b_t4], out=t4[:], in0=pr[:, 0:4], in1=Mg[:, ti, :], op=ALU.mult)
                VV("vector", "tensor_reduce", [b_t4], [b_sl], out=slotf[:, ti:ti + 1], in_=t4[:], axis=AX.X, op=ALU.add)

            Sel = sb2("Sel", [128, NT, CAP], BF16)
            b_Sel = Buf()
            hTg = sb2("hTg", [128, 8, CAP], BF16)
            b_hTg = Buf()
            Yg = sb2("Yg", [128, NST, 1024])
            b_Yg = [Buf() for _ in range(NST)]
            Ygs = sb2("Ygs", [128, NST, 1024], BF16)
            b_Ygs = Buf()
            t16 = sb2("t16", [128, 16])
            b_t16 = Buf()
            Gs = sb2("Gs", [128, NST, 8])
            b_Gs = Buf()
            hbt = [sb2("hbt%d" % i, [128, 1024], BF16) for i in range(3)]
            b_hbt = [Buf("hbt%d" % i) for i in range(3)]
            selT = [sb2("selT%d" % i, [128, 128], BF16) for i in range(4)]
            b_selT = [Buf() for _ in range(4)]
            wg = [sb2("wg%d" % i, [128, 8, 512], BF16) for i in range(2)]
            wu_ = [sb2("wup%d" % i, [128, 8, 512], BF16) for i in range(2)]
            wd = [sb2("wd%d" % i, [128, 4, 1024], BF16) for i in range(2)]
            b_wg = [Buf("wg%d" % i) for i in range(2)]
            b_wu = [Buf("wup%d" % i) for i in range(2)]
            b_wd = [Buf("wd%d" % i) for i in range(2)]
            sg = [sb2("sg%d" % i, [128, 512]) for i in range(2)]
            b_sg = [Buf() for _ in range(2)]
            act = sb2("act", [128, 4, 512], BF16)
            b_act = Buf()
            psbf = psT[7].bitcast(BF16)
            chunks = [(0, 512), (512, CAP - 512)]
            fcnt = 0
            hcnt = 0
            tcnt = 0
            for g in range(4):
                for ti in range(NT):
                    VV("vector", "tensor_scalar", [b_iota, b_sl, b_Mg], [b_Sel], out=Sel[:, ti, :], in0=iota[:], scalar1=slotf[:, ti:ti + 1],
                       scalar2=Mg[:, ti, g:g + 1], op0=ALU.is_equal, op1=ALU.mult)
                for ps_ in range(2):
                    for ti in range(NT):
                        hi = hcnt % 3
                        hcnt += 1
                        k.dma("sync", hbt[hi][:], C.hb_d[128 * ti:128 * ti + 128, :], reads=[C.b_hb_d], writes=[b_hbt[hi]])
                        for j in range(4):
                            kk = 4 * ps_ + j
                            for (bank, c0, cn) in ((j, 0, 512), (4 + j, 512, CAP - 512)):
                                fn = lambda e, bank=bank, hi=hi, kk=kk, ti=ti, c0=c0, cn=cn: e.matmul(
                                    psT[bank][:, 0:cn], lhsT=hbt[hi][:, 128 * kk:128 * kk + 128], rhs=Sel[:, ti, c0:c0 + cn], start=(ti == 0), stop=(ti == NT - 1))
                                if ti == 0:
                                    k.op("tensor", fn, reads=[b_hbt[hi], b_Sel], writes=[psB[bank]])
                                else:
                                    k.acc("tensor", fn, reads=[b_hbt[hi], b_Sel], acc=[psB[bank]])
                    for j in range(4):
                        kk = 4 * ps_ + j
                        VV("vector", "tensor_copy", [psB[j]], [b_hTg], out=hTg[:, kk, 0:512], in_=psT[j][:, 0:512])
                        VV("vector", "tensor_copy", [psB[4 + j]], [b_hTg], out=hTg[:, kk, 512:CAP], in_=psT[4 + j][:, 0:CAP - 512])
                for st in range(NST):
                    pg_, pgb_ = psT[st % 2], psB[st % 2]
                    for ti in range(NT):
                        fn = lambda e, st=st, ti=ti, g=g, pg_=pg_: e.matmul(pg_[:, 0:16], lhsT=Sel[:, ti, 128 * st:128 * st + 128], rhs=Ghl[:, ti, g, :],
                                                                           start=(ti == 0), stop=(ti == NT - 1))
                        if ti == 0:
                            k.op("tensor", fn, reads=[b_Sel, b_Ghl], writes=[pgb_])
                        else:
                            k.acc("tensor", fn, reads=[b_Sel, b_Ghl], acc=[pgb_])
                    VV("vector", "tensor_copy", [pgb_], [b_t16], out=t16[:], in_=pg_[:, 0:16])
                    VV("vector", "tensor_tensor", [b_t16], [b_Gs], out=Gs[:, st, :], in0=t16[:, 0:8], in1=t16[:, 8:16], op=ALU.add)
                for e8 in range(8):
                    ex = 8 * g + e8
                    wi = ex % 2
                    k.gload(wg[wi][:], C.w_gate[ex].rearrange("(k p) f -> p k f", p=128), writes=[b_wg[wi]])
                    k.gload(wu_[wi][:], C.w_up[ex].rearrange("(k p) f -> p k f", p=128), writes=[b_wu[wi]])
                    k.gload(wd[wi][:], C.w_down[ex].rearrange("(k p) d -> p k d", p=128), writes=[b_wd[wi]])
                    for (t0, tn) in (chunks if os.environ.get('KSKIPX', '0') == '0' else []):
                        for f in range(4):
                            pg, pgb = psT[0 + fcnt % 2], psB[0 + fcnt % 2]
                            pu, pub = psT[2 + fcnt % 2], psB[2 + fcnt % 2]
                            si = fcnt % 2
                            fcnt += 1
                            for (pp, ppb, ww, bw) in ((pg, pgb, wg[wi], b_wg[wi]), (pu, pub, wu_[wi], b_wu[wi])):
                                for kk in range(8):
                                    fn = lambda e, kk=kk, pp=pp, ww=ww, f=f, t0=t0, tn=tn: e.matmul(pp[:, 0:tn], lhsT=ww[:, kk, 128 * f:128 * f + 128], rhs=hTg[:, kk, t0:t0 + tn],
                                                                                                start=(kk == 0), stop=(kk == 7))
                                    if kk == 0:
                                        k.op("tensor", fn, reads=[bw, b_hTg], writes=[ppb])
                                    else:
                                        k.acc("tensor", fn, reads=[bw, b_hTg], acc=[ppb])
                            k.op("scalar", lambda e, si=si, pg=pg, tn=tn: e.activation(out=sg[si][:, 0:tn], in_=pg[:, 0:tn], func=AF.Silu), reads=[pgb], writes=[b_sg[si]])
                            k.op("vector", lambda e, si=si, f=f, pu=pu, tn=tn: e.tensor_tensor(out=act[:, f, 0:tn], in0=sg[si][:, 0:tn], in1=pu[:, 0:tn], op=ALU.mult),
                                 reads=[b_sg[si], pub], writes=[b_act])
                        for tt in range(tn // 128):
                            st = t0 // 128 + tt
                            for half in range(2):
                                py, pyb = psT[4 + (tt * 2 + half) % 4], psB[4 + (tt * 2 + half) % 4]
                                for f in range(4):
                                    fn = lambda e, f=f, tt=tt, half=half, py=py, wi=wi: e.matmul(py[:, :], lhsT=act[:, f, 128 * tt:128 * tt + 128],
                                                                                             rhs=wd[wi][:, f, 512 * half:512 * half + 512], start=(f == 0), stop=(f == 3))
                                    if f == 0:
                                        k.op("tensor", fn, reads=[b_act, b_wd[wi]], writes=[pyb])
                                    else:
                                        k.acc("tensor", fn, reads=[b_act, b_wd[wi]], acc=[pyb])
                                dsty = Yg[:, st, 512 * half:512 * half + 512]
                                if e8 == 0:
                                    VV("vector", "tensor_scalar", [pyb, b_Gs], [b_Yg[st]], out=dsty, in0=py[:, :], scalar1=Gs[:, st, e8:e8 + 1], scalar2=None, op0=ALU.mult)
                                else:
                                    VV("vector", "scalar_tensor_tensor", [pyb, b_Gs, b_Yg[st]], [b_Yg[st]], out=dsty, in0=py[:, :], scalar=Gs[:, st, e8:e8 + 1], in1=dsty,
                                       op0=ALU.mult, op1=ALU.add)
                for st in range(NST):
                    VV("gpsimd", "tensor_copy", [b_Yg[st]], [b_Ygs], out=Ygs[:, st, :], in_=Yg[:, st, :])
                for ti in range(NT):
                    pa = [(psT[0], psB[0]), (psT[1], psB[1])] if ti % 2 == 0 else [(psT[2], psB[2]), (psT[3], psB[3])]
                    for st in range(NST):
                        sti = tcnt % 4
                        tcnt += 1
                        pt_b = psbf[:, 128 * sti:128 * sti + 128]
                        k.op("tensor", lambda e, pt_b=pt_b, ti=ti, st=st: e.transpose(pt_b, Sel[:, ti, 128 * st:128 * st + 128], identb[:]),
                             reads=[b_Sel, b_identb], writes=[psB[7]])
                        VV("vector", "tensor_copy", [psB[7]], [b_selT[sti]], out=selT[sti][:], in_=pt_b)
                        for half in range(2):
                            fn = lambda e, half=half, sti=sti, st=st, pa=pa: e.matmul(pa[half][0][:, :], lhsT=selT[sti][:], rhs=Ygs[:, st, 512 * half:512 * half + 512],
                                                                                 start=(st == 0), stop=(st == NST - 1))
                            if st == 0:
                                k.op("tensor", fn, reads=[b_selT[sti], b_Ygs], writes=[pa[half][1]])
                            else:
                                k.acc("tensor", fn, reads=[b_selT[sti], b_Ygs], acc=[pa[half][1]])
                    for half in range(2):
                        dy = yacc[:, ti, 512 * half:512 * half + 512]
                        VV("vector", "tensor_tensor", [pa[half][1], b_y[ti]], [b_y[ti]], out=dy, in0=pa[half][0][:, :], in1=dy, op=ALU.add)
        S.barrier()

        with ExitStack() as s3:
            sb3 = lambda n, s, dt=F32: s3.enter_context(nc.sbuf_tensor(n, list(s), dt))
            lnp3 = sb3("lnp_sb3", [128, 2, 1024])
            b_lnp3 = Buf("lnp3")
            k.dma("sync", lnp3[:], C.lnp_d[:, 2:4, :], writes=[b_lnp3])
            st6, mv = st6_c3, mv_c3
            b_st = [Buf() for _ in range(2)]
            b_mv = [Buf() for _ in range(2)]
            yo = [sb3("yo%d" % i, [128, 1024]) for i in range(2)]
            b_yo = [Buf("yo%d" % i) for i in range(2)]
            for ti in range(NT if KC >= 30 else 0):
                bi = ti % 2
                for half in range(2):
                    k.op("vector", lambda e, bi=bi, half=half, ti=ti: e.bn_stats(out=st6[:, bi, half, :], in_=yacc[:, ti, 512 * half:512 * half + 512]),
                         reads=[b_y[ti]], writes=[b_st[bi]])
                k.op("vector", lambda e, bi=bi: e.bn_aggr(out=mv[:, bi, :], in_=st6[:, bi, :, :].rearrange("p a b -> p (a b)")), reads=[b_st[bi]], writes=[b_mv[bi]])
                k.op("scalar", lambda e, bi=bi: e.activation(out=mv[:, bi, 1:2], in_=mv[:, bi, 1:2], func=AF.Sqrt, bias=C.eps5[:, 0:1]), reads=[b_mv[bi], C.b_eps5], writes=[b_mv[bi]])
                k.op("vector", lambda e, bi=bi: e.reciprocal(out=mv[:, bi, 1:2], in_=mv[:, bi, 1:2]), reads=[b_mv[bi]], writes=[b_mv[bi]])
                k.op("vector", lambda e, bi=bi, ti=ti: e.tensor_scalar(out=yo[bi][:], in0=yacc[:, ti, :], scalar1=mv[:, bi, 0:1], scalar2=mv[:, bi, 1:2], op0=ALU.subtract, op1=ALU.mult),
                     reads=[b_y[ti], b_mv[bi]], writes=[b_yo[bi]])
                k.op("gpsimd", lambda e, bi=bi: e.tensor_tensor(out=yo[bi][:], in0=yo[bi][:], in1=lnp3[:, 0, :], op=ALU.mult), reads=[b_yo[bi], b_lnp3], writes=[b_yo[bi]])
                k.op("gpsimd", lambda e, bi=bi: e.tensor_tensor(out=yo[bi][:], in0=yo[bi][:], in1=lnp3[:, 1, :], op=ALU.add), reads=[b_yo[bi], b_lnp3], writes=[b_yo[bi]])
                if ti < 16:
                    final.append(k.dma("sync", C.y_out[128 * ti:128 * ti + 128, :], yo[bi][:], reads=[b_yo[bi]]))
                else:
                    final.append(k.dma("sync", C.ys_out, yo[bi][0:NS, :], reads=[b_yo[bi]]))


def phase_s(C):
    nc, k, S = C.nc, C.k, C.S
    psT, psB = C.psT, C.psB
    final = C.final
    ps_d, cs_d, ms_d = C.ps_d, C.cs_d, C.ms_d
    b_ps_d, b_cs_d, b_ms_d = Buf("ps_d"), Buf("cs_d"), Buf("ms_d")
    w_v = C.w_v

    def VV(eng, method, reads, writes, **aps):
        return k.op(eng, lambda e, aps=aps, method=method: getattr(e, method)(**aps), reads=reads, writes=writes)

    with ExitStack() as s1:
        sb1 = lambda n, s, dt=F32: s1.enter_context(nc.sbuf_tensor(n, list(s), dt))
        xsTb = sb1("xsTb", [128, 8, NS], BF16)
        b_xs = Buf("xsTb")
        k.gload(xsTb[:], C.xsT.rearrange("(k p) t -> p k t", p=128), writes=[b_xs])
        wS = [sb1("wS%d" % i, [128, 8, 512], BF16) for i in range(2)]
        b_wS = [Buf("wS%d" % i) for i in range(2)]
        p_s = sb1("p_s", [NS, IN_COLS])
        b_p = Buf("p_s")
        for cchunk in range(8):
            c0 = 512 * cchunk
            cn = min(512, IN_COLS - c0)
            wi = cchunk % 2
            k.gload(wS[wi][:, :, 0:cn], w_v[:, :, c0:c0 + cn], writes=[b_wS[wi]])
            pt_, pb_ = psT[wi], psB[wi]
            for kk in range(8):
                fn = lambda e, kk=kk, wi=wi, cn=cn, pt_=pt_: e.matmul(pt_[0:NS, 0:cn], lhsT=xsTb[:, kk, :], rhs=wS[wi][:, kk, 0:cn], start=(kk == 0), stop=(kk == 7))
                if kk == 0:
                    k.op("tensor", fn, reads=[b_xs, b_wS[wi]], writes=[pb_])
                else:
                    k.acc("tensor", fn, reads=[b_xs, b_wS[wi]], acc=[pb_])
            VV("vector", "tensor_copy", [pb_], [b_p], out=p_s[:, c0:c0 + cn], in_=pt_[0:NS, 0:cn])
        k.dma("sync", ps_d, p_s[:], reads=[b_p], writes=[b_ps_d])
        final.append(k.dma("sync", C.knew, p_s[:, COL_KA:COL_KA + 512], reads=[b_p], slot="knew"))
        final.append(k.dma("sync", C.vnew, p_s[:, COL_VA:COL_VA + 512], reads=[b_p], slot="vnew"))
        final.append(k.dma("sync", C.conv_s[:, 2, :], p_s[:, COL_UB:COL_UB + 1536], reads=[b_p], slot="convs"))
        cst_ = sb1("cst_", [NS, 3, 1536])
        b_cst_ = Buf("cst_")
        k.dma("sync", cst_[:], C.scv, writes=[b_cst_])
        final.append(k.dma("sync", C.conv_s[:, 0:2, :], cst_[:, 1:3, :], reads=[b_cst_], slot="convs"))
        cwr = sb1("cwr_sb", [NS, 4, 1536])
        b_cwr = Buf("cwr")
        k.dma("sync", cwr[:], C.cwr_d, writes=[b_cwr])
        cacc = sb1("cacc", [NS, 1536])
        ctmp = sb1("ctmp", [NS, 1536])
        b_ca, b_ct = Buf("cacc"), Buf()
        VV("vector", "tensor_tensor", [b_p, b_cwr], [b_ca], out=cacc[:], in0=p_s[:, COL_UB:COL_UB + 1536], in1=cwr[:, 3, :], op=ALU.mult)
        for i in range(3):
            VV("vector", "tensor_tensor", [b_cst_, b_cwr], [b_ct], out=ctmp[:], in0=cst_[:, i, :], in1=cwr[:, i, :], op=ALU.mult)
            VV("vector", "tensor_tensor", [b_ct, b_ca], [b_ca], out=cacc[:], in0=cacc[:], in1=ctmp[:], op=ALU.add)
        VV("scalar", "activation", [b_ca], [b_ca], out=cacc[:], in_=cacc[:], func=AF.Silu)
        k.dma("sync", cs_d, cacc[:], reads=[b_ca], writes=[b_cs_d])
    S.barrier()

    with ExitStack() as s2:
        sb2 = lambda n, s, dt=F32: s2.enter_context(nc.sbuf_tensor(n, list(s), dt))
        qkv = sb2("qkv_nh", [128, 3, 64])
        b_qkv = Buf("qkv_nh")
        for j, c0 in enumerate((COL_QA, COL_KA, COL_VA)):
            k.dma("sync", qkv[:, j, :], bass.AP(ps_d.tensor, c0, [[IN_COLS, NS], [64, 8], [1, 64]]), reads=[b_ps_d], writes=[b_qkv])
        sbias = sb2("sbias_sb", [128, 3, 129])
        b_sb = Buf("sbias")
        k.dma("sync", sbias[:], C.sbias_d, writes=[b_sb])
        Kb = sb2("Kb", [128, 128, 64])
        Vb = sb2("Vb", [128, 128, 64])
        b_Kb, b_Vb = Buf("Kb"), Buf("Vb")
        tmpS = sb2("tmpS", [128, 128, 64])
        b_tmp = Buf()
        sc = sb2("sc", [128, 3, 129])
        b_sc = Buf()
        sm = sb2("smS", [128, 16])
        b_sm = Buf()
        oacc = sb2("oacc", [128, 64])
        otmp = sb2("otmp", [128, 64])
        b_oa, b_ot = Buf("oacc"), Buf()
        VV("vector", "tensor_tensor", [b_qkv], [b_ot], out=otmp[:], in0=qkv[:, 0, :], in1=qkv[:, 1, :], op=ALU.mult)
        VV("vector", "tensor_reduce", [b_ot], [b_sm], out=sm[:, 0:1], in_=otmp[:], axis=AX.X, op=ALU.add)
        for br, (_, dil) in enumerate(BRANCHES):
            for n in range(NS):
                src = bass.AP(C.ck.tensor, n * 2048 * 512 + (2048 - 128 * dil) * 512, [[64, 8], [dil * 512, 128], [1, 64]])
                k.dma("sync", Kb[8 * n:8 * n + 8, :, :], src, writes=[b_Kb]) if n == 0 else k.S.dmaop(
                    "sync", "Kb", lambda e, n=n, src=src: e.dma_start(out=Kb[8 * n:8 * n + 8, :, :], in_=src), [])
            b_Kb.writer = ("D", "Kb", k.S.dma_sems["Kb"][1])
            VV("vector", "tensor_tensor", [b_Kb, b_qkv], [b_tmp], out=tmpS[:], in0=Kb[:], in1=qkv[:, 0, :].unsqueeze(1).to_broadcast([128, 128, 64]), op=ALU.mult)
            VV("vector", "tensor_reduce", [b_tmp], [b_sc], out=sc[:, br, 0:128], in_=tmpS[:], axis=AX.X, op=ALU.add)
            VV("vector", "tensor_copy", [b_sm], [b_sc], out=sc[:, br, 128:129], in_=sm[:, 0:1])
        VV("vector", "scalar_tensor_tensor", [b_sc, b_sb], [b_sc], out=sc[:].rearrange("p a b -> p (a b)"), in0=sc[:].rearrange("p a b -> p (a b)"),
           scalar=0.125, in1=sbias[:].rearrange("p a b -> p (a b)"), op0=ALU.mult, op1=ALU.add)
        VV("vector", "tensor_reduce", [b_sc], [b_sm], out=sm[:, 1:2], in_=sc[:].rearrange("p a b -> p (a b)"), axis=AX.X, op=ALU.max)
        VV("vector", "tensor_scalar", [b_sm], [b_sm], out=sm[:, 2:3], in0=sm[:, 1:2], scalar1=-1.0, scalar2=None, op0=ALU.mult)
        VV("scalar", "activation", [b_sc, b_sm], [b_sc, b_sm], out=sc[:].rearrange("p a b -> p (a b)"), in_=sc[:].rearrange("p a b -> p (a b)"), func=AF.Exp,
           bias=sm[:, 2:3], accum_out=sm[:, 3:4])
        VV("vector", "reciprocal", [b_sm], [b_sm], out=sm[:, 3:4], in_=sm[:, 3:4])
        VV("vector", "tensor_reduce", [b_sc], [b_sm], out=sm[:, 4:5], in_=sc[:, :, 128], axis=AX.X, op=ALU.add)
        VV("vector", "tensor_scalar", [b_qkv, b_sm], [b_oa], out=oacc[:], in0=qkv[:, 2, :], scalar1=sm[:, 4:5], scalar2=None, op0=ALU.mult)
        for br, (_, dil) in enumerate(BRANCHES):
            for n in range(NS):
                src = bass.AP(C.cv.tensor, n * 2048 * 512 + (2048 - 128 * dil) * 512, [[64, 8], [dil * 512, 128], [1, 64]])
                if n == 0:
                    k.dma("sync", Vb[8 * n:8 * n + 8, :, :], src, writes=[b_Vb])
                else:
                    k.S.dmaop("sync", "Vb", lambda e, n=n, src=src: e.dma_start(out=Vb[8 * n:8 * n + 8, :, :], in_=src), [])
            b_Vb.writer = ("D", "Vb", k.S.dma_sems["Vb"][1])
            VV("vector", "tensor_tensor", [b_Vb, b_sc], [b_tmp], out=tmpS[:], in0=Vb[:], in1=sc[:, br, 0:128].unsqueeze(2).to_broadcast([128, 128, 64]), op=ALU.mult)
            VV("vector", "tensor_reduce", [b_tmp], [b_ot], out=otmp[:], in_=tmpS[:].rearrange("p i d -> p d i"), axis=AX.X, op=ALU.add)
            VV("vector", "tensor_tensor", [b_ot, b_oa], [b_oa], out=oacc[:], in0=oacc[:], in1=otmp[:], op=ALU.add)
        VV("vector", "tensor_scalar", [b_oa, b_sm], [b_oa], out=oacc[:], in0=oacc[:], scalar1=sm[:, 3:4], scalar2=None, op0=ALU.mult)
        k.dma("sync", bass.AP(ms_d.tensor, 0, [[D, NS], [64, 8], [1, 64]]), oacc[:], reads=[b_oa], writes=[b_ms_d])
    S.barrier()

    with ExitStack() as s3:
        sb3 = lambda n, s, dt=F32: s3.enter_context(nc.sbuf_tensor(n, list(s), dt))
        NP = NS * 4
        St = sb3("St", [NP, 128, 128])
        b_St = Buf("St")
        for q4 in range(4):
            k.dma("sync", St[:, 32 * q4:32 * q4 + 32, :], C.sst[:, 32 * q4:32 * q4 + 32, :], writes=[b_St]) if q4 == 0 else k.S.dmaop(
                "sync", "St", lambda e, q4=q4: e.dma_start(out=St[:, 32 * q4:32 * q4 + 32, :], in_=C.sst[:, 32 * q4:32 * q4 + 32, :]), [])
        b_St.writer = ("D", "St", k.S.dma_sems["St"][1])
        T2 = sb3("T2", [NP, 128, 128])
        b_T2 = Buf()
        c3 = sb3("c3", [NP, 3, 128])
        b_c3 = Buf("c3")
        for ty in range(3):
            k.dma("sync", c3[:, ty, :], bass.AP(cs_d.tensor, 512 * ty, [[1536, NS], [128, 4], [1, 128]]), reads=[b_cs_d], writes=[b_c3])
        zz = sb3("zz", [NP, 128])
        b_zz = Buf("zz")
        k.dma("sync", zz[:], bass.AP(ps_d.tensor, COL_ZB, [[IN_COLS, NS], [128, 4], [1, 128]]), reads=[b_ps_d], writes=[b_zz])
        ab = sb3("ab_s", [NP, 2])
        b_ab = Buf("ab_s")
        k.dma("sync", ab[:, 0:1], bass.AP(ps_d.tensor, COL_AB, [[IN_COLS, NS], [1, 4], [1, 1]]), reads=[b_ps_d], writes=[b_ab])
        k.dma("sync", ab[:, 1:2], bass.AP(ps_d.tensor, COL_BB, [[IN_COLS, NS], [1, 4], [1, 1]]), reads=[b_ps_d], writes=[b_ab])
        sp = sb3("sprm_sb", [NP, 2 + 128])
        b_sp = Buf("sprm")
        k.dma("sync", sp[:], C.sprm_d, writes=[b_sp])
        w = sb3("wS3", [NP, 24])
        b_w = Buf()
        jk = sb3("jkS3", [NP, 128])
        b_jk = Buf()
        VV("vector", "tensor_tensor", [b_ab, b_sp], [b_w], out=w[:, 0:1], in0=ab[:, 0:1], in1=sp[:, 1:2], op=ALU.add)
        VV("scalar", "activation", [b_w], [b_w], out=w[:, 0:1], in_=w[:, 0:1], func=AF.Exp)
        VV("scalar", "activation", [b_w], [b_w], out=w[:, 0:1], in_=w[:, 0:1], func=AF.Ln, bias=1.0)
        VV("scalar", "activation", [b_sp], [b_w], out=w[:, 1:2], in_=sp[:, 0:1], func=AF.Exp)
        VV("vector", "scalar_tensor_tensor", [b_w], [b_w], out=w[:, 2:3], in0=w[:, 0:1], scalar=-1.0, in1=w[:, 1:2], op0=ALU.mult, op1=ALU.mult)
        VV("scalar", "activation", [b_w], [b_w], out=w[:, 3:4], in_=w[:, 2:3], func=AF.Exp)
        VV("scalar", "activation", [b_ab], [b_w], out=w[:, 4:5], in_=ab[:, 1:2], func=AF.Sigmoid)
        for j in range(2):
            VV("scalar", "activation", [b_c3], [b_jk, b_w], out=jk[:], in_=c3[:, j, :], func=AF.Square, accum_out=w[:, 5 + j:6 + j])
            VV("scalar", "activation", [b_w, C.b_eps], [b_w], out=w[:, 5 + j:6 + j], in_=w[:, 5 + j:6 + j], func=AF.Sqrt, bias=C.eps6[0:NP, 0:1])
            VV("vector", "reciprocal", [b_w], [b_w], out=w[:, 5 + j:6 + j], in_=w[:, 5 + j:6 + j])
        VV("vector", "tensor_scalar", [b_c3, b_w], [b_c3], out=c3[:, 0, :], in0=c3[:, 0, :], scalar1=w[:, 5:6], scalar2=128.0 ** -0.5, op0=ALU.mult, op1=ALU.mult)
        VV("vector", "tensor_scalar", [b_c3, b_w], [b_c3], out=c3[:, 1, :], in0=c3[:, 1, :], scalar1=w[:, 6:7], scalar2=None, op0=ALU.mult)
        mem = sb3("memS", [NP, 4, 128])
        b_mem = Buf()
        VV("vector", "tensor_tensor", [b_St, b_c3], [b_T2], out=T2[:], in0=St[:], in1=c3[:, 1, :].unsqueeze(2).to_broadcast([NP, 128, 128]), op=ALU.mult)
        VV("vector", "tensor_reduce", [b_T2], [b_mem], out=mem[:, 0, :], in_=T2[:].rearrange("p d e -> p e d"), axis=AX.X, op=ALU.add)
        VV("vector", "scalar_tensor_tensor", [b_mem, b_w, b_c3], [b_mem], out=mem[:, 1, :], in0=mem[:, 0, :], scalar=w[:, 3:4], in1=c3[:, 2, :], op0=ALU.mult, op1=ALU.subtract)
        VV("vector", "tensor_scalar", [b_mem, b_w], [b_mem], out=mem[:, 1, :], in0=mem[:, 1, :], scalar1=w[:, 4:5], scalar2=-1.0, op0=ALU.mult, op1=ALU.mult)
        VV("vector", "tensor_tensor", [b_c3, b_mem], [b_T2], out=T2[:], in0=c3[:, 1, :].unsqueeze(2).to_broadcast([NP, 128, 128]),
           in1=mem[:, 1, :].unsqueeze(1).to_broadcast([NP, 128, 128]), op=ALU.mult)
        VV("vector", "scalar_tensor_tensor", [b_St, b_T2, b_w], [b_St], out=St[:], in0=St[:], scalar=w[:, 3:4], in1=T2[:], op0=ALU.mult, op1=ALU.add)
        for q4 in range(4):
            final.append(k.dma("sync", C.ssm_s[:, 32 * q4:32 * q4 + 32, :], St[:, 32 * q4:32 * q4 + 32, :], reads=[b_St], slot="ssms"))
        VV("vector", "tensor_tensor", [b_St, b_c3], [b_T2], out=T2[:], in0=St[:], in1=c3[:, 0, :].unsqueeze(2).to_broadcast([NP, 128, 128]), op=ALU.mult)
        VV("vector", "tensor_reduce", [b_T2], [b_mem], out=mem[:, 2, :], in_=T2[:].rearrange("p d e -> p e d"), axis=AX.X, op=ALU.add)
        VV("scalar", "activation", [b_mem], [b_jk, b_w], out=jk[:], in_=mem[:, 2, :], func=AF.Square, accum_out=w[:, 8:9])
        VV("scalar", "activation", [b_w, C.b_eps], [b_w], out=w[:, 8:9], in_=w[:, 8:9], func=AF.Sqrt, scale=1.0 / 128.0, bias=C.eps6[0:NP, 0:1])
        VV("vector", "reciprocal", [b_w], [b_w], out=w[:, 8:9], in_=w[:, 8:9])
        VV("scalar", "activation", [b_zz], [b_zz], out=zz[:], in_=zz[:], func=AF.Silu)
        VV("vector", "tensor_tensor", [b_zz, b_sp], [b_zz], out=zz[:], in0=zz[:], in1=sp[:, 2:130], op=ALU.mult)
        VV("vector", "scalar_tensor_tensor", [b_mem, b_w, b_zz], [b_mem], out=mem[:, 3, :], in0=mem[:, 2, :], scalar=w[:, 8:9], in1=zz[:], op0=ALU.mult, op1=ALU.mult)
        k.dma("sync", bass.AP(ms_d.tensor, 512, [[D, NS], [128, 4], [1, 128]]), mem[:, 3, :], reads=[b_mem], writes=[b_ms_d])
    S.barrier()

    with ExitStack() as s4:
        sb4 = lambda n, s, dt=F32: s4.enter_context(nc.sbuf_tensor(n, list(s), dt))
        msf = sb4("msf", [128, 1024])
        b_msf = Buf("msf")
        VV("vector", "memset", [], [b_msf], ap=msf[:], constant=0.0)
        k.dma("sync", msf[0:NS, :], ms_d, reads=[b_ms_d], writes=[b_msf])
        mxS = sb4("mxS", [128, 8, 128], BF16)
        b_mxS = Buf("mxS")
        for g4 in range(2):
            pt_, pb_ = psT[2 + g4], psB[2 + g4]
            for j in range(4):
                kk = 4 * g4 + j
                fn = lambda e, kk=kk, j=j, pt_=pt_: e.transpose(pt_[:, 128 * j:128 * j + 128], msf[:, 128 * kk:128 * kk + 128], C.cst[:, 0, :])
                if j == 0:
                    k.op("tensor", fn, reads=[b_msf, C.b_cst], writes=[pb_])
                else:
                    k.acc("tensor", fn, reads=[b_msf, C.b_cst], acc=[pb_])
            VV("vector", "tensor_copy", [pb_], [b_mxS], out=mxS[:, 4 * g4:4 * g4 + 4, :], in_=pt_[:, :].rearrange("p (a c) -> p a c", a=4))
        k.dma("sync", C.mixS_d, mxS[:], reads=[b_mxS], writes=[C.b_mixS_d])
    S.barrier()


def _consts():
    t = np.arange(128)
    same = (t[:, None] // 64) == (t[None, :] // 64)
    cst = np.zeros((128, 8, 128), np.float32)
    cst[:, 0] = np.eye(128)
    cst[:, 1] = -np.eye(128)
    cst[:, 2] = (same & (t[:, None] <= t[None, :]))
    cst[:, 3] = (t[:, None] < 64) * np.ones((1, 128))
    cst[:, 4] = (t[:, None] >= 64) * np.ones((1, 128))
    cst[:, 5] = np.where(same & (t[None, :] <= t[:, None]), 0.0, NEG)
    cst[:, 6] = (same & (t[None, :] < t[:, None]))
    cst[:, 7] = (t[:, None] < t[None, :])
    return cst


def _params(inp):
    prm = np.zeros((128, 184), np.float32)
    prm[:, 0:4] = inp["a_log"][0][None, :]
    prm[:, 4:8] = inp["dt_bias"][0][None, :]
    prm[:, 8:136] = inp["o_norm_g"][0][None, :]
    cw = inp["conv_w"][0]
    prm[:, 136:184] = cw.reshape(4, 12, 128).transpose(2, 1, 0).reshape(128, 48)
    return prm


def _sample_bias(rel_bias):
    out = np.empty((128, 3, 129), np.float32)
    h = np.arange(128) % 8
    for br, (_, dil) in enumerate(BRANCHES):
        dist = np.concatenate([dil * (128 - np.arange(128)), [0]])
        out[:, br, :] = rel_bias[_rel_bucket_np(dist)][:, h].T
    return out


def _core_inputs(c, inp, bt):
    b, hf = divmod(c, 2)
    x = inp["x_prompt"][b]
    xT = np.zeros((D, EXT), np.float32)
    if hf == 1:
        xT[:, :] = x.T
    else:
        xT[:, HALF:] = x[:HALF].T
    valid = np.ones((128, 32), np.float32)
    if hf == 0:
        valid[:, :16] = 0.0
    return {
        "xT": np.ascontiguousarray(xT),
        "xo": np.ascontiguousarray(x[HALF * hf:HALF * hf + HALF]),
        "valid": valid,
        "w_in": np.ascontiguousarray(inp["w_in"][0]),
        "bt": bt,
        "cst": _consts(),
        "prm": _params(inp),
        "w_out": np.ascontiguousarray(inp["w_out"][0]),
        "lnp": np.ascontiguousarray(np.broadcast_to(np.stack([inp["ln1_g"][0], inp["ln1_b"][0], inp["ln2_g"][0], inp["ln2_b"][0]])[None], (128, 4, D))),
        "wr": np.ascontiguousarray(np.concatenate([inp["w_group"][0], inp["w_router"][0]], axis=1)),
        "rb": np.ascontiguousarray(np.broadcast_to(np.concatenate([inp["b_group"][0], inp["b_router"][0].reshape(-1)])[None], (128, 36))),
        "w_gate": np.ascontiguousarray(inp["w_gate"][0]),
        "w_up": np.ascontiguousarray(inp["w_up"][0]),
        "w_down": np.ascontiguousarray(inp["w_down"][0]),
        "xsT": np.ascontiguousarray(inp["x_sample"][NS * c:NS * c + NS, 0, :].T),
        "ck": np.ascontiguousarray(inp["cache_a_k"][0, NS * c:NS * c + NS].reshape(NS, 2048, 512)),
        "cv": np.ascontiguousarray(inp["cache_a_v"][0, NS * c:NS * c + NS].reshape(NS, 2048, 512)),
        "sst": np.ascontiguousarray(inp["state_b_ssm"][0, NS * c:NS * c + NS].reshape(NS * 4, 128, 128)),
        "scv": np.ascontiguousarray(inp["state_b_conv"][0, NS * c:NS * c + NS]),
        "sbias": _sample_bias(inp["rel_bias"].astype(np.float32)),
        "cwr": np.ascontiguousarray(np.broadcast_to(inp["conv_w"][0][None], (NS, 4, 1536))),
        "sprm": np.ascontiguousarray(np.concatenate([np.tile(inp["a_log"][0], NS)[:, None], np.tile(inp["dt_bias"][0], NS)[:, None],
                                                       np.broadcast_to(inp["o_norm_g"][0][None], (NS * 4, 128))], axis=1)),
        "iota": np.ascontiguousarray(np.broadcast_to(np.arange(CAP, dtype=np.float32)[None], (128, CAP))),
        "xs_pad": np.ascontiguousarray(np.concatenate([inp["x_sample"][NS * c:NS * c + NS, 0, :], np.zeros((128 - NS, D), np.float32)], axis=0)),
    }


def kernel(**inputs):
    inp = {k_: np.asarray(v) for k_, v in inputs.items()}
    bt = _bias_tiles(inp["rel_bias"].astype(np.float32))
    nc = build()
    in_maps = [_core_inputs(c, inp, bt) for c in range(NCORES)]
    res = run_bass_kernel_spmd(nc, in_maps, core_ids=list(range(NCORES)))
    r = res.results
    y_prompt = np.stack([np.concatenate([r[2 * b]["y_out"], r[2 * b + 1]["y_out"]], axis=0) for b in range(4)])
    k_win = np.stack([r[2 * b + 1]["kwin"].reshape(HALF, 8, 64) for b in range(4)])[None]
    v_win = np.stack([r[2 * b + 1]["vwin"].reshape(HALF, 8, 64) for b in range(4)])[None]
    ssm_p = np.stack([r[2 * b + 1]["ssm_p"] for b in range(4)])[None]
    conv_p = np.stack([r[2 * b + 1]["conv_p"] for b in range(4)])[None]
    y_sample = np.concatenate([r[c]["ys_out"] for c in range(NCORES)], axis=0)[:, None, :]
    k_new = np.concatenate([r[c]["knew"] for c in range(NCORES)], axis=0).reshape(1, 128, 1, 8, 64)
    v_new = np.concatenate([r[c]["vnew"] for c in range(NCORES)], axis=0).reshape(1, 128, 1, 8, 64)
    ssm_s = np.concatenate([r[c]["ssm_s"] for c in range(NCORES)], axis=0).reshape(1, 128, 4, 128, 128)
    conv_s = np.concatenate([r[c]["conv_s"] for c in range(NCORES)], axis=0)[None]
    return (y_prompt, y_sample, k_win, v_win, k_new, v_new, ssm_p, ssm_s, conv_p, conv_s)
```

```python
import math
import os
from contextlib import ExitStack

import numpy as np
import concourse.bass as bass
import concourse.mybir as mybir
from concourse.bass_utils import run_bass_kernel_spmd

F32 = mybir.dt.float32
BF16 = mybir.dt.bfloat16
I32 = mybir.dt.int32
U32 = mybir.dt.uint32
AF = mybir.ActivationFunctionType
ALU = mybir.AluOpType
AX = mybir.AxisListType

NCORES = 8
D = 1024
SEQ = 4096
HALF = 2048
EXT = 4096
NS = 16
A_HEADS, A_HD = 8, 64
B_HEADS, B_HD = 4, 128
COL_QA, COL_KA, COL_VA, COL_UB = 0, 512, 1024, 1536
COL_ZB = COL_UB + 1536
COL_AB = COL_ZB + 512
COL_BB = COL_AB + 4
IN_COLS = COL_BB + 4
BRANCHES = ((128, 1), (512, 4), (2048, 16))
NEG = -30000.0
CAP = 640
ENGS = ("tensor", "vector", "scalar", "gpsimd", "sync")


class Sched:
    def __init__(self, nc, stack, same_engine_wait=True):
        self.nc = nc
        self.stack = stack
        self.q = {e: [] for e in ENGS}
        self.cnt = {e: 0 for e in ENGS}
        self.sem = {e: stack.enter_context(nc.semaphore("s_" + e)) for e in ENGS}
        self.waited = {e: {} for e in ENGS}
        self.same_engine_wait = same_engine_wait
        self.dma_sems = {}
        self.ninst = 0

    def _wait(self, eng, tok):
        if tok is None:
            return
        if tok[0] == "E":
            _, src, val = tok
            if src == eng and not self.same_engine_wait:
                return
            key = "E" + src
            sem = self.sem[src]
        else:
            _, slot, val = tok
            key = "D" + slot
            sem = self.dma_sems[slot][0]
        if self.waited[eng].get(key, 0) >= val:
            return
        self.waited[eng][key] = val
        self.q[eng].append(lambda e, sem=sem, val=val: e.wait_ge(sem, val))

    def op(self, eng, fn, deps=()):
        for d in deps:
            self._wait(eng, d)
        self.cnt[eng] += 1
        c = self.cnt[eng]
        sem = self.sem[eng]
        self.q[eng].append(lambda e, fn=fn, sem=sem: fn(e).then_inc(sem, 1))
        self.ninst += 1
        return ("E", eng, c)

    def dmaop(self, eng, slot, fn, deps=()):
        for d in deps:
            self._wait(eng, d)
        if slot not in self.dma_sems:
            self.dma_sems[slot] = [self.stack.enter_context(self.nc.semaphore("d_" + slot)), 0]
        ent = self.dma_sems[slot]
        ent[1] += 16
        sem = ent[0]
        self.q[eng].append(lambda e, fn=fn, sem=sem: fn(e).then_inc(sem, 16))
        self.ninst += 1
        return ("D", slot, ent[1])

    def barrier(self):
        toks = [("E", e, self.cnt[e]) for e in ENGS if self.cnt[e] > 0]
        toks += [("D", slot, ent[1]) for slot, ent in self.dma_sems.items() if ent[1] > 0]
        for e in ENGS:
            for t in toks:
                self._wait(e, t)

    def finish(self, final_tokens):
        best = {}
        for t in final_tokens:
            if t is None:
                continue
            key = (t[0], t[1])
            if key not in best or best[key][2] < t[2]:
                best[key] = t
        for t in best.values():
            self._wait("sync", t)
        with self.nc.Block() as block:
            @block.tensor
            def _(e):
                for f in self.q["tensor"]:
                    f(e)

            @block.vector
            def _(e):
                for f in self.q["vector"]:
                    f(e)

            @block.scalar
            def _(e):
                for f in self.q["scalar"]:
                    f(e)

            @block.gpsimd
            def _(e):
                for f in self.q["gpsimd"]:
                    f(e)

            @block.sync
            def _(e):
                for f in self.q["sync"]:
                    f(e)


class Buf:
    _n = 0

    def __init__(self, name=None):
        Buf._n += 1
        self.name = name or ("b%d" % Buf._n)
        self.writer = None
        self.readers = {}

    def add_reader(self, tok):
        key = tok[1]
        if key not in self.readers or self.readers[key][2] < tok[2]:
            self.readers[key] = tok


class K:
    def __init__(self, S):
        self.S = S

    def _deps(self, reads, writes, deps):
        d = list(deps)
        for b in reads:
            d.append(b.writer)
        for b in writes:
            d.extend(b.readers.values())
            d.append(b.writer)
        return d

    def _commit(self, tok, reads, writes):
        for b in reads:
            b.add_reader(tok)
        for b in writes:
            b.writer = tok
            b.readers = {}

    def op(self, eng, fn, reads=(), writes=(), deps=()):
        tok = self.S.op(eng, fn, self._deps(reads, writes, deps))
        self._commit(tok, reads, writes)
        return tok

    def acc(self, eng, fn, reads=(), acc=(), deps=()):
        d = list(deps)
        for b in reads:
            d.append(b.writer)
        tok = self.S.op(eng, fn, d)
        for b in reads:
            b.add_reader(tok)
        for b in acc:
            b.writer = tok
        return tok

    def gload(self, dst, src, writes, reads=()):
        A, L = dst.shape[1], dst.shape[2]
        slot = writes[0].name
        d = self._deps(reads, writes, ())
        tok = None
        for a0 in range(0, A, 4):
            for l0 in range(0, L, 512):
                tok = self.S.dmaop("gpsimd", slot, lambda e, a0=a0, l0=l0, L=L: e.dma_start(
                    out=dst[:, a0:a0 + 4, l0:min(L, l0 + 512)], in_=src[:, a0:a0 + 4, l0:min(L, l0 + 512)]), d)
        self._commit(tok, reads, writes)
        return tok

    def dma(self, eng, out, in_, reads=(), writes=(), deps=(), slot=None, **kw):
        if slot is None:
            slot = (writes[0] if writes else reads[0]).name
        tok = self.S.dmaop(eng, slot, lambda e: e.dma_start(out=out, in_=in_, **kw), self._deps(reads, writes, deps))
        self._commit(tok, reads, writes)
        return tok


def _rel_bucket_np(dist):
    n = np.maximum(dist, 0)
    ratio = np.maximum(n, 1).astype(np.float32) / np.float32(16)
    large = 16 + (np.log(ratio) / np.float32(math.log(2048 / 16)) * np.float32(16)).astype(np.int32)
    return np.where(n < 16, n, np.minimum(large, 31))


def _bias_tiles(rel_bias):
    kp = np.arange(128)[:, None, None]
    kt = np.arange(2)[None, :, None]
    i = np.arange(128)[None, None, :]
    off = 128 + i - (128 * kt + kp)
    valid = (off >= 0) & (off <= 128)
    out = np.empty((128, 24, 256), np.float32)
    for h in range(A_HEADS):
        for br, (_, dil) in enumerate(BRANCHES):
            bk = _rel_bucket_np(np.maximum(off, 0) * dil)
            vals = rel_bias[bk, h]
            out[:, h * 3 + br, :] = np.where(valid, vals, np.float32(NEG)).reshape(128, 256)
    return out


def sl(start, step, n=128):
    return slice(start, start + (n - 1) * step + 1, step)


def tokset(ti, dil):
    nblk, r = divmod(ti, dil)
    start = r + dil * 128 * nblk
    return start, dil


def build(debug=(), stage=99, nexp=32, with_sample=True):
    nc = bass.Bass("TRN2", target_bir_lowering=False)
    dram = lambda n, s, dt=F32, kind="ExternalInput": nc.dram_tensor(n, list(s), dt, kind=kind).ap()
    xT = dram("xT", [D, EXT])
    xo = dram("xo", [HALF, D])
    valid = dram("valid", [128, 32])
    w_in = dram("w_in", [D, IN_COLS])
    bt = dram("bt", [128, 24, 256])
    cst_d = dram("cst", [128, 8, 128])
    prm_d = dram("prm", [128, 184])
    ssm_p = dram("ssm_p", [4, 128, 128], kind="ExternalOutput")
    conv_p = dram("conv_p", [3, 1536], kind="ExternalOutput")
    kT_d = dram("kT_d", [128, 4, EXT], BF16, kind="Internal")
    vT_d = dram("vT_d", [128, 4, EXT], BF16, kind="Internal")
    qT_d = dram("qT_d", [128, 4, HALF], BF16, kind="Internal")
    gz_d = dram("gz_d", [16, 128, 512], BF16, kind="Internal")
    mixA_d = dram("mixA_d", [128, 4, HALF], BF16, kind="Internal")
    mixB_d = dram("mixB_d", [128, 4, HALF], BF16, kind="Internal")
    w_out = dram("w_out", [D, D])
    lnp_d = dram("lnp", [128, 4, D])
    wr_d = dram("wr", [D, 36])
    rb_d = dram("rb", [128, 36])
    w_gate = dram("w_gate", [32, D, 512])
    w_up = dram("w_up", [32, D, 512])
    w_down = dram("w_down", [32, 512, D])
    xs_pad = dram("xs_pad", [128, D])
    iota_d = dram("iota", [128, CAP])
    hb_d = dram("hb_d", [17 * 128, D], BF16, kind="Internal")
    y_out = dram("y_out", [HALF, D], kind="ExternalOutput")
    ys_out = dram("ys_out", [NS, D], kind="ExternalOutput")
    mixS_d = dram("mixS_d", [128, 8, 128], BF16, kind="Internal")
    xsT = dram("xsT", [D, NS])
    ck = dram("ck", [NS, 2048, 512])
    cv = dram("cv", [NS, 2048, 512])
    sst = dram("sst", [NS * 4, 128, 128])
    scv = dram("scv", [NS, 3, 1536])
    sbias_d = dram("sbias", [128, 3, 129])
    cwr_d = dram("cwr", [NS, 4, 1536])
    sprm_d = dram("sprm", [NS * 4, 130])
    ps_d = dram("ps_d", [NS, IN_COLS], kind="Internal")
    cs_d = dram("cs_d", [NS, 1536], kind="Internal")
    ms_d = dram("ms_d", [NS, D], kind="Internal")
    knew = dram("knew", [NS, 512], kind="ExternalOutput")
    vnew = dram("vnew", [NS, 512], kind="ExternalOutput")
    ssm_s = dram("ssm_s", [NS * 4, 128, 128], kind="ExternalOutput")
    conv_s = dram("conv_s", [NS, 3, 1536], kind="ExternalOutput")
    kwin = dram("kwin", [HALF, 512], kind="ExternalOutput")
    vwin = dram("vwin", [HALF, 512], kind="ExternalOutput")
    dbg_out = {}
    for name, shape in debug:
        dbg_out[name] = dram("dbg_" + name, shape, kind="ExternalOutput")

    final = []
    with ExitStack() as st:
        S = Sched(nc, st, same_engine_wait=(os.environ.get("SEW", "1") == "1"))
        k = K(S)
        sb = lambda n, s, dt=F32: st.enter_context(nc.sbuf_tensor(n, list(s), dt))
        psT = [st.enter_context(nc.psum_tensor("ps%d" % i, [128, 512], F32)) for i in range(8)]
        psB = [Buf("ps%d" % i) for i in range(8)]

        ones_f = sb("ones_f", [128, 128])
        b_ones = Buf()
        k.op("gpsimd", lambda e: e.memset(ones_f[:], 1.0), writes=[b_ones])
        eps6 = sb("eps6", [128, 1])
        b_eps = Buf()
        k.op("gpsimd", lambda e: e.memset(eps6[:], 1e-6), writes=[b_eps])
        cst = sb("cst_sb", [128, 8, 128])
        b_cst = Buf("cst")
        k.dma("sync", cst[:], cst_d, writes=[b_cst])
        eps5 = sb("eps5", [128, 1])
        b_eps5 = Buf()
        k.op("gpsimd", lambda e: e.memset(eps5[:], 1e-5), writes=[b_eps5])
        b_mixA_d, b_mixB_d, b_mixS_d = Buf("mixA_d"), Buf("mixB_d"), Buf("mixS_d")
        valid_sb = sb("valid_sb", [128, 32])
        b_valid = Buf("valid")
        k.dma("sync", valid_sb[:], valid, writes=[b_valid])

        if with_sample:
            C = type("Ctx", (), {})()
            C.nc, C.k, C.S, C.final = nc, k, S, final
            C.psT, C.psB, C.cst, C.b_cst, C.eps6, C.b_eps = psT, psB, cst, b_cst, eps6, b_eps
            C.w_v = w_in.rearrange("(k p) c -> p k c", p=128)
            C.xsT, C.ck, C.cv, C.sst, C.scv, C.sbias_d, C.cwr_d, C.sprm_d = xsT, ck, cv, sst, scv, sbias_d, cwr_d, sprm_d
            C.ps_d, C.cs_d, C.ms_d = ps_d, cs_d, ms_d
            C.knew, C.vnew, C.ssm_s, C.conv_s = knew, vnew, ssm_s, conv_s
            C.mixS_d, C.b_mixS_d = mixS_d, b_mixS_d
            phase_s(C)
        sx = ExitStack()
        xTb = sx.enter_context(nc.sbuf_tensor("xTb", [128, 8, EXT], BF16))
        b_x = [Buf("xTb%d" % c) for c in range(8)]
        xT_v = xT.rearrange("(k p) t -> p k t", p=128)
        for c in range(8):
            k.gload(xTb[:, :, 512 * c:512 * (c + 1)], xT_v[:, :, 512 * c:512 * (c + 1)], writes=[b_x[c]])
        bx_of_tile = lambda ti_nat: b_x[ti_nat // 4]

        def x_bufs(start, step):
            lo, hi = start, start + step * 127
            return [b_x[c] for c in range(lo // 512, hi // 512 + 1)]

        with ExitStack() as sa:
            sba = lambda n, s, dt=F32: sa.enter_context(nc.sbuf_tensor(n, list(s), dt))
            mixA = sba("mixA", [128, 4, HALF], BF16)
            b_mixA = [Buf() for _ in range(4)]
            ETb = sba("ETb", [128, 24, 256], BF16)
            b_ET = Buf("ET")
            btst = sba("btst", [128, 6, 256])
            b_btst = Buf("btst")
            for g in range(4 if stage >= -1 else 0):
                k.dma("sync", btst[:], bt[:, 6 * g:6 * g + 6, :], writes=[b_btst])
                k.op("scalar", lambda e, g=g: e.activation(out=ETb[:, 6 * g:6 * g + 6, :], in_=btst[:], func=AF.Exp),
                     reads=[b_btst], writes=[b_ET])
            QT = sba("QT", [128, 2, HALF], BF16)
            KT = sba("KT", [128, 2, EXT], BF16)
            Vaug = sba("Vaug", [128, 32, 4, 65], BF16)
            acc = sba("acc", [65, 4, HALF])
            wq = sba("wq", [128, 8, 256], BF16)
            wk = sba("wk", [128, 8, 256], BF16)
            wv = sba("wv", [128, 8, 256], BF16)
            b_wq, b_wk, b_wv = Buf("wq"), Buf("wk"), Buf("wv")
            b_QT = [Buf() for _ in range(2)]
            b_KT = [Buf() for _ in range(2)]
            b_V = [Buf() for _ in range(32)]
            b_acc = [Buf() for _ in range(4)]
            b_rrow = Buf()
            stg = [sba("stg%d" % i, [128, 256]) for i in range(2)]
            b_stg = [Buf("stg%d" % i) for i in range(2)]
            exb = [sba("exb%d" % i, [128, 512]) for i in range(2)]
            b_ex = [Buf() for _ in range(2)]
            ptb = [sba("ptb%d" % i, [128, 512], BF16) for i in range(2)]
            b_pt = [Buf() for _ in range(2)]
            w_v = w_in.rearrange("(k p) c -> p k c", p=128)
            ctr = {"ps": 0, "stg": 0, "s": 0, "o": 0, "ev": 0}

            def proj_ps():
                i = ctr["ps"] % 2
                ctr["ps"] += 1
                return psT[i], psB[i]

            for hh2 in range(2):
                k.gload(wq[:], w_v[:, :, COL_QA + 256 * hh2:COL_QA + 256 * hh2 + 256], writes=[b_wq])
                k.gload(wk[:], w_v[:, :, COL_KA + 256 * hh2:COL_KA + 256 * hh2 + 256], writes=[b_wk])
                k.gload(wv[:], w_v[:, :, COL_VA + 256 * hh2:COL_VA + 256 * hh2 + 256], writes=[b_wv])
                for jj in range(2 if stage >= 0 else 0):
                    for tc in range(4 if os.environ.get('KQ','1')=='1' else 0):
                        pt_, pb_ = proj_ps()
                        for kk in range(8):
                            fn = lambda e, kk=kk, jj=jj, tc=tc, pt_=pt_: e.matmul(
                                pt_[:, :], lhsT=wq[:, kk, 128 * jj:128 * jj + 128],
                                rhs=xTb[:, kk, HALF + 512 * tc:HALF + 512 * tc + 512], start=(kk == 0), stop=(kk == 7))
                            if kk == 0:
                                k.op("tensor", fn, reads=[b_wq, b_x[4 + tc]], writes=[pb_])
                            else:
                                k.acc("tensor", fn, reads=[b_wq, b_x[4 + tc]], acc=[pb_])
                        k.op("vector", lambda e, jj=jj, tc=tc, pt_=pt_: e.tensor_scalar(
                            out=QT[:, jj, 512 * tc:512 * tc + 512], in0=pt_[:, :], scalar1=0.125, scalar2=None, op0=ALU.mult),
                            reads=[pb_], writes=[b_QT[jj]])
                    for tc in range(8 if os.environ.get('KK','1')=='1' else 0):
                        pt_, pb_ = proj_ps()
                        for kk in range(8):
                            fn = lambda e, kk=kk, jj=jj, tc=tc, pt_=pt_: e.matmul(
                                pt_[:, :], lhsT=wk[:, kk, 128 * jj:128 * jj + 128],
                                rhs=xTb[:, kk, 512 * tc:512 * tc + 512], start=(kk == 0), stop=(kk == 7))
                            if kk == 0:
                                k.op("tensor", fn, reads=[b_wk, b_x[tc]], writes=[pb_])
                            else:
                                k.acc("tensor", fn, reads=[b_wk, b_x[tc]], acc=[pb_])
                        k.op("vector", lambda e, jj=jj, tc=tc, pt_=pt_: e.tensor_copy(
                            out=KT[:, jj, 512 * tc:512 * tc + 512], in_=pt_[:, :]),
                            reads=[pb_], writes=[b_KT[jj]])
                for ti in range(16, 32 if stage >= -2 else 16):
                    pt_, pb_ = proj_ps()
                    for kk in range(8):
                        fn = lambda e, kk=kk, ti=ti, pt_=pt_: e.matmul(
                            pt_[:, 0:256], lhsT=xTb[:, kk, 128 * ti:128 * ti + 128], rhs=wk[:, kk, :],
                            start=(kk == 0), stop=(kk == 7))
                        if kk == 0:
                            k.op("tensor", fn, reads=[b_wk, b_x[ti // 4]], writes=[pb_])
                        else:
                            k.acc("tensor", fn, reads=[b_wk, b_x[ti // 4]], acc=[pb_])
                    si = ctr["stg"] % 2
                    ctr["stg"] += 1
                    k.op("scalar", lambda e, si=si, pt_=pt_: e.activation(out=stg[si][:], in_=pt_[:, 0:256], func=AF.Copy),
                         reads=[pb_], writes=[b_stg[si]])
                    final.append(k.dma("sync", kwin[128 * (ti - 16):128 * (ti - 16) + 128, 256 * hh2:256 * hh2 + 256],
                                       stg[si][:], reads=[b_stg[si]]))
                for br, (_, dil) in enumerate(BRANCHES):
                    if stage < 1:
                        break
                    k.op("gpsimd", lambda e: e.tensor_copy(
                        out=Vaug[:, :, :, 64], in_=valid_sb[:, :].unsqueeze(2).to_broadcast([128, 32, 4])),
                        reads=[b_valid], writes=b_V)
                    for ti in range(32):
                        start, step = tokset(ti, dil)
                        pt_, pb_ = proj_ps()
                        for kk in range(8):
                            fn = lambda e, kk=kk, start=start, step=step, pt_=pt_: e.matmul(
                                pt_[:, 0:256], lhsT=xTb[:, kk, sl(start, step)], rhs=wv[:, kk, :],
                                start=(kk == 0), stop=(kk == 7))
                            if kk == 0:
                                k.op("tensor", fn, reads=[b_wv] + x_bufs(start, step), writes=[pb_])
                            else:
                                k.acc("tensor", fn, reads=[b_wv] + x_bufs(start, step), acc=[pb_])
                        ev = "vector"
                        src = pt_[:, 0:256].rearrange("p (h d) -> p h d", h=4)
                        if ev == "vector":
                            k.op("vector", lambda e, ti=ti, src=src: e.tensor_copy(out=Vaug[:, ti, :, 0:64], in_=src),
                                 reads=[pb_], writes=[b_V[ti]])
                        else:
                            k.op("scalar", lambda e, ti=ti, src=src: e.activation(out=Vaug[:, ti, :, 0:64], in_=src, func=AF.Copy),
                                 reads=[pb_], writes=[b_V[ti]])
                        if br == 0 and ti >= 16:
                            si = ctr["stg"] % 2
                            ctr["stg"] += 1
                            k.op("vector", lambda e, si=si, pt_=pt_: e.tensor_copy(out=stg[si][:], in_=pt_[:, 0:256]),
                                 reads=[pb_], writes=[b_stg[si]])
                            final.append(k.dma("sync", vwin[128 * (ti - 16):128 * (ti - 16) + 128, 256 * hh2:256 * hh2 + 256],
                                               stg[si][:], reads=[b_stg[si]]))
                    def emit_S(hl, ti0):
                        jj, pb = hl // 2, 64 * (hl % 2)
                        sidx = 2 + ctr["s"] % 2
                        ctr["s"] += 1
                        pS, bS = psT[sidx], psB[sidx]
                        first = True
                        for a in range(2):
                            ti = ti0 + a
                            qs, qstep = tokset(ti, dil)
                            qs -= HALF
                            for kt in range(2):
                                tk = ti - dil * (1 - kt)
                                ks, kstep = tokset(tk, dil)
                                fn = lambda e, a=a, kt=kt, ks=ks, kstep=kstep, qs=qs, qstep=qstep, pS=pS, jj=jj, pb=pb: e.matmul(
                                    pS[:, a * 256 + kt * 128:a * 256 + kt * 128 + 128],
                                    lhsT=KT[pb:pb + 64, jj, sl(ks, kstep)],
                                    rhs=QT[pb:pb + 64, jj, sl(qs, qstep)], start=True, stop=True)
                                if first:
                                    k.op("tensor", fn, reads=[b_KT[jj], b_QT[jj]], writes=[bS])
                                    first = False
                                else:
                                    k.acc("tensor", fn, reads=[b_KT[jj], b_QT[jj]], acc=[bS])
                        return pS, bS

                    def emit_rest(hl, ti0, pS, bS):
                        h = 4 * hh2 + hl
                        jj, pb = hl // 2, 64 * (hl % 2)
                        ei = ctr["ev"] % 2
                        ctr["ev"] += 1
                        k.op("scalar", lambda e, ei=ei, pS=pS: e.activation(out=exb[ei][:], in_=pS[:, :], func=AF.Exp),
                             reads=[bS], writes=[b_ex[ei]])
                        k.op("vector", lambda e, ei=ei, h=h, br=br: e.tensor_tensor(
                            out=ptb[ei][:].rearrange("p (a c) -> p a c", a=2),
                            in0=exb[ei][:].rearrange("p (a c) -> p a c", a=2),
                            in1=ETb[:, h * 3 + br:h * 3 + br + 1, :].to_broadcast([128, 2, 256]), op=ALU.mult),
                            reads=[b_ex[ei], b_ET], writes=[b_pt[ei]])
                        oidx = 4 + ctr["o"] % 2
                        ctr["o"] += 1
                        pO, bO = psT[oidx], psB[oidx]
                        first = True
                        for a in range(2):
                            ti = ti0 + a
                            for kt in range(2):
                                tk = ti - dil * (1 - kt)
                                fn = lambda e, a=a, kt=kt, tk=tk, hl=hl, ei=ei, pO=pO: e.matmul(
                                    pO[0:65, a * 128:a * 128 + 128], lhsT=Vaug[:, tk, hl, 0:65],
                                    rhs=ptb[ei][:, a * 256 + kt * 128:a * 256 + kt * 128 + 128],
                                    start=(kt == 0), stop=(kt == 1))
                                if first:
                                    k.op("tensor", fn, reads=[b_pt[ei], b_V[tk]], writes=[bO])
                                    first = False
                                else:
                                    k.acc("tensor", fn, reads=[b_pt[ei], b_V[tk]], acc=[bO])
                        qs0, qstep = tokset(ti0, dil)
                        qs1, _ = tokset(ti0 + 1, dil)
                        qs0 -= HALF
                        qs1 -= HALF
                        dst = bass.AP(acc, hl * HALF + qs0, [[4 * HALF, 65], [qs1 - qs0, 2], [qstep, 128]])
                        srcO = pO[0:65, 0:256].rearrange("p (a c) -> p a c", a=2)
                        if br == 0:
                            k.op("vector", lambda e, dst=dst, srcO=srcO: e.tensor_copy(out=dst, in_=srcO),
                                 reads=[bO], writes=[b_acc[hl]])
                        else:
                            k.op("vector", lambda e, dst=dst, srcO=srcO: e.tensor_tensor(out=dst, in0=srcO, in1=dst, op=ALU.add),
                                 reads=[bO], writes=[b_acc[hl]])
                    items = [(hl_, t_) for hl_ in range(4) for t_ in range(16, 32, 2)] if stage >= 2 else []
                    cur = emit_S(*items[0]) if items else None
                    for idx_, it_ in enumerate(items):
                        nxt_ = emit_S(*items[idx_ + 1]) if idx_ + 1 < len(items) else None
                        emit_rest(it_[0], it_[1], *cur)
                        cur = nxt_
                if stage < 3:
                    continue
                k.op("vector", lambda e: e.reciprocal(out=acc[64:65, :, :], in_=acc[64:65, :, :]), reads=b_acc, writes=[b_rrow])
                for hl in range(4):
                    jj, pb = hl // 2, 64 * (hl % 2)
                    for c in range(4):
                        pt_, pb_ = psT[6 + c % 2], psB[6 + c % 2]
                        k.op("tensor", lambda e, hl=hl, c=c, pt_=pt_: e.matmul(
                            pt_[0:64, :], lhsT=ones_f[64:65, 0:64], rhs=acc[64:65, hl, 512 * c:512 * c + 512], start=True, stop=True),
                            reads=[b_rrow, b_ones], writes=[pb_])
                        k.op("vector", lambda e, hl=hl, c=c, pt_=pt_, pb=pb, jj=jj, hh2=hh2: e.tensor_tensor(
                            out=mixA[pb:pb + 64, 2 * hh2 + jj, 512 * c:512 * c + 512], in0=acc[0:64, hl, 512 * c:512 * c + 512],
                            in1=pt_[0:64, :], op=ALU.mult), reads=[pb_, b_acc[hl]], writes=[b_mixA[2 * hh2 + jj]])

            if stage >= 3:
                for pp in range(4):
                    k.dma("sync", mixA_d[:, pp], mixA[:, pp], reads=[b_mixA[pp]], writes=[b_mixA_d])
        S.barrier()
        if stage >= 4:
            C = type("Ctx", (), {})()
            C.nc, C.k, C.S, C.final = nc, k, S, final
            C.xTb, C.b_x, C.w_v = xTb, b_x, w_in.rearrange("(k p) c -> p k c", p=128)
            C.psT, C.psB, C.cst, C.b_cst, C.ones_f, C.b_ones = psT, psB, cst, b_cst, ones_f, b_ones
            C.eps6, C.b_eps, C.prm_d = eps6, b_eps, prm_d
            C.kT_d, C.vT_d, C.qT_d, C.gz_d = kT_d, vT_d, qT_d, gz_d
            C.b_kT_d, C.b_vT_d, C.b_qT_d, C.b_gz_d = Buf("kT_d"), Buf("vT_d"), Buf("qT_d"), Buf("gz_d")
            C.mixB_d, C.b_mixB_d, C.ssm_p, C.conv_p = mixB_d, b_mixB_d, ssm_p, conv_p
            phase_b(C)
        sx.close()
        S.barrier()
        if stage >= 5:
            C = type("Ctx", (), {})()
            C.nc, C.k, C.S, C.final = nc, k, S, final
            C.psT, C.psB, C.cst, C.b_cst = psT, psB, cst, b_cst
            C.eps5, C.b_eps5 = eps5, b_eps5
            C.NT = 17 if with_sample else 16
            C.NEXP = nexp
            C.lnp_d, C.w_out, C.wr_d, C.rb_d = lnp_d, w_out, wr_d, rb_d
            C.w_gate, C.w_up, C.w_down = w_gate, w_up, w_down
            C.mixA_d, C.mixB_d, C.b_mixA_d, C.b_mixB_d = mixA_d, mixB_d, b_mixA_d, b_mixB_d
            C.mixS_d, C.b_mixS_d, C.xs_pad, C.xo = mixS_d, b_mixS_d, xs_pad, xo
            C.y_out, C.ys_out = y_out, ys_out
            C.iota_d, C.hb_d, C.b_hb_d, C.ones_f, C.b_ones = iota_d, hb_d, Buf("hb_d"), ones_f, b_ones
            phase_c(C)
        for nm, src_d, bsrc in (("mixA", mixA_d, b_mixA_d), ("mixB", mixB_d, b_mixB_d)):
            if nm in dbg_out:
                dstg_b = sb("dstg_b" + nm, [128, HALF], BF16)
                dstg = sb("dstg" + nm, [128, HALF])
                b_db, b_df = Buf("dstg_b" + nm), Buf("dstg" + nm)
                for pp in range(4):
                    k.dma("sync", dstg_b[:], src_d[:, pp], reads=[bsrc], writes=[b_db])
                    k.op("vector", lambda e, dstg=dstg, dstg_b=dstg_b: e.tensor_copy(out=dstg[:], in_=dstg_b[:]), reads=[b_db], writes=[b_df])
                    final.append(k.dma("sync", dbg_out[nm][:, pp], dstg[:], reads=[b_df]))
        S.finish(final)
    return nc


def phase_b(C):
    nc, k, S = C.nc, C.k, C.S
    xTb, b_x, w_v = C.xTb, C.b_x, C.w_v
    psT, psB = C.psT, C.psB
    cst, b_cst = C.cst, C.b_cst
    ones_f = C.ones_f
    IDENT, NEGID, LMASK, CM0, CM1, NEGM, STRICT = range(7)
    final = C.final
    kT_d, vT_d, qT_d, gz_d = C.kT_d, C.vT_d, C.qT_d, C.gz_d

    with ExitStack() as sB:
        sbb = lambda n, s, dt=F32: sB.enter_context(nc.sbuf_tensor(n, list(s), dt))
        prm = sbb("prm_sb", [128, 8 + 128 + 48])
        b_prm = Buf("prm")
        k.dma("sync", prm[:], C.prm_d, writes=[b_prm])
        identb = sbb("identb", [128, 128], BF16)
        b_identb = Buf()
        k.op("vector", lambda e: e.tensor_copy(out=identb[:], in_=cst[:, IDENT, :]), reads=[b_cst], writes=[b_identb])
        convp_sb = sbb("convp_sb", [128, 12, 3])
        b_convp = Buf("convp")

        ab_sb = sbb("ab_sb", [128, 32, 8])
        b_ab = Buf()
        with ExitStack() as s1:
            sb1 = lambda n, s, dt=F32: s1.enter_context(nc.sbuf_tensor(n, list(s), dt))
            wab = sb1("wab", [128, 8, 8], BF16)
            wz = sb1("wz", [128, 8, 512], BF16)
            b_wab, b_wz = Buf("wab"), Buf("wz")
            k.gload(wab[:], w_v[:, :, COL_AB:COL_AB + 8], writes=[b_wab])
            k.gload(wz[:], w_v[:, :, COL_ZB:COL_ZB + 512], writes=[b_wz])
            zst = [sb1("zst%d" % i, [128, 512]) for i in range(2)]
            b_zst = [Buf() for _ in range(2)]
            gzb = [sb1("gzb%d" % i, [128, 512], BF16) for i in range(2)]
            b_gzb = [Buf("gzb%d" % i) for i in range(2)]
            for ti in range(32):
                pt_, pb_ = psT[ti % 2], psB[ti % 2]
                for kk in range(8):
                    fn = lambda e, kk=kk, ti=ti, pt_=pt_: e.matmul(pt_[:, 0:8], lhsT=xTb[:, kk, 128 * ti:128 * ti + 128],
                                                                 rhs=wab[:, kk, :], start=(kk == 0), stop=(kk == 7))
                    if kk == 0:
                        k.op("tensor", fn, reads=[b_wab, b_x[ti // 4]], writes=[pb_])
                    else:
                        k.acc("tensor", fn, reads=[b_wab, b_x[ti // 4]], acc=[pb_])
                k.op("vector", lambda e, ti=ti, pt_=pt_: e.tensor_copy(out=ab_sb[:, ti, :], in_=pt_[:, 0:8]), reads=[pb_], writes=[b_ab])
            for ti in range(16, 32):
                i2 = ti % 2
                pt_, pb_ = psT[2 + i2], psB[2 + i2]
                for kk in range(8):
                    fn = lambda e, kk=kk, ti=ti, pt_=pt_: e.matmul(pt_[:, :], lhsT=xTb[:, kk, 128 * ti:128 * ti + 128],
                                                                 rhs=wz[:, kk, :], start=(kk == 0), stop=(kk == 7))
                    if kk == 0:
                        k.op("tensor", fn, reads=[b_wz, b_x[ti // 4]], writes=[pb_])
                    else:
                        k.acc("tensor", fn, reads=[b_wz, b_x[ti // 4]], acc=[pb_])
                k.op("scalar", lambda e, i2=i2, pt_=pt_: e.activation(out=zst[i2][:], in_=pt_[:, :], func=AF.Silu), reads=[pb_], writes=[b_zst[i2]])
                k.op("gpsimd", lambda e, i2=i2: e.tensor_tensor(
                    out=gzb[i2][:].rearrange("p (h e) -> p h e", h=4), in0=zst[i2][:].rearrange("p (h e) -> p h e", h=4),
                    in1=prm[:, 8:136].unsqueeze(1).to_broadcast([128, 4, 128]), op=ALU.mult),
                    reads=[b_zst[i2], b_prm], writes=[b_gzb[i2]])
                k.dma("sync", gz_d[ti - 16], gzb[i2][:], reads=[b_gzb[i2]], writes=[C.b_gz_d])

        S.barrier()
        gt = sbb("gt", [128, 12, 128])
        b_gt = [Buf() for _ in range(12)]
        G, BETA, GC, EGC, ETAIL, BEGE, EGL0, EGL1, GCL, TMP, NEGA, TMP2 = range(12)
        v3 = lambda idx: gt[:, idx, :].rearrange("p (t h) -> p t h", h=4)
        k.op("scalar", lambda e: e.activation(out=gt[:, NEGA, 0:4], in_=prm[:, 0:4], func=AF.Exp), reads=[b_prm], writes=[b_gt[NEGA]])
        k.op("vector", lambda e: e.tensor_tensor(out=v3(TMP), in0=ab_sb[:, :, 0:4], in1=prm[:, 4:8].unsqueeze(1).to_broadcast([128, 32, 4]), op=ALU.add),
             reads=[b_ab, b_prm], writes=[b_gt[TMP]])
        k.op("scalar", lambda e: e.activation(out=gt[:, TMP, :], in_=gt[:, TMP, :], func=AF.Exp), reads=[b_gt[TMP]], writes=[b_gt[TMP]])
        k.op("scalar", lambda e: e.activation(out=gt[:, TMP, :], in_=gt[:, TMP, :], func=AF.Ln, bias=1.0), reads=[b_gt[TMP]], writes=[b_gt[TMP]])
        k.op("vector", lambda e: e.scalar_tensor_tensor(out=v3(G), in0=v3(TMP), scalar=-1.0, in1=gt[:, NEGA, 0:4].unsqueeze(1).to_broadcast([128, 32, 4]),
                                                       op0=ALU.mult, op1=ALU.mult), reads=[b_gt[TMP], b_gt[NEGA]], writes=[b_gt[G]])
        k.op("scalar", lambda e: e.activation(out=v3(BETA), in_=ab_sb[:, :, 4:8], func=AF.Sigmoid), reads=[b_ab], writes=[b_gt[BETA]])
        for (mask, dst, bank) in ((LMASK, GC, 0), (CM0, EGL0, 1), (CM1, EGL1, 2)):
            k.op("tensor", lambda e, mask=mask, bank=bank: e.matmul(psT[bank][:, 0:128], lhsT=cst[:, mask, :], rhs=gt[:, G, :], start=True, stop=True),
                 reads=[b_cst, b_gt[G]], writes=[psB[bank]])
            k.op("vector", lambda e, dst=dst, bank=bank: e.tensor_copy(out=gt[:, dst, :], in_=psT[bank][:, 0:128]), reads=[psB[bank]], writes=[b_gt[dst]])
        k.op("vector", lambda e: e.tensor_copy(out=gt[0:64, GCL, :], in_=gt[0:64, EGL0, :]), reads=[b_gt[EGL0]], writes=[b_gt[GCL]])
        k.op("vector", lambda e: e.tensor_copy(out=gt[64:128, GCL, :], in_=gt[64:128, EGL1, :]), reads=[b_gt[EGL1], b_gt[GCL]], writes=[b_gt[GCL]])
        k.op("vector", lambda e: e.tensor_tensor(out=gt[:, TMP2, :], in0=gt[:, GCL, :], in1=gt[:, GC, :], op=ALU.subtract),
             reads=[b_gt[GCL], b_gt[GC]], writes=[b_gt[TMP2]])
        k.op("scalar", lambda e: e.activation(out=gt[:, ETAIL, :], in_=gt[:, TMP2, :], func=AF.Exp), reads=[b_gt[TMP2]], writes=[b_gt[ETAIL]])
        k.op("scalar", lambda e: e.activation(out=gt[:, EGC, :], in_=gt[:, GC, :], func=AF.Exp), reads=[b_gt[GC]], writes=[b_gt[EGC]])
        k.op("scalar", lambda e: e.activation(out=gt[:, EGL0, :], in_=gt[:, EGL0, :], func=AF.Exp), reads=[b_gt[EGL0], b_gt[GCL]], writes=[b_gt[EGL0]])
        k.op("scalar", lambda e: e.activation(out=gt[:, EGL1, :], in_=gt[:, EGL1, :], func=AF.Exp), reads=[b_gt[EGL1], b_gt[GCL]], writes=[b_gt[EGL1]])
        k.op("vector", lambda e: e.tensor_tensor(out=gt[:, BEGE, :], in0=gt[:, BETA, :], in1=gt[:, EGC, :], op=ALU.mult),
             reads=[b_gt[BETA], b_gt[EGC]], writes=[b_gt[BEGE]])

        with ExitStack() as s2:
            sb2 = lambda n, s, dt=F32: s2.enter_context(nc.sbuf_tensor(n, list(s), dt))
            wu = [sb2("wu%d" % i, [128, 8, 128], BF16) for i in range(2)]
            b_wu = [Buf("wu%d" % i) for i in range(2)]
            ub = [sb2("ub%d" % i, [128, 515]) for i in range(4)]
            b_ub = [Buf() for _ in range(4)]
            cb = [sb2("cb%d" % i, [128, 512]) for i in range(4)]
            b_cb = [Buf() for _ in range(4)]
            sq = [sb2("sq%d" % i, [128, 512]) for i in range(4)]
            b_sq = [Buf() for _ in range(4)]
            rt = [sb2("rt%d" % i, [128, 512]) for i in range(4)]
            b_rt = [Buf() for _ in range(4)]
            ob = [sb2("ob%d" % i, [128, 512], BF16) for i in range(4)]
            b_ob = [Buf("ob%d" % i) for i in range(4)]
            cnt = 0
            pendY = []
            for th in range(12):
                ty, hb = divmod(th, 4)
                wi = th % 2
                c0 = COL_UB + 128 * th
                k.gload(wu[wi][:], w_v[:, :, c0:c0 + 128], writes=[b_wu[wi]])
                chunks = range(4, 8) if ty == 0 else range(8)
                dst_d = (qT_d, kT_d, vT_d)[ty]
                b_dst = (C.b_qT_d, C.b_kT_d, C.b_vT_d)[ty]
                cw = lambda i, th=th: prm[:, 136 + 4 * th + i:136 + 4 * th + i + 1]
                first = True
                for tc in chunks:
                    ci = cnt % 4
                    cnt += 1
                    pt_, pb_ = psT[ci], psB[ci]
                    if first:
                        if ty == 0:
                            hp, hpb = psT[(ci + 1) % 4], psB[(ci + 1) % 4]
                            for kk in range(8):
                                fn = lambda e, kk=kk, wi=wi, hp=hp: e.matmul(hp[:, 0:4], lhsT=wu[wi][:, kk, :], rhs=xTb[:, kk, HALF - 4:HALF],
                                                                            start=(kk == 0), stop=(kk == 7))
                                if kk == 0:
                                    k.op("tensor", fn, reads=[b_wu[wi], b_x[3]], writes=[hpb])
                                else:
                                    k.acc("tensor", fn, reads=[b_wu[wi], b_x[3]], acc=[hpb])
                            k.op("vector", lambda e, ci=ci, hp=hp: e.tensor_copy(out=ub[ci][:, 0:3], in_=hp[:, 1:4]), reads=[hpb], writes=[b_ub[ci]])
                        else:
                            k.op("vector", lambda e, ci=ci: e.memset(ub[ci][:, 0:3], 0.0), writes=[b_ub[ci]])
                        first = False
                    else:
                        k.op("vector", lambda e, ci=ci: e.tensor_copy(out=ub[ci][:, 0:3], in_=ub[(ci - 1) % 4][:, 512:515]),
                             reads=[b_ub[(ci - 1) % 4]], writes=[b_ub[ci]])
                    for kk in range(8):
                        fn = lambda e, kk=kk, wi=wi, tc=tc, pt_=pt_: e.matmul(pt_[:, :], lhsT=wu[wi][:, kk, :], rhs=xTb[:, kk, 512 * tc:512 * tc + 512],
                                                                            start=(kk == 0), stop=(kk == 7))
                        if kk == 0:
                            k.op("tensor", fn, reads=[b_wu[wi], b_x[tc]], writes=[pb_])
                        else:
                            k.acc("tensor", fn, reads=[b_wu[wi], b_x[tc]], acc=[pb_])
                    k.op("scalar", lambda e, ci=ci, pt_=pt_: e.activation(out=ub[ci][:, 3:515], in_=pt_[:, :], func=AF.Copy), reads=[pb_], writes=[b_ub[ci]])
                    if tc == 7:
                        k.op("gpsimd", lambda e, ci=ci, th=th: e.tensor_copy(out=convp_sb[:, th, :], in_=ub[ci][:, 512:515]), reads=[b_ub[ci]], writes=[b_convp])
                    k.op("vector", lambda e, ci=ci, cw=cw: e.tensor_scalar(out=cb[ci][:], in0=ub[ci][:, 3:515], scalar1=cw(3), scalar2=None, op0=ALU.mult),
                         reads=[b_ub[ci], b_prm], writes=[b_cb[ci]])
                    for i in range(3):
                        k.op("vector", lambda e, ci=ci, cw=cw, i=i: e.scalar_tensor_tensor(out=cb[ci][:], in0=ub[ci][:, i:i + 512], scalar=cw(i), in1=cb[ci][:],
                                                                                       op0=ALU.mult, op1=ALU.add), reads=[b_ub[ci], b_cb[ci]], writes=[b_cb[ci]])
                    k.op("scalar", lambda e, ci=ci: e.activation(out=cb[ci][:], in_=cb[ci][:], func=AF.Silu), reads=[b_cb[ci]], writes=[b_cb[ci]])
                    if ty == 2:
                        k.op("gpsimd", lambda e, ci=ci: e.tensor_copy(out=ob[ci][:], in_=cb[ci][:]), reads=[b_cb[ci]], writes=[b_ob[ci]])
                    else:
                        k.op("gpsimd", lambda e, ci=ci: e.tensor_tensor(out=sq[ci][:], in0=cb[ci][:], in1=cb[ci][:], op=ALU.mult), reads=[b_cb[ci]], writes=[b_sq[ci]])
                        np_, npb = psT[4 + ci], psB[4 + ci]
                        k.op("tensor", lambda e, ci=ci, np_=np_: e.matmul(np_[:, :], lhsT=ones_f[:], rhs=sq[ci][:], start=True, stop=True),
                             reads=[b_sq[ci], C.b_ones], writes=[npb])
                        k.op("scalar", lambda e, ci=ci, np_=np_: e.activation(out=rt[ci][:], in_=np_[:, :], func=AF.Sqrt, bias=C.eps6[:, 0:1]),
                             reads=[npb, C.b_eps], writes=[b_rt[ci]])
                    t0 = 512 * tc - (HALF if ty == 0 else 0)
                    sc = (128.0 ** -0.5) if ty == 0 else 1.0

                    def tailY(ci=ci, ty=ty, sc=sc, dst_d=dst_d, b_dst=b_dst, hb=hb, t0=t0):
                        if ty != 2:
                            k.op("vector", lambda e, ci=ci: e.reciprocal(out=rt[ci][:], in_=rt[ci][:]), reads=[b_rt[ci]], writes=[b_rt[ci]])
                            k.op("vector", lambda e, ci=ci, sc=sc: e.scalar_tensor_tensor(out=ob[ci][:], in0=cb[ci][:], scalar=sc, in1=rt[ci][:], op0=ALU.mult, op1=ALU.mult),
                                 reads=[b_cb[ci], b_rt[ci]], writes=[b_ob[ci]])
                        k.dma("sync", dst_d[:, hb, t0:t0 + 512], ob[ci][:], reads=[b_ob[ci]], writes=[b_dst])
                    pendY.append(tailY)
                    if len(pendY) > 2:
                        pendY.pop(0)()
            while pendY:
                pendY.pop(0)()
            convp_v = C.conv_p.rearrange("r (c p) -> p c r", p=128)
            for th in range(12):
                final.append(k.dma("sync", convp_v[:, th, :], convp_sb[:, th, :], reads=[b_convp], slot="convp", allow_slow_non_contiguous=True))

        S.barrier()
        sC = sB
        sbc = lambda n, s, dt=F32: sC.enter_context(nc.sbuf_tensor(n, list(s), dt))
        f_slots = [(psT[b][:, 0:128], psB[b]) for b in range(6)]
        psbf = [psT[6].bitcast(BF16), psT[7].bitcast(BF16)]
        h_slots = [(psbf[b][:, 0:128], psB[6 + b]) for b in range(2)]
        cnts = {"f": 0, "h": 0}

        def fslot():
            s_ = f_slots[cnts["f"] % len(f_slots)]
            cnts["f"] += 1
            return s_

        def hslot():
            s_ = h_slots[cnts["h"] % len(h_slots)]
            cnts["h"] += 1
            return s_

        class Pool_:
            def __init__(self, name, n, dt):
                self.t = [sbc("%s%d" % (name, i), [128, 128], dt) for i in range(n)]
                self.b = [Buf() for _ in range(n)]
                self.i = 0

            def get(self):
                j = self.i % len(self.t)
                self.i += 1
                return self.t[j], self.b[j]

        PF = Pool_("pf", 40, F32)
        PH = Pool_("ph", 96, BF16)
        Sst = [sbc("Sst%d" % h, [128, 128]) for h in range(4)]
        Sbf = [sbc("Sbf%d" % h, [128, 128], BF16) for h in range(4)]
        b_S = [Buf() for _ in range(4)]
        b_Sb = [Buf() for _ in range(4)]
        for h in range(4):
            k.op("vector", lambda e, h=h: e.memset(Sst[h][:], 0.0), writes=[b_S[h]])
            k.op("vector", lambda e, h=h: e.memset(Sbf[h][:], 0.0), writes=[b_Sb[h]])
        kt_t = [sbc("kt_t%d" % i, [128, 4, 128], BF16) for i in range(2)]
        vt_t = [sbc("vt_t%d" % i, [128, 4, 128], BF16) for i in range(2)]
        qt_t = [sbc("qt_t%d" % i, [128, 4, 128], BF16) for i in range(2)]
        gz_t = [sbc("gz_t%d" % i, [128, 512], BF16) for i in range(2)]
        b_kt = [Buf("kt_t%d" % i) for i in range(2)]
        b_vt = [Buf("vt_t%d" % i) for i in range(2)]
        b_qt = [Buf("qt_t%d" % i) for i in range(2)]
        b_gzt = [Buf("gz_t%d" % i) for i in range(2)]
        Ukeep = [[sbc("Uk%d_%d" % (i, h), [128, 128]) for h in range(4)] for i in range(2)]
        WTkeep = [[sbc("WTk%d_%d" % (i, h), [128, 128], BF16) for h in range(4)] for i in range(2)]
        ktlkeep = [[sbc("ktlk%d_%d" % (i, h), [128, 128], BF16) for h in range(4)] for i in range(2)]
        qkTkeep = [[sbc("qkTk%d_%d" % (i, h), [128, 128], BF16) for h in range(4)] for i in range(2)]
        b_Uk = [[Buf() for h in range(4)] for i in range(2)]
        b_WTk = [[Buf() for h in range(4)] for i in range(2)]
        b_ktlk = [[Buf() for h in range(4)] for i in range(2)]
        b_qkTk = [[Buf() for h in range(4)] for i in range(2)]
        ss = sbc("ss", [128, 8])
        b_ss = [Buf() for _ in range(8)]
        junk = sbc("junk", [128, 128])
        b_junk = Buf()
        mxs = [sbc("mxs%d" % i, [128, 4, 128], BF16) for i in range(2)]
        b_mxs = [Buf("mxs%d" % i) for i in range(2)]
        gt_ = gt
        col = lambda idx, c_: gt_[:, idx, c_:c_ + 1]

        def make_tile(ti):
            own = ti >= 16
            bi = ti % 2
            k.dma("sync", kt_t[bi][:], kT_d[:, :, 128 * ti:128 * ti + 128], reads=[C.b_kT_d], writes=[b_kt[bi]])
            k.dma("sync", vt_t[bi][:], vT_d[:, :, 128 * ti:128 * ti + 128], reads=[C.b_vT_d], writes=[b_vt[bi]])
            if own:
                k.dma("sync", qt_t[bi][:], qT_d[:, :, 128 * (ti - 16):128 * (ti - 16) + 128], reads=[C.b_qT_d], writes=[b_qt[bi]])
                k.dma("sync", gz_t[bi][:], gz_d[ti - 16], reads=[C.b_gz_d], writes=[b_gzt[bi]])
            HS = [None] * 4
            def stage1(hb):
                c_ = ti * 4 + hb
                kT = kt_t[bi][:, hb, :]
                vT = vt_t[bi][:, hb, :]
                qT = qt_t[bi][:, hb, :]
                rd_k, rd_v, rd_q = [b_kt[bi]], [b_vt[bi]], [b_qt[bi]]
                nd, b_nd = PF.get()
                k.op("gpsimd", lambda e, nd=nd, c_=c_: e.tensor_scalar(out=nd[:], in0=cst[:, NEGID, :], scalar1=col(GC, c_), scalar2=None, op0=ALU.mult),
                     reads=[b_cst, b_gt[GC]], writes=[b_nd])
                pD, bD = fslot()
                yield
                k.op("tensor", lambda e, pD=pD, nd=nd: e.matmul(pD, lhsT=ones_f[:], rhs=nd[:], start=True, stop=False), reads=[b_nd, C.b_ones], writes=[bD])
                k.acc("tensor", lambda e, pD=pD: e.matmul(pD, lhsT=cst[:, IDENT, :], rhs=cst[:, NEGM, :], start=False, stop=True), reads=[b_cst], acc=[bD])
                Ec, b_Ec = PF.get()
                k.op("scalar", lambda e, Ec=Ec, pD=pD, c_=c_: e.activation(out=Ec[:], in_=pD, func=AF.Exp, bias=col(GC, c_)),
                     reads=[bD, b_gt[GC]], writes=[b_Ec])
                Es, b_Es = PF.get()
                k.op("gpsimd", lambda e, Es=Es, Ec=Ec: e.tensor_tensor(out=Es[:], in0=Ec[:], in1=cst[:, STRICT, :], op=ALU.mult),
                     reads=[b_Ec, b_cst], writes=[b_Es])
                pK, bK = fslot()
                yield
                k.op("tensor", lambda e, pK=pK, kT=kT: e.matmul(pK, lhsT=kT, rhs=kT, start=True, stop=True), reads=rd_k, writes=[bK])
                A, b_A = PH.get()
                k.op("vector", lambda e, A=A, pK=pK, Es=Es, c_=c_: e.scalar_tensor_tensor(out=A[:], in0=pK, scalar=col(BETA, c_), in1=Es[:], op0=ALU.mult, op1=ALU.mult),
                     reads=[bK, b_Es, b_gt[BETA]], writes=[b_A])
                pT_, bT_ = hslot()
                yield
                k.op("tensor", lambda e, pT_=pT_, A=A: e.transpose(pT_, A[:], identb[:]), reads=[b_A, b_identb], writes=[bT_])
                Bm, b_Bm = PH.get()
                k.op("vector", lambda e, Bm=Bm, pT_=pT_: e.tensor_copy(out=Bm[:], in_=pT_), reads=[bT_], writes=[b_Bm])
                P, b_P = PH.get()
                k.op("vector", lambda e, P=P, pT_=pT_: e.tensor_tensor(out=P[:], in0=cst[:, IDENT, :], in1=pT_, op=ALU.subtract), reads=[bT_, b_cst], writes=[b_P])
                X, b_X, Y, b_Y = A, b_A, Bm, b_Bm
                for m in range(1, 6):
                    pX, bX = fslot()
                    yield
                    k.op("tensor", lambda e, pX=pX, X=X, Y=Y: e.matmul(pX, lhsT=Y[:], rhs=X[:], start=True, stop=True), reads=[b_X, b_Y], writes=[bX])
                    Xn, b_Xn = PH.get()
                    if os.environ.get("ACTEV", "0") == "1":
                        k.op("scalar", lambda e, Xn=Xn, pX=pX: e.activation(out=Xn[:], in_=pX, func=AF.Copy), reads=[bX], writes=[b_Xn])
                    else:
                        k.op("vector", lambda e, Xn=Xn, pX=pX: e.tensor_copy(out=Xn[:], in_=pX), reads=[bX], writes=[b_Xn])
                    if m < 5:
                        pY, bY = fslot()
                        yield
                        k.op("tensor", lambda e, pY=pY, X=X, Y=Y: e.matmul(pY, lhsT=X[:], rhs=Y[:], start=True, stop=True), reads=[b_X, b_Y], writes=[bY])
                        Yn, b_Yn = PH.get()
                        if os.environ.get("ACTEV", "0") == "1":
                            k.op("scalar", lambda e, Yn=Yn, pY=pY: e.activation(out=Yn[:], in_=pY, func=AF.Copy), reads=[bY], writes=[b_Yn])
                        else:
                            k.op("vector", lambda e, Yn=Yn, pY=pY: e.tensor_copy(out=Yn[:], in_=pY), reads=[bY], writes=[b_Yn])
                    pP, bP = fslot()
                    yield
                    k.op("tensor", lambda e, pP=pP, Xn=Xn, P=P: e.matmul(pP, lhsT=Xn[:], rhs=P[:], start=True, stop=True), reads=[b_Xn, b_P], writes=[bP])
                    Pn, b_Pn = PH.get()
                    k.op("vector", lambda e, Pn=Pn, pP=pP, P=P: e.tensor_tensor(out=Pn[:], in0=pP, in1=P[:], op=ALU.add), reads=[bP, b_P], writes=[b_Pn])
                    P, b_P = Pn, b_Pn
                    X, b_X = Xn, b_Xn
                    if m < 5:
                        Y, b_Y = Yn, b_Yn
                pk_, bk_ = hslot()
                yield
                k.op("tensor", lambda e, pk_=pk_, kT=kT: e.transpose(pk_, kT, identb[:]), reads=rd_k + [b_identb], writes=[bk_])
                Rw, b_Rw = PH.get()
                k.op("vector", lambda e, Rw=Rw, pk_=pk_, c_=c_: e.tensor_scalar(out=Rw[:], in0=pk_, scalar1=col(BEGE, c_), scalar2=None, op0=ALU.mult),
                     reads=[bk_, b_gt[BEGE]], writes=[b_Rw])
                ktl, b_ktl = ktlkeep[bi][hb], b_ktlk[bi][hb]
                k.op("vector", lambda e, ktl=ktl, pk_=pk_, c_=c_: e.tensor_scalar(out=ktl[:], in0=pk_, scalar1=col(ETAIL, c_), scalar2=None, op0=ALU.mult),
                     reads=[bk_, b_gt[ETAIL]], writes=[b_ktl])
                pv_, bv_ = hslot()
                yield
                k.op("tensor", lambda e, pv_=pv_, vT=vT: e.transpose(pv_, vT, identb[:]), reads=rd_v + [b_identb], writes=[bv_])
                Ru, b_Ru = PH.get()
                k.op("vector", lambda e, Ru=Ru, pv_=pv_, c_=c_: e.tensor_scalar(out=Ru[:], in0=pv_, scalar1=col(BETA, c_), scalar2=None, op0=ALU.mult),
                     reads=[bv_, b_gt[BETA]], writes=[b_Ru])
                pU, bU = fslot()
                yield
                k.op("tensor", lambda e, pU=pU, P=P, Ru=Ru: e.matmul(pU, lhsT=P[:], rhs=Ru[:], start=True, stop=True), reads=[b_P, b_Ru], writes=[bU])
                U, b_U = Ukeep[bi][hb], b_Uk[bi][hb]
                k.op("vector", lambda e, U=U, pU=pU: e.tensor_copy(out=U[:], in_=pU), reads=[bU], writes=[b_U])
                pW, bW = fslot()
                yield
                k.op("tensor", lambda e, pW=pW, P=P, Rw=Rw: e.matmul(pW, lhsT=Rw[:], rhs=P[:], start=True, stop=True), reads=[b_P, b_Rw], writes=[bW])
                WT, b_WT = WTkeep[bi][hb], b_WTk[bi][hb]
                k.op("vector", lambda e, WT=WT, pW=pW: e.tensor_copy(out=WT[:], in_=pW), reads=[bW], writes=[b_WT])
                qkT = b_qkT = None
                if own:
                    pQ, bQ = fslot()
                    yield
                    k.op("tensor", lambda e, pQ=pQ, qT=qT, kT=kT: e.matmul(pQ, lhsT=qT, rhs=kT, start=True, stop=True), reads=rd_q + rd_k, writes=[bQ])
                    qk, b_qk = PH.get()
                    k.op("vector", lambda e, qk=qk, pQ=pQ, Ec=Ec: e.tensor_tensor(out=qk[:], in0=pQ, in1=Ec[:], op=ALU.mult), reads=[bQ, b_Ec], writes=[b_qk])
                    pq2, bq2 = hslot()
                    yield
                    k.op("tensor", lambda e, pq2=pq2, qk=qk: e.transpose(pq2, qk[:], identb[:]), reads=[b_qk, b_identb], writes=[bq2])
                    qkT, b_qkT = qkTkeep[bi][hb], b_qkTk[bi][hb]
                    k.op("vector", lambda e, qkT=qkT, pq2=pq2: e.tensor_copy(out=qkT[:], in_=pq2), reads=[bq2], writes=[b_qkT])
                HS[hb] = (dict(U=U, b_U=b_U, WT=WT, b_WT=b_WT, ktl=ktl, b_ktl=b_ktl, qkT=qkT, b_qkT=b_qkT, qT=qT, rd_q=rd_q, c_=c_))
            def stage23():
                outs = []
                if own:
                    for hb in range(4):
                        o_, b_o = PF.get()
                        outs.append((o_, b_o))
                for ch in range(2):
                    r0 = 64 * ch
                    for hb in range(4):
                        H = HS[hb]
                        c_ = H["c_"]
                        yield
                        pV, bV = fslot()
                        k.op("tensor", lambda e, pV=pV, H=H, hb=hb, r0=r0: e.matmul(pV[r0:r0 + 64, :], lhsT=H["WT"][:, r0:r0 + 64], rhs=Sbf[hb][:], start=True, stop=True),
                             reads=[H["b_WT"], b_Sb[hb]], writes=[bV])
                        vn, b_vn = PH.get()
                        k.op("vector", lambda e, vn=vn, pV=pV, H=H, r0=r0: e.tensor_tensor(out=vn[r0:r0 + 64, :], in0=H["U"][r0:r0 + 64, :], in1=pV[r0:r0 + 64, :], op=ALU.subtract),
                             reads=[bV, H["b_U"]], writes=[b_vn])
                        if own:
                            o_, b_o = outs[hb]
                            yield
                            p1, b1 = fslot()
                            k.op("tensor", lambda e, p1=p1, H=H, hb=hb, r0=r0: e.matmul(p1[r0:r0 + 64, :], lhsT=H["qT"][:, r0:r0 + 64], rhs=Sbf[hb][:], start=True, stop=True),
                                 reads=H["rd_q"] + [b_Sb[hb]], writes=[b1])
                            p2, b2 = fslot()
                            k.op("tensor", lambda e, p2=p2, H=H, vn=vn, r0=r0: e.matmul(p2[r0:r0 + 64, :], lhsT=H["qkT"][r0:r0 + 64, r0:r0 + 64], rhs=vn[r0:r0 + 64, :], start=True, stop=True),
                                 reads=[H["b_qkT"], b_vn], writes=[b2])
                            o2, b_o2 = PF.get()
                            k.op("vector", lambda e, o2=o2, p2=p2, r0=r0: e.tensor_copy(out=o2[r0:r0 + 64, :], in_=p2[r0:r0 + 64, :]), reads=[b2], writes=[b_o2])
                            k.op("vector", lambda e, o_=o_, p1=p1, o2=o2, r0=r0, c_=c_: e.scalar_tensor_tensor(
                                out=o_[r0:r0 + 64, :], in0=p1[r0:r0 + 64, :], scalar=gt_[r0:r0 + 64, EGC, c_:c_ + 1], in1=o2[r0:r0 + 64, :], op0=ALU.mult, op1=ALU.add),
                                reads=[b1, b_o2, b_gt[EGC]], writes=[b_o])
                        yield
                        pS, bS_ = fslot()
                        k.op("tensor", lambda e, pS=pS, H=H, vn=vn, r0=r0: e.matmul(pS, lhsT=H["ktl"][r0:r0 + 64, :], rhs=vn[r0:r0 + 64, :], start=True, stop=True),
                             reads=[H["b_ktl"], b_vn], writes=[bS_])
                        egl = EGL0 if ch == 0 else EGL1
                        k.op("vector", lambda e, pS=pS, hb=hb, egl=egl, c_=c_: e.scalar_tensor_tensor(
                            out=Sst[hb][:], in0=Sst[hb][:], scalar=col(egl, c_), in1=pS, op0=ALU.mult, op1=ALU.add),
                            reads=[bS_, b_gt[egl], b_S[hb]], writes=[b_S[hb]])
                        k.op("scalar", lambda e, hb=hb: e.activation(out=Sbf[hb][:], in_=Sst[hb][:], func=AF.Copy), reads=[b_S[hb]], writes=[b_Sb[hb]])
                if own:
                    for hb in range(4):
                        o_, b_o = outs[hb]
                        si = (ti * 4 + hb) % 8
                        yield
                        k.op("scalar", lambda e, o_=o_, si=si: e.activation(out=junk[:], in_=o_[:], func=AF.Square, accum_out=ss[:, si:si + 1]),
                             reads=[b_o], writes=[b_junk, b_ss[si]])
                        k.op("scalar", lambda e, si=si: e.activation(out=ss[:, si:si + 1], in_=ss[:, si:si + 1], func=AF.Sqrt, scale=1.0 / 128.0, bias=C.eps6[:, 0:1]),
                             reads=[b_ss[si], C.b_eps], writes=[b_ss[si]])
                        k.op("vector", lambda e, si=si: e.reciprocal(out=ss[:, si:si + 1], in_=ss[:, si:si + 1]), reads=[b_ss[si]], writes=[b_ss[si]])
                        on, b_on = PH.get()
                        k.op("vector", lambda e, on=on, o_=o_, si=si, hb=hb, bi=bi: e.scalar_tensor_tensor(
                            out=on[:], in0=o_[:], scalar=ss[:, si:si + 1], in1=gz_t[bi][:, 128 * hb:128 * hb + 128], op0=ALU.mult, op1=ALU.mult),
                            reads=[b_o, b_ss[si], b_gzt[bi]], writes=[b_on])
                        yield
                        pm, bm = hslot()
                        k.op("tensor", lambda e, pm=pm, on=on: e.transpose(pm, on[:], identb[:]), reads=[b_on, b_identb], writes=[bm])
                        k.op("vector", lambda e, pm=pm, hb=hb, bi=bi: e.tensor_copy(out=mxs[bi][:, hb, :], in_=pm),
                             reads=[bm], writes=[b_mxs[bi]])
                    k.dma("sync", C.mixB_d[:, :, 128 * (ti - 16):128 * (ti - 16) + 128], mxs[bi][:], reads=[b_mxs[bi]], writes=[C.b_mixB_d])
            return [stage1(hb) for hb in range(4)], stage23


        pending = None
        for ti in range(33):
            gens = []
            nxt = None
            if ti < 32:
                s1, nxt = make_tile(ti)
                gens += s1
            if pending is not None:
                gens.append(pending())
            while gens:
                for g_ in list(gens):
                    try:
                        next(g_)
                    except StopIteration:
                        gens.remove(g_)
            pending = nxt
        for hb in range(4):
            final.append(k.dma("sync", C.ssm_p[hb], Sst[hb][:], reads=[b_S[hb]], slot="ssmp"))

def phase_c(C):
    nc, k, S = C.nc, C.k, C.S
    psT, psB = C.psT, C.psB
    cst, b_cst = C.cst, C.b_cst
    IDENT = 0
    final = C.final
    NT = C.NT
    NTOK = NT * 128
    ALPHA = 2.0 ** 0.25
    KC = int(os.environ.get('KC', '99'))

    with ExitStack() as sC:
        sbc = lambda n, s, dt=F32: sC.enter_context(nc.sbuf_tensor(n, list(s), dt))
        Mg = sbc("Mg", [128, NT, 4])
        b_Mg = Buf()
        Ghl = sbc("Ghl", [128, NT, 4, 16], BF16)
        b_Ghl = Buf()
        identb = sbc("identbC", [128, 128], BF16)
        b_identb = Buf()
        k.op("vector", lambda e: e.tensor_copy(out=identb[:], in_=cst[:, IDENT, :]), reads=[b_cst], writes=[b_identb])
        yacc = sbc("yacc", [128, NT, 1024])
        b_y = [Buf() for _ in range(NT)]
        G = sbc("G", [128, NT, 32])
        b_G = [Buf() for _ in range(NT)]
        st6_c3 = sbc("st6b", [128, 2, 2, 6])
        mv_c3 = sbc("mvb", [128, 2, 2])

        with ExitStack() as s1:
            sb1 = lambda n, s, dt=F32: s1.enter_context(nc.sbuf_tensor(n, list(s), dt))
            lnp = sb1("lnp_sb", [128, 2, 1024])
            b_lnp = Buf("lnp")
            k.dma("sync", lnp[:], C.lnp_d[:, 0:2, :], writes=[b_lnp])
            wo = sb1("wo", [128, 8, 1024], BF16)
            b_wo = Buf("wo")
            k.gload(wo[:], C.w_out.rearrange("(k p) c -> p k c", p=128), writes=[b_wo])
            wr = sb1("wr_sb", [128, 8, 36])
            b_wr = Buf("wr")
            k.dma("sync", wr[:], C.wr_d.rearrange("(k p) c -> p k c", p=128), writes=[b_wr])
            rb = sb1("rb_sb", [128, 36])
            b_rb = Buf("rb")
            k.dma("sync", rb[:], C.rb_d, writes=[b_rb])
            mx = [sb1("mx%d" % i, [128, 8, 128], BF16) for i in range(2)]
            b_mx = [Buf("mx%d" % i) for i in range(2)]
            xt = [sb1("xt%d" % i, [128, 1024]) for i in range(2)]
            b_xt = [Buf("xt%d" % i) for i in range(2)]
            rr = [sb1("rr%d" % i, [128, 1024]) for i in range(2)]
            b_rr = [Buf() for _ in range(2)]
            hh = [sb1("hh%d" % i, [128, 1024]) for i in range(2)]
            b_hh = [Buf() for _ in range(2)]
            hbb = [sb1("hbb%d" % i, [128, 1024], BF16) for i in range(2)]
            b_hbb = [Buf("hbb%d" % i) for i in range(2)]
            hTf = [sb1("hTf%d" % i, [128, 8, 128]) for i in range(2)]
            b_hTf = [Buf() for _ in range(2)]
            st6 = sb1("st6", [128, 2, 2, 6])
            mv = sb1("mv", [128, 2, 2])
            b_st = [Buf() for _ in range(2)]
            b_mv = [Buf() for _ in range(2)]
            tmpr = sb1("tmpr", [128, 2, 32])
            b_tmp = [Buf() for _ in range(2)]
            sm = sb1("sm", [128, 2, 96])
            b_sm = [Buf() for _ in range(2)]
            for ti in range(NT if KC >= 6 else 0):
                bi = ti % 2
                is_s = ti >= 16
                if not is_s:
                    k.dma("sync", mx[bi][:, 0:4, :], C.mixA_d[:, :, 128 * ti:128 * ti + 128], reads=[C.b_mixA_d], writes=[b_mx[bi]])
                    k.dma("sync", mx[bi][:, 4:8, :], C.mixB_d[:, :, 128 * ti:128 * ti + 128], reads=[C.b_mixB_d], writes=[b_mx[bi]])
                    k.dma("sync", xt[bi][:], C.xo[128 * ti:128 * ti + 128, :], writes=[b_xt[bi]])
                else:
                    k.dma("sync", mx[bi][:], C.mixS_d, reads=[C.b_mixS_d], writes=[b_mx[bi]])
                    k.dma("sync", xt[bi][:], C.xs_pad, writes=[b_xt[bi]])
                for half in range(2 if KC >= 7 else 0):
                    pt_, pb_ = psT[half], psB[half]
                    for kk in range(8):
                        fn = lambda e, kk=kk, bi=bi, half=half, pt_=pt_: e.matmul(pt_[:, :], lhsT=mx[bi][:, kk, :], rhs=wo[:, kk, 512 * half:512 * half + 512],
                                                                                start=(kk == 0), stop=(kk == 7))
                        if kk == 0:
                            k.op("tensor", fn, reads=[b_mx[bi], b_wo], writes=[pb_])
                        else:
                            k.acc("tensor", fn, reads=[b_mx[bi], b_wo], acc=[pb_])
                    if KC < 8:
                        continue
                    k.op("vector", lambda e, bi=bi, half=half, pt_=pt_: e.scalar_tensor_tensor(
                        out=rr[bi][:, 512 * half:512 * half + 512], in0=xt[bi][:, 512 * half:512 * half + 512], scalar=ALPHA, in1=pt_[:, :],
                        op0=ALU.mult, op1=ALU.add), reads=[pb_, b_xt[bi]], writes=[b_rr[bi]])
                    k.op("vector", lambda e, bi=bi, half=half: e.bn_stats(out=st6[:, bi, half, :], in_=rr[bi][:, 512 * half:512 * half + 512]),
                         reads=[b_rr[bi]], writes=[b_st[bi]])
                if KC < 11:
                    continue
                k.op("vector", lambda e, bi=bi: e.bn_aggr(out=mv[:, bi, :], in_=st6[:, bi, :, :].rearrange("p a b -> p (a b)")), reads=[b_st[bi]], writes=[b_mv[bi]])
                k.op("scalar", lambda e, bi=bi: e.activation(out=mv[:, bi, 1:2], in_=mv[:, bi, 1:2], func=AF.Sqrt, bias=C.eps5[:, 0:1]), reads=[b_mv[bi], C.b_eps5], writes=[b_mv[bi]])
                k.op("vector", lambda e, bi=bi: e.reciprocal(out=mv[:, bi, 1:2], in_=mv[:, bi, 1:2]), reads=[b_mv[bi]], writes=[b_mv[bi]])
                k.op("vector", lambda e, bi=bi: e.tensor_scalar(out=hh[bi][:], in0=rr[bi][:], scalar1=mv[:, bi, 0:1], scalar2=mv[:, bi, 1:2], op0=ALU.subtract, op1=ALU.mult),
                     reads=[b_rr[bi], b_mv[bi]], writes=[b_hh[bi]])
                k.op("gpsimd", lambda e, bi=bi: e.tensor_tensor(out=hh[bi][:], in0=hh[bi][:], in1=lnp[:, 0, :], op=ALU.mult), reads=[b_hh[bi], b_lnp], writes=[b_hh[bi]])
                k.op("gpsimd", lambda e, bi=bi: e.tensor_tensor(out=hh[bi][:], in0=hh[bi][:], in1=lnp[:, 1, :], op=ALU.add), reads=[b_hh[bi], b_lnp], writes=[b_hh[bi]])
                k.op("gpsimd", lambda e, bi=bi, ti=ti: e.tensor_scalar(out=yacc[:, ti, :], in0=hh[bi][:], scalar1=ALPHA, scalar2=None, op0=ALU.mult),
                     reads=[b_hh[bi]], writes=[b_y[ti]])
                if KC < 12:
                    continue
                k.op("gpsimd", lambda e, bi=bi: e.tensor_copy(out=hbb[bi][:], in_=hh[bi][:]), reads=[b_hh[bi]], writes=[b_hbb[bi]])
                k.dma("sync", C.hb_d[128 * ti:128 * ti + 128, :], hbb[bi][:], reads=[b_hbb[bi]], writes=[C.b_hb_d])
                for g4 in range(2):
                    pt_, pb_ = psT[2 + g4], psB[2 + g4]
                    for j in range(4):
                        kk = 4 * g4 + j
                        fn = lambda e, kk=kk, j=j, bi=bi, pt_=pt_: e.transpose(pt_[:, 128 * j:128 * j + 128], hh[bi][:, 128 * kk:128 * kk + 128], cst[:, IDENT, :])
                        if j == 0:
                            k.op("tensor", fn, reads=[b_hh[bi], b_cst], writes=[pb_])
                        else:
                            k.acc("tensor", fn, reads=[b_hh[bi], b_cst], acc=[pb_])
                    k.op("vector", lambda e, g4=g4, bi=bi, pt_=pt_: e.tensor_copy(out=hTf[bi][:, 4 * g4:4 * g4 + 4, :], in_=pt_[:, :].rearrange("p (a c) -> p a c", a=4)),
                         reads=[pb_], writes=[b_hTf[bi]])
                if KC < 13:
                    continue
                pl, plb = psT[4], psB[4]
                for kk in range(8):
                    fn = lambda e, kk=kk, bi=bi: e.matmul(pl[:, 0:36], lhsT=hTf[bi][:, kk, :], rhs=wr[:, kk, :], start=(kk == 0), stop=(kk == 7))
                    if kk == 0:
                        k.op("tensor", fn, reads=[b_hTf[bi], b_wr], writes=[plb])
                    else:
                        k.acc("tensor", fn, reads=[b_hTf[bi], b_wr], acc=[plb])
                if KC < 14:
                    continue
                R = lambda a, b_, bi=bi: sm[:, bi, a:b_]
                T3 = tmpr[:, bi, :].rearrange("p (g e) -> p g e", g=4)
                sm_b, tmp_b = b_sm[bi], b_tmp[bi]

                def VV(eng, method, reads, writes, **aps):
                    k.op(eng, lambda e, aps=aps, method=method: getattr(e, method)(**aps), reads=reads, writes=writes)
                VV("vector", "tensor_tensor", [plb, b_rb], [sm_b], out=R(0, 36), in0=pl[:, 0:36], in1=rb[:], op=ALU.add)
                VV("vector", "tensor_reduce", [sm_b], [sm_b], out=R(36, 37), in_=R(0, 4), axis=AX.X, op=ALU.max)
                VV("vector", "tensor_scalar", [sm_b], [sm_b], out=R(40, 44), in0=R(0, 4), scalar1=R(36, 37), scalar2=None, op0=ALU.is_equal)
                VV("vector", "tensor_scalar", [sm_b], [sm_b], out=R(37, 38), in0=R(36, 37), scalar1=-1.0, scalar2=None, op0=ALU.mult)
                VV("scalar", "activation", [sm_b], [sm_b], out=R(89, 93), in_=R(0, 4), func=AF.Exp, bias=R(37, 38), accum_out=R(38, 39))
                VV("vector", "reciprocal", [sm_b], [sm_b], out=R(38, 39), in_=R(38, 39))
                VV("vector", "tensor_tensor", [sm_b], [tmp_b], out=T3, in0=R(4, 36).rearrange("p (g e) -> p g e", g=4),
                   in1=R(40, 44).unsqueeze(2).to_broadcast([128, 4, 8]), op=ALU.mult)
                VV("vector", "tensor_reduce", [tmp_b], [sm_b], out=R(44, 52), in_=T3.rearrange("p g e -> p e g"), axis=AX.X, op=ALU.add)
                VV("vector", "tensor_reduce", [sm_b], [sm_b], out=R(52, 53), in_=R(44, 52), axis=AX.X, op=ALU.max)
                VV("vector", "tensor_scalar", [sm_b], [sm_b], out=R(54, 62), in0=R(44, 52), scalar1=R(52, 53), scalar2=None, op0=ALU.is_equal)
                VV("vector", "scalar_tensor_tensor", [sm_b], [sm_b], out=R(62, 70), in0=R(54, 62), scalar=-1e30, in1=R(44, 52), op0=ALU.mult, op1=ALU.add)
                VV("vector", "tensor_reduce", [sm_b], [sm_b], out=R(53, 54), in_=R(62, 70), axis=AX.X, op=ALU.max)
                VV("vector", "tensor_scalar", [sm_b], [sm_b], out=R(70, 78), in0=R(62, 70), scalar1=R(53, 54), scalar2=None, op0=ALU.is_equal)
                VV("vector", "tensor_tensor", [sm_b], [sm_b], out=R(78, 79), in0=R(53, 54), in1=R(52, 53), op=ALU.subtract)
                VV("scalar", "activation", [sm_b], [sm_b], out=R(78, 79), in_=R(78, 79), func=AF.Exp)
                VV("vector", "tensor_scalar", [sm_b], [sm_b], out=R(79, 80), in0=R(78, 79), scalar1=1.0, scalar2=None, op0=ALU.add)
                VV("vector", "reciprocal", [sm_b], [sm_b], out=R(79, 80), in_=R(79, 80))
                VV("vector", "tensor_tensor", [sm_b], [sm_b], out=R(80, 81), in0=R(78, 79), in1=R(79, 80), op=ALU.mult)
                VV("vector", "tensor_scalar", [sm_b], [sm_b], out=R(79, 81), in0=R(79, 81), scalar1=R(38, 39), scalar2=None, op0=ALU.mult)
                VV("vector", "tensor_scalar", [sm_b], [sm_b], out=R(81, 89), in0=R(54, 62), scalar1=R(79, 80), scalar2=None, op0=ALU.mult)
                VV("vector", "scalar_tensor_tensor", [sm_b], [sm_b], out=R(81, 89), in0=R(70, 78), scalar=R(80, 81), in1=R(81, 89), op0=ALU.mult, op1=ALU.add)
                VV("vector", "tensor_tensor", [sm_b], [b_G[ti]], out=G[:, ti, :].rearrange("p (g e) -> p g e", g=4),
                   in0=R(40, 44).unsqueeze(2).to_broadcast([128, 4, 8]), in1=R(81, 89).unsqueeze(1).to_broadcast([128, 4, 8]), op=ALU.mult)
                G3 = G[:, ti, :].rearrange("p (g e) -> p g e", g=4)
                VV("vector", "tensor_copy", [sm_b], [b_Mg], out=Mg[:, ti, :], in_=R(40, 44))
                VV("vector", "tensor_copy", [b_G[ti]], [b_Ghl], out=Ghl[:, ti, :, 0:8], in_=G3)
                VV("vector", "tensor_tensor", [b_G[ti], b_Ghl], [tmp_b], out=T3, in0=G3, in1=Ghl[:, ti, :, 0:8], op=ALU.subtract)
                VV("vector", "tensor_copy", [tmp_b], [b_Ghl], out=Ghl[:, ti, :, 8:16], in_=T3)
        S.barrier()

        with ExitStack() as s2:
            sb2 = lambda n, s, dt=F32: s2.enter_context(nc.sbuf_tensor(n, list(s), dt))
            NST = CAP // 128
            iota = sb2("iota_sb", [128, CAP])
            b_iota = Buf("iota_sb")
            k.dma("sync", iota[:], C.iota_d, writes=[b_iota])
            Mcum = sb2("Mcum", [128, NT, 4])
            b_Mc = Buf()
            slotf = sb2("slotf", [128, NT])
            b_sl = Buf()
            t4 = sb2("t4", [128, 4])
            b_t4 = Buf()

            def VV(eng, method, reads, writes, **aps):
                return k.op(eng, lambda e, aps=aps, method=method: getattr(e, method)(**aps), reads=reads, writes=writes)
            for ti in range(NT):
                if ti == 0:
                    VV("vector", "tensor_copy", [b_Mg], [b_Mc], out=Mcum[:, 0, :], in_=Mg[:, 0, :])
                else:
                    VV("vector", "tensor_tensor", [b_Mg, b_Mc], [b_Mc], out=Mcum[:, ti, :], in0=Mcum[:, ti - 1, :], in1=Mg[:, ti, :], op=ALU.add)
            for ti in range(NT):
                pr, prb = psT[ti % 2], psB[ti % 2]
                k.op("tensor", lambda e, ti=ti, pr=pr: e.matmul(pr[:, 0:4], lhsT=cst[:, 7, :], rhs=Mg[:, ti, :], start=True, stop=(ti == 0)),
                     reads=[b_cst, b_Mg], writes=[prb])
                if ti > 0:
                    k.acc("tensor", lambda e, ti=ti, pr=pr: e.matmul(pr[:, 0:4], lhsT=C.ones_f[:], rhs=Mcum[:, ti - 1, :], start=False, stop=True),
                          reads=[C.b_ones, b_Mc], acc=[prb])
                VV("vector", "tensor_tensor", [prb, b_Mg], [b_t4], out=t4[:], in0=pr[:, 0:4], in1=Mg[:, ti, :], op=ALU.mult)
                VV("vector", "tensor_reduce", [b_t4], [b_sl], out=slotf[:, ti:ti + 1], in_=t4[:], axis=AX.X, op=ALU.add)

            Sel = sb2("Sel", [128, NT, CAP], BF16)
            b_Sel = Buf()
            hTg = sb2("hTg", [128, 8, CAP], BF16)
            b_hTg = Buf()
            Yg = sb2("Yg", [128, NST, 1024])
            b_Yg = [Buf() for _ in range(NST)]
            Ygs = sb2("Ygs", [128, NST, 1024], BF16)
            b_Ygs = Buf()
            t16 = sb2("t16", [128, 16])
            b_t16 = Buf()
            Gs = sb2("Gs", [128, NST, 8])
            b_Gs = Buf()
            hbt = [sb2("hbt%d" % i, [128, 1024], BF16) for i in range(3)]
            b_hbt = [Buf("hbt%d" % i) for i in range(3)]
            selT = [sb2("selT%d" % i, [128, 128], BF16) for i in range(4)]
            b_selT = [Buf() for _ in range(4)]
            wg = [sb2("wg%d" % i, [128, 8, 512], BF16) for i in range(2)]
            wu_ = [sb2("wup%d" % i, [128, 8, 512], BF16) for i in range(2)]
            wd = [sb2("wd%d" % i, [128, 4, 1024], BF16) for i in range(2)]
            b_wg = [Buf("wg%d" % i) for i in range(2)]
            b_wu = [Buf("wup%d" % i) for i in range(2)]
            b_wd = [Buf("wd%d" % i) for i in range(2)]
            sg = [sb2("sg%d" % i, [128, 512]) for i in range(2)]
            b_sg = [Buf() for _ in range(2)]
            act = sb2("act", [128, 4, 512], BF16)
            b_act = Buf()
            psbf = psT[7].bitcast(BF16)
            chunks = [(0, 512), (512, CAP - 512)]
            fcnt = 0
            hcnt = 0
            tcnt = 0
            for g in range(4):
                for ti in range(NT):
                    VV("vector", "tensor_scalar", [b_iota, b_sl, b_Mg], [b_Sel], out=Sel[:, ti, :], in0=iota[:], scalar1=slotf[:, ti:ti + 1],
                       scalar2=Mg[:, ti, g:g + 1], op0=ALU.is_equal, op1=ALU.mult)
                for ps_ in range(2):
                    for ti in range(NT):
                        hi = hcnt % 3
                        hcnt += 1
                        k.dma("sync", hbt[hi][:], C.hb_d[128 * ti:128 * ti + 128, :], reads=[C.b_hb_d], writes=[b_hbt[hi]])
                        for j in range(4):
                            kk = 4 * ps_ + j
                            for (bank, c0, cn) in ((j, 0, 512), (4 + j, 512, CAP - 512)):
                                fn = lambda e, bank=bank, hi=hi, kk=kk, ti=ti, c0=c0, cn=cn: e.matmul(
                                    psT[bank][:, 0:cn], lhsT=hbt[hi][:, 128 * kk:128 * kk + 128], rhs=Sel[:, ti, c0:c0 + cn], start=(ti == 0), stop=(ti == NT - 1))
                                if ti == 0:
                                    k.op("tensor", fn, reads=[b_hbt[hi], b_Sel], writes=[psB[bank]])
                                else:
                                    k.acc("tensor", fn, reads=[b_hbt[hi], b_Sel], acc=[psB[bank]])
                    for j in range(4):
                        kk = 4 * ps_ + j
                        VV("vector", "tensor_copy", [psB[j]], [b_hTg], out=hTg[:, kk, 0:512], in_=psT[j][:, 0:512])
                        VV("vector", "tensor_copy", [psB[4 + j]], [b_hTg], out=hTg[:, kk, 512:CAP], in_=psT[4 + j][:, 0:CAP - 512])
                for st in range(NST):
                    pg_, pgb_ = psT[st % 2], psB[st % 2]
                    for ti in range(NT):
                        fn = lambda e, st=st, ti=ti, g=g, pg_=pg_: e.matmul(pg_[:, 0:16], lhsT=Sel[:, ti, 128 * st:128 * st + 128], rhs=Ghl[:, ti, g, :],
                                                                           start=(ti == 0), stop=(ti == NT - 1))
                        if ti == 0:
                            k.op("tensor", fn, reads=[b_Sel, b_Ghl], writes=[pgb_])
                        else:
                            k.acc("tensor", fn, reads=[b_Sel, b_Ghl], acc=[pgb_])
                    VV("vector", "tensor_copy", [pgb_], [b_t16], out=t16[:], in_=pg_[:, 0:16])
                    VV("vector", "tensor_tensor", [b_t16], [b_Gs], out=Gs[:, st, :], in0=t16[:, 0:8], in1=t16[:, 8:16], op=ALU.add)
                for e8 in range(8):
                    ex = 8 * g + e8
                    wi = ex % 2
                    k.gload(wg[wi][:], C.w_gate[ex].rearrange("(k p) f -> p k f", p=128), writes=[b_wg[wi]])
                    k.gload(wu_[wi][:], C.w_up[ex].rearrange("(k p) f -> p k f", p=128), writes=[b_wu[wi]])
                    k.gload(wd[wi][:], C.w_down[ex].rearrange("(k p) d -> p k d", p=128), writes=[b_wd[wi]])
                    for (t0, tn) in (chunks if os.environ.get('KSKIPX', '0') == '0' else []):
                        for f in range(4):
                            pg, pgb = psT[0 + fcnt % 2], psB[0 + fcnt % 2]
                            pu, pub = psT[2 + fcnt % 2], psB[2 + fcnt % 2]
                            si = fcnt % 2
                            fcnt += 1
                            for (pp, ppb, ww, bw) in ((pg, pgb, wg[wi], b_wg[wi]), (pu, pub, wu_[wi], b_wu[wi])):
                                for kk in range(8):
                                    fn = lambda e, kk=kk, pp=pp, ww=ww, f=f, t0=t0, tn=tn: e.matmul(pp[:, 0:tn], lhsT=ww[:, kk, 128 * f:128 * f + 128], rhs=hTg[:, kk, t0:t0 + tn],
                                                                                                start=(kk == 0), stop=(kk == 7))
                                    if kk == 0:
                                        k.op("tensor", fn, reads=[bw, b_hTg], writes=[ppb])
                                    else:
                                        k.acc("tensor", fn, reads=[bw, b_hTg], acc=[ppb])
                            k.op("scalar", lambda e, si=si, pg=pg, tn=tn: e.activation(out=sg[si][:, 0:tn], in_=pg[:, 0:tn], func=AF.Silu), reads=[pgb], writes=[b_sg[si]])
                            k.op("vector", lambda e, si=si, f=f, pu=pu, tn=tn: e.tensor_tensor(out=act[:, f, 0:tn], in0=sg[si][:, 0:tn], in1=pu[:, 0:tn], op=ALU.mult),
                                 reads=[b_sg[si], pub], writes=[b_act])
                        for tt in range(tn // 128):
                            st = t0 // 128 + tt
                            for half in range(2):
                                py, pyb = psT[4 + (tt * 2 + half) % 4], psB[4 + (tt * 2 + half) % 4]
                                for f in range(4):
                                    fn = lambda e, f=f, tt=tt, half=half, py=py, wi=wi: e.matmul(py[:, :], lhsT=act[:, f, 128 * tt:128 * tt + 128],
                                                                                             rhs=wd[wi][:, f, 512 * half:512 * half + 512], start=(f == 0), stop=(f == 3))
                                    if f == 0:
                                        k.op("tensor", fn, reads=[b_act, b_wd[wi]], writes=[pyb])
                                    else:
                                        k.acc("tensor", fn, reads=[b_act, b_wd[wi]], acc=[pyb])
                                dsty = Yg[:, st, 512 * half:512 * half + 512]
                                if e8 == 0:
                                    VV("vector", "tensor_scalar", [pyb, b_Gs], [b_Yg[st]], out=dsty, in0=py[:, :], scalar1=Gs[:, st, e8:e8 + 1], scalar2=None, op0=ALU.mult)
                                else:
                                    VV("vector", "scalar_tensor_tensor", [pyb, b_Gs, b_Yg[st]], [b_Yg[st]], out=dsty, in0=py[:, :], scalar=Gs[:, st, e8:e8 + 1], in1=dsty,
                                       op0=ALU.mult, op1=ALU.add)
                for st in range(NST):
                    VV("gpsimd", "tensor_copy", [b_Yg[st]], [b_Ygs], out=Ygs[:, st, :], in_=Yg[:, st, :])
                for ti in range(NT):
                    pa = [(psT[0], psB[0]), (psT[1], psB[1])] if ti % 2 == 0 else [(psT[2], psB[2]), (psT[3], psB[3])]
                    for st in range(NST):
                        sti = tcnt % 4
                        tcnt += 1
                        pt_b = psbf[:, 128 * sti:128 * sti + 128]
                        k.op("tensor", lambda e, pt_b=pt_b, ti=ti, st=st: e.transpose(pt_b, Sel[:, ti, 128 * st:128 * st + 128], identb[:]),
                             reads=[b_Sel, b_identb], writes=[psB[7]])
                        VV("vector", "tensor_copy", [psB[7]], [b_selT[sti]], out=selT[sti][:], in_=pt_b)
                        for half in range(2):
                            fn = lambda e, half=half, sti=sti, st=st, pa=pa: e.matmul(pa[half][0][:, :], lhsT=selT[sti][:], rhs=Ygs[:, st, 512 * half:512 * half + 512],
                                                                                 start=(st == 0), stop=(st == NST - 1))
                            if st == 0:
                                k.op("tensor", fn, reads=[b_selT[sti], b_Ygs], writes=[pa[half][1]])
                            else:
                                k.acc("tensor", fn, reads=[b_selT[sti], b_Ygs], acc=[pa[half][1]])
                    for half in range(2):
                        dy = yacc[:, ti, 512 * half:512 * half + 512]
                        VV("vector", "tensor_tensor", [pa[half][1], b_y[ti]], [b_y[ti]], out=dy, in0=pa[half][0][:, :], in1=dy, op=ALU.add)
        S.barrier()

        with ExitStack() as s3:
            sb3 = lambda n, s, dt=F32: s3.enter_context(nc.sbuf_tensor(n, list(s), dt))
            lnp3 = sb3("lnp_sb3", [128, 2, 1024])
            b_lnp3 = Buf("lnp3")
            k.dma("sync", lnp3[:], C.lnp_d[:, 2:4, :], writes=[b_lnp3])
            st6, mv = st6_c3, mv_c3
            b_st = [Buf() for _ in range(2)]
            b_mv = [Buf() for _ in range(2)]
            yo = [sb3("yo%d" % i, [128, 1024]) for i in range(2)]
            b_yo = [Buf("yo%d" % i) for i in range(2)]
            for ti in range(NT if KC >= 30 else 0):
                bi = ti % 2
                for half in range(2):
                    k.op("vector", lambda e, bi=bi, half=half, ti=ti: e.bn_stats(out=st6[:, bi, half, :], in_=yacc[:, ti, 512 * half:512 * half + 512]),
                         reads=[b_y[ti]], writes=[b_st[bi]])
                k.op("vector", lambda e, bi=bi: e.bn_aggr(out=mv[:, bi, :], in_=st6[:, bi, :, :].rearrange("p a b -> p (a b)")), reads=[b_st[bi]], writes=[b_mv[bi]])
                k.op("scalar", lambda e, bi=bi: e.activation(out=mv[:, bi, 1:2], in_=mv[:, bi, 1:2], func=AF.Sqrt, bias=C.eps5[:, 0:1]), reads=[b_mv[bi], C.b_eps5], writes=[b_mv[bi]])
                k.op("vector", lambda e, bi=bi: e.reciprocal(out=mv[:, bi, 1:2], in_=mv[:, bi, 1:2]), reads=[b_mv[bi]], writes=[b_mv[bi]])
                k.op("vector", lambda e, bi=bi, ti=ti: e.tensor_scalar(out=yo[bi][:], in0=yacc[:, ti, :], scalar1=mv[:, bi, 0:1], scalar2=mv[:, bi, 1:2], op0=ALU.subtract, op1=ALU.mult),
                     reads=[b_y[ti], b_mv[bi]], writes=[b_yo[bi]])
                k.op("gpsimd", lambda e, bi=bi: e.tensor_tensor(out=yo[bi][:], in0=yo[bi][:], in1=lnp3[:, 0, :], op=ALU.mult), reads=[b_yo[bi], b_lnp3], writes=[b_yo[bi]])
                k.op("gpsimd", lambda e, bi=bi: e.tensor_tensor(out=yo[bi][:], in0=yo[bi][:], in1=lnp3[:, 1, :], op=ALU.add), reads=[b_yo[bi], b_lnp3], writes=[b_yo[bi]])
                if ti < 16:
                    final.append(k.dma("sync", C.y_out[128 * ti:128 * ti + 128, :], yo[bi][:], reads=[b_yo[bi]]))
                else:
                    final.append(k.dma("sync", C.ys_out, yo[bi][0:NS, :], reads=[b_yo[bi]]))


def phase_s(C):
    nc, k, S = C.nc, C.k, C.S
    psT, psB = C.psT, C.psB
    final = C.final
    ps_d, cs_d, ms_d = C.ps_d, C.cs_d, C.ms_d
    b_ps_d, b_cs_d, b_ms_d = Buf("ps_d"), Buf("cs_d"), Buf("ms_d")
    w_v = C.w_v

    def VV(eng, method, reads, writes, **aps):
        return k.op(eng, lambda e, aps=aps, method=method: getattr(e, method)(**aps), reads=reads, writes=writes)

    with ExitStack() as s1:
        sb1 = lambda n, s, dt=F32: s1.enter_context(nc.sbuf_tensor(n, list(s), dt))
        xsTb = sb1("xsTb", [128, 8, NS], BF16)
        b_xs = Buf("xsTb")
        k.gload(xsTb[:], C.xsT.rearrange("(k p) t -> p k t", p=128), writes=[b_xs])
        wS = [sb1("wS%d" % i, [128, 8, 512], BF16) for i in range(2)]
        b_wS = [Buf("wS%d" % i) for i in range(2)]
        p_s = sb1("p_s", [NS, IN_COLS])
        b_p = Buf("p_s")
        for cchunk in range(8):
            c0 = 512 * cchunk
            cn = min(512, IN_COLS - c0)
            wi = cchunk % 2
            k.gload(wS[wi][:, :, 0:cn], w_v[:, :, c0:c0 + cn], writes=[b_wS[wi]])
            pt_, pb_ = psT[wi], psB[wi]
            for kk in range(8):
                fn = lambda e, kk=kk, wi=wi, cn=cn, pt_=pt_: e.matmul(pt_[0:NS, 0:cn], lhsT=xsTb[:, kk, :], rhs=wS[wi][:, kk, 0:cn], start=(kk == 0), stop=(kk == 7))
                if kk == 0:
                    k.op("tensor", fn, reads=[b_xs, b_wS[wi]], writes=[pb_])
                else:
                    k.acc("tensor", fn, reads=[b_xs, b_wS[wi]], acc=[pb_])
            VV("vector", "tensor_copy", [pb_], [b_p], out=p_s[:, c0:c0 + cn], in_=pt_[0:NS, 0:cn])
        k.dma("sync", ps_d, p_s[:], reads=[b_p], writes=[b_ps_d])
        final.append(k.dma("sync", C.knew, p_s[:, COL_KA:COL_KA + 512], reads=[b_p], slot="knew"))
        final.append(k.dma("sync", C.vnew, p_s[:, COL_VA:COL_VA + 512], reads=[b_p], slot="vnew"))
        final.append(k.dma("sync", C.conv_s[:, 2, :], p_s[:, COL_UB:COL_UB + 1536], reads=[b_p], slot="convs"))
        cst_ = sb1("cst_", [NS, 3, 1536])
        b_cst_ = Buf("cst_")
        k.dma("sync", cst_[:], C.scv, writes=[b_cst_])
        final.append(k.dma("sync", C.conv_s[:, 0:2, :], cst_[:, 1:3, :], reads=[b_cst_], slot="convs"))
        cwr = sb1("cwr_sb", [NS, 4, 1536])
        b_cwr = Buf("cwr")
        k.dma("sync", cwr[:], C.cwr_d, writes=[b_cwr])
        cacc = sb1("cacc", [NS, 1536])
        ctmp = sb1("ctmp", [NS, 1536])
        b_ca, b_ct = Buf("cacc"), Buf()
        VV("vector", "tensor_tensor", [b_p, b_cwr], [b_ca], out=cacc[:], in0=p_s[:, COL_UB:COL_UB + 1536], in1=cwr[:, 3, :], op=ALU.mult)
        for i in range(3):
            VV("vector", "tensor_tensor", [b_cst_, b_cwr], [b_ct], out=ctmp[:], in0=cst_[:, i, :], in1=cwr[:, i, :], op=ALU.mult)
            VV("vector", "tensor_tensor", [b_ct, b_ca], [b_ca], out=cacc[:], in0=cacc[:], in1=ctmp[:], op=ALU.add)
        VV("scalar", "activation", [b_ca], [b_ca], out=cacc[:], in_=cacc[:], func=AF.Silu)
        k.dma("sync", cs_d, cacc[:], reads=[b_ca], writes=[b_cs_d])
    S.barrier()

    with ExitStack() as s2:
        sb2 = lambda n, s, dt=F32: s2.enter_context(nc.sbuf_tensor(n, list(s), dt))
        qkv = sb2("qkv_nh", [128, 3, 64])
        b_qkv = Buf("qkv_nh")
        for j, c0 in enumerate((COL_QA, COL_KA, COL_VA)):
            k.dma("sync", qkv[:, j, :], bass.AP(ps_d.tensor, c0, [[IN_COLS, NS], [64, 8], [1, 64]]), reads=[b_ps_d], writes=[b_qkv])
        sbias = sb2("sbias_sb", [128, 3, 129])
        b_sb = Buf("sbias")
        k.dma("sync", sbias[:], C.sbias_d, writes=[b_sb])
        Kb = sb2("Kb", [128, 128, 64])
        Vb = sb2("Vb", [128, 128, 64])
        b_Kb, b_Vb = Buf("Kb"), Buf("Vb")
        tmpS = sb2("tmpS", [128, 128, 64])
        b_tmp = Buf()
        sc = sb2("sc", [128, 3, 129])
        b_sc = Buf()
        sm = sb2("smS", [128, 16])
        b_sm = Buf()
        oacc = sb2("oacc", [128, 64])
        otmp = sb2("otmp", [128, 64])
        b_oa, b_ot = Buf("oacc"), Buf()
        VV("vector", "tensor_tensor", [b_qkv], [b_ot], out=otmp[:], in0=qkv[:, 0, :], in1=qkv[:, 1, :], op=ALU.mult)
        VV("vector", "tensor_reduce", [b_ot], [b_sm], out=sm[:, 0:1], in_=otmp[:], axis=AX.X, op=ALU.add)
        for br, (_, dil) in enumerate(BRANCHES):
            for n in range(NS):
                src = bass.AP(C.ck.tensor, n * 2048 * 512 + (2048 - 128 * dil) * 512, [[64, 8], [dil * 512, 128], [1, 64]])
                k.dma("sync", Kb[8 * n:8 * n + 8, :, :], src, writes=[b_Kb]) if n == 0 else k.S.dmaop(
                    "sync", "Kb", lambda e, n=n, src=src: e.dma_start(out=Kb[8 * n:8 * n + 8, :, :], in_=src), [])
            b_Kb.writer = ("D", "Kb", k.S.dma_sems["Kb"][1])
            VV("vector", "tensor_tensor", [b_Kb, b_qkv], [b_tmp], out=tmpS[:], in0=Kb[:], in1=qkv[:, 0, :].unsqueeze(1).to_broadcast([128, 128, 64]), op=ALU.mult)
            VV("vector", "tensor_reduce", [b_tmp], [b_sc], out=sc[:, br, 0:128], in_=tmpS[:], axis=AX.X, op=ALU.add)
            VV("vector", "tensor_copy", [b_sm], [b_sc], out=sc[:, br, 128:129], in_=sm[:, 0:1])
        VV("vector", "scalar_tensor_tensor", [b_sc, b_sb], [b_sc], out=sc[:].rearrange("p a b -> p (a b)"), in0=sc[:].rearrange("p a b -> p (a b)"),
           scalar=0.125, in1=sbias[:].rearrange("p a b -> p (a b)"), op0=ALU.mult, op1=ALU.add)
        VV("vector", "tensor_reduce", [b_sc], [b_sm], out=sm[:, 1:2], in_=sc[:].rearrange("p a b -> p (a b)"), axis=AX.X, op=ALU.max)
        VV("vector", "tensor_scalar", [b_sm], [b_sm], out=sm[:, 2:3], in0=sm[:, 1:2], scalar1=-1.0, scalar2=None, op0=ALU.mult)
        VV("scalar", "activation", [b_sc, b_sm], [b_sc, b_sm], out=sc[:].rearrange("p a b -> p (a b)"), in_=sc[:].rearrange("p a b -> p (a b)"), func=AF.Exp,
           bias=sm[:, 2:3], accum_out=sm[:, 3:4])
        VV("vector", "reciprocal", [b_sm], [b_sm], out=sm[:, 3:4], in_=sm[:, 3:4])
        VV("vector", "tensor_reduce", [b_sc], [b_sm], out=sm[:, 4:5], in_=sc[:, :, 128], axis=AX.X, op=ALU.add)
        VV("vector", "tensor_scalar", [b_qkv, b_sm], [b_oa], out=oacc[:], in0=qkv[:, 2, :], scalar1=sm[:, 4:5], scalar2=None, op0=ALU.mult)
        for br, (_, dil) in enumerate(BRANCHES):
            for n in range(NS):
                src = bass.AP(C.cv.tensor, n * 2048 * 512 + (2048 - 128 * dil) * 512, [[64, 8], [dil * 512, 128], [1, 64]])
                if n == 0:
                    k.dma("sync", Vb[8 * n:8 * n + 8, :, :], src, writes=[b_Vb])
                else:
                    k.S.dmaop("sync", "Vb", lambda e, n=n, src=src: e.dma_start(out=Vb[8 * n:8 * n + 8, :, :], in_=src), [])
            b_Vb.writer = ("D", "Vb", k.S.dma_sems["Vb"][1])
            VV("vector", "tensor_tensor", [b_Vb, b_sc], [b_tmp], out=tmpS[:], in0=Vb[:], in1=sc[:, br, 0:128].unsqueeze(2).to_broadcast([128, 128, 64]), op=ALU.mult)
            VV("vector", "tensor_reduce", [b_tmp], [b_ot], out=otmp[:], in_=tmpS[:].rearrange("p i d -> p d i"), axis=AX.X, op=ALU.add)
            VV("vector", "tensor_tensor", [b_ot, b_oa], [b_oa], out=oacc[:], in0=oacc[:], in1=otmp[:], op=ALU.add)
        VV("vector", "tensor_scalar", [b_oa, b_sm], [b_oa], out=oacc[:], in0=oacc[:], scalar1=sm[:, 3:4], scalar2=None, op0=ALU.mult)
        k.dma("sync", bass.AP(ms_d.tensor, 0, [[D, NS], [64, 8], [1, 64]]), oacc[:], reads=[b_oa], writes=[b_ms_d])
    S.barrier()

    with ExitStack() as s3:
        sb3 = lambda n, s, dt=F32: s3.enter_context(nc.sbuf_tensor(n, list(s), dt))
        NP = NS * 4
        St = sb3("St", [NP, 128, 128])
        b_St = Buf("St")
        for q4 in range(4):
            k.dma("sync", St[:, 32 * q4:32 * q4 + 32, :], C.sst[:, 32 * q4:32 * q4 + 32, :], writes=[b_St]) if q4 == 0 else k.S.dmaop(
                "sync", "St", lambda e, q4=q4: e.dma_start(out=St[:, 32 * q4:32 * q4 + 32, :], in_=C.sst[:, 32 * q4:32 * q4 + 32, :]), [])
        b_St.writer = ("D", "St", k.S.dma_sems["St"][1])
        T2 = sb3("T2", [NP, 128, 128])
        b_T2 = Buf()
        c3 = sb3("c3", [NP, 3, 128])
        b_c3 = Buf("c3")
        for ty in range(3):
            k.dma("sync", c3[:, ty, :], bass.AP(cs_d.tensor, 512 * ty, [[1536, NS], [128, 4], [1, 128]]), reads=[b_cs_d], writes=[b_c3])
        zz = sb3("zz", [NP, 128])
        b_zz = Buf("zz")
        k.dma("sync", zz[:], bass.AP(ps_d.tensor, COL_ZB, [[IN_COLS, NS], [128, 4], [1, 128]]), reads=[b_ps_d], writes=[b_zz])
        ab = sb3("ab_s", [NP, 2])
        b_ab = Buf("ab_s")
        k.dma("sync", ab[:, 0:1], bass.AP(ps_d.tensor, COL_AB, [[IN_COLS, NS], [1, 4], [1, 1]]), reads=[b_ps_d], writes=[b_ab])
        k.dma("sync", ab[:, 1:2], bass.AP(ps_d.tensor, COL_BB, [[IN_COLS, NS], [1, 4], [1, 1]]), reads=[b_ps_d], writes=[b_ab])
        sp = sb3("sprm_sb", [NP, 2 + 128])
        b_sp = Buf("sprm")
        k.dma("sync", sp[:], C.sprm_d, writes=[b_sp])
        w = sb3("wS3", [NP, 24])
        b_w = Buf()
        jk = sb3("jkS3", [NP, 128])
        b_jk = Buf()
        VV("vector", "tensor_tensor", [b_ab, b_sp], [b_w], out=w[:, 0:1], in0=ab[:, 0:1], in1=sp[:, 1:2], op=ALU.add)
        VV("scalar", "activation", [b_w], [b_w], out=w[:, 0:1], in_=w[:, 0:1], func=AF.Exp)
        VV("scalar", "activation", [b_w], [b_w], out=w[:, 0:1], in_=w[:, 0:1], func=AF.Ln, bias=1.0)
        VV("scalar", "activation", [b_sp], [b_w], out=w[:, 1:2], in_=sp[:, 0:1], func=AF.Exp)
        VV("vector", "scalar_tensor_tensor", [b_w], [b_w], out=w[:, 2:3], in0=w[:, 0:1], scalar=-1.0, in1=w[:, 1:2], op0=ALU.mult, op1=ALU.mult)
        VV("scalar", "activation", [b_w], [b_w], out=w[:, 3:4], in_=w[:, 2:3], func=AF.Exp)
        VV("scalar", "activation", [b_ab], [b_w], out=w[:, 4:5], in_=ab[:, 1:2], func=AF.Sigmoid)
        for j in range(2):
            VV("scalar", "activation", [b_c3], [b_jk, b_w], out=jk[:], in_=c3[:, j, :], func=AF.Square, accum_out=w[:, 5 + j:6 + j])
            VV("scalar", "activation", [b_w, C.b_eps], [b_w], out=w[:, 5 + j:6 + j], in_=w[:, 5 + j:6 + j], func=AF.Sqrt, bias=C.eps6[0:NP, 0:1])
            VV("vector", "reciprocal", [b_w], [b_w], out=w[:, 5 + j:6 + j], in_=w[:, 5 + j:6 + j])
        VV("vector", "tensor_scalar", [b_c3, b_w], [b_c3], out=c3[:, 0, :], in0=c3[:, 0, :], scalar1=w[:, 5:6], scalar2=128.0 ** -0.5, op0=ALU.mult, op1=ALU.mult)
        VV("vector", "tensor_scalar", [b_c3, b_w], [b_c3], out=c3[:, 1, :], in0=c3[:, 1, :], scalar1=w[:, 6:7], scalar2=None, op0=ALU.mult)
        mem = sb3("memS", [NP, 4, 128])
        b_mem = Buf()
        VV("vector", "tensor_tensor", [b_St, b_c3], [b_T2], out=T2[:], in0=St[:], in1=c3[:, 1, :].unsqueeze(2).to_broadcast([NP, 128, 128]), op=ALU.mult)
        VV("vector", "tensor_reduce", [b_T2], [b_mem], out=mem[:, 0, :], in_=T2[:].rearrange("p d e -> p e d"), axis=AX.X, op=ALU.add)
        VV("vector", "scalar_tensor_tensor", [b_mem, b_w, b_c3], [b_mem], out=mem[:, 1, :], in0=mem[:, 0, :], scalar=w[:, 3:4], in1=c3[:, 2, :], op0=ALU.mult, op1=ALU.subtract)
        VV("vector", "tensor_scalar", [b_mem, b_w], [b_mem], out=mem[:, 1, :], in0=mem[:, 1, :], scalar1=w[:, 4:5], scalar2=-1.0, op0=ALU.mult, op1=ALU.mult)
        VV("vector", "tensor_tensor", [b_c3, b_mem], [b_T2], out=T2[:], in0=c3[:, 1, :].unsqueeze(2).to_broadcast([NP, 128, 128]),
           in1=mem[:, 1, :].unsqueeze(1).to_broadcast([NP, 128, 128]), op=ALU.mult)
        VV("vector", "scalar_tensor_tensor", [b_St, b_T2, b_w], [b_St], out=St[:], in0=St[:], scalar=w[:, 3:4], in1=T2[:], op0=ALU.mult, op1=ALU.add)
        for q4 in range(4):
            final.append(k.dma("sync", C.ssm_s[:, 32 * q4:32 * q4 + 32, :], St[:, 32 * q4:32 * q4 + 32, :], reads=[b_St], slot="ssms"))
        VV("vector", "tensor_tensor", [b_St, b_c3], [b_T2], out=T2[:], in0=St[:], in1=c3[:, 0, :].unsqueeze(2).to_broadcast([NP, 128, 128]), op=ALU.mult)
        VV("vector", "tensor_reduce", [b_T2], [b_mem], out=mem[:, 2, :], in_=T2[:].rearrange("p d e -> p e d"), axis=AX.X, op=ALU.add)
        VV("scalar", "activation", [b_mem], [b_jk, b_w], out=jk[:], in_=mem[:, 2, :], func=AF.Square, accum_out=w[:, 8:9])
        VV("scalar", "activation", [b_w, C.b_eps], [b_w], out=w[:, 8:9], in_=w[:, 8:9], func=AF.Sqrt, scale=1.0 / 128.0, bias=C.eps6[0:NP, 0:1])
        VV("vector", "reciprocal", [b_w], [b_w], out=w[:, 8:9], in_=w[:, 8:9])
        VV("scalar", "activation", [b_zz], [b_zz], out=zz[:], in_=zz[:], func=AF.Silu)
        VV("vector", "tensor_tensor", [b_zz, b_sp], [b_zz], out=zz[:], in0=zz[:], in1=sp[:, 2:130], op=ALU.mult)
        VV("vector", "scalar_tensor_tensor", [b_mem, b_w, b_zz], [b_mem], out=mem[:, 3, :], in0=mem[:, 2, :], scalar=w[:, 8:9], in1=zz[:], op0=ALU.mult, op1=ALU.mult)
        k.dma("sync", bass.AP(ms_d.tensor, 512, [[D, NS], [128, 4], [1, 128]]), mem[:, 3, :], reads=[b_mem], writes=[b_ms_d])
    S.barrier()

    with ExitStack() as s4:
        sb4 = lambda n, s, dt=F32: s4.enter_context(nc.sbuf_tensor(n, list(s), dt))
        msf = sb4("msf", [128, 1024])
        b_msf = Buf("msf")
        VV("vector", "memset", [], [b_msf], ap=msf[:], constant=0.0)
        k.dma("sync", msf[0:NS, :], ms_d, reads=[b_ms_d], writes=[b_msf])
        mxS = sb4("mxS", [128, 8, 128], BF16)
        b_mxS = Buf("mxS")
        for g4 in range(2):
            pt_, pb_ = psT[2 + g4], psB[2 + g4]
            for j in range(4):
                kk = 4 * g4 + j
                fn = lambda e, kk=kk, j=j, pt_=pt_: e.transpose(pt_[:, 128 * j:128 * j + 128], msf[:, 128 * kk:128 * kk + 128], C.cst[:, 0, :])
                if j == 0:
                    k.op("tensor", fn, reads=[b_msf, C.b_cst], writes=[pb_])
                else:
                    k.acc("tensor", fn, reads=[b_msf, C.b_cst], acc=[pb_])
            VV("vector", "tensor_copy", [pb_], [b_mxS], out=mxS[:, 4 * g4:4 * g4 + 4, :], in_=pt_[:, :].rearrange("p (a c) -> p a c", a=4))
        k.dma("sync", C.mixS_d, mxS[:], reads=[b_mxS], writes=[C.b_mixS_d])
    S.barrier()


def _consts():
    t = np.arange(128)
    same = (t[:, None] // 64) == (t[None, :] // 64)
    cst = np.zeros((128, 8, 128), np.float32)
    cst[:, 0] = np.eye(128)
    cst[:, 1] = -np.eye(128)
    cst[:, 2] = (same & (t[:, None] <= t[None, :]))
    cst[:, 3] = (t[:, None] < 64) * np.ones((1, 128))
    cst[:, 4] = (t[:, None] >= 64) * np.ones((1, 128))
    cst[:, 5] = np.where(same & (t[None, :] <= t[:, None]), 0.0, NEG)
    cst[:, 6] = (same & (t[None, :] < t[:, None]))
    cst[:, 7] = (t[:, None] < t[None, :])
    return cst


def _params(inp):
    prm = np.zeros((128, 184), np.float32)
    prm[:, 0:4] = inp["a_log"][0][None, :]
    prm[:, 4:8] = inp["dt_bias"][0][None, :]
    prm[:, 8:136] = inp["o_norm_g"][0][None, :]
    cw = inp["conv_w"][0]
    prm[:, 136:184] = cw.reshape(4, 12, 128).transpose(2, 1, 0).reshape(128, 48)
    return prm


def _sample_bias(rel_bias):
    out = np.empty((128, 3, 129), np.float32)
    h = np.arange(128) % 8
    for br, (_, dil) in enumerate(BRANCHES):
        dist = np.concatenate([dil * (128 - np.arange(128)), [0]])
        out[:, br, :] = rel_bias[_rel_bucket_np(dist)][:, h].T
    return out


def _core_inputs(c, inp, bt):
    b, hf = divmod(c, 2)
    x = inp["x_prompt"][b]
    xT = np.zeros((D, EXT), np.float32)
    if hf == 1:
        xT[:, :] = x.T
    else:
        xT[:, HALF:] = x[:HALF].T
    valid = np.ones((128, 32), np.float32)
    if hf == 0:
        valid[:, :16] = 0.0
    return {
        "xT": np.ascontiguousarray(xT),
        "xo": np.ascontiguousarray(x[HALF * hf:HALF * hf + HALF]),
        "valid": valid,
        "w_in": np.ascontiguousarray(inp["w_in"][0]),
        "bt": bt,
        "cst": _consts(),
        "prm": _params(inp),
        "w_out": np.ascontiguousarray(inp["w_out"][0]),
        "lnp": np.ascontiguousarray(np.broadcast_to(np.stack([inp["ln1_g"][0], inp["ln1_b"][0], inp["ln2_g"][0], inp["ln2_b"][0]])[None], (128, 4, D))),
        "wr": np.ascontiguousarray(np.concatenate([inp["w_group"][0], inp["w_router"][0]], axis=1)),
        "rb": np.ascontiguousarray(np.broadcast_to(np.concatenate([inp["b_group"][0], inp["b_router"][0].reshape(-1)])[None], (128, 36))),
        "w_gate": np.ascontiguousarray(inp["w_gate"][0]),
        "w_up": np.ascontiguousarray(inp["w_up"][0]),
        "w_down": np.ascontiguousarray(inp["w_down"][0]),
        "xsT": np.ascontiguousarray(inp["x_sample"][NS * c:NS * c + NS, 0, :].T),
        "ck": np.ascontiguousarray(inp["cache_a_k"][0, NS * c:NS * c + NS].reshape(NS, 2048, 512)),
        "cv": np.ascontiguousarray(inp["cache_a_v"][0, NS * c:NS * c + NS].reshape(NS, 2048, 512)),
        "sst": np.ascontiguousarray(inp["state_b_ssm"][0, NS * c:NS * c + NS].reshape(NS * 4, 128, 128)),
        "scv": np.ascontiguousarray(inp["state_b_conv"][0, NS * c:NS * c + NS]),
        "sbias": _sample_bias(inp["rel_bias"].astype(np.float32)),
        "cwr": np.ascontiguousarray(np.broadcast_to(inp["conv_w"][0][None], (NS, 4, 1536))),
        "sprm": np.ascontiguousarray(np.concatenate([np.tile(inp["a_log"][0], NS)[:, None], np.tile(inp["dt_bias"][0], NS)[:, None],
                                                       np.broadcast_to(inp["o_norm_g"][0][None], (NS * 4, 128))], axis=1)),
        "iota": np.ascontiguousarray(np.broadcast_to(np.arange(CAP, dtype=np.float32)[None], (128, CAP))),
        "xs_pad": np.ascontiguousarray(np.concatenate([inp["x_sample"][NS * c:NS * c + NS, 0, :], np.zeros((128 - NS, D), np.float32)], axis=0)),
    }


def kernel(**inputs):
    inp = {k_: np.asarray(v) for k_, v in inputs.items()}
    bt = _bias_tiles(inp["rel_bias"].astype(np.float32))
    nc = build()
    in_maps = [_core_inputs(c, inp, bt) for c in range(NCORES)]
    res = run_bass_kernel_spmd(nc, in_maps, core_ids=list(range(NCORES)))
    r = res.results
    y_prompt = np.stack([np.concatenate([r[2 * b]["y_out"], r[2 * b + 1]["y_out"]], axis=0) for b in range(4)])
    k_win = np.stack([r[2 * b + 1]["kwin"].reshape(HALF, 8, 64) for b in range(4)])[None]
    v_win = np.stack([r[2 * b + 1]["vwin"].reshape(HALF, 8, 64) for b in range(4)])[None]
    ssm_p = np.stack([r[2 * b + 1]["ssm_p"] for b in range(4)])[None]
    conv_p = np.stack([r[2 * b + 1]["conv_p"] for b in range(4)])[None]
    y_sample = np.concatenate([r[c]["ys_out"] for c in range(NCORES)], axis=0)[:, None, :]
    k_new = np.concatenate([r[c]["knew"] for c in range(NCORES)], axis=0).reshape(1, 128, 1, 8, 64)
    v_new = np.concatenate([r[c]["vnew"] for c in range(NCORES)], axis=0).reshape(1, 128, 1, 8, 64)
    ssm_s = np.concatenate([r[c]["ssm_s"] for c in range(NCORES)], axis=0).reshape(1, 128, 4, 128, 128)
    conv_s = np.concatenate([r[c]["conv_s"] for c in range(NCORES)], axis=0)[None]
    return (y_prompt, y_sample, k_win, v_win, k_new, v_new, ssm_p, ssm_s, conv_p, conv_s)
```

```python
import math
import os
from contextlib import ExitStack

import numpy as np
import concourse.bass as bass
import concourse.mybir as mybir
from concourse.bass_utils import run_bass_kernel_spmd

F32 = mybir.dt.float32
BF16 = mybir.dt.bfloat16
I32 = mybir.dt.int32
U32 = mybir.dt.uint32
AF = mybir.ActivationFunctionType
ALU = mybir.AluOpType
AX = mybir.AxisListType

NCORES = 8
D = 1024
SEQ = 4096
HALF = 2048
EXT = 4096
NS = 16
A_HEADS, A_HD = 8, 64
B_HEADS, B_HD = 4, 128
COL_QA, COL_KA, COL_VA, COL_UB = 0, 512, 1024, 1536
COL_ZB = COL_UB + 1536
COL_AB = COL_ZB + 512
COL_BB = COL_AB + 4
IN_COLS = COL_BB + 4
BRANCHES = ((128, 1), (512, 4), (2048, 16))
NEG = -30000.0
CAP = 640
ENGS = ("tensor", "vector", "scalar", "gpsimd", "sync")


class Sched:
    def __init__(self, nc, stack, same_engine_wait=True):
        self.nc = nc
        self.stack = stack
        self.q = {e: [] for e in ENGS}
        self.cnt = {e: 0 for e in ENGS}
        self.sem = {e: stack.enter_context(nc.semaphore("s_" + e)) for e in ENGS}
        self.waited = {e: {} for e in ENGS}
        self.same_engine_wait = same_engine_wait
        self.dma_sems = {}
        self.ninst = 0

    def _wait(self, eng, tok):
        if tok is None:
            return
        if tok[0] == "E":
            _, src, val = tok
            if src == eng and not self.same_engine_wait:
                return
            key = "E" + src
            sem = self.sem[src]
        else:
            _, slot, val = tok
            key = "D" + slot
            sem = self.dma_sems[slot][0]
        if self.waited[eng].get(key, 0) >= val:
            return
        self.waited[eng][key] = val
        self.q[eng].append(lambda e, sem=sem, val=val: e.wait_ge(sem, val))

    def op(self, eng, fn, deps=()):
        for d in deps:
            self._wait(eng, d)
        self.cnt[eng] += 1
        c = self.cnt[eng]
        sem = self.sem[eng]
        self.q[eng].append(lambda e, fn=fn, sem=sem: fn(e).then_inc(sem, 1))
        self.ninst += 1
        return ("E", eng, c)

    def dmaop(self, eng, slot, fn, deps=()):
        for d in deps:
            self._wait(eng, d)
        if slot not in self.dma_sems:
            self.dma_sems[slot] = [self.stack.enter_context(self.nc.semaphore("d_" + slot)), 0]
        ent = self.dma_sems[slot]
        ent[1] += 16
        sem = ent[0]
        self.q[eng].append(lambda e, fn=fn, sem=sem: fn(e).then_inc(sem, 16))
        self.ninst += 1
        return ("D", slot, ent[1])

    def barrier(self):
        toks = [("E", e, self.cnt[e]) for e in ENGS if self.cnt[e] > 0]
        toks += [("D", slot, ent[1]) for slot, ent in self.dma_sems.items() if ent[1] > 0]
        for e in ENGS:
            for t in toks:
                self._wait(e, t)

    def finish(self, final_tokens):
        best = {}
        for t in final_tokens:
            if t is None:
                continue
            key = (t[0], t[1])
            if key not in best or best[key][2] < t[2]:
                best[key] = t
        for t in best.values():
            self._wait("sync", t)
        with self.nc.Block() as block:
            @block.tensor
            def _(e):
                for f in self.q["tensor"]:
                    f(e)

            @block.vector
            def _(e):
                for f in self.q["vector"]:
                    f(e)

            @block.scalar
            def _(e):
                for f in self.q["scalar"]:
                    f(e)

            @block.gpsimd
            def _(e):
                for f in self.q["gpsimd"]:
                    f(e)

            @block.sync
            def _(e):
                for f in self.q["sync"]:
                    f(e)


class Buf:
    _n = 0

    def __init__(self, name=None):
        Buf._n += 1
        self.name = name or ("b%d" % Buf._n)
        self.writer = None
        self.readers = {}

    def add_reader(self, tok):
        key = tok[1]
        if key not in self.readers or self.readers[key][2] < tok[2]:
            self.readers[key] = tok


class K:
    def __init__(self, S):
        self.S = S

    def _deps(self, reads, writes, deps):
        d = list(deps)
        for b in reads:
            d.append(b.writer)
        for b in writes:
            d.extend(b.readers.values())
            d.append(b.writer)
        return d

    def _commit(self, tok, reads, writes):
        for b in reads:
            b.add_reader(tok)
        for b in writes:
            b.writer = tok
            b.readers = {}

    def op(self, eng, fn, reads=(), writes=(), deps=()):
        tok = self.S.op(eng, fn, self._deps(reads, writes, deps))
        self._commit(tok, reads, writes)
        return tok

    def acc(self, eng, fn, reads=(), acc=(), deps=()):
        d = list(deps)
        for b in reads:
            d.append(b.writer)
        tok = self.S.op(eng, fn, d)
        for b in reads:
            b.add_reader(tok)
        for b in acc:
            b.writer = tok
        return tok

    def gload(self, dst, src, writes, reads=()):
        A, L = dst.shape[1], dst.shape[2]
        slot = writes[0].name
        d = self._deps(reads, writes, ())
        tok = None
        for a0 in range(0, A, 4):
            for l0 in range(0, L, 512):
                tok = self.S.dmaop("gpsimd", slot, lambda e, a0=a0, l0=l0, L=L: e.dma_start(
                    out=dst[:, a0:a0 + 4, l0:min(L, l0 + 512)], in_=src[:, a0:a0 + 4, l0:min(L, l0 + 512)]), d)
        self._commit(tok, reads, writes)
        return tok

    def dma(self, eng, out, in_, reads=(), writes=(), deps=(), slot=None, **kw):
        if slot is None:
            slot = (writes[0] if writes else reads[0]).name
        tok = self.S.dmaop(eng, slot, lambda e: e.dma_start(out=out, in_=in_, **kw), self._deps(reads, writes, deps))
        self._commit(tok, reads, writes)
        return tok


def _rel_bucket_np(dist):
    n = np.maximum(dist, 0)
    ratio = np.maximum(n, 1).astype(np.float32) / np.float32(16)
    large = 16 + (np.log(ratio) / np.float32(math.log(2048 / 16)) * np.float32(16)).astype(np.int32)
    return np.where(n < 16, n, np.minimum(large, 31))


def _bias_tiles(rel_bias):
    kp = np.arange(128)[:, None, None]
    kt = np.arange(2)[None, :, None]
    i = np.arange(128)[None, None, :]
    off = 128 + i - (128 * kt + kp)
    valid = (off >= 0) & (off <= 128)
    out = np.empty((128, 24, 256), np.float32)
    for h in range(A_HEADS):
        for br, (_, dil) in enumerate(BRANCHES):
            bk = _rel_bucket_np(np.maximum(off, 0) * dil)
            vals = rel_bias[bk, h]
            out[:, h * 3 + br, :] = np.where(valid, vals, np.float32(NEG)).reshape(128, 256)
    return out


def sl(start, step, n=128):
    return slice(start, start + (n - 1) * step + 1, step)


def tokset(ti, dil):
    nblk, r = divmod(ti, dil)
    start = r + dil * 128 * nblk
    return start, dil


def build(debug=(), stage=99, nexp=32, with_sample=True):
    nc = bass.Bass("TRN2", target_bir_lowering=False)
    dram = lambda n, s, dt=F32, kind="ExternalInput": nc.dram_tensor(n, list(s), dt, kind=kind).ap()
    xT = dram("xT", [D, EXT])
    xo = dram("xo", [HALF, D])
    valid = dram("valid", [128, 32])
    w_in = dram("w_in", [D, IN_COLS])
    bt = dram("bt", [128, 24, 256])
    cst_d = dram("cst", [128, 8, 128])
    prm_d = dram("prm", [128, 184])
    ssm_p = dram("ssm_p", [4, 128, 128], kind="ExternalOutput")
    conv_p = dram("conv_p", [3, 1536], kind="ExternalOutput")
    kT_d = dram("kT_d", [128, 4, EXT], BF16, kind="Internal")
    vT_d = dram("vT_d", [128, 4, EXT], BF16, kind="Internal")
    qT_d = dram("qT_d", [128, 4, HALF], BF16, kind="Internal")
    gz_d = dram("gz_d", [16, 128, 512], BF16, kind="Internal")
    mixA_d = dram("mixA_d", [128, 4, HALF], BF16, kind="Internal")
    mixB_d = dram("mixB_d", [128, 4, HALF], BF16, kind="Internal")
    w_out = dram("w_out", [D, D])
    lnp_d = dram("lnp", [128, 4, D])
    wr_d = dram("wr", [D, 36])
    rb_d = dram("rb", [128, 36])
    w_gate = dram("w_gate", [32, D, 512])
    w_up = dram("w_up", [32, D, 512])
    w_down = dram("w_down", [32, 512, D])
    xs_pad = dram("xs_pad", [128, D])
    iota_d = dram("iota", [128, CAP])
    hb_d = dram("hb_d", [17 * 128, D], BF16, kind="Internal")
    y_out = dram("y_out", [HALF, D], kind="ExternalOutput")
    ys_out = dram("ys_out", [NS, D], kind="ExternalOutput")
    mixS_d = dram("mixS_d", [128, 8, 128], BF16, kind="Internal")
    xsT = dram("xsT", [D, NS])
    ck = dram("ck", [NS, 2048, 512])
    cv = dram("cv", [NS, 2048, 512])
    sst = dram("sst", [NS * 4, 128, 128])
    scv = dram("scv", [NS, 3, 1536])
    sbias_d = dram("sbias", [128, 3, 129])
    cwr_d = dram("cwr", [NS, 4, 1536])
    sprm_d = dram("sprm", [NS * 4, 130])
    ps_d = dram("ps_d", [NS, IN_COLS], kind="Internal")
    cs_d = dram("cs_d", [NS, 1536], kind="Internal")
    ms_d = dram("ms_d", [NS, D], kind="Internal")
    knew = dram("knew", [NS, 512], kind="ExternalOutput")
    vnew = dram("vnew", [NS, 512], kind="ExternalOutput")
    ssm_s = dram("ssm_s", [NS * 4, 128, 128], kind="ExternalOutput")
    conv_s = dram("conv_s", [NS, 3, 1536], kind="ExternalOutput")
    kwin = dram("kwin", [HALF, 512], kind="ExternalOutput")
    vwin = dram("vwin", [HALF, 512], kind="ExternalOutput")
    dbg_out = {}
    for name, shape in debug:
        dbg_out[name] = dram("dbg_" + name, shape, kind="ExternalOutput")

    final = []
    with ExitStack() as st:
        S = Sched(nc, st, same_engine_wait=(os.environ.get("SEW", "1") == "1"))
        k = K(S)
        sb = lambda n, s, dt=F32: st.enter_context(nc.sbuf_tensor(n, list(s), dt))
        psT = [st.enter_context(nc.psum_tensor("ps%d" % i, [128, 512], F32)) for i in range(8)]
        psB = [Buf("ps%d" % i) for i in range(8)]

        ones_f = sb("ones_f", [128, 128])
        b_ones = Buf()
        k.op("gpsimd", lambda e: e.memset(ones_f[:], 1.0), writes=[b_ones])
        eps6 = sb("eps6", [128, 1])
        b_eps = Buf()
        k.op("gpsimd", lambda e: e.memset(eps6[:], 1e-6), writes=[b_eps])
        cst = sb("cst_sb", [128, 8, 128])
        b_cst = Buf("cst")
        k.dma("sync", cst[:], cst_d, writes=[b_cst])
        eps5 = sb("eps5", [128, 1])
        b_eps5 = Buf()
        k.op("gpsimd", lambda e: e.memset(eps5[:], 1e-5), writes=[b_eps5])
        b_mixA_d, b_mixB_d, b_mixS_d = Buf("mixA_d"), Buf("mixB_d"), Buf("mixS_d")
        valid_sb = sb("valid_sb", [128, 32])
        b_valid = Buf("valid")
        k.dma("sync", valid_sb[:], valid, writes=[b_valid])

        if with_sample:
            C = type("Ctx", (), {})()
            C.nc, C.k, C.S, C.final = nc, k, S, final
            C.psT, C.psB, C.cst, C.b_cst, C.eps6, C.b_eps = psT, psB, cst, b_cst, eps6, b_eps
            C.w_v = w_in.rearrange("(k p) c -> p k c", p=128)
            C.xsT, C.ck, C.cv, C.sst, C.scv, C.sbias_d, C.cwr_d, C.sprm_d = xsT, ck, cv, sst, scv, sbias_d, cwr_d, sprm_d
            C.ps_d, C.cs_d, C.ms_d = ps_d, cs_d, ms_d
            C.knew, C.vnew, C.ssm_s, C.conv_s = knew, vnew, ssm_s, conv_s
            C.mixS_d, C.b_mixS_d = mixS_d, b_mixS_d
            phase_s(C)
        sx = ExitStack()
        xTb = sx.enter_context(nc.sbuf_tensor("xTb", [128, 8, EXT], BF16))
        b_x = [Buf("xTb%d" % c) for c in range(8)]
        xT_v = xT.rearrange("(k p) t -> p k t", p=128)
        for c in range(8):
            k.gload(xTb[:, :, 512 * c:512 * (c + 1)], xT_v[:, :, 512 * c:512 * (c + 1)], writes=[b_x[c]])
        bx_of_tile = lambda ti_nat: b_x[ti_nat // 4]

        def x_bufs(start, step):
            lo, hi = start, start + step * 127
            return [b_x[c] for c in range(lo // 512, hi // 512 + 1)]

        with ExitStack() as sa:
            sba = lambda n, s, dt=F32: sa.enter_context(nc.sbuf_tensor(n, list(s), dt))
            mixA = sba("mixA", [128, 4, HALF], BF16)
            b_mixA = [Buf() for _ in range(4)]
            ETb = sba("ETb", [128, 24, 256], BF16)
            b_ET = Buf("ET")
            btst = sba("btst", [128, 6, 256])
            b_btst = Buf("btst")
            for g in range(4 if stage >= -1 else 0):
                k.dma("sync", btst[:], bt[:, 6 * g:6 * g + 6, :], writes=[b_btst])
                k.op("scalar", lambda e, g=g: e.activation(out=ETb[:, 6 * g:6 * g + 6, :], in_=btst[:], func=AF.Exp),
                     reads=[b_btst], writes=[b_ET])
            QT = sba("QT", [128, 2, HALF], BF16)
            KT = sba("KT", [128, 2, EXT], BF16)
            Vaug = sba("Vaug", [128, 32, 4, 65], BF16)
            acc = sba("acc", [65, 4, HALF])
            wq = sba("wq", [128, 8, 256], BF16)
            wk = sba("wk", [128, 8, 256], BF16)
            wv = sba("wv", [128, 8, 256], BF16)
            b_wq, b_wk, b_wv = Buf("wq"), Buf("wk"), Buf("wv")
            b_QT = [Buf() for _ in range(2)]
            b_KT = [Buf() for _ in range(2)]
            b_V = [Buf() for _ in range(32)]
            b_acc = [Buf() for _ in range(4)]
            b_rrow = Buf()
            stg = [sba("stg%d" % i, [128, 256]) for i in range(2)]
            b_stg = [Buf("stg%d" % i) for i in range(2)]
            exb = [sba("exb%d" % i, [128, 512]) for i in range(2)]
            b_ex = [Buf() for _ in range(2)]
            ptb = [sba("ptb%d" % i, [128, 512], BF16) for i in range(2)]
            b_pt = [Buf() for _ in range(2)]
            w_v = w_in.rearrange("(k p) c -> p k c", p=128)
            ctr = {"ps": 0, "stg": 0, "s": 0, "o": 0, "ev": 0}

            def proj_ps():
                i = ctr["ps"] % 2
                ctr["ps"] += 1
                return psT[i], psB[i]

            for hh2 in range(2):
                k.gload(wq[:], w_v[:, :, COL_QA + 256 * hh2:COL_QA + 256 * hh2 + 256], writes=[b_wq])
                k.gload(wk[:], w_v[:, :, COL_KA + 256 * hh2:COL_KA + 256 * hh2 + 256], writes=[b_wk])
                k.gload(wv[:], w_v[:, :, COL_VA + 256 * hh2:COL_VA + 256 * hh2 + 256], writes=[b_wv])
                for jj in range(2 if stage >= 0 else 0):
                    for tc in range(4 if os.environ.get('KQ','1')=='1' else 0):
                        pt_, pb_ = proj_ps()
                        for kk in range(8):
                            fn = lambda e, kk=kk, jj=jj, tc=tc, pt_=pt_: e.matmul(
                                pt_[:, :], lhsT=wq[:, kk, 128 * jj:128 * jj + 128],
                                rhs=xTb[:, kk, HALF + 512 * tc:HALF + 512 * tc + 512], start=(kk == 0), stop=(kk == 7))
                            if kk == 0:
                                k.op("tensor", fn, reads=[b_wq, b_x[4 + tc]], writes=[pb_])
                            else:
                                k.acc("tensor", fn, reads=[b_wq, b_x[4 + tc]], acc=[pb_])
                        k.op("vector", lambda e, jj=jj, tc=tc, pt_=pt_: e.tensor_scalar(
                            out=QT[:, jj, 512 * tc:512 * tc + 512], in0=pt_[:, :], scalar1=0.125, scalar2=None, op0=ALU.mult),
                            reads=[pb_], writes=[b_QT[jj]])
                    for tc in range(8 if os.environ.get('KK','1')=='1' else 0):
                        pt_, pb_ = proj_ps()
                        for kk in range(8):
                            fn = lambda e, kk=kk, jj=jj, tc=tc, pt_=pt_: e.matmul(
                                pt_[:, :], lhsT=wk[:, kk, 128 * jj:128 * jj + 128],
                                rhs=xTb[:, kk, 512 * tc:512 * tc + 512], start=(kk == 0), stop=(kk == 7))
                            if kk == 0:
                                k.op("tensor", fn, reads=[b_wk, b_x[tc]], writes=[pb_])
                            else:
                                k.acc("tensor", fn, reads=[b_wk, b_x[tc]], acc=[pb_])
                        k.op("vector", lambda e, jj=jj, tc=tc, pt_=pt_: e.tensor_copy(
                            out=KT[:, jj, 512 * tc:512 * tc + 512], in_=pt_[:, :]),
                            reads=[pb_], writes=[b_KT[jj]])
                for ti in range(16, 32 if stage >= -2 else 16):
                    pt_, pb_ = proj_ps()
                    for kk in range(8):
                        fn = lambda e, kk=kk, ti=ti, pt_=pt_: e.matmul(
                            pt_[:, 0:256], lhsT=xTb[:, kk, 128 * ti:128 * ti + 128], rhs=wk[:, kk, :],
                            start=(kk == 0), stop=(kk == 7))
                        if kk == 0:
                            k.op("tensor", fn, reads=[b_wk, b_x[ti // 4]], writes=[pb_])
                        else:
                            k.acc("tensor", fn, reads=[b_wk, b_x[ti // 4]], acc=[pb_])
                    si = ctr["stg"] % 2
                    ctr["stg"] += 1
                    k.op("scalar", lambda e, si=si, pt_=pt_: e.activation(out=stg[si][:], in_=pt_[:, 0:256], func=AF.Copy),
                         reads=[pb_], writes=[b_stg[si]])
                    final.append(k.dma("sync", kwin[128 * (ti - 16):128 * (ti - 16) + 128, 256 * hh2:256 * hh2 + 256],
                                       stg[si][:], reads=[b_stg[si]]))
                for br, (_, dil) in enumerate(BRANCHES):
                    if stage < 1:
                        break
                    k.op("gpsimd", lambda e: e.tensor_copy(
                        out=Vaug[:, :, :, 64], in_=valid_sb[:, :].unsqueeze(2).to_broadcast([128, 32, 4])),
                        reads=[b_valid], writes=b_V)
                    for ti in range(32):
                        start, step = tokset(ti, dil)
                        pt_, pb_ = proj_ps()
                        for kk in range(8):
                            fn = lambda e, kk=kk, start=start, step=step, pt_=pt_: e.matmul(
                                pt_[:, 0:256], lhsT=xTb[:, kk, sl(start, step)], rhs=wv[:, kk, :],
                                start=(kk == 0), stop=(kk == 7))
                            if kk == 0:
                                k.op("tensor", fn, reads=[b_wv] + x_bufs(start, step), writes=[pb_])
                            else:
                                k.acc("tensor", fn, reads=[b_wv] + x_bufs(start, step), acc=[pb_])
                        ev = "vector"
                        src = pt_[:, 0:256].rearrange("p (h d) -> p h d", h=4)
                        if ev == "vector":
                            k.op("vector", lambda e, ti=ti, src=src: e.tensor_copy(out=Vaug[:, ti, :, 0:64], in_=src),
                                 reads=[pb_], writes=[b_V[ti]])
                        else:
                            k.op("scalar", lambda e, ti=ti, src=src: e.activation(out=Vaug[:, ti, :, 0:64], in_=src, func=AF.Copy),
                                 reads=[pb_], writes=[b_V[ti]])
                        if br == 0 and ti >= 16:
                            si = ctr["stg"] % 2
                            ctr["stg"] += 1
                            k.op("vector", lambda e, si=si, pt_=pt_: e.tensor_copy(out=stg[si][:], in_=pt_[:, 0:256]),
                                 reads=[pb_], writes=[b_stg[si]])
                            final.append(k.dma("sync", vwin[128 * (ti - 16):128 * (ti - 16) + 128, 256 * hh2:256 * hh2 + 256],
                                               stg[si][:], reads=[b_stg[si]]))
                    def emit_S(hl, ti0):
                        jj, pb = hl // 2, 64 * (hl % 2)
                        sidx = 2 + ctr["s"] % 2
                        ctr["s"] += 1
                        pS, bS = psT[sidx], psB[sidx]
                        first = True
                        for a in range(2):
                            ti = ti0 + a
                            qs, qstep = tokset(ti, dil)
                            qs -= HALF
                            for kt in range(2):
                                tk = ti - dil * (1 - kt)
                                ks, kstep = tokset(tk, dil)
                                fn = lambda e, a=a, kt=kt, ks=ks, kstep=kstep, qs=qs, qstep=qstep, pS=pS, jj=jj, pb=pb: e.matmul(
                                    pS[:, a * 256 + kt * 128:a * 256 + kt * 128 + 128],
                                    lhsT=KT[pb:pb + 64, jj, sl(ks, kstep)],
                                    rhs=QT[pb:pb + 64, jj, sl(qs, qstep)], start=True, stop=True)
                                if first:
                                    k.op("tensor", fn, reads=[b_KT[jj], b_QT[jj]], writes=[bS])
                                    first = False
                                else:
                                    k.acc("tensor", fn, reads=[b_KT[jj], b_QT[jj]], acc=[bS])
                        return pS, bS

                    def emit_rest(hl, ti0, pS, bS):
                        h = 4 * hh2 + hl
                        jj, pb = hl // 2, 64 * (hl % 2)
                        ei = ctr["ev"] % 2
                        ctr["ev"] += 1
                        k.op("scalar", lambda e, ei=ei, pS=pS: e.activation(out=exb[ei][:], in_=pS[:, :], func=AF.Exp),
                             reads=[bS], writes=[b_ex[ei]])
                        k.op("vector", lambda e, ei=ei, h=h, br=br: e.tensor_tensor(
                            out=ptb[ei][:].rearrange("p (a c) -> p a c", a=2),
                            in0=exb[ei][:].rearrange("p (a c) -> p a c", a=2),
                            in1=ETb[:, h * 3 + br:h * 3 + br + 1, :].to_broadcast([128, 2, 256]), op=ALU.mult),
                            reads=[b_ex[ei], b_ET], writes=[b_pt[ei]])
                        oidx = 4 + ctr["o"] % 2
                        ctr["o"] += 1
                        pO, bO = psT[oidx], psB[oidx]
                        first = True
                        for a in range(2):
                            ti = ti0 + a
                            for kt in range(2):
                                tk = ti - dil * (1 - kt)
                                fn = lambda e, a=a, kt=kt, tk=tk, hl=hl, ei=ei, pO=pO: e.matmul(
                                    pO[0:65, a * 128:a * 128 + 128], lhsT=Vaug[:, tk, hl, 0:65],
                                    rhs=ptb[ei][:, a * 256 + kt * 128:a * 256 + kt * 128 + 128],
                                    start=(kt == 0), stop=(kt == 1))
                                if first:
                                    k.op("tensor", fn, reads=[b_pt[ei], b_V[tk]], writes=[bO])
                                    first = False
                                else:
                                    k.acc("tensor", fn, reads=[b_pt[ei], b_V[tk]], acc=[bO])
                        qs0, qstep = tokset(ti0, dil)
                        qs1, _ = tokset(ti0 + 1, dil)
                        qs0 -= HALF
                        qs1 -= HALF
                        dst = bass.AP(acc, hl * HALF + qs0, [[4 * HALF, 65], [qs1 - qs0, 2], [qstep, 128]])
                        srcO = pO[0:65, 0:256].rearrange("p (a c) -> p a c", a=2)
                        if br == 0:
                            k.op("vector", lambda e, dst=dst, srcO=srcO: e.tensor_copy(out=dst, in_=srcO),
                                 reads=[bO], writes=[b_acc[hl]])
                        else:
                            k.op("vector", lambda e, dst=dst, srcO=srcO: e.tensor_tensor(out=dst, in0=srcO, in1=dst, op=ALU.add),
                                 reads=[bO], writes=[b_acc[hl]])
                    items = [(hl_, t_) for hl_ in range(4) for t_ in range(16, 32, 2)] if stage >= 2 else []
                    cur = emit_S(*items[0]) if items else None
                    for idx_, it_ in enumerate(items):
                        nxt_ = emit_S(*items[idx_ + 1]) if idx_ + 1 < len(items) else None
                        emit_rest(it_[0], it_[1], *cur)
                        cur = nxt_
                if stage < 3:
                    continue
                k.op("vector", lambda e: e.reciprocal(out=acc[64:65, :, :], in_=acc[64:65, :, :]), reads=b_acc, writes=[b_rrow])
                for hl in range(4):
                    jj, pb = hl // 2, 64 * (hl % 2)
                    for c in range(4):
                        pt_, pb_ = psT[6 + c % 2], psB[6 + c % 2]
                        k.op("tensor", lambda e, hl=hl, c=c, pt_=pt_: e.matmul(
                            pt_[0:64, :], lhsT=ones_f[64:65, 0:64], rhs=acc[64:65, hl, 512 * c:512 * c + 512], start=True, stop=True),
                            reads=[b_rrow, b_ones], writes=[pb_])
                        k.op("vector", lambda e, hl=hl, c=c, pt_=pt_, pb=pb, jj=jj, hh2=hh2: e.tensor_tensor(
                            out=mixA[pb:pb + 64, 2 * hh2 + jj, 512 * c:512 * c + 512], in0=acc[0:64, hl, 512 * c:512 * c + 512],
                            in1=pt_[0:64, :], op=ALU.mult), reads=[pb_, b_acc[hl]], writes=[b_mixA[2 * hh2 + jj]])

            if stage >= 3:
                for pp in range(4):
                    k.dma("sync", mixA_d[:, pp], mixA[:, pp], reads=[b_mixA[pp]], writes=[b_mixA_d])
        S.barrier()
        if stage >= 4:
            C = type("Ctx", (), {})()
            C.nc, C.k, C.S, C.final = nc, k, S, final
            C.xTb, C.b_x, C.w_v = xTb, b_x, w_in.rearrange("(k p) c -> p k c", p=128)
            C.psT, C.psB, C.cst, C.b_cst, C.ones_f, C.b_ones = psT, psB, cst, b_cst, ones_f, b_ones
            C.eps6, C.b_eps, C.prm_d = eps6, b_eps, prm_d
            C.kT_d, C.vT_d, C.qT_d, C.gz_d = kT_d, vT_d, qT_d, gz_d
            C.b_kT_d, C.b_vT_d, C.b_qT_d, C.b_gz_d = Buf("kT_d"), Buf("vT_d"), Buf("qT_d"), Buf("gz_d")
            C.mixB_d, C.b_mixB_d, C.ssm_p, C.conv_p = mixB_d, b_mixB_d, ssm_p, conv_p
            phase_b(C)
        sx.close()
        S.barrier()
        if stage >= 5:
            C = type("Ctx", (), {})()
            C.nc, C.k, C.S, C.final = nc, k, S, final
            C.psT, C.psB, C.cst, C.b_cst = psT, psB, cst, b_cst
            C.eps5, C.b_eps5 = eps5, b_eps5
            C.NT = 17 if with_sample else 16
            C.NEXP = nexp
            C.lnp_d, C.w_out, C.wr_d, C.rb_d = lnp_d, w_out, wr_d, rb_d
            C.w_gate, C.w_up, C.w_down = w_gate, w_up, w_down
            C.mixA_d, C.mixB_d, C.b_mixA_d, C.b_mixB_d = mixA_d, mixB_d, b_mixA_d, b_mixB_d
            C.mixS_d, C.b_mixS_d, C.xs_pad, C.xo = mixS_d, b_mixS_d, xs_pad, xo
            C.y_out, C.ys_out = y_out, ys_out
            C.iota_d, C.hb_d, C.b_hb_d, C.ones_f, C.b_ones = iota_d, hb_d, Buf("hb_d"), ones_f, b_ones
            phase_c(C)
        for nm, src_d, bsrc in (("mixA", mixA_d, b_mixA_d), ("mixB", mixB_d, b_mixB_d)):
            if nm in dbg_out:
                dstg_b = sb("dstg_b" + nm, [128, HALF], BF16)
                dstg = sb("dstg" + nm, [128, HALF])
                b_db, b_df = Buf("dstg_b" + nm), Buf("dstg" + nm)
                for pp in range(4):
                    k.dma("sync", dstg_b[:], src_d[:, pp], reads=[bsrc], writes=[b_db])
                    k.op("vector", lambda e, dstg=dstg, dstg_b=dstg_b: e.tensor_copy(out=dstg[:], in_=dstg_b[:]), reads=[b_db], writes=[b_df])
                    final.append(k.dma("sync", dbg_out[nm][:, pp], dstg[:], reads=[b_df]))
        S.finish(final)
    return nc


def phase_b(C):
    nc, k, S = C.nc, C.k, C.S
    xTb, b_x, w_v = C.xTb, C.b_x, C.w_v
    psT, psB = C.psT, C.psB
    cst, b_cst = C.cst, C.b_cst
    ones_f = C.ones_f
    IDENT, NEGID, LMASK, CM0, CM1, NEGM, STRICT = range(7)
    final = C.final
    kT_d, vT_d, qT_d, gz_d = C.kT_d, C.vT_d, C.qT_d, C.gz_d

    with ExitStack() as sB:
        sbb = lambda n, s, dt=F32: sB.enter_context(nc.sbuf_tensor(n, list(s), dt))
        prm = sbb("prm_sb", [128, 8 + 128 + 48])
        b_prm = Buf("prm")
        k.dma("sync", prm[:], C.prm_d, writes=[b_prm])
        identb = sbb("identb", [128, 128], BF16)
        b_identb = Buf()
        k.op("vector", lambda e: e.tensor_copy(out=identb[:], in_=cst[:, IDENT, :]), reads=[b_cst], writes=[b_identb])
        convp_sb = sbb("convp_sb", [128, 12, 3])
        b_convp = Buf("convp")

        ab_sb = sbb("ab_sb", [128, 32, 8])
        b_ab = Buf()
        with ExitStack() as s1:
            sb1 = lambda n, s, dt=F32: s1.enter_context(nc.sbuf_tensor(n, list(s), dt))
            wab = sb1("wab", [128, 8, 8], BF16)
            wz = sb1("wz", [128, 8, 512], BF16)
            b_wab, b_wz = Buf("wab"), Buf("wz")
            k.gload(wab[:], w_v[:, :, COL_AB:COL_AB + 8], writes=[b_wab])
            k.gload(wz[:], w_v[:, :, COL_ZB:COL_ZB + 512], writes=[b_wz])
            zst = [sb1("zst%d" % i, [128, 512]) for i in range(2)]
            b_zst = [Buf() for _ in range(2)]
            gzb = [sb1("gzb%d" % i, [128, 512], BF16) for i in range(2)]
            b_gzb = [Buf("gzb%d" % i) for i in range(2)]
            for ti in range(32):
                pt_, pb_ = psT[ti % 2], psB[ti % 2]
                for kk in range(8):
                    fn = lambda e, kk=kk, ti=ti, pt_=pt_: e.matmul(pt_[:, 0:8], lhsT=xTb[:, kk, 128 * ti:128 * ti + 128],
                                                                 rhs=wab[:, kk, :], start=(kk == 0), stop=(kk == 7))
                    if kk == 0:
                        k.op("tensor", fn, reads=[b_wab, b_x[ti // 4]], writes=[pb_])
                    else:
                        k.acc("tensor", fn, reads=[b_wab, b_x[ti // 4]], acc=[pb_])
                k.op("vector", lambda e, ti=ti, pt_=pt_: e.tensor_copy(out=ab_sb[:, ti, :], in_=pt_[:, 0:8]), reads=[pb_], writes=[b_ab])
            for ti in range(16, 32):
                i2 = ti % 2
                pt_, pb_ = psT[2 + i2], psB[2 + i2]
                for kk in range(8):
                    fn = lambda e, kk=kk, ti=ti, pt_=pt_: e.matmul(pt_[:, :], lhsT=xTb[:, kk, 128 * ti:128 * ti + 128],
                                                                 rhs=wz[:, kk, :], start=(kk == 0), stop=(kk == 7))
                    if kk == 0:
                        k.op("tensor", fn, reads=[b_wz, b_x[ti // 4]], writes=[pb_])
                    else:
                        k.acc("tensor", fn, reads=[b_wz, b_x[ti // 4]], acc=[pb_])
                k.op("scalar", lambda e, i2=i2, pt_=pt_: e.activation(out=zst[i2][:], in_=pt_[:, :], func=AF.Silu), reads=[pb_], writes=[b_zst[i2]])
                k.op("gpsimd", lambda e, i2=i2: e.tensor_tensor(
                    out=gzb[i2][:].rearrange("p (h e) -> p h e", h=4), in0=zst[i2][:].rearrange("p (h e) -> p h e", h=4),
                    in1=prm[:, 8:136].unsqueeze(1).to_broadcast([128, 4, 128]), op=ALU.mult),
                    reads=[b_zst[i2], b_prm], writes=[b_gzb[i2]])
                k.dma("sync", gz_d[ti - 16], gzb[i2][:], reads=[b_gzb[i2]], writes=[C.b_gz_d])

        S.barrier()
        gt = sbb("gt", [128, 12, 128])
        b_gt = [Buf() for _ in range(12)]
        G, BETA, GC, EGC, ETAIL, BEGE, EGL0, EGL1, GCL, TMP, NEGA, TMP2 = range(12)
        v3 = lambda idx: gt[:, idx, :].rearrange("p (t h) -> p t h", h=4)
        k.op("scalar", lambda e: e.activation(out=gt[:, NEGA, 0:4], in_=prm[:, 0:4], func=AF.Exp), reads=[b_prm], writes=[b_gt[NEGA]])
        k.op("vector", lambda e: e.tensor_tensor(out=v3(TMP), in0=ab_sb[:, :, 0:4], in1=prm[:, 4:8].unsqueeze(1).to_broadcast([128, 32, 4]), op=ALU.add),
             reads=[b_ab, b_prm], writes=[b_gt[TMP]])
        k.op("scalar", lambda e: e.activation(out=gt[:, TMP, :], in_=gt[:, TMP, :], func=AF.Exp), reads=[b_gt[TMP]], writes=[b_gt[TMP]])
        k.op("scalar", lambda e: e.activation(out=gt[:, TMP, :], in_=gt[:, TMP, :], func=AF.Ln, bias=1.0), reads=[b_gt[TMP]], writes=[b_gt[TMP]])
        k.op("vector", lambda e: e.scalar_tensor_tensor(out=v3(G), in0=v3(TMP), scalar=-1.0, in1=gt[:, NEGA, 0:4].unsqueeze(1).to_broadcast([128, 32, 4]),
                                                       op0=ALU.mult, op1=ALU.mult), reads=[b_gt[TMP], b_gt[NEGA]], writes=[b_gt[G]])
        k.op("scalar", lambda e: e.activation(out=v3(BETA), in_=ab_sb[:, :, 4:8], func=AF.Sigmoid), reads=[b_ab], writes=[b_gt[BETA]])
        for (mask, dst, bank) in ((LMASK, GC, 0), (CM0, EGL0, 1), (CM1, EGL1, 2)):
            k.op("tensor", lambda e, mask=mask, bank=bank: e.matmul(psT[bank][:, 0:128], lhsT=cst[:, mask, :], rhs=gt[:, G, :], start=True, stop=True),
                 reads=[b_cst, b_gt[G]], writes=[psB[bank]])
            k.op("vector", lambda e, dst=dst, bank=bank: e.tensor_copy(out=gt[:, dst, :], in_=psT[bank][:, 0:128]), reads=[psB[bank]], writes=[b_gt[dst]])
        k.op("vector", lambda e: e.tensor_copy(out=gt[0:64, GCL, :], in_=gt[0:64, EGL0, :]), reads=[b_gt[EGL0]], writes=[b_gt[GCL]])
        k.op("vector", lambda e: e.tensor_copy(out=gt[64:128, GCL, :], in_=gt[64:128, EGL1, :]), reads=[b_gt[EGL1], b_gt[GCL]], writes=[b_gt[GCL]])
        k.op("vector", lambda e: e.tensor_tensor(out=gt[:, TMP2, :], in0=gt[:, GCL, :], in1=gt[:, GC, :], op=ALU.subtract),
             reads=[b_gt[GCL], b_gt[GC]], writes=[b_gt[TMP2]])
        k.op("scalar", lambda e: e.activation(out=gt[:, ETAIL, :], in_=gt[:, TMP2, :], func=AF.Exp), reads=[b_gt[TMP2]], writes=[b_gt[ETAIL]])
        k.op("scalar", lambda e: e.activation(out=gt[:, EGC, :], in_=gt[:, GC, :], func=AF.Exp), reads=[b_gt[GC]], writes=[b_gt[EGC]])
        k.op("scalar", lambda e: e.activation(out=gt[:, EGL0, :], in_=gt[:, EGL0, :], func=AF.Exp), reads=[b_gt[EGL0], b_gt[GCL]], writes=[b_gt[EGL0]])
        k.op("scalar", lambda e: e.activation(out=gt[:, EGL1, :], in_=gt[:, EGL1, :], func=AF.Exp), reads=[b_gt[EGL1], b_gt[GCL]], writes=[b_gt[EGL1]])
        k.op("vector", lambda e: e.tensor_tensor(out=gt[:, BEGE, :], in0=gt[:, BETA, :], in1=gt[:, EGC, :], op=ALU.mult),
             reads=[b_gt[BETA], b_gt[EGC]], writes=[b_gt[BEGE]])

        with ExitStack() as s2:
            sb2 = lambda n, s, dt=F32: s2.enter_context(nc.sbuf_tensor(n, list(s), dt))
            wu = [sb2("wu%d" % i, [128, 8, 128], BF16) for i in range(2)]
            b_wu = [Buf("wu%d" % i) for i in range(2)]
            ub = [sb2("ub%d" % i, [128, 515]) for i in range(4)]
            b_ub = [Buf() for _ in range(4)]
            cb = [sb2("cb%d" % i, [128, 512]) for i in range(4)]
            b_cb = [Buf() for _ in range(4)]
            sq = [sb2("sq%d" % i, [128, 512]) for i in range(4)]
            b_sq = [Buf() for _ in range(4)]
            rt = [sb2("rt%d" % i, [128, 512]) for i in range(4)]
            b_rt = [Buf() for _ in range(4)]
            ob = [sb2("ob%d" % i, [128, 512], BF16) for i in range(4)]
            b_ob = [Buf("ob%d" % i) for i in range(4)]
            cnt = 0
            pendY = []
            for th in range(12):
                ty, hb = divmod(th, 4)
                wi = th % 2
                c0 = COL_UB + 128 * th
                k.gload(wu[wi][:], w_v[:, :, c0:c0 + 128], writes=[b_wu[wi]])
                chunks = range(4, 8) if ty == 0 else range(8)
                dst_d = (qT_d, kT_d, vT_d)[ty]
                b_dst = (C.b_qT_d, C.b_kT_d, C.b_vT_d)[ty]
                cw = lambda i, th=th: prm[:, 136 + 4 * th + i:136 + 4 * th + i + 1]
                first = True
                for tc in chunks:
                    ci = cnt % 4
                    cnt += 1
                    pt_, pb_ = psT[ci], psB[ci]
                    if first:
                        if ty == 0:
                            hp, hpb = psT[(ci + 1) % 4], psB[(ci + 1) % 4]
                            for kk in range(8):
                                fn = lambda e, kk=kk, wi=wi, hp=hp: e.matmul(hp[:, 0:4], lhsT=wu[wi][:, kk, :], rhs=xTb[:, kk, HALF - 4:HALF],
                                                                            start=(kk == 0), stop=(kk == 7))
                                if kk == 0:
                                    k.op("tensor", fn, reads=[b_wu[wi], b_x[3]], writes=[hpb])
                                else:
                                    k.acc("tensor", fn, reads=[b_wu[wi], b_x[3]], acc=[hpb])
                            k.op("vector", lambda e, ci=ci, hp=hp: e.tensor_copy(out=ub[ci][:, 0:3], in_=hp[:, 1:4]), reads=[hpb], writes=[b_ub[ci]])
                        else:
                            k.op("vector", lambda e, ci=ci: e.memset(ub[ci][:, 0:3], 0.0), writes=[b_ub[ci]])
                        first = False
                    else:
                        k.op("vector", lambda e, ci=ci: e.tensor_copy(out=ub[ci][:, 0:3], in_=ub[(ci - 1) % 4][:, 512:515]),
                             reads=[b_ub[(ci - 1) % 4]], writes=[b_ub[ci]])
                    for kk in range(8):
                        fn = lambda e, kk=kk, wi=wi, tc=tc, pt_=pt_: e.matmul(pt_[:, :], lhsT=wu[wi][:, kk, :], rhs=xTb[:, kk, 512 * tc:512 * tc + 512],
                                                                            start=(kk == 0), stop=(kk == 7))
                        if kk == 0:
                            k.op("tensor", fn, reads=[b_wu[wi], b_x[tc]], writes=[pb_])
                        else:
                            k.acc("tensor", fn, reads=[b_wu[wi], b_x[tc]], acc=[pb_])
                    k.op("scalar", lambda e, ci=ci, pt_=pt_: e.activation(out=ub[ci][:, 3:515], in_=pt_[:, :], func=AF.Copy), reads=[pb_], writes=[b_ub[ci]])
                    if tc == 7:
                        k.op("gpsimd", lambda e, ci=ci, th=th: e.tensor_copy(out=convp_sb[:, th, :], in_=ub[ci][:, 512:515]), reads=[b_ub[ci]], writes=[b_convp])
                    k.op("vector", lambda e, ci=ci, cw=cw: e.tensor_scalar(out=cb[ci][:], in0=ub[ci][:, 3:515], scalar1=cw(3), scalar2=None, op0=ALU.mult),
                         reads=[b_ub[ci], b_prm], writes=[b_cb[ci]])
                    for i in range(3):
                        k.op("vector", lambda e, ci=ci, cw=cw, i=i: e.scalar_tensor_tensor(out=cb[ci][:], in0=ub[ci][:, i:i + 512], scalar=cw(i), in1=cb[ci][:],
                                                                                       op0=ALU.mult, op1=ALU.add), reads=[b_ub[ci], b_cb[ci]], writes=[b_cb[ci]])
                    k.op("scalar", lambda e, ci=ci: e.activation(out=cb[ci][:], in_=cb[ci][:], func=AF.Silu), reads=[b_cb[ci]], writes=[b_cb[ci]])
                    if ty == 2:
                        k.op("gpsimd", lambda e, ci=ci: e.tensor_copy(out=ob[ci][:], in_=cb[ci][:]), reads=[b_cb[ci]], writes=[b_ob[ci]])
                    else:
                        k.op("gpsimd", lambda e, ci=ci: e.tensor_tensor(out=sq[ci][:], in0=cb[ci][:], in1=cb[ci][:], op=ALU.mult), reads=[b_cb[ci]], writes=[b_sq[ci]])
                        np_, npb = psT[4 + ci], psB[4 + ci]
                        k.op("tensor", lambda e, ci=ci, np_=np_: e.matmul(np_[:, :], lhsT=ones_f[:], rhs=sq[ci][:], start=True, stop=True),
                             reads=[b_sq[ci], C.b_ones], writes=[npb])
                        k.op("scalar", lambda e, ci=ci, np_=np_: e.activation(out=rt[ci][:], in_=np_[:, :], func=AF.Sqrt, bias=C.eps6[:, 0:1]),
                             reads=[npb, C.b_eps], writes=[b_rt[ci]])
                    t0 = 512 * tc - (HALF if ty == 0 else 0)
                    sc = (128.0 ** -0.5) if ty == 0 else 1.0

                    def tailY(ci=ci, ty=ty, sc=sc, dst_d=dst_d, b_dst=b_dst, hb=hb, t0=t0):
                        if ty != 2:
                            k.op("vector", lambda e, ci=ci: e.reciprocal(out=rt[ci][:], in_=rt[ci][:]), reads=[b_rt[ci]], writes=[b_rt[ci]])
                            k.op("vector", lambda e, ci=ci, sc=sc: e.scalar_tensor_tensor(out=ob[ci][:], in0=cb[ci][:], scalar=sc, in1=rt[ci][:], op0=ALU.mult, op1=ALU.mult),
                                 reads=[b_cb[ci], b_rt[ci]], writes=[b_ob[ci]])
                        k.dma("sync", dst_d[:, hb, t0:t0 + 512], ob[ci][:], reads=[b_ob[ci]], writes=[b_dst])
                    pendY.append(tailY)
                    if len(pendY) > 2:
                        pendY.pop(0)()
            while pendY:
                pendY.pop(0)()
            convp_v = C.conv_p.rearrange("r (c p) -> p c r", p=128)
            for th in range(12):
                final.append(k.dma("sync", convp_v[:, th, :], convp_sb[:, th, :], reads=[b_convp], slot="convp", allow_slow_non_contiguous=True))

        S.barrier()
        sC = sB
        sbc = lambda n, s, dt=F32: sC.enter_context(nc.sbuf_tensor(n, list(s), dt))
        f_slots = [(psT[b][:, 0:128], psB[b]) for b in range(6)]
        psbf = [psT[6].bitcast(BF16), psT[7].bitcast(BF16)]
        h_slots = [(psbf[b][:, 0:128], psB[6 + b]) for b in range(2)]
        cnts = {"f": 0, "h": 0}

        def fslot():
            s_ = f_slots[cnts["f"] % len(f_slots)]
            cnts["f"] += 1
            return s_

        def hslot():
            s_ = h_slots[cnts["h"] % len(h_slots)]
            cnts["h"] += 1
            return s_

        class Pool_:
            def __init__(self, name, n, dt):
                self.t = [sbc("%s%d" % (name, i), [128, 128], dt) for i in range(n)]
                self.b = [Buf() for _ in range(n)]
                self.i = 0

            def get(self):
                j = self.i % len(self.t)
                self.i += 1
                return self.t[j], self.b[j]

        PF = Pool_("pf", 40, F32)
        PH = Pool_("ph", 96, BF16)
        Sst = [sbc("Sst%d" % h, [128, 128]) for h in range(4)]
        Sbf = [sbc("Sbf%d" % h, [128, 128], BF16) for h in range(4)]
        b_S = [Buf() for _ in range(4)]
        b_Sb = [Buf() for _ in range(4)]
        for h in range(4):
            k.op("vector", lambda e, h=h: e.memset(Sst[h][:], 0.0), writes=[b_S[h]])
            k.op("vector", lambda e, h=h: e.memset(Sbf[h][:], 0.0), writes=[b_Sb[h]])
        kt_t = [sbc("kt_t%d" % i, [128, 4, 128], BF16) for i in range(2)]
        vt_t = [sbc("vt_t%d" % i, [128, 4, 128], BF16) for i in range(2)]
        qt_t = [sbc("qt_t%d" % i, [128, 4, 128], BF16) for i in range(2)]
        gz_t = [sbc("gz_t%d" % i, [128, 512], BF16) for i in range(2)]
        b_kt = [Buf("kt_t%d" % i) for i in range(2)]
        b_vt = [Buf("vt_t%d" % i) for i in range(2)]
        b_qt = [Buf("qt_t%d" % i) for i in range(2)]
        b_gzt = [Buf("gz_t%d" % i) for i in range(2)]
        Ukeep = [[sbc("Uk%d_%d" % (i, h), [128, 128]) for h in range(4)] for i in range(2)]
        WTkeep = [[sbc("WTk%d_%d" % (i, h), [128, 128], BF16) for h in range(4)] for i in range(2)]
        ktlkeep = [[sbc("ktlk%d_%d" % (i, h), [128, 128], BF16) for h in range(4)] for i in range(2)]
        qkTkeep = [[sbc("qkTk%d_%d" % (i, h), [128, 128], BF16) for h in range(4)] for i in range(2)]
        b_Uk = [[Buf() for h in range(4)] for i in range(2)]
        b_WTk = [[Buf() for h in range(4)] for i in range(2)]
        b_ktlk = [[Buf() for h in range(4)] for i in range(2)]
        b_qkTk = [[Buf() for h in range(4)] for i in range(2)]
        ss = sbc("ss", [128, 8])
        b_ss = [Buf() for _ in range(8)]
        junk = sbc("junk", [128, 128])
        b_junk = Buf()
        mxs = [sbc("mxs%d" % i, [128, 4, 128], BF16) for i in range(2)]
        b_mxs = [Buf("mxs%d" % i) for i in range(2)]
        gt_ = gt
        col = lambda idx, c_: gt_[:, idx, c_:c_ + 1]

        def make_tile(ti):
            own = ti >= 16
            bi = ti % 2
            k.dma("sync", kt_t[bi][:], kT_d[:, :, 128 * ti:128 * ti + 128], reads=[C.b_kT_d], writes=[b_kt[bi]])
            k.dma("sync", vt_t[bi][:], vT_d[:, :, 128 * ti:128 * ti + 128], reads=[C.b_vT_d], writes=[b_vt[bi]])
            if own:
                k.dma("sync", qt_t[bi][:], qT_d[:, :, 128 * (ti - 16):128 * (ti - 16) + 128], reads=[C.b_qT_d], writes=[b_qt[bi]])
                k.dma("sync", gz_t[bi][:], gz_d[ti - 16], reads=[C.b_gz_d], writes=[b_gzt[bi]])
            HS = [None] * 4
            def stage1(hb):
                c_ = ti * 4 + hb
                kT = kt_t[bi][:, hb, :]
                vT = vt_t[bi][:, hb, :]
                qT = qt_t[bi][:, hb, :]
                rd_k, rd_v, rd_q = [b_kt[bi]], [b_vt[bi]], [b_qt[bi]]
                nd, b_nd = PF.get()
                k.op("gpsimd", lambda e, nd=nd, c_=c_: e.tensor_scalar(out=nd[:], in0=cst[:, NEGID, :], scalar1=col(GC, c_), scalar2=None, op0=ALU.mult),
                     reads=[b_cst, b_gt[GC]], writes=[b_nd])
                pD, bD = fslot()
                yield
                k.op("tensor", lambda e, pD=pD, nd=nd: e.matmul(pD, lhsT=ones_f[:], rhs=nd[:], start=True, stop=False), reads=[b_nd, C.b_ones], writes=[bD])
                k.acc("tensor", lambda e, pD=pD: e.matmul(pD, lhsT=cst[:, IDENT, :], rhs=cst[:, NEGM, :], start=False, stop=True), reads=[b_cst], acc=[bD])
                Ec, b_Ec = PF.get()
                k.op("scalar", lambda e, Ec=Ec, pD=pD, c_=c_: e.activation(out=Ec[:], in_=pD, func=AF.Exp, bias=col(GC, c_)),
                     reads=[bD, b_gt[GC]], writes=[b_Ec])
                Es, b_Es = PF.get()
                k.op("gpsimd", lambda e, Es=Es, Ec=Ec: e.tensor_tensor(out=Es[:], in0=Ec[:], in1=cst[:, STRICT, :], op=ALU.mult),
                     reads=[b_Ec, b_cst], writes=[b_Es])
                pK, bK = fslot()
                yield
                k.op("tensor", lambda e, pK=pK, kT=kT: e.matmul(pK, lhsT=kT, rhs=kT, start=True, stop=True), reads=rd_k, writes=[bK])
                A, b_A = PH.get()
                k.op("vector", lambda e, A=A, pK=pK, Es=Es, c_=c_: e.scalar_tensor_tensor(out=A[:], in0=pK, scalar=col(BETA, c_), in1=Es[:], op0=ALU.mult, op1=ALU.mult),
                     reads=[bK, b_Es, b_gt[BETA]], writes=[b_A])
                pT_, bT_ = hslot()
                yield
                k.op("tensor", lambda e, pT_=pT_, A=A: e.transpose(pT_, A[:], identb[:]), reads=[b_A, b_identb], writes=[bT_])
                Bm, b_Bm = PH.get()
                k.op("vector", lambda e, Bm=Bm, pT_=pT_: e.tensor_copy(out=Bm[:], in_=pT_), reads=[bT_], writes=[b_Bm])
                P, b_P = PH.get()
                k.op("vector", lambda e, P=P, pT_=pT_: e.tensor_tensor(out=P[:], in0=cst[:, IDENT, :], in1=pT_, op=ALU.subtract), reads=[bT_, b_cst], writes=[b_P])
                X, b_X, Y, b_Y = A, b_A, Bm, b_Bm
                for m in range(1, 6):
                    pX, bX = fslot()
                    yield
                    k.op("tensor", lambda e, pX=pX, X=X, Y=Y: e.matmul(pX, lhsT=Y[:], rhs=X[:], start=True, stop=True), reads=[b_X, b_Y], writes=[bX])
                    Xn, b_Xn = PH.get()
                    if os.environ.get("ACTEV", "0") == "1":
                        k.op("scalar", lambda e, Xn=Xn, pX=pX: e.activation(out=Xn[:], in_=pX, func=AF.Copy), reads=[bX], writes=[b_Xn])
                    else:
                        k.op("vector", lambda e, Xn=Xn, pX=pX: e.tensor_copy(out=Xn[:], in_=pX), reads=[bX], writes=[b_Xn])
                    if m < 5:
                        pY, bY = fslot()
                        yield
                        k.op("tensor", lambda e, pY=pY, X=X, Y=Y: e.matmul(pY, lhsT=X[:], rhs=Y[:], start=True, stop=True), reads=[b_X, b_Y], writes=[bY])
                        Yn, b_Yn = PH.get()
                        if os.environ.get("ACTEV", "0") == "1":
                            k.op("scalar", lambda e, Yn=Yn, pY=pY: e.activation(out=Yn[:], in_=pY, func=AF.Copy), reads=[bY], writes=[b_Yn])
                        else:
                            k.op("vector", lambda e, Yn=Yn, pY=pY: e.tensor_copy(out=Yn[:], in_=pY), reads=[bY], writes=[b_Yn])
                    pP, bP = fslot()
                    yield
                    k.op("tensor", lambda e, pP=pP, Xn=Xn, P=P: e.matmul(pP, lhsT=Xn[:], rhs=P[:], start=True, stop=True), reads=[b_Xn, b_P], writes=[bP])
                    Pn, b_Pn = PH.get()
                    k.op("vector", lambda e, Pn=Pn, pP=pP, P=P: e.tensor_tensor(out=Pn[:], in0=pP, in1=P[:], op=ALU.add), reads=[bP, b_P], writes=[b_Pn])
                    P, b_P = Pn, b_Pn
                    X, b_X = Xn, b_Xn
                    if m < 5:
                        Y, b_Y = Yn, b_Yn
                pk_, bk_ = hslot()
                yield
                k.op("tensor", lambda e, pk_=pk_, kT=kT: e.transpose(pk_, kT, identb[:]), reads=rd_k + [b_identb], writes=[bk_])
                Rw, b_Rw = PH.get()
                k.op("vector", lambda e, Rw=Rw, pk_=pk_, c_=c_: e.tensor_scalar(out=Rw[:], in0=pk_, scalar1=col(BEGE, c_), scalar2=None, op0=ALU.mult),
                     reads=[bk_, b_gt[BEGE]], writes=[b_Rw])
                ktl, b_ktl = ktlkeep[bi][hb], b_ktlk[bi][hb]
                k.op("vector", lambda e, ktl=ktl, pk_=pk_, c_=c_: e.tensor_scalar(out=ktl[:], in0=pk_, scalar1=col(ETAIL, c_), scalar2=None, op0=ALU.mult),
                     reads=[bk_, b_gt[ETAIL]], writes=[b_ktl])
                pv_, bv_ = hslot()
                yield
                k.op("tensor", lambda e, pv_=pv_, vT=vT: e.transpose(pv_, vT, identb[:]), reads=rd_v + [b_identb], writes=[bv_])
                Ru, b_Ru = PH.get()
                k.op("vector", lambda e, Ru=Ru, pv_=pv_, c_=c_: e.tensor_scalar(out=Ru[:], in0=pv_, scalar1=col(BETA, c_), scalar2=None, op0=ALU.mult),
                     reads=[bv_, b_gt[BETA]], writes=[b_Ru])
                pU, bU = fslot()
                yield
                k.op("tensor", lambda e, pU=pU, P=P, Ru=Ru: e.matmul(pU, lhsT=P[:], rhs=Ru[:], start=True, stop=True), reads=[b_P, b_Ru], writes=[bU])
                U, b_U = Ukeep[bi][hb], b_Uk[bi][hb]
                k.op("vector", lambda e, U=U, pU=pU: e.tensor_copy(out=U[:], in_=pU), reads=[bU], writes=[b_U])
                pW, bW = fslot()
                yield
                k.op("tensor", lambda e, pW=pW, P=P, Rw=Rw: e.matmul(pW, lhsT=Rw[:], rhs=P[:], start=True, stop=True), reads=[b_P, b_Rw], writes=[bW])
                WT, b_WT = WTkeep[bi][hb], b_WTk[bi][hb]
                k.op("vector", lambda e, WT=WT, pW=pW: e.tensor_copy(out=WT[:], in_=pW), reads=[bW], writes=[b_WT])
                qkT = b_qkT = None
                if own:
                    pQ, bQ = fslot()
                    yield
                    k.op("tensor", lambda e, pQ=pQ, qT=qT, kT=kT: e.matmul(pQ, lhsT=qT, rhs=kT, start=True, stop=True), reads=rd_q + rd_k, writes=[bQ])
                    qk, b_qk = PH.get()
                    k.op("vector", lambda e, qk=qk, pQ=pQ, Ec=Ec: e.tensor_tensor(out=qk[:], in0=pQ, in1=Ec[:], op=ALU.mult), reads=[bQ, b_Ec], writes=[b_qk])
                    pq2, bq2 = hslot()
                    yield
                    k.op("tensor", lambda e, pq2=pq2, qk=qk: e.transpose(pq2, qk[:], identb[:]), reads=[b_qk, b_identb], writes=[bq2])
                    qkT, b_qkT = qkTkeep[bi][hb], b_qkTk[bi][hb]
                    k.op("vector", lambda e, qkT=qkT, pq2=pq2: e.tensor_copy(out=qkT[:], in_=pq2), reads=[bq2], writes=[b_qkT])
                HS[hb] = (dict(U=U, b_U=b_U, WT=WT, b_WT=b_WT, ktl=ktl, b_ktl=b_ktl, qkT=qkT, b_qkT=b_qkT, qT=qT, rd_q=rd_q, c_=c_))
            def stage23():
                outs = []
                if own:
                    for hb in range(4):
                        o_, b_o = PF.get()
                        outs.append((o_, b_o))
                for ch in range(2):
                    r0 = 64 * ch
                    for hb in range(4):
                        H = HS[hb]
                        c_ = H["c_"]
                        yield
                        pV, bV = fslot()
                        k.op("tensor", lambda e, pV=pV, H=H, hb=hb, r0=r0: e.matmul(pV[r0:r0 + 64, :], lhsT=H["WT"][:, r0:r0 + 64], rhs=Sbf[hb][:], start=True, stop=True),
                             reads=[H["b_WT"], b_Sb[hb]], writes=[bV])
                        vn, b_vn = PH.get()
                        k.op("vector", lambda e, vn=vn, pV=pV, H=H, r0=r0: e.tensor_tensor(out=vn[r0:r0 + 64, :], in0=H["U"][r0:r0 + 64, :], in1=pV[r0:r0 + 64, :], op=ALU.subtract),
                             reads=[bV, H["b_U"]], writes=[b_vn])
                        if own:
                            o_, b_o = outs[hb]
                            yield
                            p1, b1 = fslot()
                            k.op("tensor", lambda e, p1=p1, H=H, hb=hb, r0=r0: e.matmul(p1[r0:r0 + 64, :], lhsT=H["qT"][:, r0:r0 + 64], rhs=Sbf[hb][:], start=True, stop=True),
                                 reads=H["rd_q"] + [b_Sb[hb]], writes=[b1])
                            p2, b2 = fslot()
                            k.op("tensor", lambda e, p2=p2, H=H, vn=vn, r0=r0: e.matmul(p2[r0:r0 + 64, :], lhsT=H["qkT"][r0:r0 + 64, r0:r0 + 64], rhs=vn[r0:r0 + 64, :], start=True, stop=True),
                                 reads=[H["b_qkT"], b_vn], writes=[b2])
                            o2, b_o2 = PF.get()
                            k.op("vector", lambda e, o2=o2, p2=p2, r0=r0: e.tensor_copy(out=o2[r0:r0 + 64, :], in_=p2[r0:r0 + 64, :]), reads=[b2], writes=[b_o2])
                            k.op("vector", lambda e, o_=o_, p1=p1, o2=o2, r0=r0, c_=c_: e.scalar_tensor_tensor(
                                out=o_[r0:r0 + 64, :], in0=p1[r0:r0 + 64, :], scalar=gt_[r0:r0 + 64, EGC, c_:c_ + 1], in1=o2[r0:r0 + 64, :], op0=ALU.mult, op1=ALU.add),
                                reads=[b1, b_o2, b_gt[EGC]], writes=[b_o])
                        yield
                        pS, bS_ = fslot()
                        k.op("tensor", lambda e, pS=pS, H=H, vn=vn, r0=r0: e.matmul(pS, lhsT=H["ktl"][r0:r0 + 64, :], rhs=vn[r0:r0 + 64, :], start=True, stop=True),
                             reads=[H["b_ktl"], b_vn], writes=[bS_])
                        egl = EGL0 if ch == 0 else EGL1
                        k.op("vector", lambda e, pS=pS, hb=hb, egl=egl, c_=c_: e.scalar_tensor_tensor(
                            out=Sst[hb][:], in0=Sst[hb][:], scalar=col(egl, c_), in1=pS, op0=ALU.mult, op1=ALU.add),
                            reads=[bS_, b_gt[egl], b_S[hb]], writes=[b_S[hb]])
                        k.op("scalar", lambda e, hb=hb: e.activation(out=Sbf[hb][:], in_=Sst[hb][:], func=AF.Copy), reads=[b_S[hb]], writes=[b_Sb[hb]])
                if own:
                    for hb in range(4):
                        o_, b_o = outs[hb]
                        si = (ti * 4 + hb) % 8
                        yield
                        k.op("scalar", lambda e, o_=o_, si=si: e.activation(out=junk[:], in_=o_[:], func=AF.Square, accum_out=ss[:, si:si + 1]),
                             reads=[b_o], writes=[b_junk, b_ss[si]])
                        k.op("scalar", lambda e, si=si: e.activation(out=ss[:, si:si + 1], in_=ss[:, si:si + 1], func=AF.Sqrt, scale=1.0 / 128.0, bias=C.eps6[:, 0:1]),
                             reads=[b_ss[si], C.b_eps], writes=[b_ss[si]])
                        k.op("vector", lambda e, si=si: e.reciprocal(out=ss[:, si:si + 1], in_=ss[:, si:si + 1]), reads=[b_ss[si]], writes=[b_ss[si]])
                        on, b_on = PH.get()
                        k.op("vector", lambda e, on=on, o_=o_, si=si, hb=hb, bi=bi: e.scalar_tensor_tensor(
                            out=on[:], in0=o_[:], scalar=ss[:, si:si + 1], in1=gz_t[bi][:, 128 * hb:128 * hb + 128], op0=ALU.mult, op1=ALU.mult),
                            reads=[b_o, b_ss[si], b_gzt[bi]], writes=[b_on])
                        yield
                        pm, bm = hslot()
                        k.op("tensor", lambda e, pm=pm, on=on: e.transpose(pm, on[:], identb[:]), reads=[b_on, b_identb], writes=[bm])
                        k.op("vector", lambda e, pm=pm, hb=hb, bi=bi: e.tensor_copy(out=mxs[bi][:, hb, :], in_=pm),
                             reads=[bm], writes=[b_mxs[bi]])
                    k.dma("sync", C.mixB_d[:, :, 128 * (ti - 16):128 * (ti - 16) + 128], mxs[bi][:], reads=[b_mxs[bi]], writes=[C.b_mixB_d])
            return [stage1(hb) for hb in range(4)], stage23


        pending = None
        for ti in range(33):
            gens = []
            nxt = None
            if ti < 32:
                s1, nxt = make_tile(ti)
                gens += s1
            if pending is not None:
                gens.append(pending())
            while gens:
                for g_ in list(gens):
                    try:
                        next(g_)
                    except StopIteration:
                        gens.remove(g_)
            pending = nxt
        for hb in range(4):
            final.append(k.dma("sync", C.ssm_p[hb], Sst[hb][:], reads=[b_S[hb]], slot="ssmp"))

def phase_c(C):
    nc, k, S = C.nc, C.k, C.S
    psT, psB = C.psT, C.psB
    cst, b_cst = C.cst, C.b_cst
    IDENT = 0
    final = C.final
    NT = C.NT
    NTOK = NT * 128
    ALPHA = 2.0 ** 0.25
    KC = int(os.environ.get('KC', '99'))

    with ExitStack() as sC:
        sbc = lambda n, s, dt=F32: sC.enter_context(nc.sbuf_tensor(n, list(s), dt))
        Mg = sbc("Mg", [128, NT, 4])
        b_Mg = Buf()
        Ghl = sbc("Ghl", [128, NT, 4, 16], BF16)
        b_Ghl = Buf()
        identb = sbc("identbC", [128, 128], BF16)
        b_identb = Buf()
        k.op("vector", lambda e: e.tensor_copy(out=identb[:], in_=cst[:, IDENT, :]), reads=[b_cst], writes=[b_identb])
        yacc = sbc("yacc", [128, NT, 1024])
        b_y = [Buf() for _ in range(NT)]
        G = sbc("G", [128, NT, 32])
        b_G = [Buf() for _ in range(NT)]
        st6_c3 = sbc("st6b", [128, 2, 2, 6])
        mv_c3 = sbc("mvb", [128, 2, 2])

        with ExitStack() as s1:
            sb1 = lambda n, s, dt=F32: s1.enter_context(nc.sbuf_tensor(n, list(s), dt))
            lnp = sb1("lnp_sb", [128, 2, 1024])
            b_lnp = Buf("lnp")
            k.dma("sync", lnp[:], C.lnp_d[:, 0:2, :], writes=[b_lnp])
            wo = sb1("wo", [128, 8, 1024], BF16)
            b_wo = Buf("wo")
            k.gload(wo[:], C.w_out.rearrange("(k p) c -> p k c", p=128), writes=[b_wo])
            wr = sb1("wr_sb", [128, 8, 36])
            b_wr = Buf("wr")
            k.dma("sync", wr[:], C.wr_d.rearrange("(k p) c -> p k c", p=128), writes=[b_wr])
            rb = sb1("rb_sb", [128, 36])
            b_rb = Buf("rb")
            k.dma("sync", rb[:], C.rb_d, writes=[b_rb])
            mx = [sb1("mx%d" % i, [128, 8, 128], BF16) for i in range(2)]
            b_mx = [Buf("mx%d" % i) for i in range(2)]
            xt = [sb1("xt%d" % i, [128, 1024]) for i in range(2)]
            b_xt = [Buf("xt%d" % i) for i in range(2)]
            rr = [sb1("rr%d" % i, [128, 1024]) for i in range(2)]
            b_rr = [Buf() for _ in range(2)]
            hh = [sb1("hh%d" % i, [128, 1024]) for i in range(2)]
            b_hh = [Buf() for _ in range(2)]
            hbb = [sb1("hbb%d" % i, [128, 1024], BF16) for i in range(2)]
            b_hbb = [Buf("hbb%d" % i) for i in range(2)]
            hTf = [sb1("hTf%d" % i, [128, 8, 128]) for i in range(2)]
            b_hTf = [Buf() for _ in range(2)]
            st6 = sb1("st6", [128, 2, 2, 6])
            mv = sb1("mv", [128, 2, 2])
            b_st = [Buf() for _ in range(2)]
            b_mv = [Buf() for _ in range(2)]
            tmpr = sb1("tmpr", [128, 2, 32])
            b_tmp = [Buf() for _ in range(2)]
            sm = sb1("sm", [128, 2, 96])
            b_sm = [Buf() for _ in range(2)]
            def c1_tile(ti):
                bi = ti % 2
                is_s = ti >= 16
                if not is_s:
                    k.dma("sync", mx[bi][:, 0:4, :], C.mixA_d[:, :, 128 * ti:128 * ti + 128], reads=[C.b_mixA_d], writes=[b_mx[bi]])
                    k.dma("sync", mx[bi][:, 4:8, :], C.mixB_d[:, :, 128 * ti:128 * ti + 128], reads=[C.b_mixB_d], writes=[b_mx[bi]])
                    k.dma("sync", xt[bi][:], C.xo[128 * ti:128 * ti + 128, :], writes=[b_xt[bi]])
                else:
                    k.dma("sync", mx[bi][:], C.mixS_d, reads=[C.b_mixS_d], writes=[b_mx[bi]])
                    k.dma("sync", xt[bi][:], C.xs_pad, writes=[b_xt[bi]])
                for half in range(2 if KC >= 7 else 0):
                    pt_, pb_ = psT[half], psB[half]
                    for kk in range(8):
                        fn = lambda e, kk=kk, bi=bi, half=half, pt_=pt_: e.matmul(pt_[:, :], lhsT=mx[bi][:, kk, :], rhs=wo[:, kk, 512 * half:512 * half + 512],
                                                                                start=(kk == 0), stop=(kk == 7))
                        if kk == 0:
                            k.op("tensor", fn, reads=[b_mx[bi], b_wo], writes=[pb_])
                        else:
                            k.acc("tensor", fn, reads=[b_mx[bi], b_wo], acc=[pb_])
                    if KC < 8:
                        continue
                    k.op("vector", lambda e, bi=bi, half=half, pt_=pt_: e.scalar_tensor_tensor(
                        out=rr[bi][:, 512 * half:512 * half + 512], in0=xt[bi][:, 512 * half:512 * half + 512], scalar=ALPHA, in1=pt_[:, :],
                        op0=ALU.mult, op1=ALU.add), reads=[pb_, b_xt[bi]], writes=[b_rr[bi]])
                    k.op("vector", lambda e, bi=bi, half=half: e.bn_stats(out=st6[:, bi, half, :], in_=rr[bi][:, 512 * half:512 * half + 512]),
                         reads=[b_rr[bi]], writes=[b_st[bi]])
                    yield
                if KC < 11:
                    return
                k.op("vector", lambda e, bi=bi: e.bn_aggr(out=mv[:, bi, :], in_=st6[:, bi, :, :].rearrange("p a b -> p (a b)")), reads=[b_st[bi]], writes=[b_mv[bi]])
                k.op("scalar", lambda e, bi=bi: e.activation(out=mv[:, bi, 1:2], in_=mv[:, bi, 1:2], func=AF.Sqrt, bias=C.eps5[:, 0:1]), reads=[b_mv[bi], C.b_eps5], writes=[b_mv[bi]])
                k.op("vector", lambda e, bi=bi: e.reciprocal(out=mv[:, bi, 1:2], in_=mv[:, bi, 1:2]), reads=[b_mv[bi]], writes=[b_mv[bi]])
                k.op("vector", lambda e, bi=bi: e.tensor_scalar(out=hh[bi][:], in0=rr[bi][:], scalar1=mv[:, bi, 0:1], scalar2=mv[:, bi, 1:2], op0=ALU.subtract, op1=ALU.mult),
                     reads=[b_rr[bi], b_mv[bi]], writes=[b_hh[bi]])
                k.op("gpsimd", lambda e, bi=bi: e.tensor_tensor(out=hh[bi][:], in0=hh[bi][:], in1=lnp[:, 0, :], op=ALU.mult), reads=[b_hh[bi], b_lnp], writes=[b_hh[bi]])
                k.op("gpsimd", lambda e, bi=bi: e.tensor_tensor(out=hh[bi][:], in0=hh[bi][:], in1=lnp[:, 1, :], op=ALU.add), reads=[b_hh[bi], b_lnp], writes=[b_hh[bi]])
                k.op("gpsimd", lambda e, bi=bi, ti=ti: e.tensor_scalar(out=yacc[:, ti, :], in0=hh[bi][:], scalar1=ALPHA, scalar2=None, op0=ALU.mult),
                     reads=[b_hh[bi]], writes=[b_y[ti]])
                if KC < 12:
                    return
                yield
                k.op("gpsimd", lambda e, bi=bi: e.tensor_copy(out=hbb[bi][:], in_=hh[bi][:]), reads=[b_hh[bi]], writes=[b_hbb[bi]])
                k.dma("sync", C.hb_d[128 * ti:128 * ti + 128, :], hbb[bi][:], reads=[b_hbb[bi]], writes=[C.b_hb_d])
                for g4 in range(2):
                    pt_, pb_ = psT[2 + g4], psB[2 + g4]
                    for j in range(4):
                        kk = 4 * g4 + j
                        fn = lambda e, kk=kk, j=j, bi=bi, pt_=pt_: e.transpose(pt_[:, 128 * j:128 * j + 128], hh[bi][:, 128 * kk:128 * kk + 128], cst[:, IDENT, :])
                        if j == 0:
                            k.op("tensor", fn, reads=[b_hh[bi], b_cst], writes=[pb_])
                        else:
                            k.acc("tensor", fn, reads=[b_hh[bi], b_cst], acc=[pb_])
                    k.op("vector", lambda e, g4=g4, bi=bi, pt_=pt_: e.tensor_copy(out=hTf[bi][:, 4 * g4:4 * g4 + 4, :], in_=pt_[:, :].rearrange("p (a c) -> p a c", a=4)),
                         reads=[pb_], writes=[b_hTf[bi]])
                    yield
                if KC < 13:
                    return
                pl, plb = psT[4], psB[4]
                for kk in range(8):
                    fn = lambda e, kk=kk, bi=bi: e.matmul(pl[:, 0:36], lhsT=hTf[bi][:, kk, :], rhs=wr[:, kk, :], start=(kk == 0), stop=(kk == 7))
                    if kk == 0:
                        k.op("tensor", fn, reads=[b_hTf[bi], b_wr], writes=[plb])
                    else:
                        k.acc("tensor", fn, reads=[b_hTf[bi], b_wr], acc=[plb])
                if KC < 14:
                    return
                R = lambda a, b_, bi=bi: sm[:, bi, a:b_]
                T3 = tmpr[:, bi, :].rearrange("p (g e) -> p g e", g=4)
                sm_b, tmp_b = b_sm[bi], b_tmp[bi]

                def VV(eng, method, reads, writes, **aps):
                    k.op(eng, lambda e, aps=aps, method=method: getattr(e, method)(**aps), reads=reads, writes=writes)
                VV("vector", "tensor_tensor", [plb, b_rb], [sm_b], out=R(0, 36), in0=pl[:, 0:36], in1=rb[:], op=ALU.add)
                yield
                VV("vector", "tensor_reduce", [sm_b], [sm_b], out=R(36, 37), in_=R(0, 4), axis=AX.X, op=ALU.max)
                VV("vector", "tensor_scalar", [sm_b], [sm_b], out=R(40, 44), in0=R(0, 4), scalar1=R(36, 37), scalar2=None, op0=ALU.is_equal)
                VV("vector", "tensor_scalar", [sm_b], [sm_b], out=R(37, 38), in0=R(36, 37), scalar1=-1.0, scalar2=None, op0=ALU.mult)
                VV("scalar", "activation", [sm_b], [sm_b], out=R(89, 93), in_=R(0, 4), func=AF.Exp, bias=R(37, 38), accum_out=R(38, 39))
                VV("vector", "reciprocal", [sm_b], [sm_b], out=R(38, 39), in_=R(38, 39))
                VV("vector", "tensor_tensor", [sm_b], [tmp_b], out=T3, in0=R(4, 36).rearrange("p (g e) -> p g e", g=4),
                   in1=R(40, 44).unsqueeze(2).to_broadcast([128, 4, 8]), op=ALU.mult)
                VV("vector", "tensor_reduce", [tmp_b], [sm_b], out=R(44, 52), in_=T3.rearrange("p g e -> p e g"), axis=AX.X, op=ALU.add)
                yield
                VV("vector", "tensor_reduce", [sm_b], [sm_b], out=R(52, 53), in_=R(44, 52), axis=AX.X, op=ALU.max)
                VV("vector", "tensor_scalar", [sm_b], [sm_b], out=R(54, 62), in0=R(44, 52), scalar1=R(52, 53), scalar2=None, op0=ALU.is_equal)
                VV("vector", "scalar_tensor_tensor", [sm_b], [sm_b], out=R(62, 70), in0=R(54, 62), scalar=-1e30, in1=R(44, 52), op0=ALU.mult, op1=ALU.add)
                VV("vector", "tensor_reduce", [sm_b], [sm_b], out=R(53, 54), in_=R(62, 70), axis=AX.X, op=ALU.max)
                VV("vector", "tensor_scalar", [sm_b], [sm_b], out=R(70, 78), in0=R(62, 70), scalar1=R(53, 54), scalar2=None, op0=ALU.is_equal)
                VV("vector", "tensor_tensor", [sm_b], [sm_b], out=R(78, 79), in0=R(53, 54), in1=R(52, 53), op=ALU.subtract)
                VV("scalar", "activation", [sm_b], [sm_b], out=R(78, 79), in_=R(78, 79), func=AF.Exp)
                yield
                VV("vector", "tensor_scalar", [sm_b], [sm_b], out=R(79, 80), in0=R(78, 79), scalar1=1.0, scalar2=None, op0=ALU.add)
                VV("vector", "reciprocal", [sm_b], [sm_b], out=R(79, 80), in_=R(79, 80))
                VV("vector", "tensor_tensor", [sm_b], [sm_b], out=R(80, 81), in0=R(78, 79), in1=R(79, 80), op=ALU.mult)
                VV("vector", "tensor_scalar", [sm_b], [sm_b], out=R(79, 81), in0=R(79, 81), scalar1=R(38, 39), scalar2=None, op0=ALU.mult)
                VV("vector", "tensor_scalar", [sm_b], [sm_b], out=R(81, 89), in0=R(54, 62), scalar1=R(79, 80), scalar2=None, op0=ALU.mult)
                VV("vector", "scalar_tensor_tensor", [sm_b], [sm_b], out=R(81, 89), in0=R(70, 78), scalar=R(80, 81), in1=R(81, 89), op0=ALU.mult, op1=ALU.add)
                VV("vector", "tensor_tensor", [sm_b], [b_G[ti]], out=G[:, ti, :].rearrange("p (g e) -> p g e", g=4),
                   in0=R(40, 44).unsqueeze(2).to_broadcast([128, 4, 8]), in1=R(81, 89).unsqueeze(1).to_broadcast([128, 4, 8]), op=ALU.mult)
                G3 = G[:, ti, :].rearrange("p (g e) -> p g e", g=4)
                VV("vector", "tensor_copy", [sm_b], [b_Mg], out=Mg[:, ti, :], in_=R(40, 44))
                VV("vector", "tensor_copy", [b_G[ti]], [b_Ghl], out=Ghl[:, ti, :, 0:8], in_=G3)
                VV("vector", "tensor_tensor", [b_G[ti], b_Ghl], [tmp_b], out=T3, in0=G3, in1=Ghl[:, ti, :, 0:8], op=ALU.subtract)
                VV("vector", "tensor_copy", [tmp_b], [b_Ghl], out=Ghl[:, ti, :, 8:16], in_=T3)
            act_g = []
            nxt_t = 0
            n_t = NT if KC >= 6 else 0
            while act_g or nxt_t < n_t:
                while len(act_g) < 2 and nxt_t < n_t:
                    act_g.append(c1_tile(nxt_t))
                    nxt_t += 1
                for g_ in list(act_g):
                    try:
                        next(g_)
                    except StopIteration:
                        act_g.remove(g_)
        S.barrier()

        with ExitStack() as s2:
            sb2 = lambda n, s, dt=F32: s2.enter_context(nc.sbuf_tensor(n, list(s), dt))
            NST = CAP // 128
            iota = sb2("iota_sb", [128, CAP])
            b_iota = Buf("iota_sb")
            k.dma("sync", iota[:], C.iota_d, writes=[b_iota])
            Mcum = sb2("Mcum", [128, NT, 4])
            b_Mc = Buf()
            slotf = sb2("slotf", [128, NT])
            b_sl = Buf()
            t4 = sb2("t4", [128, 4])
            b_t4 = Buf()

            def VV(eng, method, reads, writes, **aps):
                return k.op(eng, lambda e, aps=aps, method=method: getattr(e, method)(**aps), reads=reads, writes=writes)
            for ti in range(NT):
                if ti == 0:
                    VV("vector", "tensor_copy", [b_Mg], [b_Mc], out=Mcum[:, 0, :], in_=Mg[:, 0, :])
                else:
                    VV("vector", "tensor_tensor", [b_Mg, b_Mc], [b_Mc], out=Mcum[:, ti, :], in0=Mcum[:, ti - 1, :], in1=Mg[:, ti, :], op=ALU.add)
            for ti in range(NT):
                pr, prb = psT[ti % 2], psB[ti % 2]
                k.op("tensor", lambda e, ti=ti, pr=pr: e.matmul(pr[:, 0:4], lhsT=cst[:, 7, :], rhs=Mg[:, ti, :], start=True, stop=(ti == 0)),
                     reads=[b_cst, b_Mg], writes=[prb])
                if ti > 0:
                    k.acc("tensor", lambda e, ti=ti, pr=pr: e.matmul(pr[:, 0:4], lhsT=C.ones_f[:], rhs=Mcum[:, ti - 1, :], start=False, stop=True),
                          reads=[C.b_ones, b_Mc], acc=[prb])
                VV("vector", "tensor_tensor", [prb, b_Mg], [b_t4], out=t4[:], in0=pr[:, 0:4], in1=Mg[:, ti, :], op=ALU.mult)
                VV("vector", "tensor_reduce", [b_t4], [b_sl], out=slotf[:, ti:ti + 1], in_=t4[:], axis=AX.X, op=ALU.add)

            Sel = sb2("Sel", [128, NT, CAP], BF16)
            b_Sel = Buf()
            hTg = sb2("hTg", [128, 8, CAP], BF16)
            b_hTg = Buf()
            Yg = sb2("Yg", [128, NST, 1024])
            b_Yg = [Buf() for _ in range(NST)]
            Ygs = sb2("Ygs", [128, NST, 1024], BF16)
            b_Ygs = Buf()
            t16 = sb2("t16", [128, 16])
            b_t16 = Buf()
            Gs = sb2("Gs", [128, NST, 8])
            b_Gs = Buf()
            hbt = [sb2("hbt%d" % i, [128, 1024], BF16) for i in range(3)]
            b_hbt = [Buf("hbt%d" % i) for i in range(3)]
            selT = [sb2("selT%d" % i, [128, 128], BF16) for i in range(4)]
            b_selT = [Buf() for _ in range(4)]
            wg = [sb2("wg%d" % i, [128, 8, 512], BF16) for i in range(2)]
            wu_ = [sb2("wup%d" % i, [128, 8, 512], BF16) for i in range(2)]
            wd = [sb2("wd%d" % i, [128, 4, 1024], BF16) for i in range(2)]
            b_wg = [Buf("wg%d" % i) for i in range(2)]
            b_wu = [Buf("wup%d" % i) for i in range(2)]
            b_wd = [Buf("wd%d" % i) for i in range(2)]
            sg = [sb2("sg%d" % i, [128, 512]) for i in range(2)]
            b_sg = [Buf() for _ in range(2)]
            act = sb2("act", [128, 4, 512], BF16)
            b_act = Buf()
            psbf = psT[7].bitcast(BF16)
            chunks = [(0, 512), (512, CAP - 512)]
            fcnt = 0
            hcnt = 0
            tcnt = 0
            for g in range(4):
                for ti in range(NT):
                    VV("vector", "tensor_scalar", [b_iota, b_sl, b_Mg], [b_Sel], out=Sel[:, ti, :], in0=iota[:], scalar1=slotf[:, ti:ti + 1],
                       scalar2=Mg[:, ti, g:g + 1], op0=ALU.is_equal, op1=ALU.mult)
                for ps_ in range(2):
                    for ti in range(NT):
                        hi = hcnt % 3
                        hcnt += 1
                        k.dma("sync", hbt[hi][:], C.hb_d[128 * ti:128 * ti + 128, :], reads=[C.b_hb_d], writes=[b_hbt[hi]])
                        for j in range(4):
                            kk = 4 * ps_ + j
                            for (bank, c0, cn) in ((j, 0, 512), (4 + j, 512, CAP - 512)):
                                fn = lambda e, bank=bank, hi=hi, kk=kk, ti=ti, c0=c0, cn=cn: e.matmul(
                                    psT[bank][:, 0:cn], lhsT=hbt[hi][:, 128 * kk:128 * kk + 128], rhs=Sel[:, ti, c0:c0 + cn], start=(ti == 0), stop=(ti == NT - 1))
                                if ti == 0:
                                    k.op("tensor", fn, reads=[b_hbt[hi], b_Sel], writes=[psB[bank]])
                                else:
                                    k.acc("tensor", fn, reads=[b_hbt[hi], b_Sel], acc=[psB[bank]])
                    for j in range(4):
                        kk = 4 * ps_ + j
                        VV("vector", "tensor_copy", [psB[j]], [b_hTg], out=hTg[:, kk, 0:512], in_=psT[j][:, 0:512])
                        VV("vector", "tensor_copy", [psB[4 + j]], [b_hTg], out=hTg[:, kk, 512:CAP], in_=psT[4 + j][:, 0:CAP - 512])
                for st in range(NST):
                    pg_, pgb_ = psT[st % 2], psB[st % 2]
                    for ti in range(NT):
                        fn = lambda e, st=st, ti=ti, g=g, pg_=pg_: e.matmul(pg_[:, 0:16], lhsT=Sel[:, ti, 128 * st:128 * st + 128], rhs=Ghl[:, ti, g, :],
                                                                           start=(ti == 0), stop=(ti == NT - 1))
                        if ti == 0:
                            k.op("tensor", fn, reads=[b_Sel, b_Ghl], writes=[pgb_])
                        else:
                            k.acc("tensor", fn, reads=[b_Sel, b_Ghl], acc=[pgb_])
                    VV("vector", "tensor_copy", [pgb_], [b_t16], out=t16[:], in_=pg_[:, 0:16])
                    VV("vector", "tensor_tensor", [b_t16], [b_Gs], out=Gs[:, st, :], in0=t16[:, 0:8], in1=t16[:, 8:16], op=ALU.add)
                for e8 in range(8):
                    ex = 8 * g + e8
                    wi = ex % 2
                    k.gload(wg[wi][:], C.w_gate[ex].rearrange("(k p) f -> p k f", p=128), writes=[b_wg[wi]])
                    k.gload(wu_[wi][:], C.w_up[ex].rearrange("(k p) f -> p k f", p=128), writes=[b_wu[wi]])
                    k.gload(wd[wi][:], C.w_down[ex].rearrange("(k p) d -> p k d", p=128), writes=[b_wd[wi]])
                    for (t0, tn) in (chunks if os.environ.get('KSKIPX', '0') == '0' else []):
                        for f in range(4):
                            pg, pgb = psT[0 + fcnt % 2], psB[0 + fcnt % 2]
                            pu, pub = psT[2 + fcnt % 2], psB[2 + fcnt % 2]
                            si = fcnt % 2
                            fcnt += 1
                            for (pp, ppb, ww, bw) in ((pg, pgb, wg[wi], b_wg[wi]), (pu, pub, wu_[wi], b_wu[wi])):
                                for kk in range(8):
                                    fn = lambda e, kk=kk, pp=pp, ww=ww, f=f, t0=t0, tn=tn: e.matmul(pp[:, 0:tn], lhsT=ww[:, kk, 128 * f:128 * f + 128], rhs=hTg[:, kk, t0:t0 + tn],
                                                                                                start=(kk == 0), stop=(kk == 7))
                                    if kk == 0:
                                        k.op("tensor", fn, reads=[bw, b_hTg], writes=[ppb])
                                    else:
                                        k.acc("tensor", fn, reads=[bw, b_hTg], acc=[ppb])
                            k.op("scalar", lambda e, si=si, pg=pg, tn=tn: e.activation(out=sg[si][:, 0:tn], in_=pg[:, 0:tn], func=AF.Silu), reads=[pgb], writes=[b_sg[si]])
                            k.op("vector", lambda e, si=si, f=f, pu=pu, tn=tn: e.tensor_tensor(out=act[:, f, 0:tn], in0=sg[si][:, 0:tn], in1=pu[:, 0:tn], op=ALU.mult),
                                 reads=[b_sg[si], pub], writes=[b_act])
                        for tt in range(tn // 128):
                            st = t0 // 128 + tt
                            for half in range(2):
                                py, pyb = psT[4 + (tt * 2 + half) % 4], psB[4 + (tt * 2 + half) % 4]
                                for f in range(4):
                                    fn = lambda e, f=f, tt=tt, half=half, py=py, wi=wi: e.matmul(py[:, :], lhsT=act[:, f, 128 * tt:128 * tt + 128],
                                                                                             rhs=wd[wi][:, f, 512 * half:512 * half + 512], start=(f == 0), stop=(f == 3))
                                    if f == 0:
                                        k.op("tensor", fn, reads=[b_act, b_wd[wi]], writes=[pyb])
                                    else:
                                        k.acc("tensor", fn, reads=[b_act, b_wd[wi]], acc=[pyb])
                                dsty = Yg[:, st, 512 * half:512 * half + 512]
                                if e8 == 0:
                                    VV("vector", "tensor_scalar", [pyb, b_Gs], [b_Yg[st]], out=dsty, in0=py[:, :], scalar1=Gs[:, st, e8:e8 + 1], scalar2=None, op0=ALU.mult)
                                else:
                                    VV("vector", "scalar_tensor_tensor", [pyb, b_Gs, b_Yg[st]], [b_Yg[st]], out=dsty, in0=py[:, :], scalar=Gs[:, st, e8:e8 + 1], in1=dsty,
                                       op0=ALU.mult, op1=ALU.add)
                for st in range(NST):
                    VV("gpsimd", "tensor_copy", [b_Yg[st]], [b_Ygs], out=Ygs[:, st, :], in_=Yg[:, st, :])
                for ti in range(NT):
                    pa = [(psT[0], psB[0]), (psT[1], psB[1])] if ti % 2 == 0 else [(psT[2], psB[2]), (psT[3], psB[3])]
                    for st in range(NST):
                        sti = tcnt % 4
                        tcnt += 1
                        pt_b = psbf[:, 128 * sti:128 * sti + 128]
                        k.op("tensor", lambda e, pt_b=pt_b, ti=ti, st=st: e.transpose(pt_b, Sel[:, ti, 128 * st:128 * st + 128], identb[:]),
                             reads=[b_Sel, b_identb], writes=[psB[7]])
                        VV("vector", "tensor_copy", [psB[7]], [b_selT[sti]], out=selT[sti][:], in_=pt_b)
                        for half in range(2):
                            fn = lambda e, half=half, sti=sti, st=st, pa=pa: e.matmul(pa[half][0][:, :], lhsT=selT[sti][:], rhs=Ygs[:, st, 512 * half:512 * half + 512],
                                                                                 start=(st == 0), stop=(st == NST - 1))
                            if st == 0:
                                k.op("tensor", fn, reads=[b_selT[sti], b_Ygs], writes=[pa[half][1]])
                            else:
                                k.acc("tensor", fn, reads=[b_selT[sti], b_Ygs], acc=[pa[half][1]])
                    for half in range(2):
                        dy = yacc[:, ti, 512 * half:512 * half + 512]
                        VV("vector", "tensor_tensor", [pa[half][1], b_y[ti]], [b_y[ti]], out=dy, in0=pa[half][0][:, :], in1=dy, op=ALU.add)
        S.barrier()

        with ExitStack() as s3:
            sb3 = lambda n, s, dt=F32: s3.enter_context(nc.sbuf_tensor(n, list(s), dt))
            lnp3 = sb3("lnp_sb3", [128, 2, 1024])
            b_lnp3 = Buf("lnp3")
            k.dma("sync", lnp3[:], C.lnp_d[:, 2:4, :], writes=[b_lnp3])
            st6, mv = st6_c3, mv_c3
            b_st = [Buf() for _ in range(2)]
            b_mv = [Buf() for _ in range(2)]
            yo = [sb3("yo%d" % i, [128, 1024]) for i in range(2)]
            b_yo = [Buf("yo%d" % i) for i in range(2)]
            for ti in range(NT if KC >= 30 else 0):
                bi = ti % 2
                for half in range(2):
                    k.op("vector", lambda e, bi=bi, half=half, ti=ti: e.bn_stats(out=st6[:, bi, half, :], in_=yacc[:, ti, 512 * half:512 * half + 512]),
                         reads=[b_y[ti]], writes=[b_st[bi]])
                k.op("vector", lambda e, bi=bi: e.bn_aggr(out=mv[:, bi, :], in_=st6[:, bi, :, :].rearrange("p a b -> p (a b)")), reads=[b_st[bi]], writes=[b_mv[bi]])
                k.op("scalar", lambda e, bi=bi: e.activation(out=mv[:, bi, 1:2], in_=mv[:, bi, 1:2], func=AF.Sqrt, bias=C.eps5[:, 0:1]), reads=[b_mv[bi], C.b_eps5], writes=[b_mv[bi]])
                k.op("vector", lambda e, bi=bi: e.reciprocal(out=mv[:, bi, 1:2], in_=mv[:, bi, 1:2]), reads=[b_mv[bi]], writes=[b_mv[bi]])
                k.op("vector", lambda e, bi=bi, ti=ti: e.tensor_scalar(out=yo[bi][:], in0=yacc[:, ti, :], scalar1=mv[:, bi, 0:1], scalar2=mv[:, bi, 1:2], op0=ALU.subtract, op1=ALU.mult),
                     reads=[b_y[ti], b_mv[bi]], writes=[b_yo[bi]])
                k.op("gpsimd", lambda e, bi=bi: e.tensor_tensor(out=yo[bi][:], in0=yo[bi][:], in1=lnp3[:, 0, :], op=ALU.mult), reads=[b_yo[bi], b_lnp3], writes=[b_yo[bi]])
                k.op("gpsimd", lambda e, bi=bi: e.tensor_tensor(out=yo[bi][:], in0=yo[bi][:], in1=lnp3[:, 1, :], op=ALU.add), reads=[b_yo[bi], b_lnp3], writes=[b_yo[bi]])
                if ti < 16:
                    final.append(k.dma("sync", C.y_out[128 * ti:128 * ti + 128, :], yo[bi][:], reads=[b_yo[bi]]))
                else:
                    final.append(k.dma("sync", C.ys_out, yo[bi][0:NS, :], reads=[b_yo[bi]]))


def phase_s(C):
    nc, k, S = C.nc, C.k, C.S
    psT, psB = C.psT, C.psB
    final = C.final
    ps_d, cs_d, ms_d = C.ps_d, C.cs_d, C.ms_d
    b_ps_d, b_cs_d, b_ms_d = Buf("ps_d"), Buf("cs_d"), Buf("ms_d")
    w_v = C.w_v

    def VV(eng, method, reads, writes, **aps):
        return k.op(eng, lambda e, aps=aps, method=method: getattr(e, method)(**aps), reads=reads, writes=writes)

    with ExitStack() as s1:
        sb1 = lambda n, s, dt=F32: s1.enter_context(nc.sbuf_tensor(n, list(s), dt))
        xsTb = sb1("xsTb", [128, 8, NS], BF16)
        b_xs = Buf("xsTb")
        k.gload(xsTb[:], C.xsT.rearrange("(k p) t -> p k t", p=128), writes=[b_xs])
        wS = [sb1("wS%d" % i, [128, 8, 512], BF16) for i in range(2)]
        b_wS = [Buf("wS%d" % i) for i in range(2)]
        p_s = sb1("p_s", [NS, IN_COLS])
        b_p = Buf("p_s")
        for cchunk in range(8):
            c0 = 512 * cchunk
            cn = min(512, IN_COLS - c0)
            wi = cchunk % 2
            k.gload(wS[wi][:, :, 0:cn], w_v[:, :, c0:c0 + cn], writes=[b_wS[wi]])
            pt_, pb_ = psT[wi], psB[wi]
            for kk in range(8):
                fn = lambda e, kk=kk, wi=wi, cn=cn, pt_=pt_: e.matmul(pt_[0:NS, 0:cn], lhsT=xsTb[:, kk, :], rhs=wS[wi][:, kk, 0:cn], start=(kk == 0), stop=(kk == 7))
                if kk == 0:
                    k.op("tensor", fn, reads=[b_xs, b_wS[wi]], writes=[pb_])
                else:
                    k.acc("tensor", fn, reads=[b_xs, b_wS[wi]], acc=[pb_])
            VV("vector", "tensor_copy", [pb_], [b_p], out=p_s[:, c0:c0 + cn], in_=pt_[0:NS, 0:cn])
        k.dma("sync", ps_d, p_s[:], reads=[b_p], writes=[b_ps_d])
        final.append(k.dma("sync", C.knew, p_s[:, COL_KA:COL_KA + 512], reads=[b_p], slot="knew"))
        final.append(k.dma("sync", C.vnew, p_s[:, COL_VA:COL_VA + 512], reads=[b_p], slot="vnew"))
        final.append(k.dma("sync", C.conv_s[:, 2, :], p_s[:, COL_UB:COL_UB + 1536], reads=[b_p], slot="convs"))
        cst_ = sb1("cst_", [NS, 3, 1536])
        b_cst_ = Buf("cst_")
        k.dma("sync", cst_[:], C.scv, writes=[b_cst_])
        final.append(k.dma("sync", C.conv_s[:, 0:2, :], cst_[:, 1:3, :], reads=[b_cst_], slot="convs"))
        cwr = sb1("cwr_sb", [NS, 4, 1536])
        b_cwr = Buf("cwr")
        k.dma("sync", cwr[:], C.cwr_d, writes=[b_cwr])
        cacc = sb1("cacc", [NS, 1536])
        ctmp = sb1("ctmp", [NS, 1536])
        b_ca, b_ct = Buf("cacc"), Buf()
        VV("vector", "tensor_tensor", [b_p, b_cwr], [b_ca], out=cacc[:], in0=p_s[:, COL_UB:COL_UB + 1536], in1=cwr[:, 3, :], op=ALU.mult)
        for i in range(3):
            VV("vector", "tensor_tensor", [b_cst_, b_cwr], [b_ct], out=ctmp[:], in0=cst_[:, i, :], in1=cwr[:, i, :], op=ALU.mult)
            VV("vector", "tensor_tensor", [b_ct, b_ca], [b_ca], out=cacc[:], in0=cacc[:], in1=ctmp[:], op=ALU.add)
        VV("scalar", "activation", [b_ca], [b_ca], out=cacc[:], in_=cacc[:], func=AF.Silu)
        k.dma("sync", cs_d, cacc[:], reads=[b_ca], writes=[b_cs_d])
    S.barrier()

    with ExitStack() as s2:
        sb2 = lambda n, s, dt=F32: s2.enter_context(nc.sbuf_tensor(n, list(s), dt))
        qkv = sb2("qkv_nh", [128, 3, 64])
        b_qkv = Buf("qkv_nh")
        for j, c0 in enumerate((COL_QA, COL_KA, COL_VA)):
            k.dma("sync", qkv[:, j, :], bass.AP(ps_d.tensor, c0, [[IN_COLS, NS], [64, 8], [1, 64]]), reads=[b_ps_d], writes=[b_qkv])
        sbias = sb2("sbias_sb", [128, 3, 129])
        b_sb = Buf("sbias")
        k.dma("sync", sbias[:], C.sbias_d, writes=[b_sb])
        Kb = sb2("Kb", [128, 128, 64])
        Vb = sb2("Vb", [128, 128, 64])
        b_Kb, b_Vb = Buf("Kb"), Buf("Vb")
        tmpS = sb2("tmpS", [128, 128, 64])
        b_tmp = Buf()
        sc = sb2("sc", [128, 3, 129])
        b_sc = Buf()
        sm = sb2("smS", [128, 16])
        b_sm = Buf()
        oacc = sb2("oacc", [128, 64])
        otmp = sb2("otmp", [128, 64])
        b_oa, b_ot = Buf("oacc"), Buf()
        VV("vector", "tensor_tensor", [b_qkv], [b_ot], out=otmp[:], in0=qkv[:, 0, :], in1=qkv[:, 1, :], op=ALU.mult)
        VV("vector", "tensor_reduce", [b_ot], [b_sm], out=sm[:, 0:1], in_=otmp[:], axis=AX.X, op=ALU.add)
        for br, (_, dil) in enumerate(BRANCHES):
            for n in range(NS):
                src = bass.AP(C.ck.tensor, n * 2048 * 512 + (2048 - 128 * dil) * 512, [[64, 8], [dil * 512, 128], [1, 64]])
                k.dma("sync", Kb[8 * n:8 * n + 8, :, :], src, writes=[b_Kb]) if n == 0 else k.S.dmaop(
                    "sync", "Kb", lambda e, n=n, src=src: e.dma_start(out=Kb[8 * n:8 * n + 8, :, :], in_=src), [])
            b_Kb.writer = ("D", "Kb", k.S.dma_sems["Kb"][1])
            VV("vector", "tensor_tensor", [b_Kb, b_qkv], [b_tmp], out=tmpS[:], in0=Kb[:], in1=qkv[:, 0, :].unsqueeze(1).to_broadcast([128, 128, 64]), op=ALU.mult)
            VV("vector", "tensor_reduce", [b_tmp], [b_sc], out=sc[:, br, 0:128], in_=tmpS[:], axis=AX.X, op=ALU.add)
            VV("vector", "tensor_copy", [b_sm], [b_sc], out=sc[:, br, 128:129], in_=sm[:, 0:1])
        VV("vector", "scalar_tensor_tensor", [b_sc, b_sb], [b_sc], out=sc[:].rearrange("p a b -> p (a b)"), in0=sc[:].rearrange("p a b -> p (a b)"),
           scalar=0.125, in1=sbias[:].rearrange("p a b -> p (a b)"), op0=ALU.mult, op1=ALU.add)
        VV("vector", "tensor_reduce", [b_sc], [b_sm], out=sm[:, 1:2], in_=sc[:].rearrange("p a b -> p (a b)"), axis=AX.X, op=ALU.max)
        VV("vector", "tensor_scalar", [b_sm], [b_sm], out=sm[:, 2:3], in0=sm[:, 1:2], scalar1=-1.0, scalar2=None, op0=ALU.mult)
        VV("scalar", "activation", [b_sc, b_sm], [b_sc, b_sm], out=sc[:].rearrange("p a b -> p (a b)"), in_=sc[:].rearrange("p a b -> p (a b)"), func=AF.Exp,
           bias=sm[:, 2:3], accum_out=sm[:, 3:4])
        VV("vector", "reciprocal", [b_sm], [b_sm], out=sm[:, 3:4], in_=sm[:, 3:4])
        VV("vector", "tensor_reduce", [b_sc], [b_sm], out=sm[:, 4:5], in_=sc[:, :, 128], axis=AX.X, op=ALU.add)
        VV("vector", "tensor_scalar", [b_qkv, b_sm], [b_oa], out=oacc[:], in0=qkv[:, 2, :], scalar1=sm[:, 4:5], scalar2=None, op0=ALU.mult)
        for br, (_, dil) in enumerate(BRANCHES):
            for n in range(NS):
                src = bass.AP(C.cv.tensor, n * 2048 * 512 + (2048 - 128 * dil) * 512, [[64, 8], [dil * 512, 128], [1, 64]])
                if n == 0:
                    k.dma("sync", Vb[8 * n:8 * n + 8, :, :], src, writes=[b_Vb])
                else:
                    k.S.dmaop("sync", "Vb", lambda e, n=n, src=src: e.dma_start(out=Vb[8 * n:8 * n + 8, :, :], in_=src), [])
            b_Vb.writer = ("D", "Vb", k.S.dma_sems["Vb"][1])
            VV("vector", "tensor_tensor", [b_Vb, b_sc], [b_tmp], out=tmpS[:], in0=Vb[:], in1=sc[:, br, 0:128].unsqueeze(2).to_broadcast([128, 128, 64]), op=ALU.mult)
            VV("vector", "tensor_reduce", [b_tmp], [b_ot], out=otmp[:], in_=tmpS[:].rearrange("p i d -> p d i"), axis=AX.X, op=ALU.add)
            VV("vector", "tensor_tensor", [b_ot, b_oa], [b_oa], out=oacc[:], in0=oacc[:], in1=otmp[:], op=ALU.add)
        VV("vector", "tensor_scalar", [b_oa, b_sm], [b_oa], out=oacc[:], in0=oacc[:], scalar1=sm[:, 3:4], scalar2=None, op0=ALU.mult)
        k.dma("sync", bass.AP(ms_d.tensor, 0, [[D, NS], [64, 8], [1, 64]]), oacc[:], reads=[b_oa], writes=[b_ms_d])
    S.barrier()

    with ExitStack() as s3:
        sb3 = lambda n, s, dt=F32: s3.enter_context(nc.sbuf_tensor(n, list(s), dt))
        NP = NS * 4
        St = sb3("St", [NP, 128, 128])
        b_St = Buf("St")
        for q4 in range(4):
            k.dma("sync", St[:, 32 * q4:32 * q4 + 32, :], C.sst[:, 32 * q4:32 * q4 + 32, :], writes=[b_St]) if q4 == 0 else k.S.dmaop(
                "sync", "St", lambda e, q4=q4: e.dma_start(out=St[:, 32 * q4:32 * q4 + 32, :], in_=C.sst[:, 32 * q4:32 * q4 + 32, :]), [])
        b_St.writer = ("D", "St", k.S.dma_sems["St"][1])
        T2 = sb3("T2", [NP, 128, 128])
        b_T2 = Buf()
        c3 = sb3("c3", [NP, 3, 128])
        b_c3 = Buf("c3")
        for ty in range(3):
            k.dma("sync", c3[:, ty, :], bass.AP(cs_d.tensor, 512 * ty, [[1536, NS], [128, 4], [1, 128]]), reads=[b_cs_d], writes=[b_c3])
        zz = sb3("zz", [NP, 128])
        b_zz = Buf("zz")
        k.dma("sync", zz[:], bass.AP(ps_d.tensor, COL_ZB, [[IN_COLS, NS], [128, 4], [1, 128]]), reads=[b_ps_d], writes=[b_zz])
        ab = sb3("ab_s", [NP, 2])
        b_ab = Buf("ab_s")
        k.dma("sync", ab[:, 0:1], bass.AP(ps_d.tensor, COL_AB, [[IN_COLS, NS], [1, 4], [1, 1]]), reads=[b_ps_d], writes=[b_ab])
        k.dma("sync", ab[:, 1:2], bass.AP(ps_d.tensor, COL_BB, [[IN_COLS, NS], [1, 4], [1, 1]]), reads=[b_ps_d], writes=[b_ab])
        sp = sb3("sprm_sb", [NP, 2 + 128])
        b_sp = Buf("sprm")
        k.dma("sync", sp[:], C.sprm_d, writes=[b_sp])
        w = sb3("wS3", [NP, 24])
        b_w = Buf()
        jk = sb3("jkS3", [NP, 128])
        b_jk = Buf()
        VV("vector", "tensor_tensor", [b_ab, b_sp], [b_w], out=w[:, 0:1], in0=ab[:, 0:1], in1=sp[:, 1:2], op=ALU.add)
        VV("scalar", "activation", [b_w], [b_w], out=w[:, 0:1], in_=w[:, 0:1], func=AF.Exp)
        VV("scalar", "activation", [b_w], [b_w], out=w[:, 0:1], in_=w[:, 0:1], func=AF.Ln, bias=1.0)
        VV("scalar", "activation", [b_sp], [b_w], out=w[:, 1:2], in_=sp[:, 0:1], func=AF.Exp)
        VV("vector", "scalar_tensor_tensor", [b_w], [b_w], out=w[:, 2:3], in0=w[:, 0:1], scalar=-1.0, in1=w[:, 1:2], op0=ALU.mult, op1=ALU.mult)
        VV("scalar", "activation", [b_w], [b_w], out=w[:, 3:4], in_=w[:, 2:3], func=AF.Exp)
        VV("scalar", "activation", [b_ab], [b_w], out=w[:, 4:5], in_=ab[:, 1:2], func=AF.Sigmoid)
        for j in range(2):
            VV("scalar", "activation", [b_c3], [b_jk, b_w], out=jk[:], in_=c3[:, j, :], func=AF.Square, accum_out=w[:, 5 + j:6 + j])
            VV("scalar", "activation", [b_w, C.b_eps], [b_w], out=w[:, 5 + j:6 + j], in_=w[:, 5 + j:6 + j], func=AF.Sqrt, bias=C.eps6[0:NP, 0:1])
            VV("vector", "reciprocal", [b_w], [b_w], out=w[:, 5 + j:6 + j], in_=w[:, 5 + j:6 + j])
        VV("vector", "tensor_scalar", [b_c3, b_w], [b_c3], out=c3[:, 0, :], in0=c3[:, 0, :], scalar1=w[:, 5:6], scalar2=128.0 ** -0.5, op0=ALU.mult, op1=ALU.mult)
        VV("vector", "tensor_scalar", [b_c3, b_w], [b_c3], out=c3[:, 1, :], in0=c3[:, 1, :], scalar1=w[:, 6:7], scalar2=None, op0=ALU.mult)
        mem = sb3("memS", [NP, 4, 128])
        b_mem = Buf()
        VV("vector", "tensor_tensor", [b_St, b_c3], [b_T2], out=T2[:], in0=St[:], in1=c3[:, 1, :].unsqueeze(2).to_broadcast([NP, 128, 128]), op=ALU.mult)
        VV("vector", "tensor_reduce", [b_T2], [b_mem], out=mem[:, 0, :], in_=T2[:].rearrange("p d e -> p e d"), axis=AX.X, op=ALU.add)
        VV("vector", "scalar_tensor_tensor", [b_mem, b_w, b_c3], [b_mem], out=mem[:, 1, :], in0=mem[:, 0, :], scalar=w[:, 3:4], in1=c3[:, 2, :], op0=ALU.mult, op1=ALU.subtract)
        VV("vector", "tensor_scalar", [b_mem, b_w], [b_mem], out=mem[:, 1, :], in0=mem[:, 1, :], scalar1=w[:, 4:5], scalar2=-1.0, op0=ALU.mult, op1=ALU.mult)
        VV("vector", "tensor_tensor", [b_c3, b_mem], [b_T2], out=T2[:], in0=c3[:, 1, :].unsqueeze(2).to_broadcast([NP, 128, 128]),
           in1=mem[:, 1, :].unsqueeze(1).to_broadcast([NP, 128, 128]), op=ALU.mult)
        VV("vector", "scalar_tensor_tensor", [b_St, b_T2, b_w], [b_St], out=St[:], in0=St[:], scalar=w[:, 3:4], in1=T2[:], op0=ALU.mult, op1=ALU.add)
        for q4 in range(4):
            final.append(k.dma("sync", C.ssm_s[:, 32 * q4:32 * q4 + 32, :], St[:, 32 * q4:32 * q4 + 32, :], reads=[b_St], slot="ssms"))
        VV("vector", "tensor_tensor", [b_St, b_c3], [b_T2], out=T2[:], in0=St[:], in1=c3[:, 0, :].unsqueeze(2).to_broadcast([NP, 128, 128]), op=ALU.mult)
        VV("vector", "tensor_reduce", [b_T2], [b_mem], out=mem[:, 2, :], in_=T2[:].rearrange("p d e -> p e d"), axis=AX.X, op=ALU.add)
        VV("scalar", "activation", [b_mem], [b_jk, b_w], out=jk[:], in_=mem[:, 2, :], func=AF.Square, accum_out=w[:, 8:9])
        VV("scalar", "activation", [b_w, C.b_eps], [b_w], out=w[:, 8:9], in_=w[:, 8:9], func=AF.Sqrt, scale=1.0 / 128.0, bias=C.eps6[0:NP, 0:1])
        VV("vector", "reciprocal", [b_w], [b_w], out=w[:, 8:9], in_=w[:, 8:9])
        VV("scalar", "activation", [b_zz], [b_zz], out=zz[:], in_=zz[:], func=AF.Silu)
        VV("vector", "tensor_tensor", [b_zz, b_sp], [b_zz], out=zz[:], in0=zz[:], in1=sp[:, 2:130], op=ALU.mult)
        VV("vector", "scalar_tensor_tensor", [b_mem, b_w, b_zz], [b_mem], out=mem[:, 3, :], in0=mem[:, 2, :], scalar=w[:, 8:9], in1=zz[:], op0=ALU.mult, op1=ALU.mult)
        k.dma("sync", bass.AP(ms_d.tensor, 512, [[D, NS], [128, 4], [1, 128]]), mem[:, 3, :], reads=[b_mem], writes=[b_ms_d])
    S.barrier()

    with ExitStack() as s4:
        sb4 = lambda n, s, dt=F32: s4.enter_context(nc.sbuf_tensor(n, list(s), dt))
        msf = sb4("msf", [128, 1024])
        b_msf = Buf("msf")
        VV("vector", "memset", [], [b_msf], ap=msf[:], constant=0.0)
        k.dma("sync", msf[0:NS, :], ms_d, reads=[b_ms_d], writes=[b_msf])
        mxS = sb4("mxS", [128, 8, 128], BF16)
        b_mxS = Buf("mxS")
        for g4 in range(2):
            pt_, pb_ = psT[2 + g4], psB[2 + g4]
            for j in range(4):
                kk = 4 * g4 + j
                fn = lambda e, kk=kk, j=j, pt_=pt_: e.transpose(pt_[:, 128 * j:128 * j + 128], msf[:, 128 * kk:128 * kk + 128], C.cst[:, 0, :])
                if j == 0:
                    k.op("tensor", fn, reads=[b_msf, C.b_cst], writes=[pb_])
                else:
                    k.acc("tensor", fn, reads=[b_msf, C.b_cst], acc=[pb_])
            VV("vector", "tensor_copy", [pb_], [b_mxS], out=mxS[:, 4 * g4:4 * g4 + 4, :], in_=pt_[:, :].rearrange("p (a c) -> p a c", a=4))
        k.dma("sync", C.mixS_d, mxS[:], reads=[b_mxS], writes=[C.b_mixS_d])
    S.barrier()


def _consts():
    t = np.arange(128)
    same = (t[:, None] // 64) == (t[None, :] // 64)
    cst = np.zeros((128, 8, 128), np.float32)
    cst[:, 0] = np.eye(128)
    cst[:, 1] = -np.eye(128)
    cst[:, 2] = (same & (t[:, None] <= t[None, :]))
    cst[:, 3] = (t[:, None] < 64) * np.ones((1, 128))
    cst[:, 4] = (t[:, None] >= 64) * np.ones((1, 128))
    cst[:, 5] = np.where(same & (t[None, :] <= t[:, None]), 0.0, NEG)
    cst[:, 6] = (same & (t[None, :] < t[:, None]))
    cst[:, 7] = (t[:, None] < t[None, :])
    return cst


def _params(inp):
    prm = np.zeros((128, 184), np.float32)
    prm[:, 0:4] = inp["a_log"][0][None, :]
    prm[:, 4:8] = inp["dt_bias"][0][None, :]
    prm[:, 8:136] = inp["o_norm_g"][0][None, :]
    cw = inp["conv_w"][0]
    prm[:, 136:184] = cw.reshape(4, 12, 128).transpose(2, 1, 0).reshape(128, 48)
    return prm


def _sample_bias(rel_bias):
    out = np.empty((128, 3, 129), np.float32)
    h = np.arange(128) % 8
    for br, (_, dil) in enumerate(BRANCHES):
        dist = np.concatenate([dil * (128 - np.arange(128)), [0]])
        out[:, br, :] = rel_bias[_rel_bucket_np(dist)][:, h].T
    return out


def _core_inputs(c, inp, bt):
    b, hf = divmod(c, 2)
    x = inp["x_prompt"][b]
    xT = np.zeros((D, EXT), np.float32)
    if hf == 1:
        xT[:, :] = x.T
    else:
        xT[:, HALF:] = x[:HALF].T
    valid = np.ones((128, 32), np.float32)
    if hf == 0:
        valid[:, :16] = 0.0
    return {
        "xT": np.ascontiguousarray(xT),
        "xo": np.ascontiguousarray(x[HALF * hf:HALF * hf + HALF]),
        "valid": valid,
        "w_in": np.ascontiguousarray(inp["w_in"][0]),
        "bt": bt,
        "cst": _consts(),
        "prm": _params(inp),
        "w_out": np.ascontiguousarray(inp["w_out"][0]),
        "lnp": np.ascontiguousarray(np.broadcast_to(np.stack([inp["ln1_g"][0], inp["ln1_b"][0], inp["ln2_g"][0], inp["ln2_b"][0]])[None], (128, 4, D))),
        "wr": np.ascontiguousarray(np.concatenate([inp["w_group"][0], inp["w_router"][0]], axis=1)),
        "rb": np.ascontiguousarray(np.broadcast_to(np.concatenate([inp["b_group"][0], inp["b_router"][0].reshape(-1)])[None], (128, 36))),
        "w_gate": np.ascontiguousarray(inp["w_gate"][0]),
        "w_up": np.ascontiguousarray(inp["w_up"][0]),
        "w_down": np.ascontiguousarray(inp["w_down"][0]),
        "xsT": np.ascontiguousarray(inp["x_sample"][NS * c:NS * c + NS, 0, :].T),
        "ck": np.ascontiguousarray(inp["cache_a_k"][0, NS * c:NS * c + NS].reshape(NS, 2048, 512)),
        "cv": np.ascontiguousarray(inp["cache_a_v"][0, NS * c:NS * c + NS].reshape(NS, 2048, 512)),
        "sst": np.ascontiguousarray(inp["state_b_ssm"][0, NS * c:NS * c + NS].reshape(NS * 4, 128, 128)),
        "scv": np.ascontiguousarray(inp["state_b_conv"][0, NS * c:NS * c + NS]),
        "sbias": _sample_bias(inp["rel_bias"].astype(np.float32)),
        "cwr": np.ascontiguousarray(np.broadcast_to(inp["conv_w"][0][None], (NS, 4, 1536))),
        "sprm": np.ascontiguousarray(np.concatenate([np.tile(inp["a_log"][0], NS)[:, None], np.tile(inp["dt_bias"][0], NS)[:, None],
                                                       np.broadcast_to(inp["o_norm_g"][0][None], (NS * 4, 128))], axis=1)),
        "iota": np.ascontiguousarray(np.broadcast_to(np.arange(CAP, dtype=np.float32)[None], (128, CAP))),
        "xs_pad": np.ascontiguousarray(np.concatenate([inp["x_sample"][NS * c:NS * c + NS, 0, :], np.zeros((128 - NS, D), np.float32)], axis=0)),
    }


def kernel(**inputs):
    inp = {k_: np.asarray(v) for k_, v in inputs.items()}
    bt = _bias_tiles(inp["rel_bias"].astype(np.float32))
    nc = build()
    in_maps = [_core_inputs(c, inp, bt) for c in range(NCORES)]
    res = run_bass_kernel_spmd(nc, in_maps, core_ids=list(range(NCORES)))
    r = res.results
    y_prompt = np.stack([np.concatenate([r[2 * b]["y_out"], r[2 * b + 1]["y_out"]], axis=0) for b in range(4)])
    k_win = np.stack([r[2 * b + 1]["kwin"].reshape(HALF, 8, 64) for b in range(4)])[None]
    v_win = np.stack([r[2 * b + 1]["vwin"].reshape(HALF, 8, 64) for b in range(4)])[None]
    ssm_p = np.stack([r[2 * b + 1]["ssm_p"] for b in range(4)])[None]
    conv_p = np.stack([r[2 * b + 1]["conv_p"] for b in range(4)])[None]
    y_sample = np.concatenate([r[c]["ys_out"] for c in range(NCORES)], axis=0)[:, None, :]
    k_new = np.concatenate([r[c]["knew"] for c in range(NCORES)], axis=0).reshape(1, 128, 1, 8, 64)
    v_new = np.concatenate([r[c]["vnew"] for c in range(NCORES)], axis=0).reshape(1, 128, 1, 8, 64)
    ssm_s = np.concatenate([r[c]["ssm_s"] for c in range(NCORES)], axis=0).reshape(1, 128, 4, 128, 128)
    conv_s = np.concatenate([r[c]["conv_s"] for c in range(NCORES)], axis=0)[None]
    return (y_prompt, y_sample, k_win, v_win, k_new, v_new, ssm_p, ssm_s, conv_p, conv_s)
```
